# Optimizing a Trainium2 kernel written in Bass

```python
import math
import jax, jax.numpy as jnp
from jax import lax
import numpy as np

D_MODEL = 1024
BATCH = 16
SEQ = 4096
DEPTH = 4

CTX_LEN = 256
GRID_W = 64
N_MIXERS = 3
CHUNK = 64
Q_BLOCK = 128
EPS = 1e-6

GDN_HEADS = 8
GDN_DK = D_MODEL // GDN_HEADS
GDN_DV = D_MODEL // GDN_HEADS
GDN_CONV = 5
GDN_IN = 2 * GDN_HEADS * GDN_DK + 2 * GDN_HEADS * GDN_DV + 4 * GDN_HEADS

MLSTM_HEADS = 8
MLSTM_DQK = D_MODEL // (2 * MLSTM_HEADS)
MLSTM_DV = D_MODEL // MLSTM_HEADS
MLSTM_IN = 2 * MLSTM_HEADS * MLSTM_DQK + 2 * MLSTM_HEADS * MLSTM_DV + 4 * MLSTM_HEADS

ATTN_HEADS = 8
ATTN_KV_HEADS = 2
ATTN_GROUP = ATTN_HEADS // ATTN_KV_HEADS
ATTN_DH = D_MODEL // ATTN_HEADS
ATTN_IN = (ATTN_HEADS + 2 * ATTN_KV_HEADS) * ATTN_DH
ROPE_THETA = 10000.0

FFN_DIM = ((8 * D_MODEL // 3 + 127) // 128) * 128
FFN_CONV = 3

kernel_name = "hybrid_gdn_mlstm_gqa_prefix_dit"

F32 = jnp.float32


def rms_norm(x, g):
    xf = x.astype(F32)
    y = xf * lax.rsqrt(jnp.mean(xf * xf, axis=-1, keepdims=True) + EPS)
    return (y * g.astype(F32)).astype(x.dtype)


def l2_norm(x):
    xf = x.astype(F32)
    return xf * lax.rsqrt(jnp.sum(xf * xf, axis=-1, keepdims=True) + EPS)


def modulate(h, shift, scale):
    return h * (1.0 + scale) + shift


def dw_conv(x, w):
    k = w.shape[0]
    return lax.conv_general_dilated(
        x, w[:, None, :].astype(x.dtype), window_strides=(1,), padding=[(k // 2, k // 2)],
        dimension_numbers=("NWC", "WIO", "NWC"), feature_group_count=x.shape[-1])


def to_chunks(t):
    b, l, h = t.shape[:3]
    t = t.reshape(b, l // CHUNK, CHUNK, h, *t.shape[3:])
    return jnp.moveaxis(t, (1, 3), (0, 2))


def from_chunks(t):
    t = jnp.moveaxis(t, (0, 2), (1, 3))
    return t.reshape(t.shape[0], t.shape[1] * t.shape[2], *t.shape[3:])


def tri_masks():
    i = jnp.arange(CHUNK)
    return i[:, None] >= i[None, :], i[:, None] > i[None, :]


def flip_seq(t):
    return jnp.flip(t, axis=1)


def identity(t):
    return t


def gated_delta_chunked(q, k, v, beta, g, s0):
    incl, strict = tri_masks()
    q, k, v, beta, g = map(to_chunks, (q, k, v, beta, g))
    gc = jnp.cumsum(g, axis=-1)
    decay = jnp.exp(jnp.where(incl, gc[..., :, None] - gc[..., None, :], -jnp.inf))
    kk = jnp.einsum("nbhid,nbhjd->nbhij", k, k)
    a_mat = jnp.where(strict, beta[..., :, None] * kk * decay, 0.0) + jnp.eye(CHUNK, dtype=kk.dtype)
    rhs = jnp.concatenate([v * beta[..., None], k * (beta * jnp.exp(gc))[..., None]], axis=-1)
    sol = lax.linalg.triangular_solve(a_mat, rhs, left_side=True, lower=True, unit_diagonal=True)
    dv = v.shape[-1]
    u, w = sol[..., :dv], sol[..., dv:]
    qk = jnp.einsum("nbhid,nbhjd->nbhij", q, k) * decay
    q_dec = q * jnp.exp(gc)[..., None]
    k_dec = k * jnp.exp(gc[..., -1:] - gc)[..., None]
    a_last = jnp.exp(gc[..., -1])

    def step(s, xs):
        qd, kd, u_c, w_c, qk_c, al = xs
        v_new = u_c - jnp.einsum("bhck,bhkv->bhcv", w_c, s)
        o = jnp.einsum("bhck,bhkv->bhcv", qd, s) + jnp.einsum("bhij,bhjv->bhiv", qk_c, v_new)
        s = s * al[..., None, None] + jnp.einsum("bhck,bhcv->bhkv", kd, v_new)
        return s, o

    s_fin, o = lax.scan(step, s0, (q_dec, k_dec, u, w, qk, a_last))
    return from_chunks(o), s_fin


def gdn_project(h, w_in, conv_w, a_log, dt_bias):
    bsz, l, _ = h.shape
    nqk, nv = GDN_HEADS * GDN_DK, GDN_HEADS * GDN_DV
    z = h @ w_in
    qkv = jax.nn.silu(dw_conv(z[..., :2 * nqk + nv], conv_w))
    q = l2_norm(qkv[..., :nqk].reshape(bsz, l, GDN_HEADS, GDN_DK)) * (GDN_DK ** -0.5)
    k = l2_norm(qkv[..., nqk:2 * nqk].reshape(bsz, l, GDN_HEADS, GDN_DK))
    v = qkv[..., 2 * nqk:].reshape(bsz, l, GDN_HEADS, GDN_DV).astype(F32)
    gate = z[..., 2 * nqk + nv:2 * nqk + 2 * nv]
    ba = z[..., 2 * nqk + 2 * nv:].reshape(bsz, l, 2, 2, GDN_HEADS).astype(F32)
    beta = jax.nn.sigmoid(ba[:, :, :, 0])
    g = -jnp.exp(a_log.astype(F32)) * jax.nn.softplus(ba[:, :, :, 1] + dt_bias.astype(F32))
    return q, k, v, beta, g, gate


def gdn_mixer(h_ctx, h_lat, want_ctx, w_in, conv_w, a_log, dt_bias, norm_g, w_out):
    qc, kc, vc, bc, gcx, gate_c = gdn_project(h_ctx, w_in, conv_w, a_log, dt_bias)
    ql, kl, vl, bl, glt, gate_l = gdn_project(h_lat, w_in, conv_w, a_log, dt_bias)
    s0 = jnp.zeros((h_ctx.shape[0], GDN_HEADS, GDN_DK, GDN_DV), F32)
    o_ctx, o_lat = [], []
    for d, f in enumerate((identity, flip_seq)):
        oc, s_ctx = gated_delta_chunked(f(qc), f(kc), f(vc), f(bc[:, :, d]), f(gcx[:, :, d]), s0)
        ol, _ = gated_delta_chunked(f(ql), f(kl), f(vl), f(bl[:, :, d]), f(glt[:, :, d]), s_ctx)
        o_ctx.append(f(oc))
        o_lat.append(f(ol))

    def finish(o_pair, gate, dtype):
        o = rms_norm(o_pair[0] + o_pair[1], norm_g)
        o = o.reshape(*o.shape[:2], GDN_HEADS * GDN_DV) * jax.nn.silu(gate.astype(F32))
        return o.astype(dtype) @ w_out

    y_ctx = finish(o_ctx, gate_c, h_ctx.dtype) if want_ctx else None
    return y_ctx, finish(o_lat, gate_l, h_lat.dtype)


def mlstm_chunked(q, k, v, ig, lf, c0, n0, m0):
    incl, _ = tri_masks()
    q, k, v, ig, lf = map(to_chunks, (q, k, v, ig, lf))
    b = jnp.cumsum(lf, axis=-1)
    dlog = jnp.where(incl, b[..., :, None] - b[..., None, :] + ig[..., None, :], -jnp.inf)
    a = jnp.max(dlog, axis=-1)
    p = jnp.exp(dlog - a[..., None]) * jnp.einsum("nbhid,nbhjd->nbhij", q, k)
    num_in = jnp.einsum("nbhij,nbhjv->nbhiv", p, v)
    den_in = jnp.sum(p, axis=-1)
    b_last = b[..., -1]
    wlog = b_last[..., None] - b + ig
    m_loc = jnp.max(wlog, axis=-1)
    kw = k * jnp.exp(wlog - m_loc[..., None])[..., None]
    dc = jnp.einsum("nbhck,nbhcv->nbhkv", kw, v)
    dn = jnp.sum(kw, axis=-2)

    def step(carry, xs):
        cm, nm, mm = carry
        qc, bc, ac, num_c, den_c, bl, ml, dcc, dnc = xs
        m_t = jnp.maximum(ac, bc + mm[..., None])
        s_inter = jnp.exp(bc + mm[..., None] - m_t)
        s_intra = jnp.exp(ac - m_t)
        num = s_inter[..., None] * jnp.einsum("bhck,bhkv->bhcv", qc, cm) + s_intra[..., None] * num_c
        den = s_inter * jnp.einsum("bhck,bhk->bhc", qc, nm) + s_intra * den_c
        h = num / jnp.maximum(jnp.abs(den), jnp.exp(-m_t))[..., None]
        m_new = jnp.maximum(bl + mm, ml)
        f_sc = jnp.exp(bl + mm - m_new)
        i_sc = jnp.exp(ml - m_new)
        cm = f_sc[..., None, None] * cm + i_sc[..., None, None] * dcc
        nm = f_sc[..., None] * nm + i_sc[..., None] * dnc
        return (cm, nm, m_new), h

    (c_f, n_f, m_f), h = lax.scan(step, (c0, n0, m0), (q, b, a, num_in, den_in, b_last, m_loc, dc, dn))
    return from_chunks(h), c_f, n_f, m_f


def mlstm_project(h, w_in, gate_b):
    bsz, l, _ = h.shape
    nqk, nv = MLSTM_HEADS * MLSTM_DQK, MLSTM_HEADS * MLSTM_DV
    z = h @ w_in
    q = z[..., :nqk].reshape(bsz, l, MLSTM_HEADS, MLSTM_DQK).astype(F32) * (MLSTM_DQK ** -0.5)
    k = z[..., nqk:2 * nqk].reshape(bsz, l, MLSTM_HEADS, MLSTM_DQK).astype(F32)
    v = z[..., 2 * nqk:2 * nqk + nv].reshape(bsz, l, MLSTM_HEADS, MLSTM_DV).astype(F32)
    o_gate = z[..., 2 * nqk + nv:2 * nqk + 2 * nv]
    gates = z[..., 2 * nqk + 2 * nv:].reshape(bsz, l, 2, 2, MLSTM_HEADS).astype(F32) + gate_b.astype(F32)
    ig = gates[:, :, :, 0]
    lf = jax.nn.log_sigmoid(gates[:, :, :, 1])
    return q, k, v, ig, lf, o_gate


def mlstm_mixer(h_ctx, h_lat, want_ctx, w_in, gate_b, norm_g, w_out):
    qc, kc, vc, igc, lfc, og_c = mlstm_project(h_ctx, w_in, gate_b)
    ql, kl, vl, igl, lfl, og_l = mlstm_project(h_lat, w_in, gate_b)
    bsz = h_ctx.shape[0]
    c0 = jnp.zeros((bsz, MLSTM_HEADS, MLSTM_DQK, MLSTM_DV), F32)
    n0 = jnp.zeros((bsz, MLSTM_HEADS, MLSTM_DQK), F32)
    m0 = jnp.zeros((bsz, MLSTM_HEADS), F32)
    o_ctx, o_lat = [], []
    for d, f in enumerate((identity, flip_seq)):
        hc, cc, nc, mc = mlstm_chunked(f(qc), f(kc), f(vc), f(igc[:, :, d]), f(lfc[:, :, d]), c0, n0, m0)
        hl, _, _, _ = mlstm_chunked(f(ql), f(kl), f(vl), f(igl[:, :, d]), f(lfl[:, :, d]), cc, nc, mc)
        o_ctx.append(f(hc))
        o_lat.append(f(hl))

    def finish(h_pair, o_gate, dtype):
        o = rms_norm(h_pair[0] + h_pair[1], norm_g)
        o = o.reshape(*o.shape[:2], MLSTM_HEADS * MLSTM_DV) * jax.nn.sigmoid(o_gate.astype(F32))
        return o.astype(dtype) @ w_out

    y_ctx = finish(o_ctx, og_c, h_ctx.dtype) if want_ctx else None
    return y_ctx, finish(o_lat, og_l, h_lat.dtype)


def axial_rope(seq_len):
    rows = seq_len // GRID_W
    row = jnp.repeat(jnp.arange(rows), GRID_W).astype(F32)
    col = jnp.tile(jnp.arange(GRID_W), rows).astype(F32)
    n_pair = ATTN_DH // 4
    inv = ROPE_THETA ** (-jnp.arange(n_pair, dtype=F32) / n_pair)
    ang = jnp.concatenate([row[:, None] * inv, col[:, None] * inv], axis=-1)
    return jnp.cos(ang), jnp.sin(ang)


def apply_rope(x, cos, sin):
    xr = x.reshape(*x.shape[:-1], -1, 2)
    x0, x1 = xr[..., 0], xr[..., 1]
    cos = cos.astype(x.dtype)
    sin = sin.astype(x.dtype)
    return jnp.stack([x0 * cos - x1 * sin, x0 * sin + x1 * cos], axis=-1).reshape(x.shape)


def attend(q, k, v):
    s = jnp.einsum("bqhgd,bkhd->bhgqk", q, k, preferred_element_type=F32) * (ATTN_DH ** -0.5)
    p = jax.nn.softmax(s, axis=-1).astype(v.dtype)
    return jnp.einsum("bhgqk,bkhd->bqhgd", p, v)


def gqa_mixer(h_ctx, h_lat, want_ctx, w_in, q_norm_g, k_norm_g, w_out):
    nq, nk = ATTN_HEADS * ATTN_DH, ATTN_KV_HEADS * ATTN_DH

    def project(h):
        bsz, l, _ = h.shape
        z = h @ w_in
        q = rms_norm(z[..., :nq].reshape(bsz, l, ATTN_KV_HEADS, ATTN_GROUP, ATTN_DH), q_norm_g)
        k = rms_norm(z[..., nq:nq + nk].reshape(bsz, l, ATTN_KV_HEADS, ATTN_DH), k_norm_g)
        v = z[..., nq + nk:].reshape(bsz, l, ATTN_KV_HEADS, ATTN_DH)
        return q, k, v

    qc, kc, vc = project(h_ctx)
    ql, kl, vl = project(h_lat)
    bsz, l = h_lat.shape[:2]
    cos, sin = axial_rope(l)
    ql = apply_rope(ql, cos[:, None, None, :], sin[:, None, None, :])
    kl = apply_rope(kl, cos[:, None, :], sin[:, None, :])
    k_all = jnp.concatenate([kc, kl], axis=1)
    v_all = jnp.concatenate([vc, vl], axis=1)
    nb = l // Q_BLOCK
    qb = jnp.moveaxis(ql.reshape(bsz, nb, Q_BLOCK, ATTN_KV_HEADS, ATTN_GROUP, ATTN_DH), 1, 0)
    ob = lax.map(lambda qq: attend(qq, k_all, v_all), qb)
    y_lat = jnp.moveaxis(ob, 0, 1).reshape(bsz, l, nq) @ w_out
    y_ctx = attend(qc, kc, vc).reshape(bsz, h_ctx.shape[1], nq) @ w_out if want_ctx else None
    return y_ctx, y_lat


def conv_glu(h, w_in, conv_w, conv_b, w_out):
    val, gate = jnp.split(h @ w_in, 2, axis=-1)
    gate = dw_conv(gate, conv_w) + conv_b
    return (val * jax.nn.silu(gate)) @ w_out


def _count(kind):
    return len(range(kind, DEPTH, N_MIXERS))


def setup_inputs(seed: int = 0) -> dict:
    key = jax.random.key(seed)
    keys = iter(jax.random.split(key, 40))

    def nrm(shape, std):
        return std * jax.random.normal(next(keys), shape, F32)

    D = D_MODEL
    n_a, n_b, n_c = _count(0), _count(1), _count(2)
    x = nrm((BATCH, SEQ, D), 1.0)
    c = nrm((BATCH, D), 1.0)
    ctx = nrm((BATCH, CTX_LEN, D), 1.0)
    c_ctx = nrm((D,), 1.0)
    norm1_g = 1.0 + nrm((DEPTH, D), 0.02)
    norm2_g = 1.0 + nrm((DEPTH, D), 0.02)
    w_mod = nrm((DEPTH, D, 6 * D), 0.5 * D ** -0.5)
    b_mod = nrm((DEPTH, 6 * D), 0.02)
    ffn_w_in = nrm((DEPTH, D, 2 * FFN_DIM), D ** -0.5)
    ffn_conv_w = nrm((DEPTH, FFN_CONV, FFN_DIM), FFN_CONV ** -0.5)
    ffn_conv_b = nrm((DEPTH, FFN_DIM), 0.02)
    ffn_w_out = nrm((DEPTH, FFN_DIM, D), FFN_DIM ** -0.5)
    gdn_w_in = nrm((n_a, D, GDN_IN), D ** -0.5)
    gdn_conv_w = nrm((n_a, GDN_CONV, 2 * GDN_HEADS * GDN_DK + GDN_HEADS * GDN_DV), GDN_CONV ** -0.5)
    gdn_a_log = jnp.log(jax.random.uniform(next(keys), (n_a, 2, GDN_HEADS), F32, 1.0, 16.0))
    dt = jnp.exp(jax.random.uniform(next(keys), (n_a, 2, GDN_HEADS), F32, math.log(1e-3), math.log(1e-1)))
    gdn_dt_bias = dt + jnp.log(-jnp.expm1(-dt))
    gdn_norm_g = 1.0 + nrm((n_a, GDN_DV), 0.02)
    gdn_w_out = nrm((n_a, GDN_HEADS * GDN_DV, D), (GDN_HEADS * GDN_DV) ** -0.5)
    mlstm_w_in = nrm((n_b, D, MLSTM_IN), D ** -0.5)
    ig_b = nrm((n_b, 2, 1, MLSTM_HEADS), 0.1)
    fg_b = jnp.linspace(3.0, 6.0, MLSTM_HEADS, dtype=F32) + nrm((n_b, 2, 1, MLSTM_HEADS), 0.1)
    mlstm_gate_b = jnp.concatenate([ig_b, fg_b], axis=2)
    mlstm_norm_g = 1.0 + nrm((n_b, MLSTM_DV), 0.02)
    mlstm_w_out = nrm((n_b, MLSTM_HEADS * MLSTM_DV, D), (MLSTM_HEADS * MLSTM_DV) ** -0.5)
    attn_w_in = nrm((n_c, D, ATTN_IN), D ** -0.5)
    attn_q_norm_g = 1.0 + nrm((n_c, ATTN_DH), 0.02)
    attn_k_norm_g = 1.0 + nrm((n_c, ATTN_DH), 0.02)
    attn_w_out = nrm((n_c, ATTN_HEADS * ATTN_DH, D), (ATTN_HEADS * ATTN_DH) ** -0.5)
    final_norm_g = 1.0 + nrm((D,), 0.02)
    return {
        "x": x, "c": c, "ctx": ctx, "c_ctx": c_ctx,
        "norm1_g": norm1_g, "norm2_g": norm2_g, "w_mod": w_mod, "b_mod": b_mod,
        "ffn_w_in": ffn_w_in, "ffn_conv_w": ffn_conv_w, "ffn_conv_b": ffn_conv_b, "ffn_w_out": ffn_w_out,
        "gdn_w_in": gdn_w_in, "gdn_conv_w": gdn_conv_w, "gdn_a_log": gdn_a_log, "gdn_dt_bias": gdn_dt_bias,
        "gdn_norm_g": gdn_norm_g, "gdn_w_out": gdn_w_out,
        "mlstm_w_in": mlstm_w_in, "mlstm_gate_b": mlstm_gate_b, "mlstm_norm_g": mlstm_norm_g,
        "mlstm_w_out": mlstm_w_out,
        "attn_w_in": attn_w_in, "attn_q_norm_g": attn_q_norm_g, "attn_k_norm_g": attn_k_norm_g,
        "attn_w_out": attn_w_out, "final_norm_g": final_norm_g,
    }


def reference(x, c, ctx, c_ctx, norm1_g, norm2_g, w_mod, b_mod, ffn_w_in, ffn_conv_w, ffn_conv_b,
              ffn_w_out, gdn_w_in, gdn_conv_w, gdn_a_log, gdn_dt_bias, gdn_norm_g, gdn_w_out,
              mlstm_w_in, mlstm_gate_b, mlstm_norm_g, mlstm_w_out, attn_w_in, attn_q_norm_g,
              attn_k_norm_g, attn_w_out, final_norm_g):
    x_lat, x_ctx = x, ctx
    s_lat = jax.nn.silu(c)[:, None, :]
    s_ctx = jax.nn.silu(c_ctx)[None, None, :]
    for i in range(DEPTH):
        last = i == DEPTH - 1
        kind, j = i % N_MIXERS, i // N_MIXERS
        mod_l = jnp.split(s_lat @ w_mod[i] + b_mod[i], 6, axis=-1)
        mod_c = jnp.split(s_ctx @ w_mod[i] + b_mod[i], 6, axis=-1)
        h_lat = modulate(rms_norm(x_lat, norm1_g[i]), mod_l[0], mod_l[1])
        h_ctx = modulate(rms_norm(x_ctx, norm1_g[i]), mod_c[0], mod_c[1])
        if kind == 0:
            y_ctx, y_lat = gdn_mixer(h_ctx, h_lat, not last, gdn_w_in[j], gdn_conv_w[j], gdn_a_log[j],
                                     gdn_dt_bias[j], gdn_norm_g[j], gdn_w_out[j])
        elif kind == 1:
            y_ctx, y_lat = mlstm_mixer(h_ctx, h_lat, not last, mlstm_w_in[j], mlstm_gate_b[j],
                                       mlstm_norm_g[j], mlstm_w_out[j])
        else:
            y_ctx, y_lat = gqa_mixer(h_ctx, h_lat, not last, attn_w_in[j], attn_q_norm_g[j],
                                     attn_k_norm_g[j], attn_w_out[j])
        x_lat = x_lat + mod_l[2] * y_lat
        h_lat = modulate(rms_norm(x_lat, norm2_g[i]), mod_l[3], mod_l[4])
        x_lat = x_lat + mod_l[5] * conv_glu(h_lat, ffn_w_in[i], ffn_conv_w[i], ffn_conv_b[i], ffn_w_out[i])
        if not last:
            x_ctx = x_ctx + mod_c[2] * y_ctx
            h_ctx = modulate(rms_norm(x_ctx, norm2_g[i]), mod_c[3], mod_c[4])
            x_ctx = x_ctx + mod_c[5] * conv_glu(h_ctx, ffn_w_in[i], ffn_conv_w[i], ffn_conv_b[i], ffn_w_out[i])
    return rms_norm(x_lat, final_norm_g)
```

```python
import contextlib
import math
import numpy as np
import concourse.bass as bass
import concourse.mybir as mybir
from concourse.bass_utils import run_bass_kernel_spmd

F32 = mybir.dt.float32
BF16 = mybir.dt.bfloat16
AF = mybir.ActivationFunctionType
ALU = mybir.AluOpType
AX = mybir.AxisListType

D = 1024
LAT = 4096
CTXL = 256
NTC = 2
NTL = 32
NT = 34
TOK = NT * 128
FFN = 2816
EPS = 1e-6
HALO = 2
REG_C = CTXL + 2 * HALO
REG_L = LAT + 2 * HALO
REG = REG_C + REG_L
KINDS = [0, 1, 2, 0]


class KB:
    def __init__(self, nc, ring=6, same_engine_sync=True):
        self.nc = nc
        self.eng = {'pe': nc.tensor, 'dve': nc.vector, 'act': nc.scalar,
                    'pool': nc.gpsimd, 'sp': nc.sync}
        self.same_engine_sync = same_engine_sync
        self.sem = {}
        self.cnt = {}
        self.seen = {e: {} for e in self.eng}
        self.res = {}
        self._ctx = []
        for e in ('pe', 'dve', 'act', 'pool'):
            self._mksem('c_' + e)
        self.rings = {}
        for q in ('sp', 'act', 'pool'):
            names = []
            for i in range(ring):
                n = 'd_%s_%d' % (q, i)
                self._mksem(n)
                names.append(n)
            self.rings[q] = [names, 0]
        self.n_inst = 0
        self.n_wait = 0

    def _mksem(self, name):
        cm = self.nc.semaphore(name)
        h = cm.__enter__()
        self._ctx.append(cm)
        self.sem[name] = h
        self.cnt[name] = 0

    def close(self):
        for cm in reversed(self._ctx):
            cm.__exit__(None, None, None)
        self._ctx = []

    def _R(self, key):
        r = self.res.get(key)
        if r is None:
            r = {'w': None, 'r': {}}
            self.res[key] = r
        return r

    def _deps(self, reads, writes):
        deps = {}

        def add(tok):
            if tok is None:
                return
            s, v = tok
            if deps.get(s, 0) < v:
                deps[s] = v
        for k in reads:
            add(self._R(k)['w'])
        for k in writes:
            r = self._R(k)
            add(r['w'])
            for s, v in r['r'].items():
                add((s, v))
        return deps

    def _wait(self, e, deps):
        own = 'c_' + e
        seen = self.seen[e]
        for s, v in deps.items():
            if s == own and (e == 'pe' or not self.same_engine_sync):
                continue
            if seen.get(s, 0) >= v:
                continue
            self.eng[e].wait_ge(self.sem[s], v)
            self.n_wait += 1
            seen[s] = v

    def _commit(self, tok, reads, writes):
        s, v = tok
        for k in reads:
            r = self._R(k)
            if r['r'].get(s, 0) < v:
                r['r'][s] = v
        for k in writes:
            r = self._R(k)
            r['w'] = tok
            r['r'] = {}

    def op(self, e, fn, reads=(), writes=()):
        deps = self._deps(reads, writes)
        self._wait(e, deps)
        inst = fn(self.eng[e])
        s = 'c_' + e
        self.cnt[s] += 1
        inst.then_inc(self.sem[s], 1)
        self.n_inst += 1
        self._commit((s, self.cnt[s]), reads, writes)
        return inst

    def dma(self, q, out, in_, reads=(), writes=(), **kw):
        names, idx = self.rings[q]
        s = names[idx % len(names)]
        self.rings[q][1] = idx + 1
        deps = self._deps(reads, writes)
        if self.cnt[s] > 0 and deps.get(s, 0) < self.cnt[s]:
            deps[s] = self.cnt[s]
        self._wait(q, deps)
        inst = self.eng[q].dma_start(out=out, in_=in_, **kw)
        self.cnt[s] += 16
        inst.then_inc(self.sem[s], 16)
        self.n_inst += 1
        self._commit((s, self.cnt[s]), reads, writes)
        return inst

    def barrier(self):
        for e in self.eng:
            for s, v in self.cnt.items():
                if v > 0 and self.seen[e].get(s, 0) < v:
                    self.eng[e].wait_ge(self.sem[s], v)
                    self.seen[e][s] = v
        self.res = {}


class MK:
    def __init__(self, nc, NB, kinds, last_flags, debug=False):
        self.debug = debug
        self.nc = nc
        self.NB = NB
        self.kinds = kinds
        self.last_flags = last_flags
        self.kb = KB(nc)
        self.stack = None
        self.uid = 0

    @contextlib.contextmanager
    def phase(self):
        prev = self.stack
        with contextlib.ExitStack() as es:
            self.stack = es
            yield
            self.kb.barrier()
        self.stack = prev

    def sb(self, name, shape, dt=F32):
        self.uid += 1
        return self.stack.enter_context(self.nc.sbuf_tensor("%s_%d" % (name, self.uid), list(shape), dt))

    def ps(self, name, shape, dt=F32):
        self.uid += 1
        return self.stack.enter_context(self.nc.psum_tensor("%s_%d" % (name, self.uid), list(shape), dt))

    def dram(self, name, shape, dt, kind="Internal"):
        return self.nc.dram_tensor(name, list(shape), dt, kind=kind).ap()

    def mm(self, out, lhsT, rhs, start, stop, reads, writes):
        return self.kb.op('pe', lambda e: e.matmul(out, lhsT=lhsT, rhs=rhs, start=start, stop=stop),
                          reads=reads, writes=writes)

    def tr(self, out, in_, ident, reads, writes):
        return self.kb.op('pe', lambda e: e.transpose(out, in_, ident), reads=reads, writes=writes)

    def declare(self):
        NB = self.NB
        n0 = sum(1 for k in self.kinds if k == 0)
        n1 = sum(1 for k in self.kinds if k == 1)
        n2 = sum(1 for k in self.kinds if k == 2)
        DEPTH = len(self.kinds)
        I = lambda n, s: self.dram(n, s, F32, kind="ExternalInput")
        self.x = I("x", [NB, LAT, D])
        self.ctx = I("ctx", [NB, CTXL, D])
        self.cvec = I("cvec", [NB + 1, D])
        self.norm1_g = I("norm1_g", [DEPTH, D])
        self.norm2_g = I("norm2_g", [DEPTH, D])
        self.w_mod = I("w_mod", [DEPTH, D, 6 * D])
        self.b_mod = I("b_mod", [DEPTH, 6 * D])
        self.ffn_w_in = I("ffn_w_in", [DEPTH, D, 2 * FFN])
        self.ffn_conv_w = I("ffn_conv_w", [DEPTH, 3, FFN])
        self.ffn_conv_b = I("ffn_conv_b", [DEPTH, FFN])
        self.ffn_w_out = I("ffn_w_out", [DEPTH, FFN, D])
        self.gdn_w_in = I("gdn_w_in", [max(n0, 1), D, 4128])
        self.gdn_conv_w = I("gdn_conv_w", [max(n0, 1), 5, 3072])
        self.gdn_a_log = I("gdn_a_log", [max(n0, 1), 16])
        self.gdn_dt_bias = I("gdn_dt_bias", [max(n0, 1), 16])
        self.gdn_norm_g = I("gdn_norm_g", [max(n0, 1), 128])
        self.gdn_w_out = I("gdn_w_out", [max(n0, 1), D, D])
        self.mlstm_w_in = I("mlstm_w_in", [max(n1, 1), D, 3104])
        self.mlstm_gate_b = I("mlstm_gate_b", [max(n1, 1), 32])
        self.mlstm_norm_g = I("mlstm_norm_g", [max(n1, 1), 128])
        self.mlstm_w_out = I("mlstm_w_out", [max(n1, 1), D, D])
        self.attn_w_in = I("attn_w_in", [max(n2, 1), D, 1536])
        self.attn_q_norm_g = I("attn_q_norm_g", [max(n2, 1), 128])
        self.attn_k_norm_g = I("attn_k_norm_g", [max(n2, 1), 128])
        self.attn_w_out = I("attn_w_out", [max(n2, 1), D, D])
        self.final_norm_g = I("final_norm_g", [1, D])
        self.c_ident = I("c_ident", [128, 128])
        self.c_rope = I("c_rope", [LAT, 128])
        self.c_masks = I("c_masks", [16, 128, 128])
        self.out = self.dram("out", [NB, LAT, D], F32, kind="ExternalOutput")
        sk = "ExternalOutput" if self.debug else "Internal"
        self.xs = self.dram("xs", [NB, TOK, D], F32, kind=sk)
        self.hTs = self.dram("hTs", [D, NB * REG], BF16, kind=sk)
        self.modv = self.dram("modv", [DEPTH, NB + 1, 6, D], F32, kind=sk)

    def src_x(self, layer0, b, j):
        if layer0:
            if j < NTC:
                return self.ctx[b, j * 128:(j + 1) * 128, :], 'in_ctx'
            return self.x[b, (j - NTC) * 128:(j - NTC + 1) * 128, :], 'in_x'
        return self.xs[b, j * 128:(j + 1) * 128, :], 'xs_%d_%d' % (b, j)

    def hcol(self, b, j):
        base = b * REG
        if j < NTC:
            return base + HALO + j * 128
        return base + REG_C + HALO + (j - NTC) * 128

    def hT_view(self):
        return self.hTs.rearrange("(k p) c -> p k c", p=128)

    def setup(self):
        kb = self.kb
        with self.phase():
            z = self.sb("zero", [128, 8, 2 * HALO], BF16)
            kb.op('dve', lambda e: e.memset(z[:], 0.0), writes=['zero'])
            hv = self.hT_view()
            for b in range(self.NB):
                base = b * REG
                for c0 in (base, base + REG_C - HALO):
                    pass
                kb.dma('pool', hv[:, :, base:base + HALO], z[:, :, 0:HALO], reads=['zero'], writes=['hTs_halo'])
                kb.dma('pool', hv[:, :, base + REG_C - HALO:base + REG_C + HALO], z[:, :, :], reads=['zero'], writes=['hTs_halo'])
                kb.dma('pool', hv[:, :, base + REG - HALO:base + REG], z[:, :, 0:HALO], reads=['zero'], writes=['hTs_halo'])

    def mod_phase(self, li):
        kb, NB = self.kb, self.NB
        R = NB + 1
        with self.phase():
            cT = self.sb("cT", [128, 8, R])
            sT = self.sb("sT", [128, 8, R])
            ones = self.sb("ones", [1, 4])
            brow = self.sb("brow", [1, 6 * D])
            mrow = self.sb("mrow", [R, 6 * D])
            g12 = self.sb("g12", [R, 2, D])
            pm = [self.ps("pm%d" % i, [R, 512]) for i in range(2)]
            wm = [self.sb("wm%d" % i, [128, 8, 512]) for i in range(2)]
            with self.nc.allow_non_contiguous_dma(reason="tiny transposed load of conditioning vectors"):
                for r in range(R):
                    kb.dma('sp', cT[:, :, r], self.cvec[r, :].rearrange("(k p) -> p k", p=128), writes=['cT'])
            kb.op('act', lambda e: e.activation(out=sT[:], in_=cT[:], func=AF.Silu), reads=['cT'], writes=['sT'])
            kb.op('dve', lambda e: e.memset(ones[:], 1.0), writes=['ones'])
            kb.dma('sp', brow[:], self.b_mod[li:li + 1, :], writes=['brow'])
            kb.dma('sp', g12[:, 0, :], self.norm1_g[li:li + 1, :].to_broadcast([R, D]), writes=['g12'])
            kb.dma('sp', g12[:, 1, :], self.norm2_g[li:li + 1, :].to_broadcast([R, D]), writes=['g12'])
            for n in range(12):
                w = wm[n % 2]
                wk = 'wm%d' % (n % 2)
                pk = 'pm%d' % (n % 2)
                kb.dma('sp', w[:], self.w_mod[li, :, n * 512:(n + 1) * 512].rearrange("(k p) c -> p k c", p=128),
                       writes=[wk])
                for k in range(8):
                    self.mm(pm[n % 2][:], sT[:, k, :], w[:, k, :], k == 0, False, ['sT', wk], [pk])
                self.mm(pm[n % 2][:], ones[0:1, 0:R], brow[0:1, n * 512:(n + 1) * 512], False, True,
                        ['ones', 'brow'], [pk])
                kb.op('act', lambda e: e.copy(out=mrow[:, n * 512:(n + 1) * 512], in_=pm[n % 2][:]),
                      reads=[pk], writes=['mrow'])
            for (gi, sc) in ((0, 1), (1, 4)):
                kb.op('dve', lambda e: e.scalar_tensor_tensor(
                    out=mrow[:, sc * D:(sc + 1) * D], in0=mrow[:, sc * D:(sc + 1) * D], scalar=1.0,
                    in1=g12[:, gi, :], op0=ALU.add, op1=ALU.mult), reads=['mrow', 'g12'], writes=['mrow'])
            kb.dma('pool', self.modv[li].rearrange("r s d -> r (s d)"), mrow[:], reads=['mrow'], writes=['modv'])

    def load_bc(self, t, li, r, s, key):
        self.kb.dma('sp', t[:], self.modv[li, r, s:s + 1, :].to_broadcast([128, D]), reads=['modv'], writes=[key])

    def norm_phase(self, li, which, layer0, ctx_needed=True):
        kb, NB = self.kb, self.NB
        s_sh, s_g = (0, 1) if which == 1 else (3, 4)
        with self.phase():
            ident = self.sb("identb", [128, 128], BF16)
            kb.dma('pool', ident[:], self.c_ident, writes=['ident'])
            Gt = [self.sb("G%d" % r, [128, D]) for r in range(NB + 1)]
            St = [self.sb("S%d" % r, [128, D]) for r in range(NB + 1)]
            for r in range(NB + 1):
                self.load_bc(Gt[r], li, r, s_g, 'G%d' % r)
                self.load_bc(St[r], li, r, s_sh, 'S%d' % r)
            NBUF = 3
            xt = [self.sb("xt%d" % i, [128, D]) for i in range(NBUF)]
            sq = self.sb("sq", [128, D])
            st = [self.sb("st%d" % i, [128, 4]) for i in range(NBUF)]
            hb = [self.sb("hb%d" % i, [128, D], BF16) for i in range(NBUF)]
            pt = [self.ps("pt%d" % i, [128, 8, 128], BF16) for i in range(2)]
            hw = [self.sb("hw%d" % i, [128, 8, 256], BF16) for i in range(2)]
            hv = self.hT_view()
            it = 0
            wi = 0
            for b in range(NB):
                for w in range(NT // 2):
                    if w == 0 and not ctx_needed:
                        continue
                    hwk = 'hw%d' % (wi % 2)
                    for t2 in range(2):
                        j = 2 * w + t2
                        r = NB if j < NTC else b
                        i = it % NBUF
                        src, skey = self.src_x(layer0, b, j)
                        kb.dma('sp', xt[i][:], src, reads=[skey], writes=['xt%d' % i])
                        kb.op('act', lambda e: e.activation(out=sq[:], in_=xt[i][:], func=AF.Square,
                                                            accum_out=st[i][:, 0:1]),
                              reads=['xt%d' % i], writes=['sq', 'st%d' % i])
                        kb.op('dve', lambda e: e.tensor_scalar(out=st[i][:, 1:2], in0=st[i][:, 0:1], scalar1=1.0 / D,
                                                               scalar2=EPS, op0=ALU.mult, op1=ALU.add),
                              reads=['st%d' % i], writes=['st%d' % i])
                        kb.op('act', lambda e: e.activation(out=st[i][:, 2:3], in_=st[i][:, 1:2], func=AF.Sqrt),
                              reads=['st%d' % i], writes=['st%d' % i])
                        kb.op('dve', lambda e: e.reciprocal(out=st[i][:, 3:4], in_=st[i][:, 2:3]),
                              reads=['st%d' % i], writes=['st%d' % i])
                        kb.op('dve', lambda e: e.scalar_tensor_tensor(out=xt[i][:], in0=xt[i][:], scalar=st[i][:, 3:4],
                                                                      in1=Gt[r][:], op0=ALU.mult, op1=ALU.mult),
                              reads=['xt%d' % i, 'st%d' % i, 'G%d' % r], writes=['xt%d' % i])
                        kb.op('pool', lambda e: e.tensor_tensor(out=hb[i][:], in0=xt[i][:], in1=St[r][:], op=ALU.add),
                              reads=['xt%d' % i, 'S%d' % r], writes=['hb%d' % i])
                        p = pt[it % 2]
                        pk = 'pt%d' % (it % 2)
                        for k in range(8):
                            self.tr(p[:, k, :], hb[i][:, k * 128:(k + 1) * 128], ident[:], ['hb%d' % i, 'ident'], [pk])
                        kb.op('act', lambda e: e.copy(out=hw[wi % 2][:, :, t2 * 128:(t2 + 1) * 128], in_=p[:]),
                              reads=[pk], writes=[hwk])
                        it += 1
                    c0 = self.hcol(b, 2 * w)
                    kb.dma('pool', hv[:, :, c0:c0 + 256], hw[wi % 2][:], reads=[hwk], writes=['hTs_%d' % wi])
                    wi += 1

    def outproj_setup(self, w_out_ap, li):
        kb, NB = self.kb, self.NB
        o = {}
        o['w'] = self.sb("wout", [128, 8, D], BF16)
        kb.dma('pool', o['w'][:], w_out_ap.rearrange("(k p) c -> p k c", p=128), writes=['wout'])
        o['ident'] = self.sb("identb", [128, 128], BF16)
        kb.dma('pool', o['ident'][:], self.c_ident, writes=['identb'])
        o['M'] = [self.sb("M2_%d" % r, [128, D]) for r in range(NB + 1)]
        for r in range(NB + 1):
            self.load_bc(o['M'][r], li, r, 2, 'M2_%d' % r)
        o['pt'] = self.ps("opt", [128, 8, 128], BF16)
        o['py'] = self.ps("opy", [128, 2, 512])
        o['oT'] = self.sb("oT", [128, 8, 128], BF16)
        o['xt'] = [self.sb("oxt%d" % i, [128, D]) for i in range(2)]
        o['n'] = 0
        return o

    def outproj_tile(self, o, ob, obkey, layer0, b, j):
        kb = self.kb
        r = self.NB if j < NTC else b
        for k in range(8):
            self.tr(o['pt'][:, k, :], ob[:, k * 128:(k + 1) * 128], o['ident'][:], [obkey, 'identb'], ['opt'])
        kb.op('act', lambda e: e.copy(out=o['oT'][:], in_=o['pt'][:]), reads=['opt'], writes=['oT'])
        for n in range(2):
            for k in range(8):
                self.mm(o['py'][:, n, :], o['oT'][:, k, :], o['w'][:, k, n * 512:(n + 1) * 512], k == 0, k == 7,
                        ['oT', 'wout'], ['opy'])
        i = o['n'] % 2
        o['n'] += 1
        xt = o['xt'][i]
        xk = 'oxt%d' % i
        src, skey = self.src_x(layer0, b, j)
        kb.dma('sp', xt[:], src, reads=[skey], writes=[xk])
        yk = 'oy%d' % i
        kb.op('dve', lambda e: e.tensor_tensor(out=o['py'][:].rearrange("p a b -> p (a b)"),
                                               in0=o['py'][:].rearrange("p a b -> p (a b)"),
                                               in1=o['M'][r][:], op=ALU.mult),
              reads=['opy', 'M2_%d' % r], writes=['opy'])
        kb.op('dve', lambda e: e.tensor_tensor(out=xt[:], in0=o['py'][:].rearrange("p a b -> p (a b)"), in1=xt[:],
                                               op=ALU.add),
              reads=['opy', xk], writes=[xk])
        kb.dma('pool', self.xs[b, j * 128:(j + 1) * 128, :], xt[:], reads=[xk], writes=['xs_%d_%d' % (b, j)])

    def ffn_phase(self, li, ctx_needed=True):
        kb, NB = self.kb, self.NB
        NF = FFN // 128
        HF = NF // 2
        hv = self.hT_view()
        for ps_ in range(2):
            with self.phase():
                f0 = ps_ * HF
                wv = self.sb("wv", [128, 8, HF * 128], BF16)
                wg = self.sb("wg", [128, 8, HF * 128], BF16)
                wo = self.sb("wo", [128, HF, D], BF16)
                win = self.ffn_w_in[li].rearrange("(k p) c -> p k c", p=128)
                for k in range(8):
                    kb.dma('pool', wv[:, k, :], win[:, k, f0 * 128:(f0 + HF) * 128], writes=['wv'])
                    kb.dma('pool', wg[:, k, :], win[:, k, FFN + f0 * 128:FFN + (f0 + HF) * 128], writes=['wg'])
                kb.dma('pool', wo[:], self.ffn_w_out[li, f0 * 128:(f0 + HF) * 128, :].rearrange("(f p) c -> p f c", p=128),
                       writes=['wo'])
                cw = self.sb("cw", [128, HF, 4])
                with self.nc.allow_non_contiguous_dma(reason="tiny per-channel conv taps"):
                    for t in range(3):
                        kb.dma('sp', cw[:, :, t], self.ffn_conv_w[li, t, f0 * 128:(f0 + HF) * 128].rearrange("(f p) -> p f", p=128),
                               writes=['cw'])
                    kb.dma('sp', cw[:, :, 3], self.ffn_conv_b[li, f0 * 128:(f0 + HF) * 128].rearrange("(f p) -> p f", p=128),
                           writes=['cw'])
                M5 = [self.sb("M5_%d" % r, [128, D]) for r in range(NB + 1)]
                for r in range(NB + 1):
                    self.load_bc(M5[r], li, r, 5, 'M5_%d' % r)
                hT = [self.sb("fh%d" % i, [128, 8, 256 + 2 * HALO], BF16) for i in range(2)]
                u = [self.sb("fu%d" % i, [128, HF, 256], BF16) for i in range(2)]
                tA = [self.sb("ftA%d" % i, [128, 256]) for i in range(2)]
                tB = [self.sb("ftB%d" % i, [128, 256]) for i in range(2)]
                pv = [self.ps("fpv%d" % i, [128, 512]) for i in range(2)]
                pg = [self.ps("fpg%d" % i, [128, 512]) for i in range(2)]
                py = [self.ps("fpy%d" % i, [128, 2, 512]) for i in range(2)]
                xt = [self.sb("fx%d" % i, [128, D]) for i in range(2)]
                wi = 0
                ci = 0
                ti = 0
                for b in range(NB):
                    for w in range(NT // 2):
                        if w == 0 and not ctx_needed:
                            continue
                        h = hT[wi % 2]
                        hk = 'fh%d' % (wi % 2)
                        uu = u[wi % 2]
                        uk = 'fu%d' % (wi % 2)
                        c0 = self.hcol(b, 2 * w)
                        kb.dma('sp', h[:], hv[:, :, c0 - HALO:c0 + 256 + HALO], reads=['hTs'], writes=[hk])
                        for f in range(HF):
                            a = ci % 2
                            ci += 1
                            for k in range(8):
                                self.mm(pv[a][:, 0:256], wv[:, k, f * 128:(f + 1) * 128], h[:, k, HALO:HALO + 256],
                                        k == 0, k == 7, ['wv', hk], ['fpv%d' % a])
                            for k in range(8):
                                self.mm(pg[a][:, 0:258], wg[:, k, f * 128:(f + 1) * 128], h[:, k, HALO - 1:HALO + 257],
                                        k == 0, k == 7, ['wg', hk], ['fpg%d' % a])
                            kb.op('dve', lambda e: e.tensor_scalar(out=tA[a][:], in0=pg[a][:, 0:256], scalar1=cw[:, f, 0:1],
                                                                   scalar2=None, op0=ALU.mult),
                                  reads=['fpg%d' % a, 'cw'], writes=['ftA%d' % a])
                            kb.op('dve', lambda e: e.scalar_tensor_tensor(out=tA[a][:], in0=pg[a][:, 1:257], scalar=cw[:, f, 1:2],
                                                                          in1=tA[a][:], op0=ALU.mult, op1=ALU.add),
                                  reads=['fpg%d' % a, 'cw', 'ftA%d' % a], writes=['ftA%d' % a])
                            kb.op('dve', lambda e: e.scalar_tensor_tensor(out=tA[a][:], in0=pg[a][:, 2:258], scalar=cw[:, f, 2:3],
                                                                          in1=tA[a][:], op0=ALU.mult, op1=ALU.add),
                                  reads=['fpg%d' % a, 'cw', 'ftA%d' % a], writes=['ftA%d' % a])
                            kb.op('act', lambda e: e.activation(out=tB[a][:], in_=tA[a][:], func=AF.Silu, bias=cw[:, f, 3:4]),
                                  reads=['ftA%d' % a, 'cw'], writes=['ftB%d' % a])
                            kb.op('dve', lambda e: e.tensor_tensor(out=uu[:, f, :], in0=pv[a][:, 0:256], in1=tB[a][:], op=ALU.mult),
                                  reads=['fpv%d' % a, 'ftB%d' % a], writes=[uk])
                        for t2 in range(2):
                            j = 2 * w + t2
                            r = NB if j < NTC else b
                            i = ti % 2
                            ti += 1
                            for n in range(2):
                                for f in range(HF):
                                    self.mm(py[i][:, n, :], uu[:, f, t2 * 128:(t2 + 1) * 128], wo[:, f, n * 512:(n + 1) * 512],
                                            f == 0, f == HF - 1, [uk, 'wo'], ['fpy%d' % i])
                            kb.dma('sp', xt[i][:], self.xs[b, j * 128:(j + 1) * 128, :], reads=['xs_%d_%d' % (b, j)], writes=['fx%d' % i])
                            pyf = py[i][:].rearrange("p a b -> p (a b)")
                            kb.op('dve', lambda e: e.tensor_tensor(out=pyf, in0=pyf, in1=M5[r][:], op=ALU.mult),
                                  reads=['fpy%d' % i, 'M5_%d' % r], writes=['fpy%d' % i])
                            kb.op('dve', lambda e: e.tensor_tensor(out=xt[i][:], in0=pyf, in1=xt[i][:], op=ALU.add),
                                  reads=['fpy%d' % i, 'fx%d' % i], writes=['fx%d' % i])
                            kb.dma('pool', self.xs[b, j * 128:(j + 1) * 128, :], xt[i][:], reads=['fx%d' % i],
                                   writes=['xs_%d_%d' % (b, j)])
                        wi += 1

    def final_phase(self):
        kb, NB = self.kb, self.NB
        with self.phase():
            G = self.sb("fG", [128, D])
            kb.dma('sp', G[:], self.final_norm_g[0:1, :].to_broadcast([128, D]), writes=['fG'])
            xt = [self.sb("fx%d" % i, [128, D]) for i in range(3)]
            sq = self.sb("fsq", [128, D])
            st = [self.sb("fst%d" % i, [128, 4]) for i in range(3)]
            it = 0
            for b in range(NB):
                for j in range(NTC, NT):
                    i = it % 3
                    it += 1
                    kb.dma('sp', xt[i][:], self.xs[b, j * 128:(j + 1) * 128, :], reads=['xs_%d_%d' % (b, j)], writes=['fx%d' % i])
                    kb.op('act', lambda e: e.activation(out=sq[:], in_=xt[i][:], func=AF.Square, accum_out=st[i][:, 0:1]),
                          reads=['fx%d' % i], writes=['fsq', 'fst%d' % i])
                    kb.op('dve', lambda e: e.tensor_scalar(out=st[i][:, 1:2], in0=st[i][:, 0:1], scalar1=1.0 / D,
                                                           scalar2=EPS, op0=ALU.mult, op1=ALU.add),
                          reads=['fst%d' % i], writes=['fst%d' % i])
                    kb.op('act', lambda e: e.activation(out=st[i][:, 2:3], in_=st[i][:, 1:2], func=AF.Sqrt),
                          reads=['fst%d' % i], writes=['fst%d' % i])
                    kb.op('dve', lambda e: e.reciprocal(out=st[i][:, 3:4], in_=st[i][:, 2:3]),
                          reads=['fst%d' % i], writes=['fst%d' % i])
                    kb.op('dve', lambda e: e.scalar_tensor_tensor(out=xt[i][:], in0=xt[i][:], scalar=st[i][:, 3:4],
                                                                  in1=G[:], op0=ALU.mult, op1=ALU.mult),
                          reads=['fx%d' % i, 'fst%d' % i, 'fG'], writes=['fx%d' % i])
                    kb.dma('pool', self.out[b, (j - NTC) * 128:(j - NTC + 1) * 128, :], xt[i][:], reads=['fx%d' % i],
                           writes=['out_%d_%d' % (b, j)])

    def attn_phase(self, li, jx, layer0, want_ctx):
        kb, NB = self.kb, self.NB
        hv = self.hT_view()
        SC = 128 ** -0.5
        for b in range(NB):
            with self.phase():
                qT = self.sb("qT", [128, 8, TOK], BF16)
                kT = self.sb("kT", [128, 2, TOK], BF16)
                V1 = self.sb("V1", [128, NT, 2, 132], BF16)
                kb.op('pool', lambda e: e.memset(V1[:, :, :, 128:129], 1.0), writes=['V1'])
                ident = self.sb("identb", [128, 128], BF16)
                kb.dma('pool', ident[:], self.c_ident, writes=['ident'])
                with self.phase():
                    win = self.sb("awin", [128, 8, 1536], BF16)
                    kb.dma('pool', win[:], self.attn_w_in[jx].rearrange("(k p) c -> p k c", p=128), writes=['awin'])
                    Gqk = self.sb("Gqk", [128, 2, 128])
                    kb.dma('sp', Gqk[:, 0, :], self.attn_q_norm_g[jx:jx + 1, :].to_broadcast([128, 128]), writes=['Gqk'])
                    kb.dma('sp', Gqk[:, 1, :], self.attn_k_norm_g[jx:jx + 1, :].to_broadcast([128, 128]), writes=['Gqk'])
                    hT = [self.sb("ah%d" % i, [128, 8, 128], BF16) for i in range(2)]
                    pz = [self.ps("apz%d" % i, [128, 512]) for i in range(3)]
                    zs = self.sb("azs", [128, 1536])
                    sq = self.sb("asq", [128, 1280])
                    st = self.sb("ast", [128, 4, 10])
                    qn = self.sb("aqn", [128, 1280])
                    qb = self.sb("aqb", [128, 1280], BF16)
                    rp = [self.sb("arp%d" % i, [128, 128]) for i in range(2)]
                    t1 = self.sb("at1", [128, 640])
                    t2 = self.sb("at2", [128, 640])
                    ptq = self.ps("aptq", [128, 8, 128], BF16)
                    ptk = self.ps("aptk", [128, 2, 128], BF16)
                    for j in range(NT):
                        i = j % 2
                        c0 = self.hcol(b, j)
                        kb.dma('sp', hT[i][:], hv[:, :, c0:c0 + 128], reads=['hTs'], writes=['ah%d' % i])
                        for n in range(3):
                            for k in range(8):
                                self.mm(pz[n][:], hT[i][:, k, :], win[:, k, n * 512:(n + 1) * 512], k == 0, k == 7,
                                        ['ah%d' % i, 'awin'], ['apz%d' % n])
                            kb.op('act', lambda e: e.copy(out=zs[:, n * 512:(n + 1) * 512], in_=pz[n][:]),
                                  reads=['apz%d' % n], writes=['azs'])
                        kb.op('pool', lambda e: e.tensor_copy(out=V1[:, j, :, 0:128],
                                                              in_=zs[:, 1280:1536].rearrange("p (g d) -> p g d", g=2)),
                              reads=['azs'], writes=['V1'])
                        kb.op('dve', lambda e: e.tensor_tensor(out=sq[:], in0=zs[:, 0:1280], in1=zs[:, 0:1280], op=ALU.mult),
                              reads=['azs'], writes=['asq'])
                        kb.op('dve', lambda e: e.tensor_reduce(out=st[:, 0, :], in_=sq[:].rearrange("p (h d) -> p h d", h=10),
                                                               axis=AX.X, op=ALU.add),
                              reads=['asq'], writes=['ast'])
                        kb.op('dve', lambda e: e.tensor_scalar(out=st[:, 1, :], in0=st[:, 0, :], scalar1=1.0 / 128, scalar2=EPS,
                                                               op0=ALU.mult, op1=ALU.add), reads=['ast'], writes=['ast'])
                        kb.op('act', lambda e: e.activation(out=st[:, 2, :], in_=st[:, 1, :], func=AF.Sqrt),
                              reads=['ast'], writes=['ast'])
                        kb.op('dve', lambda e: e.reciprocal(out=st[:, 3, :], in_=st[:, 2, :]), reads=['ast'], writes=['ast'])
                        z3 = zs[:, 0:1280].rearrange("p (h d) -> p h d", h=10)
                        q3 = qn[:].rearrange("p (h d) -> p h d", h=10)
                        kb.op('dve', lambda e: e.tensor_tensor(out=q3, in0=z3, in1=st[:, 3, :].unsqueeze(2).to_broadcast([128, 10, 128]),
                                                               op=ALU.mult), reads=['azs', 'ast'], writes=['aqn'])
                        kb.op('dve', lambda e: e.tensor_tensor(out=q3[:, 0:8, :], in0=q3[:, 0:8, :],
                                                               in1=Gqk[:, 0:1, :].to_broadcast([128, 8, 128]), op=ALU.mult),
                              reads=['aqn', 'Gqk'], writes=['aqn'])
                        kb.op('dve', lambda e: e.tensor_tensor(out=q3[:, 8:10, :], in0=q3[:, 8:10, :],
                                                               in1=Gqk[:, 1:2, :].to_broadcast([128, 2, 128]), op=ALU.mult),
                              reads=['aqn', 'Gqk'], writes=['aqn'])
                        if j >= NTC:
                            rr = rp[j % 2]
                            rk = 'arp%d' % (j % 2)
                            kb.dma('sp', rr[:], self.c_rope[(j - NTC) * 128:(j - NTC + 1) * 128, :], writes=[rk])
                            q4 = qn[:].rearrange("p (h d t) -> p h d t", h=10, t=2)
                            b4 = qb[:].rearrange("p (h d t) -> p h d t", h=10, t=2)
                            x0, x1 = q4[:, :, :, 0], q4[:, :, :, 1]
                            cosb = rr[:, 0:64].unsqueeze(1).to_broadcast([128, 10, 64])
                            sinb = rr[:, 64:128].unsqueeze(1).to_broadcast([128, 10, 64])
                            t13 = t1[:].rearrange("p (h d) -> p h d", h=10)
                            t23 = t2[:].rearrange("p (h d) -> p h d", h=10)
                            kb.op('dve', lambda e: e.tensor_tensor(out=t13, in0=x0, in1=cosb, op=ALU.mult), reads=['aqn', rk], writes=['at1'])
                            kb.op('pool', lambda e: e.tensor_tensor(out=t23, in0=x1, in1=sinb, op=ALU.mult), reads=['aqn', rk], writes=['at2'])
                            kb.op('dve', lambda e: e.tensor_tensor(out=b4[:, :, :, 0], in0=t13, in1=t23, op=ALU.subtract),
                                  reads=['at1', 'at2'], writes=['aqb'])
                            kb.op('dve', lambda e: e.tensor_tensor(out=t13, in0=x0, in1=sinb, op=ALU.mult), reads=['aqn', rk, 'aqb'], writes=['at1'])
                            kb.op('pool', lambda e: e.tensor_tensor(out=t23, in0=x1, in1=cosb, op=ALU.mult), reads=['aqn', rk, 'aqb'], writes=['at2'])
                            kb.op('dve', lambda e: e.tensor_tensor(out=b4[:, :, :, 1], in0=t13, in1=t23, op=ALU.add),
                                  reads=['at1', 'at2'], writes=['aqb'])
                        else:
                            kb.op('dve', lambda e: e.tensor_copy(out=qb[:], in_=qn[:]), reads=['aqn'], writes=['aqb'])
                        for h in range(8):
                            self.tr(ptq[:, h, :], qb[:, h * 128:(h + 1) * 128], ident[:], ['aqb', 'ident'], ['aptq'])
                        for g in range(2):
                            self.tr(ptk[:, g, :], qb[:, (8 + g) * 128:(9 + g) * 128], ident[:], ['aqb', 'ident'], ['aptk'])
                        kb.op('act', lambda e: e.copy(out=qT[:, :, j * 128:(j + 1) * 128], in_=ptq[:]), reads=['aptq'], writes=['qT'])
                        kb.op('act', lambda e: e.copy(out=kT[:, :, j * 128:(j + 1) * 128], in_=ptk[:]), reads=['aptk'], writes=['kT'])
                with self.phase():
                    o = self.outproj_setup(self.attn_w_out[jx], li)
                    E = self.sb("aE", [128, NT, 512], BF16)
                    pS = [self.ps("apS%d" % i, [128, 512]) for i in range(2)]
                    pO = [self.ps("apO%d" % i, [128, 512]) for i in range(2)]
                    ob = [self.sb("aob%d" % i, [128, D], BF16) for i in range(2)]
                    rc = self.sb("arc", [128, 8])
                    si = 0
                    oi = 0
                    for jq in range(NT):
                        if jq < NTC and not want_ctx:
                            continue
                        nk = NTC if jq < NTC else NT
                        obt = ob[jq % 2]
                        obk = 'aob%d' % (jq % 2)
                        for g in range(2):
                            for kt in range(nk):
                                p = pS[si % 2]
                                pk = 'apS%d' % (si % 2)
                                si += 1
                                self.mm(p[:].rearrange("p (h q) -> p h q", h=4), kT[:, g, kt * 128:(kt + 1) * 128], qT[:, 4 * g:4 * g + 4, jq * 128:(jq + 1) * 128],
                                        True, True, ['kT', 'qT'], [pk])
                                kb.op('act', lambda e: e.activation(out=E[:, kt, :], in_=p[:], func=AF.Exp, scale=SC),
                                      reads=[pk], writes=['aE'])
                            for hh in range(4):
                                po = pO[oi % 2]
                                pok = 'apO%d' % (oi % 2)
                                oi += 1
                                for kt in range(nk):
                                    self.mm(po[:, 0:129], E[:, kt, hh * 128:(hh + 1) * 128], V1[:, kt, g, 0:129],
                                            kt == 0, kt == nk - 1, ['aE', 'V1'], [pok])
                                hd = 4 * g + hh
                                kb.op('dve', lambda e: e.reciprocal(out=rc[:, hd:hd + 1], in_=po[:, 128:129]),
                                      reads=[pok], writes=['arc'])
                                kb.op('dve', lambda e: e.tensor_scalar(out=obt[:, hd * 128:(hd + 1) * 128], in0=po[:, 0:128],
                                                                       scalar1=rc[:, hd:hd + 1], scalar2=None, op0=ALU.mult),
                                      reads=[pok, 'arc'], writes=[obk])
                        self.outproj_tile(o, obt, obk, layer0, b, jq)

    def gdn_decl(self):
        if hasattr(self, 'g_qkT'):
            return
        NB = self.NB
        self.g_qkT = self.dram("g_qkT", [2, D, NB * TOK], F32)
        self.g_ktok = self.dram("g_ktok", [NB * TOK, D], F32)
        self.g_vtok = self.dram("g_vtok", [NB * TOK, D], F32)
        self.g_sgate = self.dram("g_sgate", [NB * TOK, D], F32)
        self.g_bg = self.dram("g_bg", [NB * TOK, 32], F32)
        self.g_odir = self.dram("g_odir", [2, NB * TOK, D], F32)

    def gdn_phase(self, li, jx, layer0, want_ctx):
        self.gdn_decl()
        self.gdn_proj(li, jx)
        self.gdn_scan(li, jx)
        self.gdn_finish(li, jx, layer0, want_ctx)

    def gdn_proj(self, li, jx):
        kb, NB = self.kb, self.NB
        hv = self.hT_view()
        with self.phase():
            win = self.sb("gwin", [128, 8, 4128], BF16)
            wsrc = self.gdn_w_in[jx].rearrange("(k p) c -> p k c", p=128)
            for k in range(8):
                kb.dma('pool', win[:, k, :], wsrc[:, k, :], writes=['gwin'])
            cw = self.sb("gcw", [128, 24, 5])
            with self.nc.allow_non_contiguous_dma(reason="tiny per-channel conv taps"):
                for t in range(5):
                    for f0 in range(0, 24, 8):
                        kb.dma('sp', cw[:, f0:f0 + 8, t], self.gdn_conv_w[jx, t, f0 * 128:(f0 + 8) * 128].rearrange("(f p) -> p f", p=128),
                               writes=['gcw'])
            identf = self.sb("identf", [128, 128])
            kb.dma('sp', identf[:], self.c_ident, writes=['identf'])
            onesf = self.sb("onesf", [128, 128])
            kb.op('dve', lambda e: e.memset(onesf[:], 1.0), writes=['onesf'])
            DTB = self.sb("gdtb", [128, 16])
            NA = self.sb("gna", [128, 16])
            kb.dma('sp', DTB[:], self.gdn_dt_bias[jx:jx + 1, :].to_broadcast([128, 16]), writes=['gdtb'])
            kb.dma('sp', NA[:], self.gdn_a_log[jx:jx + 1, :].to_broadcast([128, 16]), writes=['gna'])
            kb.op('act', lambda e: e.activation(out=NA[:], in_=NA[:], func=AF.Exp), reads=['gna'], writes=['gna'])
            kb.op('dve', lambda e: e.tensor_scalar(out=NA[:], in0=NA[:], scalar1=-1.0, scalar2=None, op0=ALU.mult),
                  reads=['gna'], writes=['gna'])
            h2 = [self.sb("gh%d" % i, [128, 8, 256 + 2 * HALO], BF16) for i in range(2)]
            pz = [self.ps("gpz%d" % i, [128, 512]) for i in range(2)]
            pn = self.ps("gpn", [128, 512])
            pT = self.ps("gpT", [128, 2, 128])
            pg = self.ps("gpg", [128, 2, 512])
            pb = self.ps("gpb", [128, 32])
            tA = [self.sb("gtA%d" % i, [128, 256]) for i in range(2)]
            sS = [self.sb("gsS%d" % i, [128, 256]) for i in range(2)]
            sq = self.sb("gsq", [128, 256])
            rs = self.sb("grs", [128, 256])
            stg = [self.sb("gstg%d" % i, [128, 256]) for i in range(2)]
            kst = self.sb("gkst", [128, 2, D])
            vst = self.sb("gvst", [128, 2, D])
            gst = [self.sb("ggst%d" % i, [128, D]) for i in range(2)]
            bgs = self.sb("gbgs", [128, 32])
            bt = self.sb("gbt", [128, 4, 16])
            qkTv = self.g_qkT.rearrange("q (h p) c -> q p h c", p=128)
            ci = 0
            wi = 0
            for b in range(NB):
                for w in range(NT // 2):
                    h = h2[wi % 2]
                    hk = 'gh%d' % (wi % 2)
                    wi += 1
                    c0 = self.hcol(b, 2 * w)
                    tok0 = b * TOK + 2 * w * 128
                    kb.dma('sp', h[:], hv[:, :, c0 - HALO:c0 + 256 + HALO], writes=[hk])
                    for f in range(24):
                        a = ci % 2
                        ci += 1
                        pzk = 'gpz%d' % a
                        for k in range(8):
                            self.mm(pz[a][:, 0:260], win[:, k, f * 128:(f + 1) * 128], h[:, k, :], k == 0, k == 7,
                                    ['gwin', hk], [pzk])
                        tk = 'gtA%d' % a
                        kb.op('dve', lambda e: e.tensor_scalar(out=tA[a][:], in0=pz[a][:, 0:256], scalar1=cw[:, f, 0:1],
                                                               scalar2=None, op0=ALU.mult), reads=[pzk, 'gcw'], writes=[tk])
                        for t in range(1, 5):
                            kb.op('dve', lambda e: e.scalar_tensor_tensor(out=tA[a][:], in0=pz[a][:, t:t + 256], scalar=cw[:, f, t:t + 1],
                                                                          in1=tA[a][:], op0=ALU.mult, op1=ALU.add),
                                  reads=[pzk, 'gcw', tk], writes=[tk])
                        sk = 'gsS%d' % a
                        kb.op('act', lambda e: e.activation(out=sS[a][:], in_=tA[a][:], func=AF.Silu), reads=[tk], writes=[sk])
                        hh = f % 8
                        if f < 16:
                            kb.op('pool', lambda e: e.tensor_tensor(out=sq[:], in0=sS[a][:], in1=sS[a][:], op=ALU.mult),
                                  reads=[sk], writes=['gsq'])
                            self.mm(pn[:, 0:256], onesf[:], sq[:], True, True, ['onesf', 'gsq'], ['gpn'])
                            kb.op('dve', lambda e: e.tensor_scalar(out=rs[:], in0=pn[:, 0:256], scalar1=EPS, scalar2=None, op0=ALU.add),
                                  reads=['gpn'], writes=['grs'])
                            kb.op('act', lambda e: e.activation(out=rs[:], in_=rs[:], func=AF.Sqrt), reads=['grs'], writes=['grs'])
                            kb.op('dve', lambda e: e.reciprocal(out=rs[:], in_=rs[:]), reads=['grs'], writes=['grs'])
                            sc = (128 ** -0.5) if f < 8 else 1.0
                            gk = 'gstg%d' % a
                            kb.op('dve', lambda e: e.scalar_tensor_tensor(out=stg[a][:], in0=sS[a][:], scalar=sc, in1=rs[:],
                                                                          op0=ALU.mult, op1=ALU.mult), reads=[sk, 'grs'], writes=[gk])
                            kb.dma('pool', qkTv[f // 8, :, hh, tok0:tok0 + 256], stg[a][:], reads=[gk], writes=['g_qkT_%d' % ci])
                            src = stg[a]
                            srck = gk
                        else:
                            src = sS[a]
                            srck = sk
                        if f >= 8:
                            dst = kst if f < 16 else vst
                            dk = 'gkst' if f < 16 else 'gvst'
                            for t2 in range(2):
                                self.tr(pT[:, t2, :], src[:, t2 * 128:(t2 + 1) * 128], identf[:], [srck, 'identf'], ['gpT'])
                            kb.op('act', lambda e: e.copy(out=dst[:, :, hh * 128:(hh + 1) * 128], in_=pT[:]), reads=['gpT'], writes=[dk])
                    for t2 in range(2):
                        r0 = tok0 + t2 * 128
                        kb.dma('pool', self.g_ktok[r0:r0 + 128, :], kst[:, t2, :], reads=['gkst'], writes=['g_ktok_%d' % r0])
                        kb.dma('pool', self.g_vtok[r0:r0 + 128, :], vst[:, t2, :], reads=['gvst'], writes=['g_vtok_%d' % r0])
                        hs = h[:, :, HALO + t2 * 128:HALO + (t2 + 1) * 128]
                        for n in range(2):
                            for k in range(8):
                                self.mm(pg[:, n, :], hs[:, k, :], win[:, k, 3072 + n * 512:3072 + (n + 1) * 512], k == 0, k == 7,
                                        [hk, 'gwin'], ['gpg'])
                        g_ = gst[t2]
                        gk2 = 'ggst%d' % t2
                        kb.op('act', lambda e: e.activation(out=g_[:], in_=pg[:].rearrange("p a b -> p (a b)"), func=AF.Silu),
                              reads=['gpg'], writes=[gk2])
                        kb.dma('pool', self.g_sgate[r0:r0 + 128, :], g_[:], reads=[gk2], writes=['g_sgate_%d' % r0])
                        for k in range(8):
                            self.mm(pb[:], hs[:, k, :], win[:, k, 4096:4128], k == 0, k == 7, [hk, 'gwin'], ['gpb'])
                        pb4 = pb[:].rearrange("p (d t h) -> p d t h", d=2, t=2)
                        kb.op('act', lambda e: e.activation(out=bgs[:, 0:16].rearrange("p (d h) -> p d h", d=2), in_=pb4[:, :, 0, :],
                                                            func=AF.Sigmoid), reads=['gpb'], writes=['gbgs'])
                        kb.op('dve', lambda e: e.tensor_tensor(out=bt[:, 0, :].rearrange("p (d h) -> p d h", d=2), in0=pb4[:, :, 1, :],
                                                               in1=DTB[:].rearrange("p (d h) -> p d h", d=2), op=ALU.add),
                              reads=['gpb', 'gdtb'], writes=['gbt'])
                        kb.op('dve', lambda e: e.scalar_tensor_tensor(out=bt[:, 1, :], in0=bt[:, 0, :], scalar=-1.0, in1=bt[:, 0, :],
                                                                      op0=ALU.mult, op1=ALU.max), reads=['gbt'], writes=['gbt'])
                        kb.op('act', lambda e: e.activation(out=bt[:, 2, :], in_=bt[:, 1, :], func=AF.Exp, scale=-1.0),
                              reads=['gbt'], writes=['gbt'])
                        kb.op('act', lambda e: e.activation(out=bt[:, 3, :], in_=bt[:, 2, :], func=AF.Ln, bias=1.0),
                              reads=['gbt'], writes=['gbt'])
                        kb.op('dve', lambda e: e.scalar_tensor_tensor(out=bt[:, 1, :], in0=bt[:, 0, :], scalar=0.0, in1=bt[:, 3, :],
                                                                      op0=ALU.max, op1=ALU.add), reads=['gbt'], writes=['gbt'])
                        kb.op('dve', lambda e: e.tensor_tensor(out=bgs[:, 16:32], in0=bt[:, 1, :], in1=NA[:], op=ALU.mult),
                              reads=['gbt', 'gna'], writes=['gbgs'])
                        kb.dma('pool', self.g_bg[r0:r0 + 128, :], bgs[:], reads=['gbgs'], writes=['g_bg_%d' % r0])

    def gdn_scan(self, li, jx):
        kb, NB = self.kb, self.NB
        HB = 4
        qkTv = self.g_qkT.rearrange("q (h p) c -> q p h c", p=128)
        order = [list(range(NT)), [1, 0] + list(range(NT - 1, NTC - 1, -1))]
        with self.phase():
            MS = self.sb("gmask", [128, 8, 128])
            kb.dma('sp', MS[:], self.c_masks[0:8].rearrange("m p c -> p m c"), writes=['gmask'])
            identf = self.sb("identf", [128, 128])
            kb.dma('sp', identf[:], self.c_ident, writes=['identf'])
            PS = [self.ps("gps%d" % i, [128, 512]) for i in range(8)]
            PK = ['gps%d' % i for i in range(8)]

            def v4(t):
                return t[:].rearrange("p (h c) -> p h c", h=HB)
            names = ['qT', 'kT', 'ktok', 'vtok', 'Gbc', 'Dm', 'E', 'DT', 'DTs', 'EG', 'qd', 'W', 'WT', 'Wa0', 'Wa1', 'Wb0', 'Wb1',
                     'QKm', 'kdec', 'wT', 'vn']
            sets = []
            for si in range(2):
                T = {}
                for nm in names:
                    T[nm] = self.sb("g%s%d" % (nm, si), [128, HB * 128])
                for nm in ('y0', 'y1', 'UW'):
                    T[nm] = self.sb("g%s%d" % (nm, si), [128, HB, 256])
                T['cols'] = self.sb("gcols%d" % si, [128, 6, HB])
                T['alast'] = self.sb("galast%d" % si, [128, HB, 2])
                T['k'] = 's%d_' % si
                sets.append(T)
            bgt = [self.sb("gbg%d" % i, [128, 32]) for i in range(4)]
            ost = [self.sb("gost%d" % i, [128, D]) for i in range(4)]
            S = [[self.sb("gS_%d_%d" % (d, hf), [128, HB, 128]) for hf in range(2)] for d in range(2)]
            stepi = 0
            bi = 0
            for b in range(NB):
                for d in range(2):
                    for hf in range(2):
                        kb.op('pool', lambda e: e.memset(S[d][hf][:], 0.0), writes=['gS_%d_%d' % (d, hf)])
                for n in range(NT):
                    for d in range(2):
                        j = order[d][n]
                        tok0 = b * TOK + j * 128
                        bg = bgt[bi % 4]
                        bgk = 'gbg%d' % (bi % 4)
                        os_ = ost[bi % 4]
                        osk = 'gost%d' % (bi % 4)
                        bi += 1
                        kb.dma('sp', bg[:], self.g_bg[tok0:tok0 + 128, :], writes=[bgk])
                        Ud, Usd, nUd, BO = MS[:, d, :], MS[:, 2 + d, :], MS[:, 4 + d, :], MS[:, 6, :]
                        for hf in range(2):
                            T = sets[stepi % 2]
                            K = lambda nm: T['k'] + nm
                            stepi += 1
                            h0 = hf * HB
                            Sk = 'gS_%d_%d' % (d, hf)
                            St = S[d][hf]
                            kb.dma('sp', v4(T['qT']), qkTv[0, :, h0:h0 + HB, tok0:tok0 + 128], writes=[K('qT')])
                            kb.dma('sp', v4(T['kT']), qkTv[1, :, h0:h0 + HB, tok0:tok0 + 128], writes=[K('kT')])
                            kb.dma('sp', T['ktok'][:], self.g_ktok[tok0:tok0 + 128, h0 * 128:(h0 + HB) * 128], writes=[K('ktok')])
                            kb.dma('sp', T['vtok'][:], self.g_vtok[tok0:tok0 + 128, h0 * 128:(h0 + HB) * 128], writes=[K('vtok')])
                            g4 = bg[:, 16 + d * 8 + h0:16 + d * 8 + h0 + HB]
                            be4 = bg[:, d * 8 + h0:d * 8 + h0 + HB]
                            bc4 = lambda ap: ap.unsqueeze(2).to_broadcast([128, HB, 128])
                            mk4 = lambda ap: ap.unsqueeze(1).to_broadcast([128, HB, 128])
                            kb.op('dve', lambda e: e.tensor_copy(out=v4(T['Gbc']), in_=bc4(g4)), reads=[bgk], writes=[K('Gbc')])
                            Gbc = v4(T['Gbc'])
                            for h in range(HB):
                                self.mm(PS[0][:, h * 128:(h + 1) * 128], Gbc[:, h, :], Ud, True, True, [K('Gbc'), 'gmask'], [PK[0]])
                            for h in range(HB):
                                self.mm(PS[1][:, h * 128:(h + 1) * 128], Gbc[:, h, :], Ud, True, False, [K('Gbc'), 'gmask'], [PK[1]])
                                self.mm(PS[1][:, h * 128:(h + 1) * 128], nUd, Gbc[:, h, :], False, True, [K('Gbc'), 'gmask'], [PK[1]])
                            self.mm(PS[5][:, 0:HB], Ud, g4, True, True, ['gmask', bgk], [PK[5]])
                            self.mm(PS[5][:, HB:2 * HB], BO, g4, True, True, ['gmask', bgk], [PK[5]])
                            for h in range(HB):
                                self.mm(PS[5][:, 8 + 2 * h:10 + 2 * h], Gbc[:, h, :], MS[:, 6, 0:128:64], True, True,
                                        [K('Gbc'), 'gmask'], [PK[5]])
                            cols = T['cols']
                            kb.op('act', lambda e: e.copy(out=cols[:, 0, :], in_=PS[5][:, 0:HB]), reads=[PK[5]], writes=[K('cols')])
                            kb.op('dve', lambda e: e.tensor_tensor(out=cols[:, 1, :], in0=PS[5][:, HB:2 * HB], in1=cols[:, 0, :], op=ALU.subtract),
                                  reads=[PK[5], K('cols')], writes=[K('cols')])
                            kb.op('act', lambda e: e.activation(out=cols[:, 2, :], in_=cols[:, 0, :], func=AF.Exp), reads=[K('cols')], writes=[K('cols')])
                            kb.op('act', lambda e: e.activation(out=cols[:, 3, :], in_=cols[:, 1, :], func=AF.Exp), reads=[K('cols')], writes=[K('cols')])
                            kb.op('act', lambda e: e.activation(out=T['alast'][:].rearrange("p h c -> p (h c)"), in_=PS[5][:, 8:8 + 2 * HB], func=AF.Exp),
                                  reads=[PK[5]], writes=[K('alast')])
                            kb.op('dve', lambda e: e.tensor_scalar(out=T['Dm'][:], in0=PS[1][:], scalar1=0.0, scalar2=None, op0=ALU.min),
                                  reads=[PK[1]], writes=[K('Dm')])
                            kb.op('act', lambda e: e.activation(out=T['E'][:], in_=T['Dm'][:], func=AF.Exp), reads=[K('Dm')], writes=[K('E')])
                            kb.op('dve', lambda e: e.tensor_tensor(out=v4(T['DT']), in0=v4(T['E']), in1=mk4(Ud), op=ALU.mult),
                                  reads=[K('E'), 'gmask'], writes=[K('DT')])
                            kb.op('pool', lambda e: e.tensor_tensor(out=v4(T['DTs']), in0=v4(T['E']), in1=mk4(Usd), op=ALU.mult),
                                  reads=[K('E'), 'gmask'], writes=[K('DTs')])
                            kb.op('act', lambda e: e.activation(out=T['EG'][:], in_=PS[0][:], func=AF.Exp), reads=[PK[0]], writes=[K('EG')])
                            kb.op('pool', lambda e: e.tensor_tensor(out=T['qd'][:], in0=T['qT'][:], in1=T['EG'][:], op=ALU.mult),
                                  reads=[K('qT'), K('EG')], writes=[K('qd')])
                            kT4, qT4 = v4(T['kT']), v4(T['qT'])
                            for h in range(HB):
                                self.mm(PS[2][:, h * 128:(h + 1) * 128], kT4[:, h, :], kT4[:, h, :], True, True, [K('kT')], [PK[2]])
                            for h in range(HB):
                                self.mm(PS[3][:, h * 128:(h + 1) * 128], kT4[:, h, :], qT4[:, h, :], True, True, [K('kT'), K('qT')], [PK[3]])
                            kb.op('dve', lambda e: e.tensor_tensor(out=T['W'][:], in0=PS[2][:], in1=T['DTs'][:], op=ALU.mult),
                                  reads=[PK[2], K('DTs')], writes=[K('W')])
                            kb.op('dve', lambda e: e.tensor_tensor(out=v4(T['W']), in0=v4(T['W']), in1=bc4(be4), op=ALU.mult),
                                  reads=[K('W'), bgk], writes=[K('W')])
                            kb.op('dve', lambda e: e.tensor_tensor(out=T['QKm'][:], in0=PS[3][:], in1=T['DT'][:], op=ALU.mult),
                                  reads=[PK[3], K('DT')], writes=[K('QKm')])
                            kb.op('pool', lambda e: e.tensor_copy(out=T['y0'][:, :, 0:128], in_=v4(T['vtok'])), reads=[K('vtok')], writes=[K('y0')])
                            kb.op('pool', lambda e: e.tensor_tensor(out=T['y0'][:, :, 128:256], in0=v4(T['ktok']), in1=bc4(cols[:, 2, :]), op=ALU.mult),
                                  reads=[K('ktok'), K('cols')], writes=[K('y0')])
                            kb.op('pool', lambda e: e.tensor_tensor(out=v4(T['kdec']), in0=v4(T['ktok']), in1=bc4(cols[:, 3, :]), op=ALU.mult),
                                  reads=[K('ktok'), K('cols')], writes=[K('kdec')])
                            W4 = v4(T['W'])
                            for h in range(HB):
                                self.tr(PS[4][:, h * 128:(h + 1) * 128], W4[:, h, :], identf[:], [K('W'), 'identf'], [PK[4]])
                            kb.op('act', lambda e: e.copy(out=T['WT'][:], in_=PS[4][:]), reads=[PK[4]], writes=[K('WT')])
                            pA = [PS[6], PS[7]]
                            ycur, ynxt = 'y0', 'y1'
                            for h in range(HB):
                                self.mm(pA[h // 2][:, (h % 2) * 256:(h % 2 + 1) * 256], W4[:, h, :], T[ycur][:, h, :], True, True,
                                        [K('W'), K(ycur)], [PK[6 + h // 2]])
                            for q in range(2):
                                kb.op('dve', lambda e: e.tensor_tensor(out=T[ynxt][:, 2 * q:2 * q + 2, :].rearrange("p h c -> p (h c)"),
                                                                       in0=T[ycur][:, 2 * q:2 * q + 2, :].rearrange("p h c -> p (h c)"),
                                                                       in1=pA[q][:], op=ALU.subtract),
                                      reads=[K(ycur), PK[6 + q]], writes=[K(ynxt)])
                            ycur, ynxt = ynxt, ycur
                            cur, curT = 'W', 'WT'
                            for lvl in range(1, 6):
                                na, nb_ = 'Wa%d' % (lvl % 2), 'Wb%d' % (lvl % 2)
                                c4, cT4 = v4(T[cur]), v4(T[curT])
                                for h in range(HB):
                                    self.mm(PS[4][:, h * 128:(h + 1) * 128], cT4[:, h, :], c4[:, h, :], True, True, [K(cur), K(curT)], [PK[4]])
                                if lvl < 5:
                                    for h in range(HB):
                                        self.mm(PS[5][:, h * 128:(h + 1) * 128], c4[:, h, :], cT4[:, h, :], True, True, [K(cur), K(curT)], [PK[5]])
                                kb.op('act', lambda e: e.copy(out=T[na][:], in_=PS[4][:]), reads=[PK[4]], writes=[K(na)])
                                if lvl < 5:
                                    kb.op('dve', lambda e: e.tensor_copy(out=T[nb_][:], in_=PS[5][:]), reads=[PK[5]], writes=[K(nb_)])
                                n4 = v4(T[na])
                                for h in range(HB):
                                    self.mm(pA[h // 2][:, (h % 2) * 256:(h % 2 + 1) * 256], n4[:, h, :], T[ycur][:, h, :], True, True,
                                            [K(na), K(ycur)], [PK[6 + h // 2]])
                                for q in range(2):
                                    kb.op('dve', lambda e: e.tensor_tensor(out=T[ynxt][:, 2 * q:2 * q + 2, :].rearrange("p h c -> p (h c)"),
                                                                           in0=T[ycur][:, 2 * q:2 * q + 2, :].rearrange("p h c -> p (h c)"),
                                                                           in1=pA[q][:], op=ALU.add),
                                          reads=[K(ycur), PK[6 + q]], writes=[K(ynxt)])
                                ycur, ynxt = ynxt, ycur
                                cur, curT = na, nb_
                            kb.op('dve', lambda e: e.tensor_tensor(out=T['UW'][:], in0=T[ycur][:], in1=be4.unsqueeze(2).to_broadcast([128, HB, 256]),
                                                                   op=ALU.mult), reads=[K(ycur), bgk], writes=[K('UW')])
                            UW = T['UW']
                            for h in range(HB):
                                self.tr(PS[3][:, h * 128:(h + 1) * 128], UW[:, h, 128:256], identf[:], [K('UW'), 'identf'], [PK[3]])
                            kb.op('act', lambda e: e.copy(out=T['wT'][:], in_=PS[3][:]), reads=[PK[3]], writes=[K('wT')])
                            wT4, qd4, QK4, kd4, vn4 = v4(T['wT']), v4(T['qd']), v4(T['QKm']), v4(T['kdec']), v4(T['vn'])
                            for c in ([0, 1] if d == 0 else [1, 0]):
                                rw = slice(64 * c, 64 * c + 64)
                                for h in range(HB):
                                    self.mm(PS[0][rw, h * 128:(h + 1) * 128], wT4[:, h, rw], St[:, h, :], True, True, [K('wT'), Sk], [PK[0]])
                                kb.op('dve', lambda e: e.tensor_tensor(out=vn4[rw], in0=UW[rw, :, 0:128],
                                                                       in1=PS[0][rw, :].rearrange("p (h c) -> p h c", h=HB), op=ALU.subtract),
                                      reads=[K('UW'), PK[0]], writes=[K('vn')])
                                for h in range(HB):
                                    self.mm(PS[1][rw, h * 128:(h + 1) * 128], qd4[:, h, rw], St[:, h, :], True, False, [K('qd'), Sk], [PK[1]])
                                    self.mm(PS[1][rw, h * 128:(h + 1) * 128], QK4[rw, h, rw], vn4[rw, h, :], False, True, [K('QKm'), K('vn')], [PK[1]])
                                kb.op('act', lambda e: e.copy(out=os_[rw, h0 * 128:(h0 + HB) * 128], in_=PS[1][rw, :]), reads=[PK[1]], writes=[osk])
                                for h in range(HB):
                                    self.mm(PS[2][:, h * 128:(h + 1) * 128], kd4[rw, h, :], vn4[rw, h, :], True, True, [K('kdec'), K('vn')], [PK[2]])
                                kb.op('dve', lambda e: e.tensor_tensor(out=St[:], in0=St[:], in1=T['alast'][:, :, c].unsqueeze(2).to_broadcast([128, HB, 128]),
                                                                       op=ALU.mult), reads=[Sk, K('alast')], writes=[Sk])
                                kb.op('dve', lambda e: e.tensor_tensor(out=St[:].rearrange("p h c -> p (h c)"), in0=St[:].rearrange("p h c -> p (h c)"),
                                                                       in1=PS[2][:], op=ALU.add), reads=[Sk, PK[2]], writes=[Sk])
                        kb.dma('pool', self.g_odir[d, tok0:tok0 + 128, :], os_[:], reads=[osk], writes=['g_odir_%d_%d' % (d, tok0)])

    def gdn_finish(self, li, jx, layer0, want_ctx):
        kb, NB = self.kb, self.NB
        with self.phase():
            o = self.outproj_setup(self.gdn_w_out[jx], li)
            NG = self.sb("gng", [128, 128])
            kb.dma('sp', NG[:], self.gdn_norm_g[jx:jx + 1, :].to_broadcast([128, 128]), writes=['gng'])
            self.rec_finish(o, NG, 'gng', self.g_odir, self.g_sgate, layer0, want_ctx)

    def rec_finish(self, o, NG, ngk, odir, sgate, layer0, want_ctx):
        kb, NB = self.kb, self.NB
        oa = [self.sb("foa%d" % i, [128, D]) for i in range(2)]
        obt = [self.sb("fob%d" % i, [128, D]) for i in range(2)]
        sg = [self.sb("fsg%d" % i, [128, D]) for i in range(2)]
        sq = self.sb("fsq", [128, D])
        st = self.sb("fst", [128, 4, 8])
        ob = [self.sb("fobb%d" % i, [128, D], BF16) for i in range(2)]
        it = 0
        for b in range(NB):
            for j in range(NT):
                if j < NTC and not want_ctx:
                    continue
                i = it % 2
                it += 1
                tok0 = b * TOK + j * 128
                kb.dma('sp', oa[i][:], odir[0, tok0:tok0 + 128, :], writes=['foa%d' % i])
                kb.dma('sp', obt[i][:], odir[1, tok0:tok0 + 128, :], writes=['fob%d' % i])
                kb.dma('sp', sg[i][:], sgate[tok0:tok0 + 128, :], writes=['fsg%d' % i])
                kb.op('dve', lambda e: e.tensor_tensor(out=oa[i][:], in0=oa[i][:], in1=obt[i][:], op=ALU.add),
                      reads=['foa%d' % i, 'fob%d' % i], writes=['foa%d' % i])
                kb.op('pool', lambda e: e.tensor_tensor(out=sq[:], in0=oa[i][:], in1=oa[i][:], op=ALU.mult), reads=['foa%d' % i], writes=['fsq'])
                kb.op('dve', lambda e: e.tensor_reduce(out=st[:, 0, :], in_=sq[:].rearrange("p (h d) -> p h d", h=8), axis=AX.X, op=ALU.add),
                      reads=['fsq'], writes=['fst'])
                kb.op('dve', lambda e: e.tensor_scalar(out=st[:, 1, :], in0=st[:, 0, :], scalar1=1.0 / 128, scalar2=EPS, op0=ALU.mult, op1=ALU.add),
                      reads=['fst'], writes=['fst'])
                kb.op('act', lambda e: e.activation(out=st[:, 2, :], in_=st[:, 1, :], func=AF.Sqrt), reads=['fst'], writes=['fst'])
                kb.op('dve', lambda e: e.reciprocal(out=st[:, 3, :], in_=st[:, 2, :]), reads=['fst'], writes=['fst'])
                o3 = oa[i][:].rearrange("p (h d) -> p h d", h=8)
                kb.op('dve', lambda e: e.tensor_tensor(out=o3, in0=o3, in1=st[:, 3, :].unsqueeze(2).to_broadcast([128, 8, 128]), op=ALU.mult),
                      reads=['foa%d' % i, 'fst'], writes=['foa%d' % i])
                kb.op('pool', lambda e: e.tensor_tensor(out=o3, in0=o3, in1=NG[:].unsqueeze(1).to_broadcast([128, 8, 128]), op=ALU.mult),
                      reads=['foa%d' % i, ngk], writes=['foa%d' % i])
                kb.op('dve', lambda e: e.tensor_tensor(out=ob[i][:], in0=oa[i][:], in1=sg[i][:], op=ALU.mult),
                      reads=['foa%d' % i, 'fsg%d' % i], writes=['fobb%d' % i])
                self.outproj_tile(o, ob[i], 'fobb%d' % i, layer0, b, j)

    def mlstm_decl(self):
        self.gdn_decl()
        if hasattr(self, 'm_qkT'):
            return
        NB = self.NB
        self.m_qkT = self.dram("m_qkT", [2, 512, NB * TOK], F32)
        self.m_ktok = self.dram("m_ktok", [NB * TOK, 512], F32)

    def mlstm_phase(self, li, jx, layer0, want_ctx):
        self.mlstm_decl()
        self.mlstm_proj(li, jx)
        self.mlstm_scan(li, jx)
        kb = self.kb
        with self.phase():
            o = self.outproj_setup(self.mlstm_w_out[jx], li)
            NG = self.sb("mng", [128, 128])
            kb.dma('sp', NG[:], self.mlstm_norm_g[jx:jx + 1, :].to_broadcast([128, 128]), writes=['mng'])
            self.rec_finish(o, NG, 'mng', self.g_odir, self.g_sgate, layer0, want_ctx)

    def mlstm_proj(self, li, jx):
        kb, NB = self.kb, self.NB
        hv = self.hT_view()
        with self.phase():
            win = self.sb("mwin", [128, 8, 3104], BF16)
            wsrc = self.mlstm_w_in[jx].rearrange("(k p) c -> p k c", p=128)
            for k in range(8):
                kb.dma('pool', win[:, k, :], wsrc[:, k, :], writes=['mwin'])
            identf = self.sb("identf", [128, 128])
            kb.dma('sp', identf[:], self.c_ident, writes=['identf'])
            GB = self.sb("mgb", [128, 32])
            kb.dma('sp', GB[:], self.mlstm_gate_b[jx:jx + 1, :].to_broadcast([128, 32]), writes=['mgb'])
            h2 = [self.sb("mh%d" % i, [128, 8, 256], BF16) for i in range(2)]
            pz = [self.ps("mpz%d" % i, [128, 512]) for i in range(2)]
            pT = self.ps("mpT", [128, 2, 128])
            pg = [self.ps("mpg%d" % i, [128, 2, 512]) for i in range(2)]
            pb = self.ps("mpb", [128, 32])
            stg = [self.sb("mstg%d" % i, [128, 256]) for i in range(2)]
            kst = self.sb("mkst", [128, 2, 512])
            gst = [self.sb("mgst%d" % i, [128, D]) for i in range(4)]
            gls = self.sb("mgls", [128, 32])
            bt = self.sb("mbt", [128, 4, 16])
            ci = 0
            wi = 0
            gi = 0
            for b in range(NB):
                for w in range(NT // 2):
                    h = h2[wi % 2]
                    hk = 'mh%d' % (wi % 2)
                    wi += 1
                    c0 = self.hcol(b, 2 * w)
                    tok0 = b * TOK + 2 * w * 128
                    kb.dma('sp', h[:], hv[:, :, c0:c0 + 256], writes=[hk])
                    for f in range(8):
                        a = ci % 2
                        ci += 1
                        pzk = 'mpz%d' % a
                        for k in range(8):
                            self.mm(pz[a][:, 0:256], win[:, k, f * 128:(f + 1) * 128], h[:, k, :], k == 0, k == 7, ['mwin', hk], [pzk])
                        gk = 'mstg%d' % a
                        kb.op('act', lambda e: e.activation(out=stg[a][:], in_=pz[a][:, 0:256], func=AF.Identity, scale=(0.125 if f < 4 else 1.0)),
                              reads=[pzk], writes=[gk])
                        kb.dma('pool', self.m_qkT[f // 4, (f % 4) * 128:(f % 4 + 1) * 128, tok0:tok0 + 256], stg[a][:], reads=[gk],
                               writes=['m_qkT_%d' % ci])
                        if f >= 4:
                            for t2 in range(2):
                                self.tr(pT[:, t2, :], stg[a][:, t2 * 128:(t2 + 1) * 128], identf[:], [gk, 'identf'], ['mpT'])
                            kb.op('dve', lambda e: e.tensor_copy(out=kst[:, :, (f - 4) * 128:(f - 3) * 128], in_=pT[:]), reads=['mpT'], writes=['mkst'])
                    for t2 in range(2):
                        r0 = tok0 + t2 * 128
                        kb.dma('pool', self.m_ktok[r0:r0 + 128, :], kst[:, t2, :], reads=['mkst'], writes=['m_ktok_%d' % r0])
                        hs = h[:, :, t2 * 128:(t2 + 1) * 128]
                        for part in range(2):
                            p_ = pg[part]
                            pk = 'mpg%d' % part
                            for n in range(2):
                                for k in range(8):
                                    c1 = 1024 + part * 1024 + n * 512
                                    self.mm(p_[:, n, :], hs[:, k, :], win[:, k, c1:c1 + 512], k == 0, k == 7, [hk, 'mwin'], [pk])
                            g_ = gst[gi % 4]
                            gk2 = 'mgst%d' % (gi % 4)
                            gi += 1
                            if part == 0:
                                kb.op('dve', lambda e: e.tensor_copy(out=g_[:], in_=p_[:].rearrange("p a b -> p (a b)")), reads=[pk], writes=[gk2])
                                kb.dma('pool', self.g_vtok[r0:r0 + 128, :], g_[:], reads=[gk2], writes=['g_vtok_%d' % r0])
                            else:
                                kb.op('act', lambda e: e.activation(out=g_[:], in_=p_[:].rearrange("p a b -> p (a b)"), func=AF.Sigmoid),
                                      reads=[pk], writes=[gk2])
                                kb.dma('pool', self.g_sgate[r0:r0 + 128, :], g_[:], reads=[gk2], writes=['g_sgate_%d' % r0])
                        for k in range(8):
                            self.mm(pb[:], hs[:, k, :], win[:, k, 3072:3104], k == 0, k == 7, [hk, 'mwin'], ['mpb'])
                        x4 = bt[:, 0:2, :].rearrange("p a c -> p (a c)")
                        kb.op('dve', lambda e: e.tensor_tensor(out=x4, in0=pb[:], in1=GB[:], op=ALU.add), reads=['mpb', 'mgb'], writes=['mbt'])
                        x5 = x4.rearrange("p (d t h) -> p d t h", d=2, t=2)
                        kb.op('pool', lambda e: e.tensor_copy(out=gls[:, 0:16].rearrange("p (d h) -> p d h", d=2), in_=x5[:, :, 0, :]),
                              reads=['mbt'], writes=['mgls'])
                        xf = x5[:, :, 1, :]
                        t3 = lambda i_: bt[:, i_, :].rearrange("p (d h) -> p d h", d=2)
                        kb.op('dve', lambda e: e.scalar_tensor_tensor(out=t3(2), in0=xf, scalar=-1.0, in1=xf, op0=ALU.mult, op1=ALU.max),
                              reads=['mbt'], writes=['mbt'])
                        kb.op('act', lambda e: e.activation(out=bt[:, 2, :], in_=bt[:, 2, :], func=AF.Exp, scale=-1.0), reads=['mbt'], writes=['mbt'])
                        kb.op('act', lambda e: e.activation(out=bt[:, 3, :], in_=bt[:, 2, :], func=AF.Ln, bias=1.0), reads=['mbt'], writes=['mbt'])
                        kb.op('dve', lambda e: e.scalar_tensor_tensor(out=gls[:, 16:32].rearrange("p (d h) -> p d h", d=2), in0=xf, scalar=0.0,
                                                                      in1=t3(3), op0=ALU.min, op1=ALU.subtract), reads=['mbt'], writes=['mgls'])
                        kb.dma('pool', self.g_bg[r0:r0 + 128, :], gls[:], reads=['mgls'], writes=['g_bg_%d' % r0])

    def mlstm_scan(self, li, jx):
        kb, NB = self.kb, self.NB
        HB = 4
        qkTv = self.m_qkT.rearrange("q (h p) c -> q p h c", p=64)
        order = [list(range(NT)), [1, 0] + list(range(NT - 1, NTC - 1, -1))]
        with self.phase():
            MS = self.sb("mmask", [128, 13, 128])
            for m0 in range(0, 13, 4):
                m1 = min(13, m0 + 4)
                kb.dma('sp', MS[:, m0:m1, :], self.c_masks[m0:m1].rearrange("m p c -> p m c"), writes=['mmask'])
            identf = self.sb("identf", [128, 128])
            kb.dma('sp', identf[:], self.c_ident, writes=['identf'])
            onesf = self.sb("onesf", [128, 128])
            kb.op('dve', lambda e: e.memset(onesf[:], 1.0), writes=['onesf'])
            PS = [self.ps("mps%d" % i, [128, 512]) for i in range(8)]
            PK = ['mps%d' % i for i in range(8)]

            def v4(t):
                return t[:].rearrange("p (h c) -> p h c", h=HB)
            sets = []
            for si in range(2):
                T = {}
                for nm in ('LF', 'Dig', 'DL', 'P', 'PT'):
                    T[nm] = self.sb("m%s%d" % (nm, si), [128, HB * 128])
                T['qT'] = self.sb("mqT%d" % si, [64, HB, 128])
                T['kT'] = self.sb("mkT%d" % si, [64, HB, 128])
                T['ktok'] = self.sb("mktok%d" % si, [128, HB, 64])
                T['kw'] = self.sb("mkw%d" % si, [128, HB, 64])
                T['v1'] = self.sb("mv1%d" % si, [128, HB, 132])
                T['NI'] = self.sb("mNI%d" % si, [128, HB, 132])
                T['t1'] = self.sb("mt1%d" % si, [128, HB, 132])
                T['t2'] = self.sb("mt2%d" % si, [128, HB, 132])
                T['c'] = self.sb("mc%d" % si, [128, 16, HB])
                T['mloc'] = self.sb("mmloc%d" % si, [128, HB, 2])
                T['blast'] = self.sb("mblast%d" % si, [128, 2, HB])
                T['e3'] = self.sb("me3%d" % si, [128, 3, HB])
                T['fi'] = self.sb("mfi%d" % si, [128, 2, HB])
                T['k'] = 'm%d_' % si
                kb.op('pool', lambda e: e.memset(T['v1'][:, :, 128:129], 1.0), writes=[T['k'] + 'v1'])
                sets.append(T)
            glt = [self.sb("mgl%d" % i, [128, 32]) for i in range(4)]
            ost = [self.sb("most%d" % i, [128, D]) for i in range(4)]
            Cn = [[self.sb("mCn_%d_%d" % (d, hf), [64, HB, 132]) for hf in range(2)] for d in range(2)]
            Mm = [[self.sb("mM_%d_%d" % (d, hf), [128, HB]) for hf in range(2)] for d in range(2)]
            stepi = 0
            bi = 0
            for b in range(NB):
                for d in range(2):
                    for hf in range(2):
                        kb.op('pool', lambda e: e.memset(Cn[d][hf][:], 0.0), writes=['mCn_%d_%d' % (d, hf)])
                        kb.op('pool', lambda e: e.memset(Mm[d][hf][:], 0.0), writes=['mM_%d_%d' % (d, hf)])
                for n in range(NT):
                    for d in range(2):
                        j = order[d][n]
                        tok0 = b * TOK + j * 128
                        gl = glt[bi % 4]
                        glk = 'mgl%d' % (bi % 4)
                        os_ = ost[bi % 4]
                        osk = 'most%d' % (bi % 4)
                        bi += 1
                        kb.dma('sp', gl[:], self.g_bg[tok0:tok0 + 128, :], writes=[glk])
                        Ud, nUd, BO, BmU, NEG = MS[:, d, :], MS[:, 4 + d, :], MS[:, 6, :], MS[:, 9 + d, :], MS[:, 11 + d, :]
                        for hf in range(2):
                            T = sets[stepi % 2]
                            K = lambda nm: T['k'] + nm
                            stepi += 1
                            h0 = hf * HB
                            Ck, Mk = 'mCn_%d_%d' % (d, hf), 'mM_%d_%d' % (d, hf)
                            Ct, Mt = Cn[d][hf], Mm[d][hf]
                            c = T['c']
                            kb.dma('sp', T['qT'][:], qkTv[0, :, h0:h0 + HB, tok0:tok0 + 128], writes=[K('qT')])
                            kb.dma('sp', T['kT'][:], qkTv[1, :, h0:h0 + HB, tok0:tok0 + 128], writes=[K('kT')])
                            kb.dma('sp', T['ktok'][:], self.m_ktok[tok0:tok0 + 128, h0 * 64:(h0 + HB) * 64].rearrange("p (h c) -> p h c", h=HB),
                                   writes=[K('ktok')])
                            kb.dma('sp', T['v1'][:, :, 0:128], self.g_vtok[tok0:tok0 + 128, h0 * 128:(h0 + HB) * 128].rearrange("p (h c) -> p h c", h=HB),
                                   writes=[K('v1')])
                            ig4 = gl[:, d * 8 + h0:d * 8 + h0 + HB]
                            lf4 = gl[:, 16 + d * 8 + h0:16 + d * 8 + h0 + HB]
                            bc4 = lambda ap: ap.unsqueeze(2).to_broadcast([128, HB, 128])
                            mk4 = lambda ap: ap.unsqueeze(1).to_broadcast([128, HB, 128])
                            kb.op('dve', lambda e: e.tensor_copy(out=v4(T['LF']), in_=bc4(lf4)), reads=[glk], writes=[K('LF')])
                            kb.op('pool', lambda e: e.tensor_tensor(out=v4(T['Dig']), in0=mk4(identf[:]), in1=bc4(ig4), op=ALU.mult),
                                  reads=[glk, 'identf'], writes=[K('Dig')])
                            LF, Dig = v4(T['LF']), v4(T['Dig'])
                            for h in range(HB):
                                o_ = PS[0][:, h * 128:(h + 1) * 128]
                                self.mm(o_, Ud, LF[:, h, :], True, False, ['mmask', K('LF')], [PK[0]])
                                self.mm(o_, LF[:, h, :], nUd, False, False, ['mmask', K('LF')], [PK[0]])
                                self.mm(o_, onesf[:], Dig[:, h, :], False, True, ['onesf', K('Dig')], [PK[0]])
                            for h in range(HB):
                                o_ = PS[1][:, h * 128:(h + 1) * 128]
                                self.mm(o_, LF[:, h, :], BmU, True, False, ['mmask', K('LF')], [PK[1]])
                                self.mm(o_, onesf[:], Dig[:, h, :], False, True, ['onesf', K('Dig')], [PK[1]])
                            self.mm(PS[4][:, 0:HB], Ud, lf4, True, True, ['mmask', glk], [PK[4]])
                            self.mm(PS[4][:, HB:2 * HB], BO, lf4, True, True, ['mmask', glk], [PK[4]])
                            for cc in range(2):
                                self.mm(PS[4][:, 8 + cc * HB:8 + (cc + 1) * HB], MS[:, 7 + cc, :], lf4, True, True, ['mmask', glk], [PK[4]])
                            for h in range(HB):
                                self.mm(PS[2][:, h * 128:(h + 1) * 128], T['qT'][:, h, :], T['kT'][:, h, :], True, True, [K('qT'), K('kT')], [PK[2]])
                            kb.op('act', lambda e: e.copy(out=c[:, 0:2, :].rearrange("p a h -> p (a h)"), in_=PS[4][:, 0:2 * HB]), reads=[PK[4]], writes=[K('c')])
                            kb.op('act', lambda e: e.copy(out=T['blast'][:].rearrange("p a h -> p (a h)"), in_=PS[4][:, 8:8 + 2 * HB]),
                                  reads=[PK[4]], writes=[K('blast')])
                            kb.op('dve', lambda e: e.tensor_tensor(out=v4(T['DL']), in0=PS[0][:].rearrange("p (h c) -> p h c", h=HB), in1=mk4(NEG), op=ALU.add),
                                  reads=[PK[0], 'mmask'], writes=[K('DL')])
                            kb.op('dve', lambda e: e.tensor_reduce(out=c[:, 2, :], in_=v4(T['DL']), axis=AX.X, op=ALU.max), reads=[K('DL')], writes=[K('c')])
                            kb.op('dve', lambda e: e.tensor_tensor(out=v4(T['DL']), in0=v4(T['DL']), in1=bc4(c[:, 2, :]), op=ALU.subtract),
                                  reads=[K('DL'), K('c')], writes=[K('DL')])
                            kb.op('act', lambda e: e.activation(out=T['P'][:], in_=T['DL'][:], func=AF.Exp), reads=[K('DL')], writes=[K('P')])
                            kb.op('dve', lambda e: e.tensor_tensor(out=T['P'][:], in0=T['P'][:], in1=PS[2][:], op=ALU.mult), reads=[K('P'), PK[2]], writes=[K('P')])
                            P4 = v4(T['P'])
                            for h in range(HB):
                                self.tr(PS[3][:, h * 128:(h + 1) * 128], P4[:, h, :], identf[:], [K('P'), 'identf'], [PK[3]])
                            kb.op('act', lambda e: e.copy(out=T['PT'][:], in_=PS[3][:]), reads=[PK[3]], writes=[K('PT')])
                            PT4 = v4(T['PT'])
                            for h in range(HB):
                                self.mm(PS[6 + h // 2][:, (h % 2) * 129:(h % 2) * 129 + 129], PT4[:, h, :], T['v1'][:, h, 0:129], True, True,
                                        [K('PT'), K('v1')], [PK[6 + h // 2]])
                            for q in range(2):
                                kb.op('act' if q == 0 else 'dve',
                                      (lambda e: e.copy(out=T['NI'][:, 0:2, 0:129], in_=PS[6][:, 0:258].rearrange("p (h c) -> p h c", h=2))) if q == 0 else
                                      (lambda e: e.tensor_copy(out=T['NI'][:, 2:4, 0:129], in_=PS[7][:, 0:258].rearrange("p (h c) -> p h c", h=2))),
                                      reads=[PK[6 + q]], writes=[K('NI')])
                            kb.op('dve', lambda e: e.tensor_reduce(out=T['mloc'][:], in_=PS[1][:].rearrange("p (h c j) -> p h c j", h=HB, c=2), axis=AX.X, op=ALU.max),
                                  reads=[PK[1]], writes=[K('mloc')])
                            kb.op('dve', lambda e: e.tensor_tensor(out=c[:, 3, :], in0=c[:, 1, :], in1=c[:, 0, :], op=ALU.subtract), reads=[K('c')], writes=[K('c')])
                            kb.op('dve', lambda e: e.tensor_tensor(out=c[:, 3, :], in0=c[:, 3, :], in1=ig4, op=ALU.add), reads=[K('c'), glk], writes=[K('c')])
                            for cc in range(2):
                                rw = slice(64 * cc, 64 * cc + 64)
                                kb.op('dve', lambda e: e.tensor_tensor(out=c[rw, 4, :], in0=c[rw, 3, :], in1=T['mloc'][rw, :, cc], op=ALU.subtract),
                                      reads=[K('c'), K('mloc')], writes=[K('c')])
                            kb.op('act', lambda e: e.activation(out=c[:, 5, :], in_=c[:, 4, :], func=AF.Exp), reads=[K('c')], writes=[K('c')])
                            kb.op('pool', lambda e: e.tensor_tensor(out=T['kw'][:], in0=T['ktok'][:], in1=c[:, 5, :].unsqueeze(2).to_broadcast([128, HB, 64]), op=ALU.mult),
                                  reads=[K('ktok'), K('c')], writes=[K('kw')])
                            for cc in ([0, 1] if d == 0 else [1, 0]):
                                rw = slice(64 * cc, 64 * cc + 64)
                                for h in range(HB):
                                    self.mm(PS[h // 2][rw, (h % 2) * 129:(h % 2) * 129 + 129], T['qT'][:, h, rw], Ct[:, h, 0:129], True, True,
                                            [K('qT'), Ck], [PK[h // 2]])
                                kb.op('dve', lambda e: e.tensor_tensor(out=c[rw, 6, :], in0=c[rw, 0, :], in1=Mt[rw, :], op=ALU.add), reads=[K('c'), Mk], writes=[K('c')])
                                kb.op('dve', lambda e: e.tensor_tensor(out=c[rw, 7, :], in0=c[rw, 2, :], in1=c[rw, 6, :], op=ALU.max), reads=[K('c')], writes=[K('c')])
                                e3 = T['e3']
                                kb.op('dve', lambda e: e.tensor_tensor(out=e3[rw, 0, :], in0=c[rw, 6, :], in1=c[rw, 7, :], op=ALU.subtract), reads=[K('c')], writes=[K('e3')])
                                kb.op('dve', lambda e: e.tensor_tensor(out=e3[rw, 1, :], in0=c[rw, 2, :], in1=c[rw, 7, :], op=ALU.subtract), reads=[K('c')], writes=[K('e3')])
                                kb.op('dve', lambda e: e.tensor_scalar(out=e3[rw, 2, :], in0=c[rw, 7, :], scalar1=-1.0, scalar2=None, op0=ALU.mult), reads=[K('c')], writes=[K('e3')])
                                kb.op('act', lambda e: e.activation(out=e3[rw, :, :], in_=e3[rw, :, :], func=AF.Exp), reads=[K('e3')], writes=[K('e3')])
                                for q in range(2):
                                    kb.op('dve', lambda e: e.tensor_tensor(out=T['t1'][rw, 2 * q:2 * q + 2, 0:129], in0=PS[q][rw, 0:258].rearrange("p (h c) -> p h c", h=2),
                                                                           in1=e3[rw, 0, 2 * q:2 * q + 2].unsqueeze(2).to_broadcast([64, 2, 129]), op=ALU.mult),
                                          reads=[PK[q], K('e3')], writes=[K('t1')])
                                kb.op('pool', lambda e: e.tensor_tensor(out=T['t2'][rw, :, 0:129], in0=T['NI'][rw, :, 0:129],
                                                                        in1=e3[rw, 1, :].unsqueeze(2).to_broadcast([64, HB, 129]), op=ALU.mult),
                                      reads=[K('NI'), K('e3')], writes=[K('t2')])
                                kb.op('dve', lambda e: e.tensor_tensor(out=T['t1'][rw, :, 0:129], in0=T['t1'][rw, :, 0:129], in1=T['t2'][rw, :, 0:129], op=ALU.add),
                                      reads=[K('t1'), K('t2')], writes=[K('t1')])
                                den = T['t1'][rw, :, 128]
                                kb.op('dve', lambda e: e.scalar_tensor_tensor(out=c[rw, 8, :], in0=den, scalar=-1.0, in1=den, op0=ALU.mult, op1=ALU.max),
                                      reads=[K('t1')], writes=[K('c')])
                                kb.op('dve', lambda e: e.tensor_tensor(out=c[rw, 9, :], in0=c[rw, 8, :], in1=e3[rw, 2, :], op=ALU.max), reads=[K('c'), K('e3')], writes=[K('c')])
                                kb.op('dve', lambda e: e.reciprocal(out=c[rw, 10, :], in_=c[rw, 9, :]), reads=[K('c')], writes=[K('c')])
                                kb.op('dve', lambda e: e.tensor_tensor(out=os_[rw, h0 * 128:(h0 + HB) * 128].rearrange("p (h c) -> p h c", h=HB), in0=T['t1'][rw, :, 0:128],
                                                                       in1=c[rw, 10, :].unsqueeze(2).to_broadcast([64, HB, 128]), op=ALU.mult),
                                      reads=[K('t1'), K('c')], writes=[osk])
                                for h in range(HB):
                                    self.mm(PS[2 + h // 2][0:64, (h % 2) * 129:(h % 2) * 129 + 129], T['kw'][rw, h, :], T['v1'][rw, h, 0:129], True, True,
                                            [K('kw'), K('v1')], [PK[2 + h // 2]])
                                fi = T['fi']
                                kb.op('dve', lambda e: e.tensor_tensor(out=c[:, 11, :], in0=T['blast'][:, cc, :], in1=Mt[:], op=ALU.add), reads=[K('blast'), Mk], writes=[K('c')])
                                kb.op('dve', lambda e: e.tensor_tensor(out=c[:, 12, :], in0=c[:, 11, :], in1=T['mloc'][:, :, cc], op=ALU.max), reads=[K('c'), K('mloc')], writes=[K('c')])
                                kb.op('dve', lambda e: e.tensor_tensor(out=fi[:, 0, :], in0=c[:, 11, :], in1=c[:, 12, :], op=ALU.subtract), reads=[K('c')], writes=[K('fi')])
                                kb.op('dve', lambda e: e.tensor_tensor(out=fi[:, 1, :], in0=T['mloc'][:, :, cc], in1=c[:, 12, :], op=ALU.subtract), reads=[K('c'), K('mloc')], writes=[K('fi')])
                                kb.op('act', lambda e: e.activation(out=fi[:], in_=fi[:], func=AF.Exp), reads=[K('fi')], writes=[K('fi')])
                                kb.op('dve', lambda e: e.tensor_copy(out=Mt[:], in_=c[:, 12, :]), reads=[K('c')], writes=[Mk])
                                kb.op('dve', lambda e: e.tensor_tensor(out=Ct[:, :, 0:129], in0=Ct[:, :, 0:129], in1=fi[0:64, 0, :].unsqueeze(2).to_broadcast([64, HB, 129]), op=ALU.mult),
                                      reads=[Ck, K('fi')], writes=[Ck])
                                for q in range(2):
                                    kb.op('dve', lambda e: e.tensor_tensor(out=T['t2'][0:64, 2 * q:2 * q + 2, 0:129], in0=PS[2 + q][0:64, 0:258].rearrange("p (h c) -> p h c", h=2),
                                                                           in1=fi[0:64, 1, 2 * q:2 * q + 2].unsqueeze(2).to_broadcast([64, 2, 129]), op=ALU.mult),
                                          reads=[PK[2 + q], K('fi'), K('t2')], writes=[K('t2')])
                                kb.op('dve', lambda e: e.tensor_tensor(out=Ct[:, :, 0:129], in0=Ct[:, :, 0:129], in1=T['t2'][0:64, :, 0:129], op=ALU.add),
                                      reads=[Ck, K('t2')], writes=[Ck])
                        kb.dma('pool', self.g_odir[d, tok0:tok0 + 128, :], os_[:], reads=[osk], writes=['g_odir_%d_%d' % (d, tok0)])

    def build(self):
        self.declare()
        self.setup()
        cnt = {0: 0, 1: 0, 2: 0}
        for li, kind in enumerate(self.kinds):
            last = self.last_flags[li]
            layer0 = (li == 0)
            jx = cnt[kind]
            cnt[kind] += 1
            self.mod_phase(li)
            self.norm_phase(li, 1, layer0)
            if kind == 2:
                self.attn_phase(li, jx, layer0, not last)
            elif kind == 0:
                self.gdn_phase(li, jx, layer0, not last)
            else:
                self.mlstm_phase(li, jx, layer0, not last)
            self.norm_phase(li, 2, False, ctx_needed=not last)
            self.ffn_phase(li, ctx_needed=not last)
        self.final_phase()
        self.kb.barrier()
        self.kb.close()


def host_consts():
    ident = np.eye(128, dtype=np.float32)
    n_pair = 32
    inv = (10000.0 ** (-np.arange(n_pair, dtype=np.float32) / n_pair)).astype(np.float32)
    pos = np.arange(LAT)
    row = (pos // 64).astype(np.float32)
    col = (pos % 64).astype(np.float32)
    ang = np.concatenate([row[:, None] * inv, col[:, None] * inv], axis=-1).astype(np.float32)
    rope = np.concatenate([np.cos(ang), np.sin(ang)], axis=-1).astype(np.float32)
    masks = np.zeros((16, 128, 128), np.float32)
    t = np.arange(128)
    same = (t[:, None] // 64) == (t[None, :] // 64)
    uf = (same & (t[:, None] <= t[None, :])).astype(np.float32)
    ub = (same & (t[:, None] >= t[None, :])).astype(np.float32)
    masks[0], masks[1] = uf, ub
    masks[2], masks[3] = uf - np.eye(128, dtype=np.float32), ub - np.eye(128, dtype=np.float32)
    masks[4], masks[5] = -uf, -ub
    masks[6] = same.astype(np.float32)
    masks[7] = np.repeat((t < 64).astype(np.float32)[:, None], 128, axis=1)
    masks[8] = np.repeat((t >= 64).astype(np.float32)[:, None], 128, axis=1)
    masks[9], masks[10] = masks[6] - uf, masks[6] - ub
    masks[11] = (1.0 - ub) * np.float32(-1e30)
    masks[12] = (1.0 - uf) * np.float32(-1e30)
    return {"c_ident": ident, "c_rope": rope, "c_masks": masks}


def make_in_maps(inputs, NB, n_cores, kinds):
    f = lambda a: np.ascontiguousarray(np.asarray(a, dtype=np.float32))
    consts = host_consts()
    shared = {}
    for k in ("norm1_g", "norm2_g", "w_mod", "b_mod", "ffn_w_in", "ffn_conv_w", "ffn_conv_b", "ffn_w_out",
              "gdn_w_in", "gdn_conv_w", "gdn_norm_g", "gdn_w_out", "mlstm_w_in", "mlstm_norm_g", "mlstm_w_out",
              "attn_w_in", "attn_q_norm_g", "attn_k_norm_g", "attn_w_out"):
        shared[k] = f(inputs[k])
    shared["gdn_a_log"] = f(inputs["gdn_a_log"]).reshape(-1, 16)
    shared["gdn_dt_bias"] = f(inputs["gdn_dt_bias"]).reshape(-1, 16)
    shared["mlstm_gate_b"] = f(inputs["mlstm_gate_b"]).reshape(-1, 32)
    shared["final_norm_g"] = f(inputs["final_norm_g"]).reshape(1, D)
    shared.update(consts)
    x, c, ctx, c_ctx = f(inputs["x"]), f(inputs["c"]), f(inputs["ctx"]), f(inputs["c_ctx"])
    maps = []
    for i in range(n_cores):
        m = dict(shared)
        m["x"] = x[i * NB:(i + 1) * NB]
        m["ctx"] = ctx[i * NB:(i + 1) * NB]
        m["cvec"] = np.ascontiguousarray(np.concatenate([c[i * NB:(i + 1) * NB], c_ctx[None, :]], axis=0))
        maps.append(m)
    return maps


def kernel(**inputs):
    NB = 2
    n_cores = 8
    nc = bass.Bass("TRN2", target_bir_lowering=False)
    mk = MK(nc, NB, KINDS, [False, False, False, True])
    mk.build()
    maps = make_in_maps(inputs, NB, n_cores, KINDS)
    res = run_bass_kernel_spmd(nc, maps, core_ids=list(range(n_cores)))
    return np.concatenate([r["out"] for r in res.results], axis=0).astype(np.float32)
```

```python
import contextlib
import math
import numpy as np
import concourse.bass as bass
import concourse.mybir as mybir
from concourse.bass_utils import run_bass_kernel_spmd

F32 = mybir.dt.float32
F32R = mybir.dt.float32r
BF16 = mybir.dt.bfloat16
AF = mybir.ActivationFunctionType
ALU = mybir.AluOpType
AX = mybir.AxisListType

D = 1024
LAT = 4096
CTXL = 256
NTC = 2
NTL = 32
NT = 34
TOK = NT * 128
FFN = 2816
EPS = 1e-6
HALO = 2
REG_C = CTXL + 2 * HALO
REG_L = LAT + 2 * HALO
REG = REG_C + REG_L
KINDS = [0, 1, 2, 0]


class KB:
    def __init__(self, nc, ring=6, same_engine_sync=True):
        self.nc = nc
        self.eng = {'pe': nc.tensor, 'dve': nc.vector, 'act': nc.scalar,
                    'pool': nc.gpsimd, 'sp': nc.sync}
        self.same_engine_sync = same_engine_sync
        self.sem = {}
        self.cnt = {}
        self.seen = {e: {} for e in self.eng}
        self.res = {}
        self._ctx = []
        for e in ('pe', 'dve', 'act', 'pool'):
            self._mksem('c_' + e)
        self.rings = {}
        for q in ('sp', 'act', 'pool'):
            names = []
            for i in range(ring):
                n = 'd_%s_%d' % (q, i)
                self._mksem(n)
                names.append(n)
            self.rings[q] = [names, 0]
        self.n_inst = 0
        self.n_wait = 0

    def _mksem(self, name):
        cm = self.nc.semaphore(name)
        h = cm.__enter__()
        self._ctx.append(cm)
        self.sem[name] = h
        self.cnt[name] = 0

    def close(self):
        for cm in reversed(self._ctx):
            cm.__exit__(None, None, None)
        self._ctx = []

    def _R(self, key):
        r = self.res.get(key)
        if r is None:
            r = {'w': None, 'r': {}}
            self.res[key] = r
        return r

    def _deps(self, reads, writes):
        deps = {}

        def add(tok):
            if tok is None:
                return
            s, v = tok
            if deps.get(s, 0) < v:
                deps[s] = v
        for k in reads:
            add(self._R(k)['w'])
        for k in writes:
            r = self._R(k)
            add(r['w'])
            for s, v in r['r'].items():
                add((s, v))
        return deps

    def _wait(self, e, deps, is_dma=False):
        own = 'c_' + e
        seen = self.seen[e]
        for s, v in deps.items():
            if s == own and not is_dma and (e == 'pe' or not self.same_engine_sync):
                continue
            if seen.get(s, 0) >= v:
                continue
            self.eng[e].wait_ge(self.sem[s], v)
            self.n_wait += 1
            seen[s] = v

    def _commit(self, tok, reads, writes):
        s, v = tok
        for k in reads:
            r = self._R(k)
            if r['r'].get(s, 0) < v:
                r['r'][s] = v
        for k in writes:
            r = self._R(k)
            r['w'] = tok
            r['r'] = {}

    def op(self, e, fn, reads=(), writes=()):
        deps = self._deps(reads, writes)
        self._wait(e, deps)
        inst = fn(self.eng[e])
        s = 'c_' + e
        self.cnt[s] += 1
        inst.then_inc(self.sem[s], 1)
        self.n_inst += 1
        self._commit((s, self.cnt[s]), reads, writes)
        return inst

    def dma(self, q, out, in_, reads=(), writes=(), **kw):
        names, idx = self.rings[q]
        s = names[idx % len(names)]
        self.rings[q][1] = idx + 1
        deps = self._deps(reads, writes)
        if self.cnt[s] > 0 and deps.get(s, 0) < self.cnt[s]:
            deps[s] = self.cnt[s]
        self._wait(q, deps, is_dma=True)
        inst = self.eng[q].dma_start(out=out, in_=in_, **kw)
        self.cnt[s] += 16
        inst.then_inc(self.sem[s], 16)
        self.n_inst += 1
        self._commit((s, self.cnt[s]), reads, writes)
        return inst

    def barrier(self):
        for e in self.eng:
            for s, v in self.cnt.items():
                if v > 0 and self.seen[e].get(s, 0) < v:
                    self.eng[e].wait_ge(self.sem[s], v)
                    self.seen[e][s] = v
        self.res = {}


class MK:
    def __init__(self, nc, NB, kinds, last_flags, debug=False):
        self.debug = debug
        self.nc = nc
        self.NB = NB
        self.kinds = kinds
        self.last_flags = last_flags
        self.kb = KB(nc)
        self.stack = None
        self.uid = 0

    @contextlib.contextmanager
    def phase(self):
        prev = self.stack
        with contextlib.ExitStack() as es:
            self.stack = es
            yield
            self.kb.barrier()
        self.stack = prev

    @contextlib.contextmanager
    def nosame(self):
        yield

    def sb(self, name, shape, dt=F32):
        self.uid += 1
        return self.stack.enter_context(self.nc.sbuf_tensor("%s_%d" % (name, self.uid), list(shape), dt))

    def ps(self, name, shape, dt=F32):
        self.uid += 1
        return self.stack.enter_context(self.nc.psum_tensor("%s_%d" % (name, self.uid), list(shape), dt))

    def dram(self, name, shape, dt, kind="Internal"):
        return self.nc.dram_tensor(name, list(shape), dt, kind=kind).ap()

    def mm(self, out, lhsT, rhs, start, stop, reads, writes):
        return self.kb.op('pe', lambda e: e.matmul(out, lhsT=lhsT, rhs=rhs, start=start, stop=stop),
                          reads=reads, writes=writes)

    def mmr(self, out, lhsT, rhs, start, stop, reads, writes):
        F32R = mybir.dt.float32r
        return self.kb.op('pe', lambda e: e.matmul(out, lhsT=lhsT.bitcast(F32R), rhs=rhs.bitcast(F32R), start=start, stop=stop),
                          reads=reads, writes=writes)

    def tr(self, out, in_, ident, reads, writes):
        return self.kb.op('pe', lambda e: e.transpose(out, in_, ident), reads=reads, writes=writes)

    def declare(self):
        NB = self.NB
        n0 = sum(1 for k in self.kinds if k == 0)
        n1 = sum(1 for k in self.kinds if k == 1)
        n2 = sum(1 for k in self.kinds if k == 2)
        DEPTH = len(self.kinds)
        I = lambda n, s: self.dram(n, s, F32, kind="ExternalInput")
        self.x = I("x", [NB, LAT, D])
        self.ctx = I("ctx", [NB, CTXL, D])
        self.cvec = I("cvec", [NB + 1, D])
        self.norm1_g = I("norm1_g", [DEPTH, D])
        self.norm2_g = I("norm2_g", [DEPTH, D])
        self.w_mod = I("w_mod", [DEPTH, D, 6 * D])
        self.b_mod = I("b_mod", [DEPTH, 6 * D])
        self.ffn_w_in = I("ffn_w_in", [DEPTH, D, 2 * FFN])
        self.ffn_conv_w = I("ffn_conv_w", [DEPTH, 3, FFN])
        self.ffn_conv_b = I("ffn_conv_b", [DEPTH, FFN])
        self.ffn_w_out = I("ffn_w_out", [DEPTH, FFN, D])
        self.gdn_w_in = I("gdn_w_in", [max(n0, 1), D, 4128])
        self.gdn_conv_w = I("gdn_conv_w", [max(n0, 1), 5, 3072])
        self.gdn_a_log = I("gdn_a_log", [max(n0, 1), 16])
        self.gdn_dt_bias = I("gdn_dt_bias", [max(n0, 1), 16])
        self.gdn_norm_g = I("gdn_norm_g", [max(n0, 1), 128])
        self.gdn_w_out = I("gdn_w_out", [max(n0, 1), D, D])
        self.mlstm_w_in = I("mlstm_w_in", [max(n1, 1), D, 3104])
        self.mlstm_gate_b = I("mlstm_gate_b", [max(n1, 1), 32])
        self.mlstm_norm_g = I("mlstm_norm_g", [max(n1, 1), 128])
        self.mlstm_w_out = I("mlstm_w_out", [max(n1, 1), D, D])
        self.attn_w_in = I("attn_w_in", [max(n2, 1), D, 1536])
        self.attn_q_norm_g = I("attn_q_norm_g", [max(n2, 1), 128])
        self.attn_k_norm_g = I("attn_k_norm_g", [max(n2, 1), 128])
        self.attn_w_out = I("attn_w_out", [max(n2, 1), D, D])
        self.final_norm_g = I("final_norm_g", [1, D])
        self.c_ident = I("c_ident", [128, 128])
        self.c_rope = I("c_rope", [LAT, 128])
        self.c_masks = I("c_masks", [16, 128, 128])
        self.out = self.dram("out", [NB, LAT, D], F32, kind="ExternalOutput")
        sk = "ExternalOutput" if self.debug else "Internal"
        self.xs = self.dram("xs", [NB, TOK, D], F32, kind=sk)
        self.hTs = self.dram("hTs", [D, NB * REG], BF16, kind=sk)
        self.modv = self.dram("modv", [DEPTH, NB + 1, 6, D], F32, kind=sk)

    def src_x(self, layer0, b, j):
        if layer0:
            if j < NTC:
                return self.ctx[b, j * 128:(j + 1) * 128, :], 'in_ctx'
            return self.x[b, (j - NTC) * 128:(j - NTC + 1) * 128, :], 'in_x'
        return self.xs[b, j * 128:(j + 1) * 128, :], 'xs_%d_%d' % (b, j)

    def hcol(self, b, j):
        base = b * REG
        if j < NTC:
            return base + HALO + j * 128
        return base + REG_C + HALO + (j - NTC) * 128

    def hT_view(self):
        return self.hTs.rearrange("(k p) c -> p k c", p=128)

    def setup(self):
        kb = self.kb
        with self.phase():
            z = self.sb("zero", [128, 8, 2 * HALO], BF16)
            kb.op('dve', lambda e: e.memset(z[:], 0.0), writes=['zero'])
            hv = self.hT_view()
            for b in range(self.NB):
                base = b * REG
                for c0 in (base, base + REG_C - HALO):
                    pass
                kb.dma('pool', hv[:, :, base:base + HALO], z[:, :, 0:HALO], reads=['zero'], writes=['hTs_halo'])
                kb.dma('pool', hv[:, :, base + REG_C - HALO:base + REG_C + HALO], z[:, :, :], reads=['zero'], writes=['hTs_halo'])
                kb.dma('pool', hv[:, :, base + REG - HALO:base + REG], z[:, :, 0:HALO], reads=['zero'], writes=['hTs_halo'])

    def mod_phase(self, li):
        kb, NB = self.kb, self.NB
        R = NB + 1
        with self.phase():
            cT = self.sb("cT", [128, 8, R])
            sT = self.sb("sT", [128, 8, R])
            ones = self.sb("ones", [1, 4])
            brow = self.sb("brow", [1, 6 * D])
            mrow = self.sb("mrow", [R, 6 * D])
            g12 = self.sb("g12", [R, 2, D])
            pm = [self.ps("pm%d" % i, [R, 512]) for i in range(2)]
            wm = [self.sb("wm%d" % i, [128, 8, 512]) for i in range(2)]
            with self.nc.allow_non_contiguous_dma(reason="tiny transposed load of conditioning vectors"):
                for r in range(R):
                    kb.dma('sp', cT[:, :, r], self.cvec[r, :].rearrange("(k p) -> p k", p=128), writes=['cT'])
            kb.op('act', lambda e: e.activation(out=sT[:], in_=cT[:], func=AF.Silu), reads=['cT'], writes=['sT'])
            kb.op('dve', lambda e: e.memset(ones[:], 1.0), writes=['ones'])
            kb.dma('sp', brow[:], self.b_mod[li:li + 1, :], writes=['brow'])
            kb.dma('sp', g12[:, 0, :], self.norm1_g[li:li + 1, :].to_broadcast([R, D]), writes=['g12'])
            kb.dma('sp', g12[:, 1, :], self.norm2_g[li:li + 1, :].to_broadcast([R, D]), writes=['g12'])
            for n in range(12):
                w = wm[n % 2]
                wk = 'wm%d' % (n % 2)
                pk = 'pm%d' % (n % 2)
                kb.dma('sp', w[:], self.w_mod[li, :, n * 512:(n + 1) * 512].rearrange("(k p) c -> p k c", p=128),
                       writes=[wk])
                for k in range(8):
                    self.mm(pm[n % 2][:], sT[:, k, :], w[:, k, :], k == 0, False, ['sT', wk], [pk])
                self.mm(pm[n % 2][:], ones[0:1, 0:R], brow[0:1, n * 512:(n + 1) * 512], False, True,
                        ['ones', 'brow'], [pk])
                kb.op('act', lambda e: e.copy(out=mrow[:, n * 512:(n + 1) * 512], in_=pm[n % 2][:]),
                      reads=[pk], writes=['mrow'])
            for (gi, sc) in ((0, 1), (1, 4)):
                kb.op('dve', lambda e: e.scalar_tensor_tensor(
                    out=mrow[:, sc * D:(sc + 1) * D], in0=mrow[:, sc * D:(sc + 1) * D], scalar=1.0,
                    in1=g12[:, gi, :], op0=ALU.add, op1=ALU.mult), reads=['mrow', 'g12'], writes=['mrow'])
            kb.dma('pool', self.modv[li].rearrange("r s d -> r (s d)"), mrow[:], reads=['mrow'], writes=['modv'])

    def load_bc(self, t, li, r, s, key):
        self.kb.dma('sp', t[:], self.modv[li, r, s:s + 1, :].to_broadcast([128, D]), reads=['modv'], writes=[key])

    def norm_phase(self, li, which, layer0, ctx_needed=True):
        kb, NB = self.kb, self.NB
        s_sh, s_g = (0, 1) if which == 1 else (3, 4)
        with self.phase():
            ident = self.sb("identb", [128, 128], BF16)
            kb.dma('pool', ident[:], self.c_ident, writes=['ident'])
            Gt = [self.sb("G%d" % r, [128, D]) for r in range(NB + 1)]
            St = [self.sb("S%d" % r, [128, D]) for r in range(NB + 1)]
            for r in range(NB + 1):
                self.load_bc(Gt[r], li, r, s_g, 'G%d' % r)
                self.load_bc(St[r], li, r, s_sh, 'S%d' % r)
            NBUF = 3
            xt = [self.sb("xt%d" % i, [128, D]) for i in range(NBUF)]
            sq = self.sb("sq", [128, D])
            st = [self.sb("st%d" % i, [128, 4]) for i in range(NBUF)]
            hb = [self.sb("hb%d" % i, [128, D], BF16) for i in range(NBUF)]
            pt = [self.ps("pt%d" % i, [128, 8, 128], BF16) for i in range(2)]
            hw = [self.sb("hw%d" % i, [128, 8, 256], BF16) for i in range(2)]
            hv = self.hT_view()
            it = 0
            wi = 0
            for b in range(NB):
                for w in range(NT // 2):
                    if w == 0 and not ctx_needed:
                        continue
                    hwk = 'hw%d' % (wi % 2)
                    for t2 in range(2):
                        j = 2 * w + t2
                        r = NB if j < NTC else b
                        i = it % NBUF
                        src, skey = self.src_x(layer0, b, j)
                        kb.dma('sp', xt[i][:], src, reads=[skey], writes=['xt%d' % i])
                        kb.op('act', lambda e: e.activation(out=sq[:], in_=xt[i][:], func=AF.Square,
                                                            accum_out=st[i][:, 0:1]),
                              reads=['xt%d' % i], writes=['sq', 'st%d' % i])
                        kb.op('dve', lambda e: e.tensor_scalar(out=st[i][:, 1:2], in0=st[i][:, 0:1], scalar1=1.0 / D,
                                                               scalar2=EPS, op0=ALU.mult, op1=ALU.add),
                              reads=['st%d' % i], writes=['st%d' % i])
                        kb.op('act', lambda e: e.activation(out=st[i][:, 2:3], in_=st[i][:, 1:2], func=AF.Sqrt),
                              reads=['st%d' % i], writes=['st%d' % i])
                        kb.op('dve', lambda e: e.reciprocal(out=st[i][:, 3:4], in_=st[i][:, 2:3]),
                              reads=['st%d' % i], writes=['st%d' % i])
                        kb.op('dve', lambda e: e.scalar_tensor_tensor(out=xt[i][:], in0=xt[i][:], scalar=st[i][:, 3:4],
                                                                      in1=Gt[r][:], op0=ALU.mult, op1=ALU.mult),
                              reads=['xt%d' % i, 'st%d' % i, 'G%d' % r], writes=['xt%d' % i])
                        kb.op('pool', lambda e: e.tensor_tensor(out=hb[i][:], in0=xt[i][:], in1=St[r][:], op=ALU.add),
                              reads=['xt%d' % i, 'S%d' % r], writes=['hb%d' % i])
                        p = pt[it % 2]
                        pk = 'pt%d' % (it % 2)
                        for k in range(8):
                            self.tr(p[:, k, :], hb[i][:, k * 128:(k + 1) * 128], ident[:], ['hb%d' % i, 'ident'], [pk])
                        kb.op('act', lambda e: e.copy(out=hw[wi % 2][:, :, t2 * 128:(t2 + 1) * 128], in_=p[:]),
                              reads=[pk], writes=[hwk])
                        it += 1
                    c0 = self.hcol(b, 2 * w)
                    kb.dma('pool', hv[:, :, c0:c0 + 256], hw[wi % 2][:], reads=[hwk], writes=['hTs_%d' % wi])
                    wi += 1

    def outproj_setup(self, w_out_ap, li):
        kb, NB = self.kb, self.NB
        o = {}
        o['w'] = self.sb("wout", [128, 8, D], BF16)
        kb.dma('pool', o['w'][:], w_out_ap.rearrange("(k p) c -> p k c", p=128), writes=['wout'])
        o['ident'] = self.sb("identb", [128, 128], BF16)
        kb.dma('pool', o['ident'][:], self.c_ident, writes=['identb'])
        o['M'] = [self.sb("M2_%d" % r, [128, D]) for r in range(NB + 1)]
        for r in range(NB + 1):
            self.load_bc(o['M'][r], li, r, 2, 'M2_%d' % r)
        o['pt'] = self.ps("opt", [128, 8, 128], BF16)
        o['py'] = self.ps("opy", [128, 2, 512])
        o['oT'] = self.sb("oT", [128, 8, 128], BF16)
        o['xt'] = [self.sb("oxt%d" % i, [128, D]) for i in range(2)]
        o['n'] = 0
        return o

    def outproj_tile(self, o, ob, obkey, layer0, b, j):
        kb = self.kb
        r = self.NB if j < NTC else b
        for k in range(8):
            self.tr(o['pt'][:, k, :], ob[:, k * 128:(k + 1) * 128], o['ident'][:], [obkey, 'identb'], ['opt'])
        kb.op('act', lambda e: e.copy(out=o['oT'][:], in_=o['pt'][:]), reads=['opt'], writes=['oT'])
        for n in range(2):
            for k in range(8):
                self.mm(o['py'][:, n, :], o['oT'][:, k, :], o['w'][:, k, n * 512:(n + 1) * 512], k == 0, k == 7,
                        ['oT', 'wout'], ['opy'])
        i = o['n'] % 2
        o['n'] += 1
        xt = o['xt'][i]
        xk = 'oxt%d' % i
        src, skey = self.src_x(layer0, b, j)
        kb.dma('sp', xt[:], src, reads=[skey], writes=[xk])
        yk = 'oy%d' % i
        kb.op('dve', lambda e: e.tensor_tensor(out=o['py'][:].rearrange("p a b -> p (a b)"),
                                               in0=o['py'][:].rearrange("p a b -> p (a b)"),
                                               in1=o['M'][r][:], op=ALU.mult),
              reads=['opy', 'M2_%d' % r], writes=['opy'])
        kb.op('dve', lambda e: e.tensor_tensor(out=xt[:], in0=o['py'][:].rearrange("p a b -> p (a b)"), in1=xt[:],
                                               op=ALU.add),
              reads=['opy', xk], writes=[xk])
        kb.dma('pool', self.xs[b, j * 128:(j + 1) * 128, :], xt[:], reads=[xk], writes=['xs_%d_%d' % (b, j)])

    def ffn_phase(self, li, ctx_needed=True):
        kb, NB = self.kb, self.NB
        NF = FFN // 128
        HF = NF // 2
        hv = self.hT_view()
        for ps_ in range(2):
            with self.phase():
                f0 = ps_ * HF
                wv = self.sb("wv", [128, 8, HF * 128], BF16)
                wg = self.sb("wg", [128, 8, HF * 128], BF16)
                wo = self.sb("wo", [128, HF, D], BF16)
                win = self.ffn_w_in[li].rearrange("(k p) c -> p k c", p=128)
                for k in range(8):
                    kb.dma('pool', wv[:, k, :], win[:, k, f0 * 128:(f0 + HF) * 128], writes=['wv'])
                    kb.dma('pool', wg[:, k, :], win[:, k, FFN + f0 * 128:FFN + (f0 + HF) * 128], writes=['wg'])
                kb.dma('pool', wo[:], self.ffn_w_out[li, f0 * 128:(f0 + HF) * 128, :].rearrange("(f p) c -> p f c", p=128),
                       writes=['wo'])
                cw = self.sb("cw", [128, HF, 4])
                with self.nc.allow_non_contiguous_dma(reason="tiny per-channel conv taps"):
                    for t in range(3):
                        kb.dma('sp', cw[:, :, t], self.ffn_conv_w[li, t, f0 * 128:(f0 + HF) * 128].rearrange("(f p) -> p f", p=128),
                               writes=['cw'])
                    kb.dma('sp', cw[:, :, 3], self.ffn_conv_b[li, f0 * 128:(f0 + HF) * 128].rearrange("(f p) -> p f", p=128),
                           writes=['cw'])
                M5 = [self.sb("M5_%d" % r, [128, D]) for r in range(NB + 1)]
                for r in range(NB + 1):
                    self.load_bc(M5[r], li, r, 5, 'M5_%d' % r)
                hT = [self.sb("fh%d" % i, [128, 8, 256 + 2 * HALO], BF16) for i in range(2)]
                u = [self.sb("fu%d" % i, [128, HF, 256], BF16) for i in range(2)]
                tA = [self.sb("ftA%d" % i, [128, 256]) for i in range(2)]
                tB = [self.sb("ftB%d" % i, [128, 256]) for i in range(2)]
                pv = [self.ps("fpv%d" % i, [128, 512]) for i in range(2)]
                pg = [self.ps("fpg%d" % i, [128, 512]) for i in range(2)]
                py = [self.ps("fpy%d" % i, [128, 2, 512]) for i in range(2)]
                xt = [self.sb("fx%d" % i, [128, D]) for i in range(2)]
                wi = 0
                ci = 0
                ti = 0
                for b in range(NB):
                    for w in range(NT // 2):
                        if w == 0 and not ctx_needed:
                            continue
                        h = hT[wi % 2]
                        hk = 'fh%d' % (wi % 2)
                        uu = u[wi % 2]
                        uk = 'fu%d' % (wi % 2)
                        c0 = self.hcol(b, 2 * w)
                        kb.dma('sp', h[:], hv[:, :, c0 - HALO:c0 + 256 + HALO], reads=['hTs'], writes=[hk])
                        with self.nosame():
                            for f in range(HF):
                                a = ci % 2
                                ci += 1
                                for k in range(8):
                                    self.mm(pv[a][:, 0:256], wv[:, k, f * 128:(f + 1) * 128], h[:, k, HALO:HALO + 256],
                                            k == 0, k == 7, ['wv', hk], ['fpv%d' % a])
                                for k in range(8):
                                    self.mm(pg[a][:, 0:258], wg[:, k, f * 128:(f + 1) * 128], h[:, k, HALO - 1:HALO + 257],
                                            k == 0, k == 7, ['wg', hk], ['fpg%d' % a])
                                kb.op('dve', lambda e: e.tensor_scalar(out=tA[a][:], in0=pg[a][:, 0:256], scalar1=cw[:, f, 0:1],
                                                                       scalar2=None, op0=ALU.mult),
                                      reads=['fpg%d' % a, 'cw'], writes=['ftA%d' % a])
                                kb.op('dve', lambda e: e.scalar_tensor_tensor(out=tA[a][:], in0=pg[a][:, 1:257], scalar=cw[:, f, 1:2],
                                                                              in1=tA[a][:], op0=ALU.mult, op1=ALU.add),
                                      reads=['fpg%d' % a, 'cw', 'ftA%d' % a], writes=['ftA%d' % a])
                                kb.op('dve', lambda e: e.scalar_tensor_tensor(out=tA[a][:], in0=pg[a][:, 2:258], scalar=cw[:, f, 2:3],
                                                                              in1=tA[a][:], op0=ALU.mult, op1=ALU.add),
                                      reads=['fpg%d' % a, 'cw', 'ftA%d' % a], writes=['ftA%d' % a])
                                kb.op('act', lambda e: e.activation(out=tB[a][:], in_=tA[a][:], func=AF.Silu, bias=cw[:, f, 3:4]),
                                      reads=['ftA%d' % a, 'cw'], writes=['ftB%d' % a])
                                kb.op('dve', lambda e: e.tensor_tensor(out=uu[:, f, :], in0=pv[a][:, 0:256], in1=tB[a][:], op=ALU.mult),
                                      reads=['fpv%d' % a, 'ftB%d' % a], writes=[uk])
                        for t2 in range(2):
                            j = 2 * w + t2
                            r = NB if j < NTC else b
                            i = ti % 2
                            ti += 1
                            for n in range(2):
                                for f in range(HF):
                                    self.mm(py[i][:, n, :], uu[:, f, t2 * 128:(t2 + 1) * 128], wo[:, f, n * 512:(n + 1) * 512],
                                            f == 0, f == HF - 1, [uk, 'wo'], ['fpy%d' % i])
                            kb.dma('sp', xt[i][:], self.xs[b, j * 128:(j + 1) * 128, :], reads=['xs_%d_%d' % (b, j)], writes=['fx%d' % i])
                            pyf = py[i][:].rearrange("p a b -> p (a b)")
                            kb.op('dve', lambda e: e.tensor_tensor(out=pyf, in0=pyf, in1=M5[r][:], op=ALU.mult),
                                  reads=['fpy%d' % i, 'M5_%d' % r], writes=['fpy%d' % i])
                            kb.op('dve', lambda e: e.tensor_tensor(out=xt[i][:], in0=pyf, in1=xt[i][:], op=ALU.add),
                                  reads=['fpy%d' % i, 'fx%d' % i], writes=['fx%d' % i])
                            kb.dma('pool', self.xs[b, j * 128:(j + 1) * 128, :], xt[i][:], reads=['fx%d' % i],
                                   writes=['xs_%d_%d' % (b, j)])
                        wi += 1

    def final_phase(self):
        kb, NB = self.kb, self.NB
        with self.phase():
            G = self.sb("fG", [128, D])
            kb.dma('sp', G[:], self.final_norm_g[0:1, :].to_broadcast([128, D]), writes=['fG'])
            xt = [self.sb("fx%d" % i, [128, D]) for i in range(3)]
            sq = self.sb("fsq", [128, D])
            st = [self.sb("fst%d" % i, [128, 4]) for i in range(3)]
            it = 0
            for b in range(NB):
                for j in range(NTC, NT):
                    i = it % 3
                    it += 1
                    kb.dma('sp', xt[i][:], self.xs[b, j * 128:(j + 1) * 128, :], reads=['xs_%d_%d' % (b, j)], writes=['fx%d' % i])
                    kb.op('act', lambda e: e.activation(out=sq[:], in_=xt[i][:], func=AF.Square, accum_out=st[i][:, 0:1]),
                          reads=['fx%d' % i], writes=['fsq', 'fst%d' % i])
                    kb.op('dve', lambda e: e.tensor_scalar(out=st[i][:, 1:2], in0=st[i][:, 0:1], scalar1=1.0 / D,
                                                           scalar2=EPS, op0=ALU.mult, op1=ALU.add),
                          reads=['fst%d' % i], writes=['fst%d' % i])
                    kb.op('act', lambda e: e.activation(out=st[i][:, 2:3], in_=st[i][:, 1:2], func=AF.Sqrt),
                          reads=['fst%d' % i], writes=['fst%d' % i])
                    kb.op('dve', lambda e: e.reciprocal(out=st[i][:, 3:4], in_=st[i][:, 2:3]),
                          reads=['fst%d' % i], writes=['fst%d' % i])
                    kb.op('dve', lambda e: e.scalar_tensor_tensor(out=xt[i][:], in0=xt[i][:], scalar=st[i][:, 3:4],
                                                                  in1=G[:], op0=ALU.mult, op1=ALU.mult),
                          reads=['fx%d' % i, 'fst%d' % i, 'fG'], writes=['fx%d' % i])
                    kb.dma('pool', self.out[b, (j - NTC) * 128:(j - NTC + 1) * 128, :], xt[i][:], reads=['fx%d' % i],
                           writes=['out_%d_%d' % (b, j)])

    def attn_phase(self, li, jx, layer0, want_ctx):
        kb, NB = self.kb, self.NB
        hv = self.hT_view()
        SC = 128 ** -0.5
        for b in range(NB):
            with self.phase():
                qT = self.sb("qT", [128, 8, TOK], BF16)
                kT = self.sb("kT", [128, 2, TOK], BF16)
                V1 = self.sb("V1", [128, NT, 2, 132], BF16)
                kb.op('pool', lambda e: e.memset(V1[:, :, :, 128:129], 1.0), writes=['V1'])
                ident = self.sb("identb", [128, 128], BF16)
                kb.dma('pool', ident[:], self.c_ident, writes=['ident'])
                with self.phase():
                    win = self.sb("awin", [128, 8, 1536], BF16)
                    kb.dma('pool', win[:], self.attn_w_in[jx].rearrange("(k p) c -> p k c", p=128), writes=['awin'])
                    Gqk = self.sb("Gqk", [128, 2, 128])
                    kb.dma('sp', Gqk[:, 0, :], self.attn_q_norm_g[jx:jx + 1, :].to_broadcast([128, 128]), writes=['Gqk'])
                    kb.dma('sp', Gqk[:, 1, :], self.attn_k_norm_g[jx:jx + 1, :].to_broadcast([128, 128]), writes=['Gqk'])
                    hT = [self.sb("ah%d" % i, [128, 8, 128], BF16) for i in range(2)]
                    pz = [self.ps("apz%d" % i, [128, 512]) for i in range(3)]
                    zs = self.sb("azs", [128, 1536])
                    sq = self.sb("asq", [128, 1280])
                    st = self.sb("ast", [128, 4, 10])
                    qn = self.sb("aqn", [128, 1280])
                    qb = self.sb("aqb", [128, 1280], BF16)
                    rp = [self.sb("arp%d" % i, [128, 128]) for i in range(2)]
                    t1 = self.sb("at1", [128, 640])
                    t2 = self.sb("at2", [128, 640])
                    ptq = self.ps("aptq", [128, 8, 128], BF16)
                    ptk = self.ps("aptk", [128, 2, 128], BF16)
                    for j in range(NT):
                        i = j % 2
                        c0 = self.hcol(b, j)
                        kb.dma('sp', hT[i][:], hv[:, :, c0:c0 + 128], reads=['hTs'], writes=['ah%d' % i])
                        for n in range(3):
                            for k in range(8):
                                self.mm(pz[n][:], hT[i][:, k, :], win[:, k, n * 512:(n + 1) * 512], k == 0, k == 7,
                                        ['ah%d' % i, 'awin'], ['apz%d' % n])
                            kb.op('act', lambda e: e.copy(out=zs[:, n * 512:(n + 1) * 512], in_=pz[n][:]),
                                  reads=['apz%d' % n], writes=['azs'])
                        kb.op('pool', lambda e: e.tensor_copy(out=V1[:, j, :, 0:128],
                                                              in_=zs[:, 1280:1536].rearrange("p (g d) -> p g d", g=2)),
                              reads=['azs'], writes=['V1'])
                        kb.op('dve', lambda e: e.tensor_tensor(out=sq[:], in0=zs[:, 0:1280], in1=zs[:, 0:1280], op=ALU.mult),
                              reads=['azs'], writes=['asq'])
                        kb.op('dve', lambda e: e.tensor_reduce(out=st[:, 0, :], in_=sq[:].rearrange("p (h d) -> p h d", h=10),
                                                               axis=AX.X, op=ALU.add),
                              reads=['asq'], writes=['ast'])
                        kb.op('dve', lambda e: e.tensor_scalar(out=st[:, 1, :], in0=st[:, 0, :], scalar1=1.0 / 128, scalar2=EPS,
                                                               op0=ALU.mult, op1=ALU.add), reads=['ast'], writes=['ast'])
                        kb.op('act', lambda e: e.activation(out=st[:, 2, :], in_=st[:, 1, :], func=AF.Sqrt),
                              reads=['ast'], writes=['ast'])
                        kb.op('dve', lambda e: e.reciprocal(out=st[:, 3, :], in_=st[:, 2, :]), reads=['ast'], writes=['ast'])
                        z3 = zs[:, 0:1280].rearrange("p (h d) -> p h d", h=10)
                        q3 = qn[:].rearrange("p (h d) -> p h d", h=10)
                        kb.op('dve', lambda e: e.tensor_tensor(out=q3, in0=z3, in1=st[:, 3, :].unsqueeze(2).to_broadcast([128, 10, 128]),
                                                               op=ALU.mult), reads=['azs', 'ast'], writes=['aqn'])
                        kb.op('dve', lambda e: e.tensor_tensor(out=q3[:, 0:8, :], in0=q3[:, 0:8, :],
                                                               in1=Gqk[:, 0:1, :].to_broadcast([128, 8, 128]), op=ALU.mult),
                              reads=['aqn', 'Gqk'], writes=['aqn'])
                        kb.op('dve', lambda e: e.tensor_tensor(out=q3[:, 8:10, :], in0=q3[:, 8:10, :],
                                                               in1=Gqk[:, 1:2, :].to_broadcast([128, 2, 128]), op=ALU.mult),
                              reads=['aqn', 'Gqk'], writes=['aqn'])
                        if j >= NTC:
                            rr = rp[j % 2]
                            rk = 'arp%d' % (j % 2)
                            kb.dma('sp', rr[:], self.c_rope[(j - NTC) * 128:(j - NTC + 1) * 128, :], writes=[rk])
                            q4 = qn[:].rearrange("p (h d t) -> p h d t", h=10, t=2)
                            b4 = qb[:].rearrange("p (h d t) -> p h d t", h=10, t=2)
                            x0, x1 = q4[:, :, :, 0], q4[:, :, :, 1]
                            cosb = rr[:, 0:64].unsqueeze(1).to_broadcast([128, 10, 64])
                            sinb = rr[:, 64:128].unsqueeze(1).to_broadcast([128, 10, 64])
                            t13 = t1[:].rearrange("p (h d) -> p h d", h=10)
                            t23 = t2[:].rearrange("p (h d) -> p h d", h=10)
                            kb.op('dve', lambda e: e.tensor_tensor(out=t13, in0=x0, in1=cosb, op=ALU.mult), reads=['aqn', rk], writes=['at1'])
                            kb.op('pool', lambda e: e.tensor_tensor(out=t23, in0=x1, in1=sinb, op=ALU.mult), reads=['aqn', rk], writes=['at2'])
                            kb.op('dve', lambda e: e.tensor_tensor(out=b4[:, :, :, 0], in0=t13, in1=t23, op=ALU.subtract),
                                  reads=['at1', 'at2'], writes=['aqb'])
                            kb.op('dve', lambda e: e.tensor_tensor(out=t13, in0=x0, in1=sinb, op=ALU.mult), reads=['aqn', rk, 'aqb'], writes=['at1'])
                            kb.op('pool', lambda e: e.tensor_tensor(out=t23, in0=x1, in1=cosb, op=ALU.mult), reads=['aqn', rk, 'aqb'], writes=['at2'])
                            kb.op('dve', lambda e: e.tensor_tensor(out=b4[:, :, :, 1], in0=t13, in1=t23, op=ALU.add),
                                  reads=['at1', 'at2'], writes=['aqb'])
                        else:
                            kb.op('dve', lambda e: e.tensor_copy(out=qb[:], in_=qn[:]), reads=['aqn'], writes=['aqb'])
                        for h in range(8):
                            self.tr(ptq[:, h, :], qb[:, h * 128:(h + 1) * 128], ident[:], ['aqb', 'ident'], ['aptq'])
                        for g in range(2):
                            self.tr(ptk[:, g, :], qb[:, (8 + g) * 128:(9 + g) * 128], ident[:], ['aqb', 'ident'], ['aptk'])
                        kb.op('act', lambda e: e.copy(out=qT[:, :, j * 128:(j + 1) * 128], in_=ptq[:]), reads=['aptq'], writes=['qT'])
                        kb.op('act', lambda e: e.copy(out=kT[:, :, j * 128:(j + 1) * 128], in_=ptk[:]), reads=['aptk'], writes=['kT'])
                with self.phase():
                    o = self.outproj_setup(self.attn_w_out[jx], li)
                    E = self.sb("aE", [128, NT, 512], BF16)
                    pS = [self.ps("apS%d" % i, [128, 512]) for i in range(2)]
                    pO = [self.ps("apO%d" % i, [128, 512]) for i in range(2)]
                    ob = [self.sb("aob%d" % i, [128, D], BF16) for i in range(2)]
                    rc = self.sb("arc", [128, 8])
                    si = 0
                    oi = 0
                    for jq in range(NT):
                        if jq < NTC and not want_ctx:
                            continue
                        nk = NTC if jq < NTC else NT
                        obt = ob[jq % 2]
                        obk = 'aob%d' % (jq % 2)
                        for g in range(2):
                            for kt in range(nk):
                                p = pS[si % 2]
                                pk = 'apS%d' % (si % 2)
                                si += 1
                                self.mm(p[:].rearrange("p (h q) -> p h q", h=4), kT[:, g, kt * 128:(kt + 1) * 128], qT[:, 4 * g:4 * g + 4, jq * 128:(jq + 1) * 128],
                                        True, True, ['kT', 'qT'], [pk])
                                kb.op('act', lambda e: e.activation(out=E[:, kt, :], in_=p[:], func=AF.Exp, scale=SC),
                                      reads=[pk], writes=['aE'])
                            for hh in range(4):
                                po = pO[oi % 2]
                                pok = 'apO%d' % (oi % 2)
                                oi += 1
                                for kt in range(nk):
                                    self.mm(po[:, 0:129], E[:, kt, hh * 128:(hh + 1) * 128], V1[:, kt, g, 0:129],
                                            kt == 0, kt == nk - 1, ['aE', 'V1'], [pok])
                                hd = 4 * g + hh
                                kb.op('dve', lambda e: e.reciprocal(out=rc[:, hd:hd + 1], in_=po[:, 128:129]),
                                      reads=[pok], writes=['arc'])
                                kb.op('dve', lambda e: e.tensor_scalar(out=obt[:, hd * 128:(hd + 1) * 128], in0=po[:, 0:128],
                                                                       scalar1=rc[:, hd:hd + 1], scalar2=None, op0=ALU.mult),
                                      reads=[pok, 'arc'], writes=[obk])
                        self.outproj_tile(o, obt, obk, layer0, b, jq)

    def gdn_decl(self):
        if hasattr(self, 'g_qkT'):
            return
        NB = self.NB
        self.g_qkT = self.dram("g_qkT", [2, D, NB * TOK], F32)
        self.g_ktok = self.dram("g_ktok", [NB * TOK, D], F32)
        self.g_vtok = self.dram("g_vtok", [NB * TOK, D], F32)
        self.g_sgate = self.dram("g_sgate", [NB * TOK, D], F32)
        self.g_bg = self.dram("g_bg", [NB * TOK, 32], F32)
        self.g_odir = self.dram("g_odir", [2, NB * TOK, D], F32)

    def gdn_phase(self, li, jx, layer0, want_ctx):
        self.gdn_decl()
        self.gdn_proj(li, jx)
        self.gdn_scan(li, jx)
        self.gdn_finish(li, jx, layer0, want_ctx)

    def gdn_proj(self, li, jx):
        kb, NB = self.kb, self.NB
        hv = self.hT_view()
        with self.phase():
            win = self.sb("gwin", [128, 8, 4128], BF16)
            wsrc = self.gdn_w_in[jx].rearrange("(k p) c -> p k c", p=128)
            for k in range(8):
                kb.dma('pool', win[:, k, :], wsrc[:, k, :], writes=['gwin'])
            cw = self.sb("gcw", [128, 24, 5])
            with self.nc.allow_non_contiguous_dma(reason="tiny per-channel conv taps"):
                for t in range(5):
                    for f0 in range(0, 24, 8):
                        kb.dma('sp', cw[:, f0:f0 + 8, t], self.gdn_conv_w[jx, t, f0 * 128:(f0 + 8) * 128].rearrange("(f p) -> p f", p=128),
                               writes=['gcw'])
            identf = self.sb("identf", [128, 128])
            kb.dma('sp', identf[:], self.c_ident, writes=['identf'])
            onesf = self.sb("onesf", [128, 128])
            kb.op('dve', lambda e: e.memset(onesf[:], 1.0), writes=['onesf'])
            DTB = self.sb("gdtb", [128, 16])
            NA = self.sb("gna", [128, 16])
            kb.dma('sp', DTB[:], self.gdn_dt_bias[jx:jx + 1, :].to_broadcast([128, 16]), writes=['gdtb'])
            kb.dma('sp', NA[:], self.gdn_a_log[jx:jx + 1, :].to_broadcast([128, 16]), writes=['gna'])
            kb.op('act', lambda e: e.activation(out=NA[:], in_=NA[:], func=AF.Exp), reads=['gna'], writes=['gna'])
            kb.op('dve', lambda e: e.tensor_scalar(out=NA[:], in0=NA[:], scalar1=-1.0, scalar2=None, op0=ALU.mult),
                  reads=['gna'], writes=['gna'])
            h2 = [self.sb("gh%d" % i, [128, 8, 256 + 2 * HALO], BF16) for i in range(2)]
            NPZ = 2
            NB4 = 4
            pz = [self.ps("gpz%d" % i, [128, 512]) for i in range(NPZ)]
            pnbk = [self.ps("gpnb%d" % i, [128, 512]) for i in range(2)]
            pnl = [pnbk[0][:, 0:256], pnbk[1][:, 0:256]]
            pXb = [self.ps("gpX%d" % i, [128, 512]) for i in range(2)]
            pTl = [pXb[0][:, 0:256].rearrange("p (a b) -> p a b", a=2), pXb[1][:, 0:256].rearrange("p (a b) -> p a b", a=2)]
            pg = self.ps("gpg", [128, 2, 512])
            pb = pXb[1][:, 256:288]
            tA = [self.sb("gtA%d" % i, [128, 256]) for i in range(NB4)]
            sAll = [self.sb("gsAll%d" % i, [128, 24, 256]) for i in range(2)]
            rAll = [self.sb("grAll%d" % i, [128, 16, 256]) for i in range(2)]
            sqb = [self.sb("gsqb%d" % i, [128, 256], BF16) for i in range(NB4)]
            onesb = self.sb("gonesb", [128, 128], BF16)
            kb.op('dve', lambda e: e.memset(onesb[:], 1.0), writes=['onesb'])
            kst = self.sb("gkst", [128, 2, D])
            vst = self.sb("gvst", [128, 2, D])
            gst = [self.sb("ggst%d" % i, [128, D]) for i in range(2)]
            bgs = self.sb("gbgs", [128, 32])
            bt = self.sb("gbt", [128, 4, 16])
            qkTv = self.g_qkT.rearrange("q (h p) c -> q p h c", p=128)
            ci = 0
            wi = 0
            for b in range(NB):
                for w in range(NT // 2):
                    h = h2[wi % 2]
                    hk = 'gh%d' % (wi % 2)
                    wi += 1
                    c0 = self.hcol(b, 2 * w)
                    tok0 = b * TOK + 2 * w * 128
                    kb.dma('sp', h[:], hv[:, :, c0 - HALO:c0 + 256 + HALO], writes=[hk])
                    ws = wi % 2
                    sA, rA = sAll[ws], rAll[ws]
                    sAk = lambda f_: 'gsA%d_%d' % (ws, f_)
                    rAk = 'grA%d' % ws

                    def stage_ones(f_):
                        an_ = f_ % 2
                        self.mm(pnl[an_], onesb[:], sqb[f_ % NB4][:], True, True, ['onesb', 'gsqb%d' % (f_ % NB4)], ['gpn%d' % an_])
                        m_ = 128.0 if f_ < 8 else 1.0
                        kb.op('dve', lambda e: e.tensor_scalar(out=rA[:, f_, :], in0=pnl[an_], scalar1=m_, scalar2=EPS * m_,
                                                               op0=ALU.mult, op1=ALU.add), reads=['gpn%d' % an_], writes=[rAk])

                    def stage_tr(f_):
                        dst = kst if f_ < 16 else vst
                        dk = 'gkst' if f_ < 16 else 'gvst'
                        an_ = f_ % 2
                        pT, pTk = pTl[an_], 'gpX%d' % an_
                        for t2_ in range(2):
                            self.tr(pT[:, t2_, :], sA[:, f_, t2_ * 128:(t2_ + 1) * 128], identf[:], [sAk(f_), 'identf'], [pTk])
                        kb.op('act', lambda e: e.copy(out=dst[:, :, (f_ % 8) * 128:(f_ % 8 + 1) * 128], in_=pT), reads=[pTk], writes=[dk])

                    with self.nosame():
                        for f in range(24):
                            a = ci % NB4
                            az = ci % NPZ
                            ci += 1
                            pzk = 'gpz%d' % az
                            for k in range(8):
                                self.mm(pz[az][:, 0:260], win[:, k, f * 128:(f + 1) * 128], h[:, k, :], k == 0, k == 7,
                                        ['gwin', hk], [pzk])
                            tk = 'gtA%d' % a
                            kb.op('dve', lambda e: e.tensor_scalar(out=tA[a][:], in0=pz[az][:, 0:256], scalar1=cw[:, f, 0:1],
                                                                   scalar2=None, op0=ALU.mult), reads=[pzk, 'gcw'], writes=[tk])
                            for t in range(1, 5):
                                kb.op('dve', lambda e: e.scalar_tensor_tensor(out=tA[a][:], in0=pz[az][:, t:t + 256], scalar=cw[:, f, t:t + 1],
                                                                              in1=tA[a][:], op0=ALU.mult, op1=ALU.add),
                                      reads=[pzk, 'gcw', tk], writes=[tk])
                            kb.op('act', lambda e: e.activation(out=sA[:, f, :], in_=tA[a][:], func=AF.Silu), reads=[tk], writes=[sAk(f)])
                            if f < 16:
                                kb.op('pool', lambda e: e.tensor_tensor(out=sqb[f % NB4][:], in0=sA[:, f, :], in1=sA[:, f, :], op=ALU.mult),
                                      reads=[sAk(f)], writes=['gsqb%d' % (f % NB4)])
                            if 2 <= f < 18:
                                stage_ones(f - 2)
                            if f >= 19:
                                stage_tr(f - 3)
                        for f_ in (21, 22, 23):
                            stage_tr(f_)
                        kb.op('act', lambda e: e.activation(out=rA[:], in_=rA[:], func=AF.Sqrt), reads=[rAk], writes=[rAk])
                        for q4 in range(4):
                            kb.op('dve', lambda e: e.reciprocal(out=rA[:, 4 * q4:4 * q4 + 4, :], in_=rA[:, 4 * q4:4 * q4 + 4, :]),
                                  reads=[rAk], writes=[rAk])
                        for f in range(16):
                            kb.op('pool', lambda e: e.tensor_tensor(out=sA[:, f, :], in0=sA[:, f, :], in1=rA[:, f, :], op=ALU.mult),
                                  reads=[sAk(f), rAk], writes=[sAk(f)])
                            kb.dma('pool', qkTv[f // 8, :, f % 8, tok0:tok0 + 256], sA[:, f, :], reads=[sAk(f)], writes=['g_qkT_%d_%d' % (wi, f)])
                            if f >= 8:
                                stage_tr(f)
                    for t2 in range(2):
                        r0 = tok0 + t2 * 128
                        kb.dma('pool', self.g_ktok[r0:r0 + 128, :], kst[:, t2, :], reads=['gkst'], writes=['g_ktok_%d' % r0])
                        kb.dma('pool', self.g_vtok[r0:r0 + 128, :], vst[:, t2, :], reads=['gvst'], writes=['g_vtok_%d' % r0])
                        hs = h[:, :, HALO + t2 * 128:HALO + (t2 + 1) * 128]
                        for n in range(2):
                            for k in range(8):
                                self.mm(pg[:, n, :], hs[:, k, :], win[:, k, 3072 + n * 512:3072 + (n + 1) * 512], k == 0, k == 7,
                                        [hk, 'gwin'], ['gpg'])
                        g_ = gst[t2]
                        gk2 = 'ggst%d' % t2
                        kb.op('act', lambda e: e.activation(out=g_[:], in_=pg[:].rearrange("p a b -> p (a b)"), func=AF.Silu),
                              reads=['gpg'], writes=[gk2])
                        kb.dma('pool', self.g_sgate[r0:r0 + 128, :], g_[:], reads=[gk2], writes=['g_sgate_%d' % r0])
                        for k in range(8):
                            self.mm(pb, hs[:, k, :], win[:, k, 4096:4128], k == 0, k == 7, [hk, 'gwin'], ['gpX1'])
                        pb4 = pb.rearrange("p (d t h) -> p d t h", d=2, t=2)
                        kb.op('act', lambda e: e.activation(out=bgs[:, 0:16].rearrange("p (d h) -> p d h", d=2), in_=pb4[:, :, 0, :],
                                                            func=AF.Sigmoid), reads=['gpX1'], writes=['gbgs'])
                        kb.op('dve', lambda e: e.tensor_tensor(out=bt[:, 0, :].rearrange("p (d h) -> p d h", d=2), in0=pb4[:, :, 1, :],
                                                               in1=DTB[:].rearrange("p (d h) -> p d h", d=2), op=ALU.add),
                              reads=['gpX1', 'gdtb'], writes=['gbt'])
                        kb.op('dve', lambda e: e.scalar_tensor_tensor(out=bt[:, 1, :], in0=bt[:, 0, :], scalar=-1.0, in1=bt[:, 0, :],
                                                                      op0=ALU.mult, op1=ALU.max), reads=['gbt'], writes=['gbt'])
                        kb.op('act', lambda e: e.activation(out=bt[:, 2, :], in_=bt[:, 1, :], func=AF.Exp, scale=-1.0),
                              reads=['gbt'], writes=['gbt'])
                        kb.op('act', lambda e: e.activation(out=bt[:, 3, :], in_=bt[:, 2, :], func=AF.Ln, bias=1.0),
                              reads=['gbt'], writes=['gbt'])
                        kb.op('dve', lambda e: e.scalar_tensor_tensor(out=bt[:, 1, :], in0=bt[:, 0, :], scalar=0.0, in1=bt[:, 3, :],
                                                                      op0=ALU.max, op1=ALU.add), reads=['gbt'], writes=['gbt'])
                        kb.op('dve', lambda e: e.tensor_tensor(out=bgs[:, 16:32], in0=bt[:, 1, :], in1=NA[:], op=ALU.mult),
                              reads=['gbt', 'gna'], writes=['gbgs'])
                        kb.dma('pool', self.g_bg[r0:r0 + 128, :], bgs[:], reads=['gbgs'], writes=['g_bg_%d' % r0])

    def gdn_scan(self, li, jx):
        kb, NB = self.kb, self.NB
        HB = 4
        qkTv = self.g_qkT.rearrange("q (h p) c -> q p h c", p=128)
        order = [list(range(NT)), [1, 0] + list(range(NT - 1, NTC - 1, -1))]
        with self.phase():
            MS = self.sb("gmask", [128, 8, 128])
            kb.dma('sp', MS[:], self.c_masks[0:8].rearrange("m p c -> p m c"), writes=['gmask'])
            identf = self.sb("identf", [128, 128])
            kb.dma('sp', identf[:], self.c_ident, writes=['identf'])
            PS = [self.ps("gps%d" % i, [128, 512]) for i in range(8)]
            PK = ['gps%d' % i for i in range(8)]

            def v4(t):
                return t[:].rearrange("p (h c) -> p h c", h=HB)
            names = ['qT', 'kT', 'ktok', 'vtok', 'Gbc', 'Dm', 'E', 'DT', 'DTs', 'EG', 'qd', 'W', 'WT', 'Wa0', 'Wa1', 'Wb0', 'Wb1',
                     'QKm', 'kdec', 'wT', 'vn']
            sets = []
            for si in range(2):
                T = {}
                for nm in names:
                    T[nm] = self.sb("g%s%d" % (nm, si), [128, HB * 128])
                for nm in ('y0', 'y1', 'UW'):
                    T[nm] = self.sb("g%s%d" % (nm, si), [128, HB, 256])
                T['cols'] = self.sb("gcols%d" % si, [128, 6, HB])
                T['alast'] = self.sb("galast%d" % si, [128, HB, 2])
                T['k'] = 's%d_' % si
                sets.append(T)
            bgt = [self.sb("gbg%d" % i, [128, 32]) for i in range(4)]
            ost = [self.sb("gost%d" % i, [128, D]) for i in range(4)]
            S = [[self.sb("gS_%d_%d" % (d, hf), [128, HB, 128]) for hf in range(2)] for d in range(2)]
            stepi = 0
            bi = 0
            for b in range(NB):
                for d in range(2):
                    for hf in range(2):
                        kb.op('pool', lambda e: e.memset(S[d][hf][:], 0.0), writes=['gS_%d_%d' % (d, hf)])
                for n in range(NT):
                    for d in range(2):
                        j = order[d][n]
                        tok0 = b * TOK + j * 128
                        bg = bgt[bi % 4]
                        bgk = 'gbg%d' % (bi % 4)
                        os_ = ost[bi % 4]
                        osk = 'gost%d' % (bi % 4)
                        bi += 1
                        kb.dma('sp', bg[:], self.g_bg[tok0:tok0 + 128, :], writes=[bgk])
                        Ud, Usd, nUd, BO = MS[:, d, :], MS[:, 2 + d, :], MS[:, 4 + d, :], MS[:, 6, :]
                        for hf in range(2):
                            T = sets[stepi % 2]
                            K = lambda nm: T['k'] + nm
                            stepi += 1
                            h0 = hf * HB
                            Sk = 'gS_%d_%d' % (d, hf)
                            St = S[d][hf]
                            kb.dma('sp', v4(T['qT']), qkTv[0, :, h0:h0 + HB, tok0:tok0 + 128], writes=[K('qT')])
                            kb.dma('sp', v4(T['kT']), qkTv[1, :, h0:h0 + HB, tok0:tok0 + 128], writes=[K('kT')])
                            kb.dma('sp', T['ktok'][:], self.g_ktok[tok0:tok0 + 128, h0 * 128:(h0 + HB) * 128], writes=[K('ktok')])
                            kb.dma('sp', T['vtok'][:], self.g_vtok[tok0:tok0 + 128, h0 * 128:(h0 + HB) * 128], writes=[K('vtok')])
                            g4 = bg[:, 16 + d * 8 + h0:16 + d * 8 + h0 + HB]
                            be4 = bg[:, d * 8 + h0:d * 8 + h0 + HB]
                            bc4 = lambda ap: ap.unsqueeze(2).to_broadcast([128, HB, 128])
                            mk4 = lambda ap: ap.unsqueeze(1).to_broadcast([128, HB, 128])
                            kb.op('dve', lambda e: e.tensor_copy(out=v4(T['Gbc']), in_=bc4(g4)), reads=[bgk], writes=[K('Gbc')])
                            Gbc = v4(T['Gbc'])
                            for h in range(HB):
                                self.mm(PS[0][:, h * 128:(h + 1) * 128], Gbc[:, h, :], Ud, True, True, [K('Gbc'), 'gmask'], [PK[0]])
                            for h in range(HB):
                                self.mm(PS[1][:, h * 128:(h + 1) * 128], Gbc[:, h, :], Ud, True, False, [K('Gbc'), 'gmask'], [PK[1]])
                                self.mm(PS[1][:, h * 128:(h + 1) * 128], nUd, Gbc[:, h, :], False, True, [K('Gbc'), 'gmask'], [PK[1]])
                            self.mm(PS[5][:, 0:HB], Ud, g4, True, True, ['gmask', bgk], [PK[5]])
                            self.mm(PS[5][:, HB:2 * HB], BO, g4, True, True, ['gmask', bgk], [PK[5]])
                            for h in range(HB):
                                self.mm(PS[5][:, 8 + 2 * h:10 + 2 * h], Gbc[:, h, :], MS[:, 6, 0:128:64], True, True,
                                        [K('Gbc'), 'gmask'], [PK[5]])
                            cols = T['cols']
                            kb.op('act', lambda e: e.copy(out=cols[:, 0, :], in_=PS[5][:, 0:HB]), reads=[PK[5]], writes=[K('cols')])
                            kb.op('dve', lambda e: e.tensor_tensor(out=cols[:, 1, :], in0=PS[5][:, HB:2 * HB], in1=cols[:, 0, :], op=ALU.subtract),
                                  reads=[PK[5], K('cols')], writes=[K('cols')])
                            kb.op('act', lambda e: e.activation(out=cols[:, 2, :], in_=cols[:, 0, :], func=AF.Exp), reads=[K('cols')], writes=[K('cols')])
                            kb.op('act', lambda e: e.activation(out=cols[:, 3, :], in_=cols[:, 1, :], func=AF.Exp), reads=[K('cols')], writes=[K('cols')])
                            kb.op('act', lambda e: e.activation(out=T['alast'][:].rearrange("p h c -> p (h c)"), in_=PS[5][:, 8:8 + 2 * HB], func=AF.Exp),
                                  reads=[PK[5]], writes=[K('alast')])
                            kb.op('dve', lambda e: e.tensor_scalar(out=T['Dm'][:], in0=PS[1][:], scalar1=0.0, scalar2=None, op0=ALU.min),
                                  reads=[PK[1]], writes=[K('Dm')])
                            kb.op('act', lambda e: e.activation(out=T['E'][:], in_=T['Dm'][:], func=AF.Exp), reads=[K('Dm')], writes=[K('E')])
                            kb.op('dve', lambda e: e.tensor_tensor(out=v4(T['DT']), in0=v4(T['E']), in1=mk4(Ud), op=ALU.mult),
                                  reads=[K('E'), 'gmask'], writes=[K('DT')])
                            kb.op('pool', lambda e: e.tensor_tensor(out=v4(T['DTs']), in0=v4(T['E']), in1=mk4(Usd), op=ALU.mult),
                                  reads=[K('E'), 'gmask'], writes=[K('DTs')])
                            kb.op('act', lambda e: e.activation(out=T['EG'][:], in_=PS[0][:], func=AF.Exp), reads=[PK[0]], writes=[K('EG')])
                            kb.op('pool', lambda e: e.tensor_tensor(out=T['qd'][:], in0=T['qT'][:], in1=T['EG'][:], op=ALU.mult),
                                  reads=[K('qT'), K('EG')], writes=[K('qd')])
                            kT4, qT4 = v4(T['kT']), v4(T['qT'])
                            for h in range(HB):
                                self.mm(PS[2][:, h * 128:(h + 1) * 128], kT4[:, h, :], kT4[:, h, :], True, True, [K('kT')], [PK[2]])
                            for h in range(HB):
                                self.mm(PS[3][:, h * 128:(h + 1) * 128], kT4[:, h, :], qT4[:, h, :], True, True, [K('kT'), K('qT')], [PK[3]])
                            kb.op('dve', lambda e: e.tensor_tensor(out=T['DTs'][:], in0=PS[2][:], in1=T['DTs'][:], op=ALU.mult),
                                  reads=[PK[2], K('DTs')], writes=[K('DTs')])
                            kb.op('dve', lambda e: e.tensor_tensor(out=v4(T['W']).bitcast(F32R), in0=v4(T['DTs']), in1=bc4(be4), op=ALU.mult),
                                  reads=[K('DTs'), bgk], writes=[K('W')])
                            kb.op('dve', lambda e: e.tensor_tensor(out=T['QKm'][:], in0=PS[3][:], in1=T['DT'][:], op=ALU.mult),
                                  reads=[PK[3], K('DT')], writes=[K('QKm')])
                            kb.op('act', lambda e: e.copy(out=T['y0'][:, :, 0:128].bitcast(F32R), in_=v4(T['vtok'])), reads=[K('vtok')], writes=[K('y0')])
                            kb.op('dve', lambda e: e.tensor_tensor(out=T['y0'][:, :, 128:256].bitcast(F32R), in0=v4(T['ktok']), in1=bc4(cols[:, 2, :]), op=ALU.mult),
                                  reads=[K('ktok'), K('cols')], writes=[K('y0')])
                            kb.op('pool', lambda e: e.tensor_tensor(out=v4(T['kdec']), in0=v4(T['ktok']), in1=bc4(cols[:, 3, :]), op=ALU.mult),
                                  reads=[K('ktok'), K('cols')], writes=[K('kdec')])
                            with self.nosame():
                                W4 = v4(T['W'])
                                for h in range(HB):
                                    self.tr(PS[4][:, h * 128:(h + 1) * 128], W4[:, h, :], identf[:], [K('W'), 'identf'], [PK[4]])
                                kb.op('act', lambda e: e.copy(out=T['WT'][:].bitcast(F32R), in_=PS[4][:]), reads=[PK[4]], writes=[K('WT')])
                                pA = [PS[6], PS[7]]
                                ycur, ynxt = 'y0', 'y1'
                                for h in range(HB):
                                    self.mmr(pA[h // 2][:, (h % 2) * 256:(h % 2 + 1) * 256], W4[:, h, :], T[ycur][:, h, :], True, True,
                                             [K('W'), K(ycur)], [PK[6 + h // 2]])
                                for q in range(2):
                                    kb.op('dve', lambda e: e.tensor_tensor(out=T[ynxt][:, 2 * q:2 * q + 2, :].rearrange("p h c -> p (h c)").bitcast(F32R),
                                                                           in0=T[ycur][:, 2 * q:2 * q + 2, :].rearrange("p h c -> p (h c)"),
                                                                           in1=pA[q][:], op=ALU.subtract),
                                          reads=[K(ycur), PK[6 + q]], writes=[K(ynxt)])
                                ycur, ynxt = ynxt, ycur
                                cur, curT = 'W', 'WT'
                                for lvl in range(1, 6):
                                    na, nb_ = 'Wa%d' % (lvl % 2), 'Wb%d' % (lvl % 2)
                                    c4, cT4 = v4(T[cur]), v4(T[curT])
                                    for h in range(HB):
                                        self.mmr(PS[4][:, h * 128:(h + 1) * 128], cT4[:, h, :], c4[:, h, :], True, True, [K(cur), K(curT)], [PK[4]])
                                    if lvl < 5:
                                        for h in range(HB):
                                            self.mmr(PS[5][:, h * 128:(h + 1) * 128], c4[:, h, :], cT4[:, h, :], True, True, [K(cur), K(curT)], [PK[5]])
                                    kb.op('act', lambda e: e.copy(out=T[na][:].bitcast(F32R), in_=PS[4][:]), reads=[PK[4]], writes=[K(na)])
                                    if lvl < 5:
                                        kb.op('dve', lambda e: e.tensor_copy(out=T[nb_][:].bitcast(F32R), in_=PS[5][:]), reads=[PK[5]], writes=[K(nb_)])
                                    n4 = v4(T[na])
                                    for h in range(HB):
                                        self.mmr(pA[h // 2][:, (h % 2) * 256:(h % 2 + 1) * 256], n4[:, h, :], T[ycur][:, h, :], True, True,
                                                 [K(na), K(ycur)], [PK[6 + h // 2]])
                                    for q in range(2):
                                        kb.op('dve', lambda e: e.tensor_tensor(out=T[ynxt][:, 2 * q:2 * q + 2, :].rearrange("p h c -> p (h c)").bitcast(F32R),
                                                                               in0=T[ycur][:, 2 * q:2 * q + 2, :].rearrange("p h c -> p (h c)"),
                                                                               in1=pA[q][:], op=ALU.add),
                                              reads=[K(ycur), PK[6 + q]], writes=[K(ynxt)])
                                    ycur, ynxt = ynxt, ycur
                                    cur, curT = na, nb_
                                kb.op('dve', lambda e: e.tensor_tensor(out=T['UW'][:], in0=T[ycur][:], in1=be4.unsqueeze(2).to_broadcast([128, HB, 256]),
                                                                       op=ALU.mult), reads=[K(ycur), bgk], writes=[K('UW')])
                            UW = T['UW']
                            for h in range(HB):
                                self.tr(PS[3][:, h * 128:(h + 1) * 128], UW[:, h, 128:256], identf[:], [K('UW'), 'identf'], [PK[3]])
                            kb.op('act', lambda e: e.copy(out=T['wT'][:], in_=PS[3][:]), reads=[PK[3]], writes=[K('wT')])
                            wT4, qd4, QK4, kd4, vn4 = v4(T['wT']), v4(T['qd']), v4(T['QKm']), v4(T['kdec']), v4(T['vn'])
                            for c in ([0, 1] if d == 0 else [1, 0]):
                                rw = slice(64 * c, 64 * c + 64)
                                for h in range(HB):
                                    self.mm(PS[0][rw, h * 128:(h + 1) * 128], wT4[:, h, rw], St[:, h, :], True, True, [K('wT'), Sk], [PK[0]])
                                kb.op('dve', lambda e: e.tensor_tensor(out=vn4[rw], in0=UW[rw, :, 0:128],
                                                                       in1=PS[0][rw, :].rearrange("p (h c) -> p h c", h=HB), op=ALU.subtract),
                                      reads=[K('UW'), PK[0]], writes=[K('vn')])
                                for h in range(HB):
                                    self.mm(PS[1][rw, h * 128:(h + 1) * 128], qd4[:, h, rw], St[:, h, :], True, False, [K('qd'), Sk], [PK[1]])
                                    self.mm(PS[1][rw, h * 128:(h + 1) * 128], QK4[rw, h, rw], vn4[rw, h, :], False, True, [K('QKm'), K('vn')], [PK[1]])
                                kb.op('act', lambda e: e.copy(out=os_[rw, h0 * 128:(h0 + HB) * 128], in_=PS[1][rw, :]), reads=[PK[1]], writes=[osk])
                                for h in range(HB):
                                    self.mm(PS[2][:, h * 128:(h + 1) * 128], kd4[rw, h, :], vn4[rw, h, :], True, True, [K('kdec'), K('vn')], [PK[2]])
                                kb.op('dve', lambda e: e.tensor_tensor(out=St[:], in0=St[:], in1=T['alast'][:, :, c].unsqueeze(2).to_broadcast([128, HB, 128]),
                                                                       op=ALU.mult), reads=[Sk, K('alast')], writes=[Sk])
                                kb.op('dve', lambda e: e.tensor_tensor(out=St[:].rearrange("p h c -> p (h c)"), in0=St[:].rearrange("p h c -> p (h c)"),
                                                                       in1=PS[2][:], op=ALU.add), reads=[Sk, PK[2]], writes=[Sk])
                        kb.dma('pool', self.g_odir[d, tok0:tok0 + 128, :], os_[:], reads=[osk], writes=['g_odir_%d_%d' % (d, tok0)])

    def gdn_finish(self, li, jx, layer0, want_ctx):
        kb, NB = self.kb, self.NB
        with self.phase():
            o = self.outproj_setup(self.gdn_w_out[jx], li)
            NG = self.sb("gng", [128, 128])
            kb.dma('sp', NG[:], self.gdn_norm_g[jx:jx + 1, :].to_broadcast([128, 128]), writes=['gng'])
            self.rec_finish(o, NG, 'gng', self.g_odir, self.g_sgate, layer0, want_ctx)

    def rec_finish(self, o, NG, ngk, odir, sgate, layer0, want_ctx):
        kb, NB = self.kb, self.NB
        oa = [self.sb("foa%d" % i, [128, D]) for i in range(2)]
        obt = [self.sb("fob%d" % i, [128, D]) for i in range(2)]
        sg = [self.sb("fsg%d" % i, [128, D]) for i in range(2)]
        sq = self.sb("fsq", [128, D])
        st = self.sb("fst", [128, 4, 8])
        ob = [self.sb("fobb%d" % i, [128, D], BF16) for i in range(2)]
        it = 0
        for b in range(NB):
            for j in range(NT):
                if j < NTC and not want_ctx:
                    continue
                i = it % 2
                it += 1
                tok0 = b * TOK + j * 128
                kb.dma('sp', oa[i][:], odir[0, tok0:tok0 + 128, :], writes=['foa%d' % i])
                kb.dma('sp', obt[i][:], odir[1, tok0:tok0 + 128, :], writes=['fob%d' % i])
                kb.dma('sp', sg[i][:], sgate[tok0:tok0 + 128, :], writes=['fsg%d' % i])
                kb.op('dve', lambda e: e.tensor_tensor(out=oa[i][:], in0=oa[i][:], in1=obt[i][:], op=ALU.add),
                      reads=['foa%d' % i, 'fob%d' % i], writes=['foa%d' % i])
                kb.op('pool', lambda e: e.tensor_tensor(out=sq[:], in0=oa[i][:], in1=oa[i][:], op=ALU.mult), reads=['foa%d' % i], writes=['fsq'])
                kb.op('dve', lambda e: e.tensor_reduce(out=st[:, 0, :], in_=sq[:].rearrange("p (h d) -> p h d", h=8), axis=AX.X, op=ALU.add),
                      reads=['fsq'], writes=['fst'])
                kb.op('dve', lambda e: e.tensor_scalar(out=st[:, 1, :], in0=st[:, 0, :], scalar1=1.0 / 128, scalar2=EPS, op0=ALU.mult, op1=ALU.add),
                      reads=['fst'], writes=['fst'])
                kb.op('act', lambda e: e.activation(out=st[:, 2, :], in_=st[:, 1, :], func=AF.Sqrt), reads=['fst'], writes=['fst'])
                kb.op('dve', lambda e: e.reciprocal(out=st[:, 3, :], in_=st[:, 2, :]), reads=['fst'], writes=['fst'])
                o3 = oa[i][:].rearrange("p (h d) -> p h d", h=8)
                kb.op('dve', lambda e: e.tensor_tensor(out=o3, in0=o3, in1=st[:, 3, :].unsqueeze(2).to_broadcast([128, 8, 128]), op=ALU.mult),
                      reads=['foa%d' % i, 'fst'], writes=['foa%d' % i])
                kb.op('pool', lambda e: e.tensor_tensor(out=o3, in0=o3, in1=NG[:].unsqueeze(1).to_broadcast([128, 8, 128]), op=ALU.mult),
                      reads=['foa%d' % i, ngk], writes=['foa%d' % i])
                kb.op('dve', lambda e: e.tensor_tensor(out=ob[i][:], in0=oa[i][:], in1=sg[i][:], op=ALU.mult),
                      reads=['foa%d' % i, 'fsg%d' % i], writes=['fobb%d' % i])
                self.outproj_tile(o, ob[i], 'fobb%d' % i, layer0, b, j)

    def mlstm_decl(self):
        self.gdn_decl()
        if hasattr(self, 'm_qkT'):
            return
        NB = self.NB
        self.m_qkT = self.dram("m_qkT", [2, 512, NB * TOK], F32)
        self.m_ktok = self.dram("m_ktok", [NB * TOK, 512], F32)

    def mlstm_phase(self, li, jx, layer0, want_ctx):
        self.mlstm_decl()
        self.mlstm_proj(li, jx)
        self.mlstm_scan(li, jx)
        kb = self.kb
        with self.phase():
            o = self.outproj_setup(self.mlstm_w_out[jx], li)
            NG = self.sb("mng", [128, 128])
            kb.dma('sp', NG[:], self.mlstm_norm_g[jx:jx + 1, :].to_broadcast([128, 128]), writes=['mng'])
            self.rec_finish(o, NG, 'mng', self.g_odir, self.g_sgate, layer0, want_ctx)

    def mlstm_proj(self, li, jx):
        kb, NB = self.kb, self.NB
        hv = self.hT_view()
        with self.phase():
            win = self.sb("mwin", [128, 8, 3104], BF16)
            wsrc = self.mlstm_w_in[jx].rearrange("(k p) c -> p k c", p=128)
            for k in range(8):
                kb.dma('pool', win[:, k, :], wsrc[:, k, :], writes=['mwin'])
            identf = self.sb("identf", [128, 128])
            kb.dma('sp', identf[:], self.c_ident, writes=['identf'])
            GB = self.sb("mgb", [128, 32])
            kb.dma('sp', GB[:], self.mlstm_gate_b[jx:jx + 1, :].to_broadcast([128, 32]), writes=['mgb'])
            h2 = [self.sb("mh%d" % i, [128, 8, 256], BF16) for i in range(2)]
            pz = [self.ps("mpz%d" % i, [128, 512]) for i in range(2)]
            pT = self.ps("mpT", [128, 2, 128])
            pg = [self.ps("mpg%d" % i, [128, 2, 512]) for i in range(2)]
            pb = self.ps("mpb", [128, 32])
            stg = [self.sb("mstg%d" % i, [128, 256]) for i in range(2)]
            kst = self.sb("mkst", [128, 2, 512])
            gst = [self.sb("mgst%d" % i, [128, D]) for i in range(4)]
            gls = self.sb("mgls", [128, 32])
            bt = self.sb("mbt", [128, 4, 16])
            ci = 0
            wi = 0
            gi = 0
            for b in range(NB):
                for w in range(NT // 2):
                    h = h2[wi % 2]
                    hk = 'mh%d' % (wi % 2)
                    wi += 1
                    c0 = self.hcol(b, 2 * w)
                    tok0 = b * TOK + 2 * w * 128
                    kb.dma('sp', h[:], hv[:, :, c0:c0 + 256], writes=[hk])
                    for f in range(8):
                        a = ci % 2
                        ci += 1
                        pzk = 'mpz%d' % a
                        for k in range(8):
                            self.mm(pz[a][:, 0:256], win[:, k, f * 128:(f + 1) * 128], h[:, k, :], k == 0, k == 7, ['mwin', hk], [pzk])
                        gk = 'mstg%d' % a
                        kb.op('act', lambda e: e.activation(out=stg[a][:], in_=pz[a][:, 0:256], func=AF.Identity, scale=(0.125 if f < 4 else 1.0)),
                              reads=[pzk], writes=[gk])
                        kb.dma('pool', self.m_qkT[f // 4, (f % 4) * 128:(f % 4 + 1) * 128, tok0:tok0 + 256], stg[a][:], reads=[gk],
                               writes=['m_qkT_%d' % ci])
                        if f >= 4:
                            for t2 in range(2):
                                self.tr(pT[:, t2, :], stg[a][:, t2 * 128:(t2 + 1) * 128], identf[:], [gk, 'identf'], ['mpT'])
                            kb.op('dve', lambda e: e.tensor_copy(out=kst[:, :, (f - 4) * 128:(f - 3) * 128], in_=pT[:]), reads=['mpT'], writes=['mkst'])
                    for t2 in range(2):
                        r0 = tok0 + t2 * 128
                        kb.dma('pool', self.m_ktok[r0:r0 + 128, :], kst[:, t2, :], reads=['mkst'], writes=['m_ktok_%d' % r0])
                        hs = h[:, :, t2 * 128:(t2 + 1) * 128]
                        for part in range(2):
                            p_ = pg[part]
                            pk = 'mpg%d' % part
                            for n in range(2):
                                for k in range(8):
                                    c1 = 1024 + part * 1024 + n * 512
                                    self.mm(p_[:, n, :], hs[:, k, :], win[:, k, c1:c1 + 512], k == 0, k == 7, [hk, 'mwin'], [pk])
                            g_ = gst[gi % 4]
                            gk2 = 'mgst%d' % (gi % 4)
                            gi += 1
                            if part == 0:
                                kb.op('dve', lambda e: e.tensor_copy(out=g_[:], in_=p_[:].rearrange("p a b -> p (a b)")), reads=[pk], writes=[gk2])
                                kb.dma('pool', self.g_vtok[r0:r0 + 128, :], g_[:], reads=[gk2], writes=['g_vtok_%d' % r0])
                            else:
                                kb.op('act', lambda e: e.activation(out=g_[:], in_=p_[:].rearrange("p a b -> p (a b)"), func=AF.Sigmoid),
                                      reads=[pk], writes=[gk2])
                                kb.dma('pool', self.g_sgate[r0:r0 + 128, :], g_[:], reads=[gk2], writes=['g_sgate_%d' % r0])
                        for k in range(8):
                            self.mm(pb[:], hs[:, k, :], win[:, k, 3072:3104], k == 0, k == 7, [hk, 'mwin'], ['mpb'])
                        x4 = bt[:, 0:2, :].rearrange("p a c -> p (a c)")
                        kb.op('dve', lambda e: e.tensor_tensor(out=x4, in0=pb[:], in1=GB[:], op=ALU.add), reads=['mpb', 'mgb'], writes=['mbt'])
                        x5 = x4.rearrange("p (d t h) -> p d t h", d=2, t=2)
                        kb.op('pool', lambda e: e.tensor_copy(out=gls[:, 0:16].rearrange("p (d h) -> p d h", d=2), in_=x5[:, :, 0, :]),
                              reads=['mbt'], writes=['mgls'])
                        xf = x5[:, :, 1, :]
                        t3 = lambda i_: bt[:, i_, :].rearrange("p (d h) -> p d h", d=2)
                        kb.op('dve', lambda e: e.scalar_tensor_tensor(out=t3(2), in0=xf, scalar=-1.0, in1=xf, op0=ALU.mult, op1=ALU.max),
                              reads=['mbt'], writes=['mbt'])
                        kb.op('act', lambda e: e.activation(out=bt[:, 2, :], in_=bt[:, 2, :], func=AF.Exp, scale=-1.0), reads=['mbt'], writes=['mbt'])
                        kb.op('act', lambda e: e.activation(out=bt[:, 3, :], in_=bt[:, 2, :], func=AF.Ln, bias=1.0), reads=['mbt'], writes=['mbt'])
                        kb.op('dve', lambda e: e.scalar_tensor_tensor(out=gls[:, 16:32].rearrange("p (d h) -> p d h", d=2), in0=xf, scalar=0.0,
                                                                      in1=t3(3), op0=ALU.min, op1=ALU.subtract), reads=['mbt'], writes=['mgls'])
                        kb.dma('pool', self.g_bg[r0:r0 + 128, :], gls[:], reads=['mgls'], writes=['g_bg_%d' % r0])

    def mlstm_scan(self, li, jx):
        kb, NB = self.kb, self.NB
        HB = 4
        qkTv = self.m_qkT.rearrange("q (h p) c -> q p h c", p=64)
        order = [list(range(NT)), [1, 0] + list(range(NT - 1, NTC - 1, -1))]
        with self.phase():
            MS = self.sb("mmask", [128, 13, 128])
            for m0 in range(0, 13, 4):
                m1 = min(13, m0 + 4)
                kb.dma('sp', MS[:, m0:m1, :], self.c_masks[m0:m1].rearrange("m p c -> p m c"), writes=['mmask'])
            identf = self.sb("identf", [128, 128])
            kb.dma('sp', identf[:], self.c_ident, writes=['identf'])
            onesf = self.sb("onesf", [128, 128])
            kb.op('dve', lambda e: e.memset(onesf[:], 1.0), writes=['onesf'])
            PS = [self.ps("mps%d" % i, [128, 512]) for i in range(8)]
            PK = ['mps%d' % i for i in range(8)]

            def v4(t):
                return t[:].rearrange("p (h c) -> p h c", h=HB)
            sets = []
            for si in range(2):
                T = {}
                for nm in ('LF', 'Dig', 'DL', 'P', 'PT'):
                    T[nm] = self.sb("m%s%d" % (nm, si), [128, HB * 128])
                T['qT'] = self.sb("mqT%d" % si, [64, HB, 128])
                T['kT'] = self.sb("mkT%d" % si, [64, HB, 128])
                T['ktok'] = self.sb("mktok%d" % si, [128, HB, 64])
                T['kw'] = self.sb("mkw%d" % si, [128, HB, 64])
                T['v1'] = self.sb("mv1%d" % si, [128, HB, 132])
                T['NI'] = self.sb("mNI%d" % si, [128, HB, 132])
                T['t1'] = self.sb("mt1%d" % si, [128, HB, 132])
                T['t2'] = self.sb("mt2%d" % si, [128, HB, 132])
                T['c'] = self.sb("mc%d" % si, [128, 16, HB])
                T['mloc'] = self.sb("mmloc%d" % si, [128, HB, 2])
                T['blast'] = self.sb("mblast%d" % si, [128, 2, HB])
                T['e3'] = self.sb("me3%d" % si, [128, 3, HB])
                T['fi'] = self.sb("mfi%d" % si, [128, 2, HB])
                T['k'] = 'm%d_' % si
                kb.op('pool', lambda e: e.memset(T['v1'][:, :, 128:129], 1.0), writes=[T['k'] + 'v1'])
                sets.append(T)
            glt = [self.sb("mgl%d" % i, [128, 32]) for i in range(4)]
            ost = [self.sb("most%d" % i, [128, D]) for i in range(4)]
            Cn = [[self.sb("mCn_%d_%d" % (d, hf), [64, HB, 132]) for hf in range(2)] for d in range(2)]
            Mm = [[self.sb("mM_%d_%d" % (d, hf), [128, HB]) for hf in range(2)] for d in range(2)]
            stepi = 0
            bi = 0
            for b in range(NB):
                for d in range(2):
                    for hf in range(2):
                        kb.op('pool', lambda e: e.memset(Cn[d][hf][:], 0.0), writes=['mCn_%d_%d' % (d, hf)])
                        kb.op('pool', lambda e: e.memset(Mm[d][hf][:], 0.0), writes=['mM_%d_%d' % (d, hf)])
                for n in range(NT):
                    for d in range(2):
                        j = order[d][n]
                        tok0 = b * TOK + j * 128
                        gl = glt[bi % 4]
                        glk = 'mgl%d' % (bi % 4)
                        os_ = ost[bi % 4]
                        osk = 'most%d' % (bi % 4)
                        bi += 1
                        kb.dma('sp', gl[:], self.g_bg[tok0:tok0 + 128, :], writes=[glk])
                        Ud, nUd, BO, BmU, NEG = MS[:, d, :], MS[:, 4 + d, :], MS[:, 6, :], MS[:, 9 + d, :], MS[:, 11 + d, :]
                        for hf in range(2):
                            T = sets[stepi % 2]
                            K = lambda nm: T['k'] + nm
                            stepi += 1
                            h0 = hf * HB
                            Ck, Mk = 'mCn_%d_%d' % (d, hf), 'mM_%d_%d' % (d, hf)
                            Ct, Mt = Cn[d][hf], Mm[d][hf]
                            c = T['c']
                            kb.dma('sp', T['qT'][:], qkTv[0, :, h0:h0 + HB, tok0:tok0 + 128], writes=[K('qT')])
                            kb.dma('sp', T['kT'][:], qkTv[1, :, h0:h0 + HB, tok0:tok0 + 128], writes=[K('kT')])
                            kb.dma('sp', T['ktok'][:], self.m_ktok[tok0:tok0 + 128, h0 * 64:(h0 + HB) * 64].rearrange("p (h c) -> p h c", h=HB),
                                   writes=[K('ktok')])
                            kb.dma('sp', T['v1'][:, :, 0:128], self.g_vtok[tok0:tok0 + 128, h0 * 128:(h0 + HB) * 128].rearrange("p (h c) -> p h c", h=HB),
                                   writes=[K('v1')])
                            ig4 = gl[:, d * 8 + h0:d * 8 + h0 + HB]
                            lf4 = gl[:, 16 + d * 8 + h0:16 + d * 8 + h0 + HB]
                            bc4 = lambda ap: ap.unsqueeze(2).to_broadcast([128, HB, 128])
                            mk4 = lambda ap: ap.unsqueeze(1).to_broadcast([128, HB, 128])
                            kb.op('dve', lambda e: e.tensor_copy(out=v4(T['LF']), in_=bc4(lf4)), reads=[glk], writes=[K('LF')])
                            kb.op('pool', lambda e: e.tensor_tensor(out=v4(T['Dig']), in0=mk4(identf[:]), in1=bc4(ig4), op=ALU.mult),
                                  reads=[glk, 'identf'], writes=[K('Dig')])
                            LF, Dig = v4(T['LF']), v4(T['Dig'])
                            for h in range(HB):
                                o_ = PS[0][:, h * 128:(h + 1) * 128]
                                self.mm(o_, Ud, LF[:, h, :], True, False, ['mmask', K('LF')], [PK[0]])
                                self.mm(o_, LF[:, h, :], nUd, False, False, ['mmask', K('LF')], [PK[0]])
                                self.mm(o_, onesf[:], Dig[:, h, :], False, True, ['onesf', K('Dig')], [PK[0]])
                            for h in range(HB):
                                o_ = PS[1][:, h * 128:(h + 1) * 128]
                                self.mm(o_, LF[:, h, :], BmU, True, False, ['mmask', K('LF')], [PK[1]])
                                self.mm(o_, onesf[:], Dig[:, h, :], False, True, ['onesf', K('Dig')], [PK[1]])
                            self.mm(PS[4][:, 0:HB], Ud, lf4, True, True, ['mmask', glk], [PK[4]])
                            self.mm(PS[4][:, HB:2 * HB], BO, lf4, True, True, ['mmask', glk], [PK[4]])
                            for cc in range(2):
                                self.mm(PS[4][:, 8 + cc * HB:8 + (cc + 1) * HB], MS[:, 7 + cc, :], lf4, True, True, ['mmask', glk], [PK[4]])
                            for h in range(HB):
                                self.mm(PS[2][:, h * 128:(h + 1) * 128], T['qT'][:, h, :], T['kT'][:, h, :], True, True, [K('qT'), K('kT')], [PK[2]])
                            kb.op('act', lambda e: e.copy(out=c[:, 0:2, :].rearrange("p a h -> p (a h)"), in_=PS[4][:, 0:2 * HB]), reads=[PK[4]], writes=[K('c')])
                            kb.op('act', lambda e: e.copy(out=T['blast'][:].rearrange("p a h -> p (a h)"), in_=PS[4][:, 8:8 + 2 * HB]),
                                  reads=[PK[4]], writes=[K('blast')])
                            kb.op('dve', lambda e: e.tensor_tensor(out=v4(T['DL']), in0=PS[0][:].rearrange("p (h c) -> p h c", h=HB), in1=mk4(NEG), op=ALU.add),
                                  reads=[PK[0], 'mmask'], writes=[K('DL')])
                            kb.op('dve', lambda e: e.tensor_reduce(out=c[:, 2, :], in_=v4(T['DL']), axis=AX.X, op=ALU.max), reads=[K('DL')], writes=[K('c')])
                            kb.op('dve', lambda e: e.tensor_tensor(out=v4(T['DL']), in0=v4(T['DL']), in1=bc4(c[:, 2, :]), op=ALU.subtract),
                                  reads=[K('DL'), K('c')], writes=[K('DL')])
                            kb.op('act', lambda e: e.activation(out=T['P'][:], in_=T['DL'][:], func=AF.Exp), reads=[K('DL')], writes=[K('P')])
                            kb.op('dve', lambda e: e.tensor_tensor(out=T['P'][:], in0=T['P'][:], in1=PS[2][:], op=ALU.mult), reads=[K('P'), PK[2]], writes=[K('P')])
                            P4 = v4(T['P'])
                            for h in range(HB):
                                self.tr(PS[3][:, h * 128:(h + 1) * 128], P4[:, h, :], identf[:], [K('P'), 'identf'], [PK[3]])
                            kb.op('act', lambda e: e.copy(out=T['PT'][:], in_=PS[3][:]), reads=[PK[3]], writes=[K('PT')])
                            PT4 = v4(T['PT'])
                            for h in range(HB):
                                self.mm(PS[6 + h // 2][:, (h % 2) * 129:(h % 2) * 129 + 129], PT4[:, h, :], T['v1'][:, h, 0:129], True, True,
                                        [K('PT'), K('v1')], [PK[6 + h // 2]])
                            for q in range(2):
                                kb.op('act' if q == 0 else 'dve',
                                      (lambda e: e.copy(out=T['NI'][:, 0:2, 0:129], in_=PS[6][:, 0:258].rearrange("p (h c) -> p h c", h=2))) if q == 0 else
                                      (lambda e: e.tensor_copy(out=T['NI'][:, 2:4, 0:129], in_=PS[7][:, 0:258].rearrange("p (h c) -> p h c", h=2))),
                                      reads=[PK[6 + q]], writes=[K('NI')])
                            kb.op('dve', lambda e: e.tensor_reduce(out=T['mloc'][:], in_=PS[1][:].rearrange("p (h c j) -> p h c j", h=HB, c=2), axis=AX.X, op=ALU.max),
                                  reads=[PK[1]], writes=[K('mloc')])
                            kb.op('dve', lambda e: e.tensor_tensor(out=c[:, 3, :], in0=c[:, 1, :], in1=c[:, 0, :], op=ALU.subtract), reads=[K('c')], writes=[K('c')])
                            kb.op('dve', lambda e: e.tensor_tensor(out=c[:, 3, :], in0=c[:, 3, :], in1=ig4, op=ALU.add), reads=[K('c'), glk], writes=[K('c')])
                            for cc in range(2):
                                rw = slice(64 * cc, 64 * cc + 64)
                                kb.op('dve', lambda e: e.tensor_tensor(out=c[rw, 4, :], in0=c[rw, 3, :], in1=T['mloc'][rw, :, cc], op=ALU.subtract),
                                      reads=[K('c'), K('mloc')], writes=[K('c')])
                            kb.op('act', lambda e: e.activation(out=c[:, 5, :], in_=c[:, 4, :], func=AF.Exp), reads=[K('c')], writes=[K('c')])
                            kb.op('pool', lambda e: e.tensor_tensor(out=T['kw'][:], in0=T['ktok'][:], in1=c[:, 5, :].unsqueeze(2).to_broadcast([128, HB, 64]), op=ALU.mult),
                                  reads=[K('ktok'), K('c')], writes=[K('kw')])
                            for cc in ([0, 1] if d == 0 else [1, 0]):
                                rw = slice(64 * cc, 64 * cc + 64)
                                for h in range(HB):
                                    self.mm(PS[h // 2][rw, (h % 2) * 129:(h % 2) * 129 + 129], T['qT'][:, h, rw], Ct[:, h, 0:129], True, True,
                                            [K('qT'), Ck], [PK[h // 2]])
                                kb.op('dve', lambda e: e.tensor_tensor(out=c[rw, 6, :], in0=c[rw, 0, :], in1=Mt[rw, :], op=ALU.add), reads=[K('c'), Mk], writes=[K('c')])
                                kb.op('dve', lambda e: e.tensor_tensor(out=c[rw, 7, :], in0=c[rw, 2, :], in1=c[rw, 6, :], op=ALU.max), reads=[K('c')], writes=[K('c')])
                                e3 = T['e3']
                                kb.op('dve', lambda e: e.tensor_tensor(out=e3[rw, 0, :], in0=c[rw, 6, :], in1=c[rw, 7, :], op=ALU.subtract), reads=[K('c')], writes=[K('e3')])
                                kb.op('dve', lambda e: e.tensor_tensor(out=e3[rw, 1, :], in0=c[rw, 2, :], in1=c[rw, 7, :], op=ALU.subtract), reads=[K('c')], writes=[K('e3')])
                                kb.op('dve', lambda e: e.tensor_scalar(out=e3[rw, 2, :], in0=c[rw, 7, :], scalar1=-1.0, scalar2=None, op0=ALU.mult), reads=[K('c')], writes=[K('e3')])
                                kb.op('act', lambda e: e.activation(out=e3[rw, :, :], in_=e3[rw, :, :], func=AF.Exp), reads=[K('e3')], writes=[K('e3')])
                                for q in range(2):
                                    kb.op('dve', lambda e: e.tensor_tensor(out=T['t1'][rw, 2 * q:2 * q + 2, 0:129], in0=PS[q][rw, 0:258].rearrange("p (h c) -> p h c", h=2),
                                                                           in1=e3[rw, 0, 2 * q:2 * q + 2].unsqueeze(2).to_broadcast([64, 2, 129]), op=ALU.mult),
                                          reads=[PK[q], K('e3')], writes=[K('t1')])
                                kb.op('pool', lambda e: e.tensor_tensor(out=T['t2'][rw, :, 0:129], in0=T['NI'][rw, :, 0:129],
                                                                        in1=e3[rw, 1, :].unsqueeze(2).to_broadcast([64, HB, 129]), op=ALU.mult),
                                      reads=[K('NI'), K('e3')], writes=[K('t2')])
                                kb.op('dve', lambda e: e.tensor_tensor(out=T['t1'][rw, :, 0:129], in0=T['t1'][rw, :, 0:129], in1=T['t2'][rw, :, 0:129], op=ALU.add),
                                      reads=[K('t1'), K('t2')], writes=[K('t1')])
                                den = T['t1'][rw, :, 128]
                                kb.op('dve', lambda e: e.scalar_tensor_tensor(out=c[rw, 8, :], in0=den, scalar=-1.0, in1=den, op0=ALU.mult, op1=ALU.max),
                                      reads=[K('t1')], writes=[K('c')])
                                kb.op('dve', lambda e: e.tensor_tensor(out=c[rw, 9, :], in0=c[rw, 8, :], in1=e3[rw, 2, :], op=ALU.max), reads=[K('c'), K('e3')], writes=[K('c')])
                                kb.op('dve', lambda e: e.reciprocal(out=c[rw, 10, :], in_=c[rw, 9, :]), reads=[K('c')], writes=[K('c')])
                                kb.op('dve', lambda e: e.tensor_tensor(out=os_[rw, h0 * 128:(h0 + HB) * 128].rearrange("p (h c) -> p h c", h=HB), in0=T['t1'][rw, :, 0:128],
                                                                       in1=c[rw, 10, :].unsqueeze(2).to_broadcast([64, HB, 128]), op=ALU.mult),
                                      reads=[K('t1'), K('c')], writes=[osk])
                                for h in range(HB):
                                    self.mm(PS[2 + h // 2][0:64, (h % 2) * 129:(h % 2) * 129 + 129], T['kw'][rw, h, :], T['v1'][rw, h, 0:129], True, True,
                                            [K('kw'), K('v1')], [PK[2 + h // 2]])
                                fi = T['fi']
                                kb.op('dve', lambda e: e.tensor_tensor(out=c[:, 11, :], in0=T['blast'][:, cc, :], in1=Mt[:], op=ALU.add), reads=[K('blast'), Mk], writes=[K('c')])
                                kb.op('dve', lambda e: e.tensor_tensor(out=c[:, 12, :], in0=c[:, 11, :], in1=T['mloc'][:, :, cc], op=ALU.max), reads=[K('c'), K('mloc')], writes=[K('c')])
                                kb.op('dve', lambda e: e.tensor_tensor(out=fi[:, 0, :], in0=c[:, 11, :], in1=c[:, 12, :], op=ALU.subtract), reads=[K('c')], writes=[K('fi')])
                                kb.op('dve', lambda e: e.tensor_tensor(out=fi[:, 1, :], in0=T['mloc'][:, :, cc], in1=c[:, 12, :], op=ALU.subtract), reads=[K('c'), K('mloc')], writes=[K('fi')])
                                kb.op('act', lambda e: e.activation(out=fi[:], in_=fi[:], func=AF.Exp), reads=[K('fi')], writes=[K('fi')])
                                kb.op('dve', lambda e: e.tensor_copy(out=Mt[:], in_=c[:, 12, :]), reads=[K('c')], writes=[Mk])
                                kb.op('dve', lambda e: e.tensor_tensor(out=Ct[:, :, 0:129], in0=Ct[:, :, 0:129], in1=fi[0:64, 0, :].unsqueeze(2).to_broadcast([64, HB, 129]), op=ALU.mult),
                                      reads=[Ck, K('fi')], writes=[Ck])
                                for q in range(2):
                                    kb.op('dve', lambda e: e.tensor_tensor(out=T['t2'][0:64, 2 * q:2 * q + 2, 0:129], in0=PS[2 + q][0:64, 0:258].rearrange("p (h c) -> p h c", h=2),
                                                                           in1=fi[0:64, 1, 2 * q:2 * q + 2].unsqueeze(2).to_broadcast([64, 2, 129]), op=ALU.mult),
                                          reads=[PK[2 + q], K('fi'), K('t2')], writes=[K('t2')])
                                kb.op('dve', lambda e: e.tensor_tensor(out=Ct[:, :, 0:129], in0=Ct[:, :, 0:129], in1=T['t2'][0:64, :, 0:129], op=ALU.add),
                                      reads=[Ck, K('t2')], writes=[Ck])
                        kb.dma('pool', self.g_odir[d, tok0:tok0 + 128, :], os_[:], reads=[osk], writes=['g_odir_%d_%d' % (d, tok0)])

    def build(self):
        self.declare()
        self.setup()
        cnt = {0: 0, 1: 0, 2: 0}
        for li, kind in enumerate(self.kinds):
            last = self.last_flags[li]
            layer0 = (li == 0)
            jx = cnt[kind]
            cnt[kind] += 1
            self.mod_phase(li)
            self.norm_phase(li, 1, layer0)
            if kind == 2:
                self.attn_phase(li, jx, layer0, not last)
            elif kind == 0:
                self.gdn_phase(li, jx, layer0, not last)
            else:
                self.mlstm_phase(li, jx, layer0, not last)
            self.norm_phase(li, 2, False, ctx_needed=not last)
            self.ffn_phase(li, ctx_needed=not last)
        self.final_phase()
        self.kb.barrier()
        self.kb.close()


def host_consts():
    ident = np.eye(128, dtype=np.float32)
    n_pair = 32
    inv = (10000.0 ** (-np.arange(n_pair, dtype=np.float32) / n_pair)).astype(np.float32)
    pos = np.arange(LAT)
    row = (pos // 64).astype(np.float32)
    col = (pos % 64).astype(np.float32)
    ang = np.concatenate([row[:, None] * inv, col[:, None] * inv], axis=-1).astype(np.float32)
    rope = np.concatenate([np.cos(ang), np.sin(ang)], axis=-1).astype(np.float32)
    masks = np.zeros((16, 128, 128), np.float32)
    t = np.arange(128)
    same = (t[:, None] // 64) == (t[None, :] // 64)
    uf = (same & (t[:, None] <= t[None, :])).astype(np.float32)
    ub = (same & (t[:, None] >= t[None, :])).astype(np.float32)
    masks[0], masks[1] = uf, ub
    masks[2], masks[3] = uf - np.eye(128, dtype=np.float32), ub - np.eye(128, dtype=np.float32)
    masks[4], masks[5] = -uf, -ub
    masks[6] = same.astype(np.float32)
    masks[7] = np.repeat((t < 64).astype(np.float32)[:, None], 128, axis=1)
    masks[8] = np.repeat((t >= 64).astype(np.float32)[:, None], 128, axis=1)
    masks[9], masks[10] = masks[6] - uf, masks[6] - ub
    masks[11] = (1.0 - ub) * np.float32(-1e30)
    masks[12] = (1.0 - uf) * np.float32(-1e30)
    return {"c_ident": ident, "c_rope": rope, "c_masks": masks}


def make_in_maps(inputs, NB, n_cores, kinds):
    f = lambda a: np.ascontiguousarray(np.asarray(a, dtype=np.float32))
    consts = host_consts()
    shared = {}
    for k in ("norm1_g", "norm2_g", "w_mod", "b_mod", "ffn_w_in", "ffn_conv_w", "ffn_conv_b", "ffn_w_out",
              "gdn_w_in", "gdn_conv_w", "gdn_norm_g", "gdn_w_out", "mlstm_w_in", "mlstm_norm_g", "mlstm_w_out",
              "attn_w_in", "attn_q_norm_g", "attn_k_norm_g", "attn_w_out"):
        shared[k] = f(inputs[k])
    shared["gdn_a_log"] = f(inputs["gdn_a_log"]).reshape(-1, 16)
    shared["gdn_dt_bias"] = f(inputs["gdn_dt_bias"]).reshape(-1, 16)
    shared["mlstm_gate_b"] = f(inputs["mlstm_gate_b"]).reshape(-1, 32)
    shared["final_norm_g"] = f(inputs["final_norm_g"]).reshape(1, D)
    shared.update(consts)
    x, c, ctx, c_ctx = f(inputs["x"]), f(inputs["c"]), f(inputs["ctx"]), f(inputs["c_ctx"])
    maps = []
    for i in range(n_cores):
        m = dict(shared)
        m["x"] = x[i * NB:(i + 1) * NB]
        m["ctx"] = ctx[i * NB:(i + 1) * NB]
        m["cvec"] = np.ascontiguousarray(np.concatenate([c[i * NB:(i + 1) * NB], c_ctx[None, :]], axis=0))
        maps.append(m)
    return maps


def kernel(**inputs):
    NB = 2
    n_cores = 8
    nc = bass.Bass("TRN2", target_bir_lowering=False)
    mk = MK(nc, NB, KINDS, [False, False, False, True])
    mk.build()
    maps = make_in_maps(inputs, NB, n_cores, KINDS)
    res = run_bass_kernel_spmd(nc, maps, core_ids=list(range(n_cores)))
    return np.concatenate([r["out"] for r in res.results], axis=0).astype(np.float32)
```

```python
import contextlib
import math
import numpy as np
import concourse.bass as bass
import concourse.mybir as mybir
from concourse.bass_utils import run_bass_kernel_spmd

F32 = mybir.dt.float32
F32R = mybir.dt.float32r
BF16 = mybir.dt.bfloat16
AF = mybir.ActivationFunctionType
ALU = mybir.AluOpType
AX = mybir.AxisListType

D = 1024
LAT = 4096
CTXL = 256
NTC = 2
NTL = 32
NT = 34
TOK = NT * 128
FFN = 2816
EPS = 1e-6
HALO = 2
REG_C = CTXL + 2 * HALO
REG_L = LAT + 2 * HALO
REG = REG_C + REG_L
KINDS = [0, 1, 2, 0]


class KB:
    def __init__(self, nc, ring=6, same_engine_sync=True):
        self.nc = nc
        self.eng = {'pe': nc.tensor, 'dve': nc.vector, 'act': nc.scalar,
                    'pool': nc.gpsimd, 'sp': nc.sync}
        self.same_engine_sync = same_engine_sync
        self.sem = {}
        self.cnt = {}
        self.seen = {e: {} for e in self.eng}
        self.res = {}
        self._ctx = []
        for e in ('pe', 'dve', 'act', 'pool'):
            self._mksem('c_' + e)
        self.rings = {}
        for q in ('sp', 'act', 'pool'):
            names = []
            for i in range(ring):
                n = 'd_%s_%d' % (q, i)
                self._mksem(n)
                names.append(n)
            self.rings[q] = [names, 0]
        self.n_inst = 0
        self.n_wait = 0

    def _mksem(self, name):
        cm = self.nc.semaphore(name)
        h = cm.__enter__()
        self._ctx.append(cm)
        self.sem[name] = h
        self.cnt[name] = 0

    def close(self):
        for cm in reversed(self._ctx):
            cm.__exit__(None, None, None)
        self._ctx = []

    def _R(self, key):
        r = self.res.get(key)
        if r is None:
            r = {'w': None, 'r': {}}
            self.res[key] = r
        return r

    def _deps(self, reads, writes):
        deps = {}

        def add(tok):
            if tok is None:
                return
            s, v = tok
            if deps.get(s, 0) < v:
                deps[s] = v
        for k in reads:
            add(self._R(k)['w'])
        for k in writes:
            r = self._R(k)
            add(r['w'])
            for s, v in r['r'].items():
                add((s, v))
        return deps

    def _wait(self, e, deps, is_dma=False):
        own = 'c_' + e
        seen = self.seen[e]
        for s, v in deps.items():
            if s == own and not is_dma and (e == 'pe' or not self.same_engine_sync):
                continue
            if seen.get(s, 0) >= v:
                continue
            self.eng[e].wait_ge(self.sem[s], v)
            self.n_wait += 1
            seen[s] = v

    def _commit(self, tok, reads, writes):
        s, v = tok
        for k in reads:
            r = self._R(k)
            if r['r'].get(s, 0) < v:
                r['r'][s] = v
        for k in writes:
            r = self._R(k)
            r['w'] = tok
            r['r'] = {}

    def op(self, e, fn, reads=(), writes=()):
        deps = self._deps(reads, writes)
        self._wait(e, deps)
        inst = fn(self.eng[e])
        s = 'c_' + e
        self.cnt[s] += 1
        inst.then_inc(self.sem[s], 1)
        self.n_inst += 1
        self._commit((s, self.cnt[s]), reads, writes)
        return inst

    def dma(self, q, out, in_, reads=(), writes=(), **kw):
        names, idx = self.rings[q]
        s = names[idx % len(names)]
        self.rings[q][1] = idx + 1
        deps = self._deps(reads, writes)
        if self.cnt[s] > 0 and deps.get(s, 0) < self.cnt[s]:
            deps[s] = self.cnt[s]
        self._wait(q, deps, is_dma=True)
        inst = self.eng[q].dma_start(out=out, in_=in_, **kw)
        self.cnt[s] += 16
        inst.then_inc(self.sem[s], 16)
        self.n_inst += 1
        self._commit((s, self.cnt[s]), reads, writes)
        return inst

    def barrier(self):
        for e in self.eng:
            for s, v in self.cnt.items():
                if v > 0 and self.seen[e].get(s, 0) < v:
                    self.eng[e].wait_ge(self.sem[s], v)
                    self.seen[e][s] = v
        self.res = {}


class MK:
    def __init__(self, nc, NB, kinds, last_flags, debug=False):
        self.debug = debug
        self.nc = nc
        self.NB = NB
        self.kinds = kinds
        self.last_flags = last_flags
        self.kb = KB(nc)
        self.stack = None
        self.uid = 0

    @contextlib.contextmanager
    def phase(self):
        prev = self.stack
        with contextlib.ExitStack() as es:
            self.stack = es
            yield
            self.kb.barrier()
        self.stack = prev

    @contextlib.contextmanager
    def nosame(self):
        yield

    def sb(self, name, shape, dt=F32):
        self.uid += 1
        return self.stack.enter_context(self.nc.sbuf_tensor("%s_%d" % (name, self.uid), list(shape), dt))

    def ps(self, name, shape, dt=F32):
        self.uid += 1
        return self.stack.enter_context(self.nc.psum_tensor("%s_%d" % (name, self.uid), list(shape), dt))

    def dram(self, name, shape, dt, kind="Internal"):
        return self.nc.dram_tensor(name, list(shape), dt, kind=kind).ap()

    def mm(self, out, lhsT, rhs, start, stop, reads, writes):
        return self.kb.op('pe', lambda e: e.matmul(out, lhsT=lhsT, rhs=rhs, start=start, stop=stop),
                          reads=reads, writes=writes)

    def mmr(self, out, lhsT, rhs, start, stop, reads, writes):
        F32R = mybir.dt.float32r
        return self.kb.op('pe', lambda e: e.matmul(out, lhsT=lhsT.bitcast(F32R), rhs=rhs.bitcast(F32R), start=start, stop=stop),
                          reads=reads, writes=writes)

    def tr(self, out, in_, ident, reads, writes):
        return self.kb.op('pe', lambda e: e.transpose(out, in_, ident), reads=reads, writes=writes)

    def declare(self):
        NB = self.NB
        n0 = sum(1 for k in self.kinds if k == 0)
        n1 = sum(1 for k in self.kinds if k == 1)
        n2 = sum(1 for k in self.kinds if k == 2)
        DEPTH = len(self.kinds)
        I = lambda n, s: self.dram(n, s, F32, kind="ExternalInput")
        self.x = I("x", [NB, LAT, D])
        self.ctx = I("ctx", [NB, CTXL, D])
        self.cvec = I("cvec", [NB + 1, D])
        self.norm1_g = I("norm1_g", [DEPTH, D])
        self.norm2_g = I("norm2_g", [DEPTH, D])
        self.w_mod = I("w_mod", [DEPTH, D, 6 * D])
        self.b_mod = I("b_mod", [DEPTH, 6 * D])
        self.ffn_w_in = I("ffn_w_in", [DEPTH, D, 2 * FFN])
        self.ffn_conv_w = I("ffn_conv_w", [DEPTH, 3, FFN])
        self.ffn_conv_b = I("ffn_conv_b", [DEPTH, FFN])
        self.ffn_w_out = I("ffn_w_out", [DEPTH, FFN, D])
        self.gdn_w_in = I("gdn_w_in", [max(n0, 1), D, 4128])
        self.gdn_conv_w = I("gdn_conv_w", [max(n0, 1), 5, 3072])
        self.gdn_a_log = I("gdn_a_log", [max(n0, 1), 16])
        self.gdn_dt_bias = I("gdn_dt_bias", [max(n0, 1), 16])
        self.gdn_norm_g = I("gdn_norm_g", [max(n0, 1), 128])
        self.gdn_w_out = I("gdn_w_out", [max(n0, 1), D, D])
        self.mlstm_w_in = I("mlstm_w_in", [max(n1, 1), D, 3104])
        self.mlstm_gate_b = I("mlstm_gate_b", [max(n1, 1), 32])
        self.mlstm_norm_g = I("mlstm_norm_g", [max(n1, 1), 128])
        self.mlstm_w_out = I("mlstm_w_out", [max(n1, 1), D, D])
        self.attn_w_in = I("attn_w_in", [max(n2, 1), D, 1536])
        self.attn_q_norm_g = I("attn_q_norm_g", [max(n2, 1), 128])
        self.attn_k_norm_g = I("attn_k_norm_g", [max(n2, 1), 128])
        self.attn_w_out = I("attn_w_out", [max(n2, 1), D, D])
        self.final_norm_g = I("final_norm_g", [1, D])
        self.c_ident = I("c_ident", [128, 128])
        self.c_rope = I("c_rope", [LAT, 128])
        self.c_masks = I("c_masks", [16, 128, 128])
        self.out = self.dram("out", [NB, LAT, D], F32, kind="ExternalOutput")
        sk = "ExternalOutput" if self.debug else "Internal"
        self.xs = self.dram("xs", [NB, TOK, D], F32, kind=sk)
        self.hTs = self.dram("hTs", [D, NB * REG], BF16, kind=sk)
        self.modv = self.dram("modv", [DEPTH, NB + 1, 6, D], F32, kind=sk)

    def src_x(self, layer0, b, j):
        if layer0:
            if j < NTC:
                return self.ctx[b, j * 128:(j + 1) * 128, :], 'in_ctx'
            return self.x[b, (j - NTC) * 128:(j - NTC + 1) * 128, :], 'in_x'
        return self.xs[b, j * 128:(j + 1) * 128, :], 'xs_%d_%d' % (b, j)

    def hcol(self, b, j):
        base = b * REG
        if j < NTC:
            return base + HALO + j * 128
        return base + REG_C + HALO + (j - NTC) * 128

    def hT_view(self):
        return self.hTs.rearrange("(k p) c -> p k c", p=128)

    def setup(self):
        kb = self.kb
        with self.phase():
            z = self.sb("zero", [128, 8, 2 * HALO], BF16)
            kb.op('dve', lambda e: e.memset(z[:], 0.0), writes=['zero'])
            hv = self.hT_view()
            for b in range(self.NB):
                base = b * REG
                for c0 in (base, base + REG_C - HALO):
                    pass
                kb.dma('pool', hv[:, :, base:base + HALO], z[:, :, 0:HALO], reads=['zero'], writes=['hTs_halo'])
                kb.dma('pool', hv[:, :, base + REG_C - HALO:base + REG_C + HALO], z[:, :, :], reads=['zero'], writes=['hTs_halo'])
                kb.dma('pool', hv[:, :, base + REG - HALO:base + REG], z[:, :, 0:HALO], reads=['zero'], writes=['hTs_halo'])

    def mod_phase(self, li):
        kb, NB = self.kb, self.NB
        R = NB + 1
        with self.phase():
            cT = self.sb("cT", [128, 8, R])
            sT = self.sb("sT", [128, 8, R])
            ones = self.sb("ones", [1, 4])
            brow = self.sb("brow", [1, 6 * D])
            mrow = self.sb("mrow", [R, 6 * D])
            g12 = self.sb("g12", [R, 2, D])
            pm = [self.ps("pm%d" % i, [R, 512]) for i in range(2)]
            wm = [self.sb("wm%d" % i, [128, 8, 512]) for i in range(2)]
            with self.nc.allow_non_contiguous_dma(reason="tiny transposed load of conditioning vectors"):
                for r in range(R):
                    kb.dma('sp', cT[:, :, r], self.cvec[r, :].rearrange("(k p) -> p k", p=128), writes=['cT'])
            kb.op('act', lambda e: e.activation(out=sT[:], in_=cT[:], func=AF.Silu), reads=['cT'], writes=['sT'])
            kb.op('dve', lambda e: e.memset(ones[:], 1.0), writes=['ones'])
            kb.dma('sp', brow[:], self.b_mod[li:li + 1, :], writes=['brow'])
            kb.dma('sp', g12[:, 0, :], self.norm1_g[li:li + 1, :].to_broadcast([R, D]), writes=['g12'])
            kb.dma('sp', g12[:, 1, :], self.norm2_g[li:li + 1, :].to_broadcast([R, D]), writes=['g12'])
            for n in range(12):
                w = wm[n % 2]
                wk = 'wm%d' % (n % 2)
                pk = 'pm%d' % (n % 2)
                kb.dma('sp', w[:], self.w_mod[li, :, n * 512:(n + 1) * 512].rearrange("(k p) c -> p k c", p=128),
                       writes=[wk])
                for k in range(8):
                    self.mm(pm[n % 2][:], sT[:, k, :], w[:, k, :], k == 0, False, ['sT', wk], [pk])
                self.mm(pm[n % 2][:], ones[0:1, 0:R], brow[0:1, n * 512:(n + 1) * 512], False, True,
                        ['ones', 'brow'], [pk])
                kb.op('act', lambda e: e.copy(out=mrow[:, n * 512:(n + 1) * 512], in_=pm[n % 2][:]),
                      reads=[pk], writes=['mrow'])
            for (gi, sc) in ((0, 1), (1, 4)):
                kb.op('dve', lambda e: e.scalar_tensor_tensor(
                    out=mrow[:, sc * D:(sc + 1) * D], in0=mrow[:, sc * D:(sc + 1) * D], scalar=1.0,
                    in1=g12[:, gi, :], op0=ALU.add, op1=ALU.mult), reads=['mrow', 'g12'], writes=['mrow'])
            kb.dma('pool', self.modv[li].rearrange("r s d -> r (s d)"), mrow[:], reads=['mrow'], writes=['modv'])

    def load_bc(self, t, li, r, s, key):
        self.kb.dma('sp', t[:], self.modv[li, r, s:s + 1, :].to_broadcast([128, D]), reads=['modv'], writes=[key])

    def norm_phase(self, li, which, layer0, ctx_needed=True):
        kb, NB = self.kb, self.NB
        s_sh, s_g = (0, 1) if which == 1 else (3, 4)
        with self.phase():
            ident = self.sb("identb", [128, 128], BF16)
            kb.dma('pool', ident[:], self.c_ident, writes=['ident'])
            Gt = [self.sb("G%d" % r, [128, D]) for r in range(NB + 1)]
            St = [self.sb("S%d" % r, [128, D]) for r in range(NB + 1)]
            for r in range(NB + 1):
                self.load_bc(Gt[r], li, r, s_g, 'G%d' % r)
                self.load_bc(St[r], li, r, s_sh, 'S%d' % r)
            NBUF = 3
            xt = [self.sb("xt%d" % i, [128, D]) for i in range(NBUF)]
            sq = self.sb("sq", [128, D])
            st = [self.sb("st%d" % i, [128, 4]) for i in range(NBUF)]
            hb = [self.sb("hb%d" % i, [128, D], BF16) for i in range(NBUF)]
            pt = [self.ps("pt%d" % i, [128, 8, 128], BF16) for i in range(2)]
            hw = [self.sb("hw%d" % i, [128, 8, 256], BF16) for i in range(2)]
            hv = self.hT_view()
            it = 0
            wi = 0
            pending = []

            def make_b(i, itv, wiv, t2, b, w):
                def stage_b():
                    p = pt[itv % 2]
                    pk = 'pt%d' % (itv % 2)
                    hwk = 'hw%d' % (wiv % 2)
                    for k in range(8):
                        self.tr(p[:, k, :], hb[i][:, k * 128:(k + 1) * 128], ident[:], ['hb%d' % i, 'ident'], [pk])
                    kb.op('act', lambda e: e.copy(out=hw[wiv % 2][:, :, t2 * 128:(t2 + 1) * 128], in_=p[:]),
                          reads=[pk], writes=[hwk])
                    if t2 == 1:
                        c0 = self.hcol(b, 2 * w)
                        kb.dma('pool', hv[:, :, c0:c0 + 256], hw[wiv % 2][:], reads=[hwk], writes=['hTs_%d' % wiv])
                return stage_b

            for b in range(NB):
                for w in range(NT // 2):
                    if w == 0 and not ctx_needed:
                        continue
                    for t2 in range(2):
                        j = 2 * w + t2
                        r = NB if j < NTC else b
                        i = it % NBUF
                        src, skey = self.src_x(layer0, b, j)
                        kb.dma('sp', xt[i][:], src, reads=[skey], writes=['xt%d' % i])
                        kb.op('act', lambda e: e.activation(out=sq[:], in_=xt[i][:], func=AF.Square,
                                                            accum_out=st[i][:, 0:1]),
                              reads=['xt%d' % i], writes=['sq', 'st%d' % i])
                        kb.op('dve', lambda e: e.tensor_scalar(out=st[i][:, 1:2], in0=st[i][:, 0:1], scalar1=1.0 / D,
                                                               scalar2=EPS, op0=ALU.mult, op1=ALU.add),
                              reads=['st%d' % i], writes=['st%d' % i])
                        kb.op('act', lambda e: e.activation(out=st[i][:, 2:3], in_=st[i][:, 1:2], func=AF.Sqrt),
                              reads=['st%d' % i], writes=['st%d' % i])
                        kb.op('dve', lambda e: e.reciprocal(out=st[i][:, 3:4], in_=st[i][:, 2:3]),
                              reads=['st%d' % i], writes=['st%d' % i])
                        kb.op('dve', lambda e: e.scalar_tensor_tensor(out=xt[i][:], in0=xt[i][:], scalar=st[i][:, 3:4],
                                                                      in1=Gt[r][:], op0=ALU.mult, op1=ALU.mult),
                              reads=['xt%d' % i, 'st%d' % i, 'G%d' % r], writes=['xt%d' % i])
                        kb.op('pool', lambda e: e.tensor_tensor(out=hb[i][:], in0=xt[i][:], in1=St[r][:], op=ALU.add),
                              reads=['xt%d' % i, 'S%d' % r], writes=['hb%d' % i])
                        while pending:
                            pending.pop(0)()
                        pending.append(make_b(i, it, wi, t2, b, w))
                        it += 1
                    wi += 1
            while pending:
                pending.pop(0)()

    def outproj_setup(self, w_out_ap, li):
        kb, NB = self.kb, self.NB
        o = {}
        o['w'] = self.sb("wout", [128, 8, D], BF16)
        kb.dma('pool', o['w'][:], w_out_ap.rearrange("(k p) c -> p k c", p=128), writes=['wout'])
        o['ident'] = self.sb("identb", [128, 128], BF16)
        kb.dma('pool', o['ident'][:], self.c_ident, writes=['identb'])
        o['M'] = [self.sb("M2_%d" % r, [128, D]) for r in range(NB + 1)]
        for r in range(NB + 1):
            self.load_bc(o['M'][r], li, r, 2, 'M2_%d' % r)
        o['pt'] = self.ps("opt", [128, 8, 128], BF16)
        o['py'] = self.ps("opy", [128, 2, 512])
        o['oT'] = self.sb("oT", [128, 8, 128], BF16)
        o['xt'] = [self.sb("oxt%d" % i, [128, D]) for i in range(2)]
        o['n'] = 0
        return o

    def outproj_tile(self, o, ob, obkey, layer0, b, j):
        kb = self.kb
        r = self.NB if j < NTC else b
        for k in range(8):
            self.tr(o['pt'][:, k, :], ob[:, k * 128:(k + 1) * 128], o['ident'][:], [obkey, 'identb'], ['opt'])
        kb.op('act', lambda e: e.copy(out=o['oT'][:], in_=o['pt'][:]), reads=['opt'], writes=['oT'])
        for n in range(2):
            for k in range(8):
                self.mm(o['py'][:, n, :], o['oT'][:, k, :], o['w'][:, k, n * 512:(n + 1) * 512], k == 0, k == 7,
                        ['oT', 'wout'], ['opy'])
        i = o['n'] % 2
        o['n'] += 1
        xt = o['xt'][i]
        xk = 'oxt%d' % i
        src, skey = self.src_x(layer0, b, j)
        kb.dma('sp', xt[:], src, reads=[skey], writes=[xk])
        yk = 'oy%d' % i
        kb.op('dve', lambda e: e.tensor_tensor(out=o['py'][:].rearrange("p a b -> p (a b)"),
                                               in0=o['py'][:].rearrange("p a b -> p (a b)"),
                                               in1=o['M'][r][:], op=ALU.mult),
              reads=['opy', 'M2_%d' % r], writes=['opy'])
        kb.op('dve', lambda e: e.tensor_tensor(out=xt[:], in0=o['py'][:].rearrange("p a b -> p (a b)"), in1=xt[:],
                                               op=ALU.add),
              reads=['opy', xk], writes=[xk])
        kb.dma('pool', self.xs[b, j * 128:(j + 1) * 128, :], xt[:], reads=[xk], writes=['xs_%d_%d' % (b, j)])

    def ffn_phase(self, li, ctx_needed=True):
        kb, NB = self.kb, self.NB
        NF = FFN // 128
        HF = NF // 2
        hv = self.hT_view()
        for ps_ in range(2):
            with self.phase():
                f0 = ps_ * HF
                wv = self.sb("wv", [128, 8, HF * 128], BF16)
                wg = self.sb("wg", [128, 8, HF * 128], BF16)
                wo = self.sb("wo", [128, HF, D], BF16)
                win = self.ffn_w_in[li].rearrange("(k p) c -> p k c", p=128)
                for k in range(8):
                    kb.dma('pool', wv[:, k, :], win[:, k, f0 * 128:(f0 + HF) * 128], writes=['wv'])
                    kb.dma('pool', wg[:, k, :], win[:, k, FFN + f0 * 128:FFN + (f0 + HF) * 128], writes=['wg'])
                kb.dma('pool', wo[:], self.ffn_w_out[li, f0 * 128:(f0 + HF) * 128, :].rearrange("(f p) c -> p f c", p=128),
                       writes=['wo'])
                cw = self.sb("cw", [128, HF, 4])
                with self.nc.allow_non_contiguous_dma(reason="tiny per-channel conv taps"):
                    for t in range(3):
                        kb.dma('sp', cw[:, :, t], self.ffn_conv_w[li, t, f0 * 128:(f0 + HF) * 128].rearrange("(f p) -> p f", p=128),
                               writes=['cw'])
                    kb.dma('sp', cw[:, :, 3], self.ffn_conv_b[li, f0 * 128:(f0 + HF) * 128].rearrange("(f p) -> p f", p=128),
                           writes=['cw'])
                M5 = [self.sb("M5_%d" % r, [128, D]) for r in range(NB + 1)]
                for r in range(NB + 1):
                    self.load_bc(M5[r], li, r, 5, 'M5_%d' % r)
                hT = [self.sb("fh%d" % i, [128, 8, 256 + 2 * HALO], BF16) for i in range(2)]
                u = [self.sb("fu%d" % i, [128, HF, 256], BF16) for i in range(2)]
                tA = [self.sb("ftA%d" % i, [128, 256]) for i in range(2)]
                tB = [self.sb("ftB%d" % i, [128, 256]) for i in range(2)]
                pv = [self.ps("fpv%d" % i, [128, 512]) for i in range(2)]
                pg = [self.ps("fpg%d" % i, [128, 512]) for i in range(2)]
                py = [self.ps("fpy%d" % i, [128, 2, 512]) for i in range(2)]
                xt = [self.sb("fx%d" % i, [128, D]) for i in range(2)]
                wi = 0
                ci = 0
                ti = 0
                for b in range(NB):
                    for w in range(NT // 2):
                        if w == 0 and not ctx_needed:
                            continue
                        h = hT[wi % 2]
                        hk = 'fh%d' % (wi % 2)
                        uu = u[wi % 2]
                        uk = 'fu%d' % (wi % 2)
                        c0 = self.hcol(b, 2 * w)
                        kb.dma('sp', h[:], hv[:, :, c0 - HALO:c0 + 256 + HALO], reads=['hTs'], writes=[hk])
                        with self.nosame():
                            for f in range(HF):
                                a = ci % 2
                                ci += 1
                                for k in range(8):
                                    self.mm(pv[a][:, 0:256], wv[:, k, f * 128:(f + 1) * 128], h[:, k, HALO:HALO + 256],
                                            k == 0, k == 7, ['wv', hk], ['fpv%d' % a])
                                for k in range(8):
                                    self.mm(pg[a][:, 0:258], wg[:, k, f * 128:(f + 1) * 128], h[:, k, HALO - 1:HALO + 257],
                                            k == 0, k == 7, ['wg', hk], ['fpg%d' % a])
                                kb.op('dve', lambda e: e.tensor_scalar(out=tA[a][:], in0=pg[a][:, 0:256], scalar1=cw[:, f, 0:1],
                                                                       scalar2=None, op0=ALU.mult),
                                      reads=['fpg%d' % a, 'cw'], writes=['ftA%d' % a])
                                kb.op('dve', lambda e: e.scalar_tensor_tensor(out=tA[a][:], in0=pg[a][:, 1:257], scalar=cw[:, f, 1:2],
                                                                              in1=tA[a][:], op0=ALU.mult, op1=ALU.add),
                                      reads=['fpg%d' % a, 'cw', 'ftA%d' % a], writes=['ftA%d' % a])
                                kb.op('dve', lambda e: e.scalar_tensor_tensor(out=tA[a][:], in0=pg[a][:, 2:258], scalar=cw[:, f, 2:3],
                                                                              in1=tA[a][:], op0=ALU.mult, op1=ALU.add),
                                      reads=['fpg%d' % a, 'cw', 'ftA%d' % a], writes=['ftA%d' % a])
                                kb.op('act', lambda e: e.activation(out=tB[a][:], in_=tA[a][:], func=AF.Silu, bias=cw[:, f, 3:4]),
                                      reads=['ftA%d' % a, 'cw'], writes=['ftB%d' % a])
                                kb.op('dve', lambda e: e.tensor_tensor(out=uu[:, f, :], in0=pv[a][:, 0:256], in1=tB[a][:], op=ALU.mult),
                                      reads=['fpv%d' % a, 'ftB%d' % a], writes=[uk])
                        for t2 in range(2):
                            j = 2 * w + t2
                            r = NB if j < NTC else b
                            i = ti % 2
                            ti += 1
                            for n in range(2):
                                for f in range(HF):
                                    self.mm(py[i][:, n, :], uu[:, f, t2 * 128:(t2 + 1) * 128], wo[:, f, n * 512:(n + 1) * 512],
                                            f == 0, f == HF - 1, [uk, 'wo'], ['fpy%d' % i])
                            kb.dma('sp', xt[i][:], self.xs[b, j * 128:(j + 1) * 128, :], reads=['xs_%d_%d' % (b, j)], writes=['fx%d' % i])
                            pyf = py[i][:].rearrange("p a b -> p (a b)")
                            kb.op('dve', lambda e: e.tensor_tensor(out=pyf, in0=pyf, in1=M5[r][:], op=ALU.mult),
                                  reads=['fpy%d' % i, 'M5_%d' % r], writes=['fpy%d' % i])
                            kb.op('dve', lambda e: e.tensor_tensor(out=xt[i][:], in0=pyf, in1=xt[i][:], op=ALU.add),
                                  reads=['fpy%d' % i, 'fx%d' % i], writes=['fx%d' % i])
                            kb.dma('pool', self.xs[b, j * 128:(j + 1) * 128, :], xt[i][:], reads=['fx%d' % i],
                                   writes=['xs_%d_%d' % (b, j)])
                        wi += 1

    def final_phase(self):
        kb, NB = self.kb, self.NB
        with self.phase():
            G = self.sb("fG", [128, D])
            kb.dma('sp', G[:], self.final_norm_g[0:1, :].to_broadcast([128, D]), writes=['fG'])
            xt = [self.sb("fx%d" % i, [128, D]) for i in range(3)]
            sq = self.sb("fsq", [128, D])
            st = [self.sb("fst%d" % i, [128, 4]) for i in range(3)]
            it = 0
            for b in range(NB):
                for j in range(NTC, NT):
                    i = it % 3
                    it += 1
                    kb.dma('sp', xt[i][:], self.xs[b, j * 128:(j + 1) * 128, :], reads=['xs_%d_%d' % (b, j)], writes=['fx%d' % i])
                    kb.op('act', lambda e: e.activation(out=sq[:], in_=xt[i][:], func=AF.Square, accum_out=st[i][:, 0:1]),
                          reads=['fx%d' % i], writes=['fsq', 'fst%d' % i])
                    kb.op('dve', lambda e: e.tensor_scalar(out=st[i][:, 1:2], in0=st[i][:, 0:1], scalar1=1.0 / D,
                                                           scalar2=EPS, op0=ALU.mult, op1=ALU.add),
                          reads=['fst%d' % i], writes=['fst%d' % i])
                    kb.op('act', lambda e: e.activation(out=st[i][:, 2:3], in_=st[i][:, 1:2], func=AF.Sqrt),
                          reads=['fst%d' % i], writes=['fst%d' % i])
                    kb.op('dve', lambda e: e.reciprocal(out=st[i][:, 3:4], in_=st[i][:, 2:3]),
                          reads=['fst%d' % i], writes=['fst%d' % i])
                    kb.op('dve', lambda e: e.scalar_tensor_tensor(out=xt[i][:], in0=xt[i][:], scalar=st[i][:, 3:4],
                                                                  in1=G[:], op0=ALU.mult, op1=ALU.mult),
                          reads=['fx%d' % i, 'fst%d' % i, 'fG'], writes=['fx%d' % i])
                    kb.dma('pool', self.out[b, (j - NTC) * 128:(j - NTC + 1) * 128, :], xt[i][:], reads=['fx%d' % i],
                           writes=['out_%d_%d' % (b, j)])

    def attn_phase(self, li, jx, layer0, want_ctx):
        kb, NB = self.kb, self.NB
        hv = self.hT_view()
        SC = 128 ** -0.5
        for b in range(NB):
            with self.phase():
                qT = self.sb("qT", [128, 8, TOK], BF16)
                kT = self.sb("kT", [128, 2, TOK], BF16)
                V1 = self.sb("V1", [128, NT, 2, 132], BF16)
                kb.op('pool', lambda e: e.memset(V1[:, :, :, 128:129], 1.0), writes=['V1'])
                ident = self.sb("identb", [128, 128], BF16)
                kb.dma('pool', ident[:], self.c_ident, writes=['ident'])
                with self.phase():
                    win = self.sb("awin", [128, 8, 1536], BF16)
                    kb.dma('pool', win[:], self.attn_w_in[jx].rearrange("(k p) c -> p k c", p=128), writes=['awin'])
                    Gqk = self.sb("Gqk", [128, 2, 128])
                    kb.dma('sp', Gqk[:, 0, :], self.attn_q_norm_g[jx:jx + 1, :].to_broadcast([128, 128]), writes=['Gqk'])
                    kb.dma('sp', Gqk[:, 1, :], self.attn_k_norm_g[jx:jx + 1, :].to_broadcast([128, 128]), writes=['Gqk'])
                    hT = [self.sb("ah%d" % i, [128, 8, 128], BF16) for i in range(2)]
                    pz = [self.ps("apz%d" % i, [128, 512]) for i in range(3)]
                    zs = self.sb("azs", [128, 1536])
                    sq = self.sb("asq", [128, 1280])
                    st = self.sb("ast", [128, 4, 10])
                    qn = self.sb("aqn", [128, 1280])
                    qb = self.sb("aqb", [128, 1280], BF16)
                    rp = [self.sb("arp%d" % i, [128, 128]) for i in range(2)]
                    t1 = self.sb("at1", [128, 640])
                    t2 = self.sb("at2", [128, 640])
                    ptq = self.ps("aptq", [128, 8, 128], BF16)
                    ptk = self.ps("aptk", [128, 2, 128], BF16)
                    for j in range(NT):
                        i = j % 2
                        c0 = self.hcol(b, j)
                        kb.dma('sp', hT[i][:], hv[:, :, c0:c0 + 128], reads=['hTs'], writes=['ah%d' % i])
                        for n in range(3):
                            for k in range(8):
                                self.mm(pz[n][:], hT[i][:, k, :], win[:, k, n * 512:(n + 1) * 512], k == 0, k == 7,
                                        ['ah%d' % i, 'awin'], ['apz%d' % n])
                            kb.op('act', lambda e: e.copy(out=zs[:, n * 512:(n + 1) * 512], in_=pz[n][:]),
                                  reads=['apz%d' % n], writes=['azs'])
                        kb.op('pool', lambda e: e.tensor_copy(out=V1[:, j, :, 0:128],
                                                              in_=zs[:, 1280:1536].rearrange("p (g d) -> p g d", g=2)),
                              reads=['azs'], writes=['V1'])
                        kb.op('dve', lambda e: e.tensor_tensor(out=sq[:], in0=zs[:, 0:1280], in1=zs[:, 0:1280], op=ALU.mult),
                              reads=['azs'], writes=['asq'])
                        kb.op('dve', lambda e: e.tensor_reduce(out=st[:, 0, :], in_=sq[:].rearrange("p (h d) -> p h d", h=10),
                                                               axis=AX.X, op=ALU.add),
                              reads=['asq'], writes=['ast'])
                        kb.op('dve', lambda e: e.tensor_scalar(out=st[:, 1, :], in0=st[:, 0, :], scalar1=1.0 / 128, scalar2=EPS,
                                                               op0=ALU.mult, op1=ALU.add), reads=['ast'], writes=['ast'])
                        kb.op('act', lambda e: e.activation(out=st[:, 2, :], in_=st[:, 1, :], func=AF.Sqrt),
                              reads=['ast'], writes=['ast'])
                        kb.op('dve', lambda e: e.reciprocal(out=st[:, 3, :], in_=st[:, 2, :]), reads=['ast'], writes=['ast'])
                        z3 = zs[:, 0:1280].rearrange("p (h d) -> p h d", h=10)
                        q3 = qn[:].rearrange("p (h d) -> p h d", h=10)
                        kb.op('dve', lambda e: e.tensor_tensor(out=q3, in0=z3, in1=st[:, 3, :].unsqueeze(2).to_broadcast([128, 10, 128]),
                                                               op=ALU.mult), reads=['azs', 'ast'], writes=['aqn'])
                        kb.op('dve', lambda e: e.tensor_tensor(out=q3[:, 0:8, :], in0=q3[:, 0:8, :],
                                                               in1=Gqk[:, 0:1, :].to_broadcast([128, 8, 128]), op=ALU.mult),
                              reads=['aqn', 'Gqk'], writes=['aqn'])
                        kb.op('dve', lambda e: e.tensor_tensor(out=q3[:, 8:10, :], in0=q3[:, 8:10, :],
                                                               in1=Gqk[:, 1:2, :].to_broadcast([128, 2, 128]), op=ALU.mult),
                              reads=['aqn', 'Gqk'], writes=['aqn'])
                        if j >= NTC:
                            rr = rp[j % 2]
                            rk = 'arp%d' % (j % 2)
                            kb.dma('sp', rr[:], self.c_rope[(j - NTC) * 128:(j - NTC + 1) * 128, :], writes=[rk])
                            q4 = qn[:].rearrange("p (h d t) -> p h d t", h=10, t=2)
                            b4 = qb[:].rearrange("p (h d t) -> p h d t", h=10, t=2)
                            x0, x1 = q4[:, :, :, 0], q4[:, :, :, 1]
                            cosb = rr[:, 0:64].unsqueeze(1).to_broadcast([128, 10, 64])
                            sinb = rr[:, 64:128].unsqueeze(1).to_broadcast([128, 10, 64])
                            t13 = t1[:].rearrange("p (h d) -> p h d", h=10)
                            t23 = t2[:].rearrange("p (h d) -> p h d", h=10)
                            kb.op('dve', lambda e: e.tensor_tensor(out=t13, in0=x0, in1=cosb, op=ALU.mult), reads=['aqn', rk], writes=['at1'])
                            kb.op('pool', lambda e: e.tensor_tensor(out=t23, in0=x1, in1=sinb, op=ALU.mult), reads=['aqn', rk], writes=['at2'])
                            kb.op('dve', lambda e: e.tensor_tensor(out=b4[:, :, :, 0], in0=t13, in1=t23, op=ALU.subtract),
                                  reads=['at1', 'at2'], writes=['aqb'])
                            kb.op('dve', lambda e: e.tensor_tensor(out=t13, in0=x0, in1=sinb, op=ALU.mult), reads=['aqn', rk, 'aqb'], writes=['at1'])
                            kb.op('pool', lambda e: e.tensor_tensor(out=t23, in0=x1, in1=cosb, op=ALU.mult), reads=['aqn', rk, 'aqb'], writes=['at2'])
                            kb.op('dve', lambda e: e.tensor_tensor(out=b4[:, :, :, 1], in0=t13, in1=t23, op=ALU.add),
                                  reads=['at1', 'at2'], writes=['aqb'])
                        else:
                            kb.op('dve', lambda e: e.tensor_copy(out=qb[:], in_=qn[:]), reads=['aqn'], writes=['aqb'])
                        for h in range(8):
                            self.tr(ptq[:, h, :], qb[:, h * 128:(h + 1) * 128], ident[:], ['aqb', 'ident'], ['aptq'])
                        for g in range(2):
                            self.tr(ptk[:, g, :], qb[:, (8 + g) * 128:(9 + g) * 128], ident[:], ['aqb', 'ident'], ['aptk'])
                        kb.op('act', lambda e: e.copy(out=qT[:, :, j * 128:(j + 1) * 128], in_=ptq[:]), reads=['aptq'], writes=['qT'])
                        kb.op('act', lambda e: e.copy(out=kT[:, :, j * 128:(j + 1) * 128], in_=ptk[:]), reads=['aptk'], writes=['kT'])
                with self.phase():
                    o = self.outproj_setup(self.attn_w_out[jx], li)
                    E = self.sb("aE", [128, NT, 512], BF16)
                    pS = [self.ps("apS%d" % i, [128, 512]) for i in range(2)]
                    pO = [self.ps("apO%d" % i, [128, 512]) for i in range(2)]
                    ob = [self.sb("aob%d" % i, [128, D], BF16) for i in range(2)]
                    rc = self.sb("arc", [128, 8])
                    si = 0
                    oi = 0
                    for jq in range(NT):
                        if jq < NTC and not want_ctx:
                            continue
                        nk = NTC if jq < NTC else NT
                        obt = ob[jq % 2]
                        obk = 'aob%d' % (jq % 2)
                        for g in range(2):
                            for kt in range(nk):
                                p = pS[si % 2]
                                pk = 'apS%d' % (si % 2)
                                si += 1
                                self.mm(p[:].rearrange("p (h q) -> p h q", h=4), kT[:, g, kt * 128:(kt + 1) * 128], qT[:, 4 * g:4 * g + 4, jq * 128:(jq + 1) * 128],
                                        True, True, ['kT', 'qT'], [pk])
                                kb.op('act', lambda e: e.activation(out=E[:, kt, :], in_=p[:], func=AF.Exp, scale=SC),
                                      reads=[pk], writes=['aE'])
                            for hh in range(4):
                                po = pO[oi % 2]
                                pok = 'apO%d' % (oi % 2)
                                oi += 1
                                for kt in range(nk):
                                    self.mm(po[:, 0:129], E[:, kt, hh * 128:(hh + 1) * 128], V1[:, kt, g, 0:129],
                                            kt == 0, kt == nk - 1, ['aE', 'V1'], [pok])
                                hd = 4 * g + hh
                                kb.op('dve', lambda e: e.reciprocal(out=rc[:, hd:hd + 1], in_=po[:, 128:129]),
                                      reads=[pok], writes=['arc'])
                                kb.op('dve', lambda e: e.tensor_scalar(out=obt[:, hd * 128:(hd + 1) * 128], in0=po[:, 0:128],
                                                                       scalar1=rc[:, hd:hd + 1], scalar2=None, op0=ALU.mult),
                                      reads=[pok, 'arc'], writes=[obk])
                        self.outproj_tile(o, obt, obk, layer0, b, jq)

    def gdn_decl(self):
        if hasattr(self, 'g_qkT'):
            return
        NB = self.NB
        self.g_qkT = self.dram("g_qkT", [2, D, NB * TOK], F32)
        self.g_ktok = self.dram("g_ktok", [NB * TOK, D], F32)
        self.g_vtok = self.dram("g_vtok", [NB * TOK, D], F32)
        self.g_sgate = self.dram("g_sgate", [NB * TOK, D], F32)
        self.g_bg = self.dram("g_bg", [NB * TOK, 32], F32)
        self.g_odir = self.dram("g_odir", [2, NB * TOK, D], F32)

    def gdn_phase(self, li, jx, layer0, want_ctx):
        self.gdn_decl()
        self.gdn_proj(li, jx)
        self.gdn_scan(li, jx)
        self.gdn_finish(li, jx, layer0, want_ctx)

    def gdn_proj(self, li, jx):
        kb, NB = self.kb, self.NB
        hv = self.hT_view()
        with self.phase():
            win = self.sb("gwin", [128, 8, 4128], BF16)
            wsrc = self.gdn_w_in[jx].rearrange("(k p) c -> p k c", p=128)
            for k in range(8):
                kb.dma('pool', win[:, k, :], wsrc[:, k, :], writes=['gwin'])
            cw = self.sb("gcw", [128, 24, 5])
            with self.nc.allow_non_contiguous_dma(reason="tiny per-channel conv taps"):
                for t in range(5):
                    for f0 in range(0, 24, 8):
                        kb.dma('sp', cw[:, f0:f0 + 8, t], self.gdn_conv_w[jx, t, f0 * 128:(f0 + 8) * 128].rearrange("(f p) -> p f", p=128),
                               writes=['gcw'])
            identf = self.sb("identf", [128, 128])
            kb.dma('sp', identf[:], self.c_ident, writes=['identf'])
            onesf = self.sb("onesf", [128, 128])
            kb.op('dve', lambda e: e.memset(onesf[:], 1.0), writes=['onesf'])
            DTB = self.sb("gdtb", [128, 16])
            NA = self.sb("gna", [128, 16])
            kb.dma('sp', DTB[:], self.gdn_dt_bias[jx:jx + 1, :].to_broadcast([128, 16]), writes=['gdtb'])
            kb.dma('sp', NA[:], self.gdn_a_log[jx:jx + 1, :].to_broadcast([128, 16]), writes=['gna'])
            kb.op('act', lambda e: e.activation(out=NA[:], in_=NA[:], func=AF.Exp), reads=['gna'], writes=['gna'])
            kb.op('dve', lambda e: e.tensor_scalar(out=NA[:], in0=NA[:], scalar1=-1.0, scalar2=None, op0=ALU.mult),
                  reads=['gna'], writes=['gna'])
            h2 = [self.sb("gh%d" % i, [128, 8, 256 + 2 * HALO], BF16) for i in range(2)]
            NPZ = 2
            NB4 = 4
            pz = [self.ps("gpz%d" % i, [128, 512]) for i in range(NPZ)]
            pnbk = [self.ps("gpnb%d" % i, [128, 512]) for i in range(2)]
            pnl = [pnbk[0][:, 0:256], pnbk[1][:, 0:256]]
            pXb = [self.ps("gpX%d" % i, [128, 512]) for i in range(2)]
            pTl = [pXb[0][:, 0:256].rearrange("p (a b) -> p a b", a=2), pXb[1][:, 0:256].rearrange("p (a b) -> p a b", a=2)]
            pg = self.ps("gpg", [128, 2, 512])
            pb = pXb[1][:, 256:288]
            tA = [self.sb("gtA%d" % i, [128, 256]) for i in range(NB4)]
            sAll = [self.sb("gsAll%d" % i, [128, 24, 256]) for i in range(2)]
            rAll = [self.sb("grAll%d" % i, [128, 16, 256]) for i in range(2)]
            sqb = [self.sb("gsqb%d" % i, [128, 256], BF16) for i in range(NB4)]
            onesb = self.sb("gonesb", [128, 128], BF16)
            kb.op('dve', lambda e: e.memset(onesb[:], 1.0), writes=['onesb'])
            kst = self.sb("gkst", [128, 2, D])
            vst = self.sb("gvst", [128, 2, D])
            gst = [self.sb("ggst%d" % i, [128, D]) for i in range(2)]
            bgs = self.sb("gbgs", [128, 32])
            bt = self.sb("gbt", [128, 4, 16])
            qkTv = self.g_qkT.rearrange("q (h p) c -> q p h c", p=128)
            ci = 0
            wi = 0
            for b in range(NB):
                for w in range(NT // 2):
                    h = h2[wi % 2]
                    hk = 'gh%d' % (wi % 2)
                    wi += 1
                    c0 = self.hcol(b, 2 * w)
                    tok0 = b * TOK + 2 * w * 128
                    kb.dma('sp', h[:], hv[:, :, c0 - HALO:c0 + 256 + HALO], writes=[hk])
                    ws = wi % 2
                    sA, rA = sAll[ws], rAll[ws]
                    sAk = lambda f_: 'gsA%d_%d' % (ws, f_)
                    rAk = 'grA%d' % ws

                    def stage_ones(f_):
                        an_ = f_ % 2
                        self.mm(pnl[an_], onesb[:], sqb[f_ % NB4][:], True, True, ['onesb', 'gsqb%d' % (f_ % NB4)], ['gpn%d' % an_])
                        m_ = 128.0 if f_ < 8 else 1.0
                        kb.op('dve', lambda e: e.tensor_scalar(out=rA[:, f_, :], in0=pnl[an_], scalar1=m_, scalar2=EPS * m_,
                                                               op0=ALU.mult, op1=ALU.add), reads=['gpn%d' % an_], writes=[rAk])

                    def stage_tr(f_):
                        dst = kst if f_ < 16 else vst
                        dk = 'gkst' if f_ < 16 else 'gvst'
                        an_ = f_ % 2
                        pT, pTk = pTl[an_], 'gpX%d' % an_
                        for t2_ in range(2):
                            self.tr(pT[:, t2_, :], sA[:, f_, t2_ * 128:(t2_ + 1) * 128], identf[:], [sAk(f_), 'identf'], [pTk])
                        kb.op('act', lambda e: e.copy(out=dst[:, :, (f_ % 8) * 128:(f_ % 8 + 1) * 128], in_=pT), reads=[pTk], writes=[dk])

                    with self.nosame():
                        for f in range(24):
                            a = ci % NB4
                            az = ci % NPZ
                            ci += 1
                            pzk = 'gpz%d' % az
                            for k in range(8):
                                self.mm(pz[az][:, 0:260], win[:, k, f * 128:(f + 1) * 128], h[:, k, :], k == 0, k == 7,
                                        ['gwin', hk], [pzk])
                            tk = 'gtA%d' % a
                            kb.op('dve', lambda e: e.tensor_scalar(out=tA[a][:], in0=pz[az][:, 0:256], scalar1=cw[:, f, 0:1],
                                                                   scalar2=None, op0=ALU.mult), reads=[pzk, 'gcw'], writes=[tk])
                            for t in range(1, 5):
                                kb.op('dve', lambda e: e.scalar_tensor_tensor(out=tA[a][:], in0=pz[az][:, t:t + 256], scalar=cw[:, f, t:t + 1],
                                                                              in1=tA[a][:], op0=ALU.mult, op1=ALU.add),
                                      reads=[pzk, 'gcw', tk], writes=[tk])
                            kb.op('act', lambda e: e.activation(out=sA[:, f, :], in_=tA[a][:], func=AF.Silu), reads=[tk], writes=[sAk(f)])
                            if f < 16:
                                kb.op('pool', lambda e: e.tensor_tensor(out=sqb[f % NB4][:], in0=sA[:, f, :], in1=sA[:, f, :], op=ALU.mult),
                                      reads=[sAk(f)], writes=['gsqb%d' % (f % NB4)])
                            if 2 <= f < 18:
                                stage_ones(f - 2)
                            if f >= 19:
                                stage_tr(f - 3)
                        for f_ in (21, 22, 23):
                            stage_tr(f_)
                        kb.op('act', lambda e: e.activation(out=rA[:], in_=rA[:], func=AF.Sqrt), reads=[rAk], writes=[rAk])
                        for q4 in range(4):
                            kb.op('dve', lambda e: e.reciprocal(out=rA[:, 4 * q4:4 * q4 + 4, :], in_=rA[:, 4 * q4:4 * q4 + 4, :]),
                                  reads=[rAk], writes=[rAk])
                        for f in range(16):
                            kb.op('pool', lambda e: e.tensor_tensor(out=sA[:, f, :], in0=sA[:, f, :], in1=rA[:, f, :], op=ALU.mult),
                                  reads=[sAk(f), rAk], writes=[sAk(f)])
                            kb.dma('pool', qkTv[f // 8, :, f % 8, tok0:tok0 + 256], sA[:, f, :], reads=[sAk(f)], writes=['g_qkT_%d_%d' % (wi, f)])
                            if f >= 8:
                                stage_tr(f)
                    for t2 in range(2):
                        r0 = tok0 + t2 * 128
                        kb.dma('pool', self.g_ktok[r0:r0 + 128, :], kst[:, t2, :], reads=['gkst'], writes=['g_ktok_%d' % r0])
                        kb.dma('pool', self.g_vtok[r0:r0 + 128, :], vst[:, t2, :], reads=['gvst'], writes=['g_vtok_%d' % r0])
                        hs = h[:, :, HALO + t2 * 128:HALO + (t2 + 1) * 128]
                        for n in range(2):
                            for k in range(8):
                                self.mm(pg[:, n, :], hs[:, k, :], win[:, k, 3072 + n * 512:3072 + (n + 1) * 512], k == 0, k == 7,
                                        [hk, 'gwin'], ['gpg'])
                        g_ = gst[t2]
                        gk2 = 'ggst%d' % t2
                        kb.op('act', lambda e: e.activation(out=g_[:], in_=pg[:].rearrange("p a b -> p (a b)"), func=AF.Silu),
                              reads=['gpg'], writes=[gk2])
                        kb.dma('pool', self.g_sgate[r0:r0 + 128, :], g_[:], reads=[gk2], writes=['g_sgate_%d' % r0])
                        for k in range(8):
                            self.mm(pb, hs[:, k, :], win[:, k, 4096:4128], k == 0, k == 7, [hk, 'gwin'], ['gpX1'])
                        pb4 = pb.rearrange("p (d t h) -> p d t h", d=2, t=2)
                        kb.op('act', lambda e: e.activation(out=bgs[:, 0:16].rearrange("p (d h) -> p d h", d=2), in_=pb4[:, :, 0, :],
                                                            func=AF.Sigmoid), reads=['gpX1'], writes=['gbgs'])
                        kb.op('dve', lambda e: e.tensor_tensor(out=bt[:, 0, :].rearrange("p (d h) -> p d h", d=2), in0=pb4[:, :, 1, :],
                                                               in1=DTB[:].rearrange("p (d h) -> p d h", d=2), op=ALU.add),
                              reads=['gpX1', 'gdtb'], writes=['gbt'])
                        kb.op('dve', lambda e: e.scalar_tensor_tensor(out=bt[:, 1, :], in0=bt[:, 0, :], scalar=-1.0, in1=bt[:, 0, :],
                                                                      op0=ALU.mult, op1=ALU.max), reads=['gbt'], writes=['gbt'])
                        kb.op('act', lambda e: e.activation(out=bt[:, 2, :], in_=bt[:, 1, :], func=AF.Exp, scale=-1.0),
                              reads=['gbt'], writes=['gbt'])
                        kb.op('act', lambda e: e.activation(out=bt[:, 3, :], in_=bt[:, 2, :], func=AF.Ln, bias=1.0),
                              reads=['gbt'], writes=['gbt'])
                        kb.op('dve', lambda e: e.scalar_tensor_tensor(out=bt[:, 1, :], in0=bt[:, 0, :], scalar=0.0, in1=bt[:, 3, :],
                                                                      op0=ALU.max, op1=ALU.add), reads=['gbt'], writes=['gbt'])
                        kb.op('dve', lambda e: e.tensor_tensor(out=bgs[:, 16:32], in0=bt[:, 1, :], in1=NA[:], op=ALU.mult),
                              reads=['gbt', 'gna'], writes=['gbgs'])
                        kb.dma('pool', self.g_bg[r0:r0 + 128, :], bgs[:], reads=['gbgs'], writes=['g_bg_%d' % r0])

    def gdn_scan(self, li, jx):
        kb, NB = self.kb, self.NB
        HB = 4
        qkTv = self.g_qkT.rearrange("q (h p) c -> q p h c", p=128)
        order = [list(range(NT)), [1, 0] + list(range(NT - 1, NTC - 1, -1))]
        with self.phase():
            MS = self.sb("gmask", [128, 8, 128])
            kb.dma('sp', MS[:], self.c_masks[0:8].rearrange("m p c -> p m c"), writes=['gmask'])
            identf = self.sb("identf", [128, 128])
            kb.dma('sp', identf[:], self.c_ident, writes=['identf'])
            PS = [self.ps("gps%d" % i, [128, 512]) for i in range(8)]
            PK = ['gps%d' % i for i in range(8)]

            def v4(t):
                return t[:].rearrange("p (h c) -> p h c", h=HB)
            names = ['qT', 'kT', 'ktok', 'vtok', 'Gbc', 'Dm', 'E', 'DT', 'DTs', 'EG', 'qd', 'W', 'WT', 'Wa0', 'Wa1', 'Wb0', 'Wb1',
                     'QKm', 'kdec', 'wT', 'vn']
            sets = []
            for si in range(2):
                T = {}
                for nm in names:
                    T[nm] = self.sb("g%s%d" % (nm, si), [128, HB * 128])
                for nm in ('y0', 'y1', 'UW'):
                    T[nm] = self.sb("g%s%d" % (nm, si), [128, HB, 256])
                T['cols'] = self.sb("gcols%d" % si, [128, 6, HB])
                T['alast'] = self.sb("galast%d" % si, [128, HB, 2])
                T['k'] = 's%d_' % si
                sets.append(T)
            bgt = [self.sb("gbg%d" % i, [128, 32]) for i in range(4)]
            ost = [self.sb("gost%d" % i, [128, D]) for i in range(4)]
            S = [[self.sb("gS_%d_%d" % (d, hf), [128, HB, 128]) for hf in range(2)] for d in range(2)]
            stepi = 0
            bi = 0
            for b in range(NB):
                for d in range(2):
                    for hf in range(2):
                        kb.op('pool', lambda e: e.memset(S[d][hf][:], 0.0), writes=['gS_%d_%d' % (d, hf)])
                for n in range(NT):
                    for d in range(2):
                        j = order[d][n]
                        tok0 = b * TOK + j * 128
                        bg = bgt[bi % 4]
                        bgk = 'gbg%d' % (bi % 4)
                        os_ = ost[bi % 4]
                        osk = 'gost%d' % (bi % 4)
                        bi += 1
                        kb.dma('sp', bg[:], self.g_bg[tok0:tok0 + 128, :], writes=[bgk])
                        Ud, Usd, nUd, BO = MS[:, d, :], MS[:, 2 + d, :], MS[:, 4 + d, :], MS[:, 6, :]
                        for hf in range(2):
                            T = sets[stepi % 2]
                            K = lambda nm: T['k'] + nm
                            stepi += 1
                            h0 = hf * HB
                            Sk = 'gS_%d_%d' % (d, hf)
                            St = S[d][hf]
                            kb.dma('sp', v4(T['qT']), qkTv[0, :, h0:h0 + HB, tok0:tok0 + 128], writes=[K('qT')])
                            kb.dma('sp', v4(T['kT']), qkTv[1, :, h0:h0 + HB, tok0:tok0 + 128], writes=[K('kT')])
                            kb.dma('sp', T['ktok'][:], self.g_ktok[tok0:tok0 + 128, h0 * 128:(h0 + HB) * 128], writes=[K('ktok')])
                            kb.dma('sp', T['vtok'][:], self.g_vtok[tok0:tok0 + 128, h0 * 128:(h0 + HB) * 128], writes=[K('vtok')])
                            g4 = bg[:, 16 + d * 8 + h0:16 + d * 8 + h0 + HB]
                            be4 = bg[:, d * 8 + h0:d * 8 + h0 + HB]
                            bc4 = lambda ap: ap.unsqueeze(2).to_broadcast([128, HB, 128])
                            mk4 = lambda ap: ap.unsqueeze(1).to_broadcast([128, HB, 128])
                            kb.op('dve', lambda e: e.tensor_copy(out=v4(T['Gbc']), in_=bc4(g4)), reads=[bgk], writes=[K('Gbc')])
                            Gbc = v4(T['Gbc'])
                            for h in range(HB):
                                self.mm(PS[0][:, h * 128:(h + 1) * 128], Gbc[:, h, :], Ud, True, True, [K('Gbc'), 'gmask'], [PK[0]])
                            for h in range(HB):
                                self.mm(PS[1][:, h * 128:(h + 1) * 128], Gbc[:, h, :], Ud, True, False, [K('Gbc'), 'gmask'], [PK[1]])
                                self.mm(PS[1][:, h * 128:(h + 1) * 128], nUd, Gbc[:, h, :], False, True, [K('Gbc'), 'gmask'], [PK[1]])
                            self.mm(PS[5][:, 0:HB], Ud, g4, True, True, ['gmask', bgk], [PK[5]])
                            self.mm(PS[5][:, HB:2 * HB], BO, g4, True, True, ['gmask', bgk], [PK[5]])
                            for h in range(HB):
                                self.mm(PS[5][:, 8 + 2 * h:10 + 2 * h], Gbc[:, h, :], MS[:, 6, 0:128:64], True, True,
                                        [K('Gbc'), 'gmask'], [PK[5]])
                            cols = T['cols']
                            kb.op('act', lambda e: e.copy(out=cols[:, 0, :], in_=PS[5][:, 0:HB]), reads=[PK[5]], writes=[K('cols')])
                            kb.op('dve', lambda e: e.tensor_tensor(out=cols[:, 1, :], in0=PS[5][:, HB:2 * HB], in1=cols[:, 0, :], op=ALU.subtract),
                                  reads=[PK[5], K('cols')], writes=[K('cols')])
                            kb.op('act', lambda e: e.activation(out=cols[:, 2, :], in_=cols[:, 0, :], func=AF.Exp), reads=[K('cols')], writes=[K('cols')])
                            kb.op('act', lambda e: e.activation(out=cols[:, 3, :], in_=cols[:, 1, :], func=AF.Exp), reads=[K('cols')], writes=[K('cols')])
                            kb.op('act', lambda e: e.activation(out=T['alast'][:].rearrange("p h c -> p (h c)"), in_=PS[5][:, 8:8 + 2 * HB], func=AF.Exp),
                                  reads=[PK[5]], writes=[K('alast')])
                            kb.op('dve', lambda e: e.tensor_scalar(out=T['Dm'][:], in0=PS[1][:], scalar1=0.0, scalar2=None, op0=ALU.min),
                                  reads=[PK[1]], writes=[K('Dm')])
                            kb.op('act', lambda e: e.activation(out=T['E'][:], in_=T['Dm'][:], func=AF.Exp), reads=[K('Dm')], writes=[K('E')])
                            kb.op('dve', lambda e: e.tensor_tensor(out=v4(T['DT']), in0=v4(T['E']), in1=mk4(Ud), op=ALU.mult),
                                  reads=[K('E'), 'gmask'], writes=[K('DT')])
                            kb.op('pool', lambda e: e.tensor_tensor(out=v4(T['DTs']), in0=v4(T['E']), in1=mk4(Usd), op=ALU.mult),
                                  reads=[K('E'), 'gmask'], writes=[K('DTs')])
                            kb.op('act', lambda e: e.activation(out=T['EG'][:], in_=PS[0][:], func=AF.Exp), reads=[PK[0]], writes=[K('EG')])
                            kb.op('pool', lambda e: e.tensor_tensor(out=T['qd'][:], in0=T['qT'][:], in1=T['EG'][:], op=ALU.mult),
                                  reads=[K('qT'), K('EG')], writes=[K('qd')])
                            kT4, qT4 = v4(T['kT']), v4(T['qT'])
                            for h in range(HB):
                                self.mm(PS[2][:, h * 128:(h + 1) * 128], kT4[:, h, :], kT4[:, h, :], True, True, [K('kT')], [PK[2]])
                            for h in range(HB):
                                self.mm(PS[3][:, h * 128:(h + 1) * 128], kT4[:, h, :], qT4[:, h, :], True, True, [K('kT'), K('qT')], [PK[3]])
                            kb.op('dve', lambda e: e.tensor_tensor(out=T['DTs'][:], in0=PS[2][:], in1=T['DTs'][:], op=ALU.mult),
                                  reads=[PK[2], K('DTs')], writes=[K('DTs')])
                            kb.op('dve', lambda e: e.tensor_tensor(out=v4(T['W']).bitcast(F32R), in0=v4(T['DTs']), in1=bc4(be4), op=ALU.mult),
                                  reads=[K('DTs'), bgk], writes=[K('W')])
                            kb.op('dve', lambda e: e.tensor_tensor(out=T['QKm'][:], in0=PS[3][:], in1=T['DT'][:], op=ALU.mult),
                                  reads=[PK[3], K('DT')], writes=[K('QKm')])
                            kb.op('act', lambda e: e.copy(out=T['y0'][:, :, 0:128].bitcast(F32R), in_=v4(T['vtok'])), reads=[K('vtok')], writes=[K('y0')])
                            kb.op('dve', lambda e: e.tensor_tensor(out=T['y0'][:, :, 128:256].bitcast(F32R), in0=v4(T['ktok']), in1=bc4(cols[:, 2, :]), op=ALU.mult),
                                  reads=[K('ktok'), K('cols')], writes=[K('y0')])
                            kb.op('pool', lambda e: e.tensor_tensor(out=v4(T['kdec']), in0=v4(T['ktok']), in1=bc4(cols[:, 3, :]), op=ALU.mult),
                                  reads=[K('ktok'), K('cols')], writes=[K('kdec')])
                            with self.nosame():
                                W4 = v4(T['W'])
                                for h in range(HB):
                                    self.tr(PS[4][:, h * 128:(h + 1) * 128], W4[:, h, :], identf[:], [K('W'), 'identf'], [PK[4]])
                                kb.op('act', lambda e: e.copy(out=T['WT'][:].bitcast(F32R), in_=PS[4][:]), reads=[PK[4]], writes=[K('WT')])
                                pA = [PS[6], PS[7]]
                                ycur, ynxt = 'y0', 'y1'
                                for h in range(HB):
                                    self.mmr(pA[h // 2][:, (h % 2) * 256:(h % 2 + 1) * 256], W4[:, h, :], T[ycur][:, h, :], True, True,
                                             [K('W'), K(ycur)], [PK[6 + h // 2]])
                                for q in range(2):
                                    kb.op('dve', lambda e: e.tensor_tensor(out=T[ynxt][:, 2 * q:2 * q + 2, :].rearrange("p h c -> p (h c)").bitcast(F32R),
                                                                           in0=T[ycur][:, 2 * q:2 * q + 2, :].rearrange("p h c -> p (h c)"),
                                                                           in1=pA[q][:], op=ALU.subtract),
                                          reads=[K(ycur), PK[6 + q]], writes=[K(ynxt)])
                                ycur, ynxt = ynxt, ycur
                                cur, curT = 'W', 'WT'
                                for lvl in range(1, 6):
                                    na, nb_ = 'Wa%d' % (lvl % 2), 'Wb%d' % (lvl % 2)
                                    c4, cT4 = v4(T[cur]), v4(T[curT])
                                    for h in range(HB):
                                        self.mmr(PS[4][:, h * 128:(h + 1) * 128], cT4[:, h, :], c4[:, h, :], True, True, [K(cur), K(curT)], [PK[4]])
                                    if lvl < 5:
                                        for h in range(HB):
                                            self.mmr(PS[5][:, h * 128:(h + 1) * 128], c4[:, h, :], cT4[:, h, :], True, True, [K(cur), K(curT)], [PK[5]])
                                    kb.op('act', lambda e: e.copy(out=T[na][:].bitcast(F32R), in_=PS[4][:]), reads=[PK[4]], writes=[K(na)])
                                    if lvl < 5:
                                        kb.op('dve', lambda e: e.tensor_copy(out=T[nb_][:].bitcast(F32R), in_=PS[5][:]), reads=[PK[5]], writes=[K(nb_)])
                                    n4 = v4(T[na])
                                    for h in range(HB):
                                        self.mmr(pA[h // 2][:, (h % 2) * 256:(h % 2 + 1) * 256], n4[:, h, :], T[ycur][:, h, :], True, True,
                                                 [K(na), K(ycur)], [PK[6 + h // 2]])
                                    for q in range(2):
                                        kb.op('dve', lambda e: e.tensor_tensor(out=T[ynxt][:, 2 * q:2 * q + 2, :].rearrange("p h c -> p (h c)").bitcast(F32R),
                                                                               in0=T[ycur][:, 2 * q:2 * q + 2, :].rearrange("p h c -> p (h c)"),
                                                                               in1=pA[q][:], op=ALU.add),
                                              reads=[K(ycur), PK[6 + q]], writes=[K(ynxt)])
                                    ycur, ynxt = ynxt, ycur
                                    cur, curT = na, nb_
                                kb.op('dve', lambda e: e.tensor_tensor(out=T['UW'][:], in0=T[ycur][:], in1=be4.unsqueeze(2).to_broadcast([128, HB, 256]),
                                                                       op=ALU.mult), reads=[K(ycur), bgk], writes=[K('UW')])
                            UW = T['UW']
                            for h in range(HB):
                                self.tr(PS[3][:, h * 128:(h + 1) * 128], UW[:, h, 128:256], identf[:], [K('UW'), 'identf'], [PK[3]])
                            kb.op('act', lambda e: e.copy(out=T['wT'][:], in_=PS[3][:]), reads=[PK[3]], writes=[K('wT')])
                            wT4, qd4, QK4, kd4, vn4 = v4(T['wT']), v4(T['qd']), v4(T['QKm']), v4(T['kdec']), v4(T['vn'])
                            for c in ([0, 1] if d == 0 else [1, 0]):
                                rw = slice(64 * c, 64 * c + 64)
                                for h in range(HB):
                                    self.mm(PS[0][rw, h * 128:(h + 1) * 128], wT4[:, h, rw], St[:, h, :], True, True, [K('wT'), Sk], [PK[0]])
                                kb.op('dve', lambda e: e.tensor_tensor(out=vn4[rw], in0=UW[rw, :, 0:128],
                                                                       in1=PS[0][rw, :].rearrange("p (h c) -> p h c", h=HB), op=ALU.subtract),
                                      reads=[K('UW'), PK[0]], writes=[K('vn')])
                                for h in range(HB):
                                    self.mm(PS[1][rw, h * 128:(h + 1) * 128], qd4[:, h, rw], St[:, h, :], True, False, [K('qd'), Sk], [PK[1]])
                                    self.mm(PS[1][rw, h * 128:(h + 1) * 128], QK4[rw, h, rw], vn4[rw, h, :], False, True, [K('QKm'), K('vn')], [PK[1]])
                                kb.op('act', lambda e: e.copy(out=os_[rw, h0 * 128:(h0 + HB) * 128], in_=PS[1][rw, :]), reads=[PK[1]], writes=[osk])
                                for h in range(HB):
                                    self.mm(PS[2][:, h * 128:(h + 1) * 128], kd4[rw, h, :], vn4[rw, h, :], True, True, [K('kdec'), K('vn')], [PK[2]])
                                kb.op('dve', lambda e: e.tensor_tensor(out=St[:], in0=St[:], in1=T['alast'][:, :, c].unsqueeze(2).to_broadcast([128, HB, 128]),
                                                                       op=ALU.mult), reads=[Sk, K('alast')], writes=[Sk])
                                kb.op('dve', lambda e: e.tensor_tensor(out=St[:].rearrange("p h c -> p (h c)"), in0=St[:].rearrange("p h c -> p (h c)"),
                                                                       in1=PS[2][:], op=ALU.add), reads=[Sk, PK[2]], writes=[Sk])
                        kb.dma('pool', self.g_odir[d, tok0:tok0 + 128, :], os_[:], reads=[osk], writes=['g_odir_%d_%d' % (d, tok0)])

    def gdn_finish(self, li, jx, layer0, want_ctx):
        kb, NB = self.kb, self.NB
        with self.phase():
            o = self.outproj_setup(self.gdn_w_out[jx], li)
            NG = self.sb("gng", [128, 128])
            kb.dma('sp', NG[:], self.gdn_norm_g[jx:jx + 1, :].to_broadcast([128, 128]), writes=['gng'])
            self.rec_finish(o, NG, 'gng', self.g_odir, self.g_sgate, layer0, want_ctx)

    def rec_finish(self, o, NG, ngk, odir, sgate, layer0, want_ctx):
        kb, NB = self.kb, self.NB
        oa = [self.sb("foa%d" % i, [128, D]) for i in range(2)]
        obt = [self.sb("fob%d" % i, [128, D]) for i in range(2)]
        sg = [self.sb("fsg%d" % i, [128, D]) for i in range(2)]
        sq = self.sb("fsq", [128, D])
        st = self.sb("fst", [128, 4, 8])
        ob = [self.sb("fobb%d" % i, [128, D], BF16) for i in range(2)]
        it = 0
        pend = []
        for b in range(NB):
            for j in range(NT):
                if j < NTC and not want_ctx:
                    continue
                i = it % 2
                it += 1
                tok0 = b * TOK + j * 128
                kb.dma('sp', oa[i][:], odir[0, tok0:tok0 + 128, :], writes=['foa%d' % i])
                kb.dma('sp', obt[i][:], odir[1, tok0:tok0 + 128, :], writes=['fob%d' % i])
                kb.dma('sp', sg[i][:], sgate[tok0:tok0 + 128, :], writes=['fsg%d' % i])
                kb.op('dve', lambda e: e.tensor_tensor(out=oa[i][:], in0=oa[i][:], in1=obt[i][:], op=ALU.add),
                      reads=['foa%d' % i, 'fob%d' % i], writes=['foa%d' % i])
                kb.op('pool', lambda e: e.tensor_tensor(out=sq[:], in0=oa[i][:], in1=oa[i][:], op=ALU.mult), reads=['foa%d' % i], writes=['fsq'])
                kb.op('dve', lambda e: e.tensor_reduce(out=st[:, 0, :], in_=sq[:].rearrange("p (h d) -> p h d", h=8), axis=AX.X, op=ALU.add),
                      reads=['fsq'], writes=['fst'])
                kb.op('dve', lambda e: e.tensor_scalar(out=st[:, 1, :], in0=st[:, 0, :], scalar1=1.0 / 128, scalar2=EPS, op0=ALU.mult, op1=ALU.add),
                      reads=['fst'], writes=['fst'])
                kb.op('act', lambda e: e.activation(out=st[:, 2, :], in_=st[:, 1, :], func=AF.Sqrt), reads=['fst'], writes=['fst'])
                kb.op('dve', lambda e: e.reciprocal(out=st[:, 3, :], in_=st[:, 2, :]), reads=['fst'], writes=['fst'])
                o3 = oa[i][:].rearrange("p (h d) -> p h d", h=8)
                kb.op('dve', lambda e: e.tensor_tensor(out=o3, in0=o3, in1=st[:, 3, :].unsqueeze(2).to_broadcast([128, 8, 128]), op=ALU.mult),
                      reads=['foa%d' % i, 'fst'], writes=['foa%d' % i])
                kb.op('pool', lambda e: e.tensor_tensor(out=o3, in0=o3, in1=NG[:].unsqueeze(1).to_broadcast([128, 8, 128]), op=ALU.mult),
                      reads=['foa%d' % i, ngk], writes=['foa%d' % i])
                kb.op('dve', lambda e: e.tensor_tensor(out=ob[i][:], in0=oa[i][:], in1=sg[i][:], op=ALU.mult),
                      reads=['foa%d' % i, 'fsg%d' % i], writes=['fobb%d' % i])
                while pend:
                    pend.pop(0)()
                pend.append((lambda i_, b_, j_: (lambda: self.outproj_tile(o, ob[i_], 'fobb%d' % i_, layer0, b_, j_)))(i, b, j))
        while pend:
            pend.pop(0)()

    def mlstm_decl(self):
        self.gdn_decl()
        if hasattr(self, 'm_qkT'):
            return
        NB = self.NB
        self.m_qkT = self.dram("m_qkT", [2, 512, NB * TOK], F32)
        self.m_ktok = self.dram("m_ktok", [NB * TOK, 512], F32)

    def mlstm_phase(self, li, jx, layer0, want_ctx):
        self.mlstm_decl()
        self.mlstm_proj(li, jx)
        self.mlstm_scan(li, jx)
        kb = self.kb
        with self.phase():
            o = self.outproj_setup(self.mlstm_w_out[jx], li)
            NG = self.sb("mng", [128, 128])
            kb.dma('sp', NG[:], self.mlstm_norm_g[jx:jx + 1, :].to_broadcast([128, 128]), writes=['mng'])
            self.rec_finish(o, NG, 'mng', self.g_odir, self.g_sgate, layer0, want_ctx)

    def mlstm_proj(self, li, jx):
        kb, NB = self.kb, self.NB
        hv = self.hT_view()
        with self.phase():
            win = self.sb("mwin", [128, 8, 3104], BF16)
            wsrc = self.mlstm_w_in[jx].rearrange("(k p) c -> p k c", p=128)
            for k in range(8):
                kb.dma('pool', win[:, k, :], wsrc[:, k, :], writes=['mwin'])
            identf = self.sb("identf", [128, 128])
            kb.dma('sp', identf[:], self.c_ident, writes=['identf'])
            GB = self.sb("mgb", [128, 32])
            kb.dma('sp', GB[:], self.mlstm_gate_b[jx:jx + 1, :].to_broadcast([128, 32]), writes=['mgb'])
            h2 = [self.sb("mh%d" % i, [128, 8, 256], BF16) for i in range(2)]
            pz = [self.ps("mpz%d" % i, [128, 512]) for i in range(2)]
            pT = self.ps("mpT", [128, 2, 128])
            pg = [self.ps("mpg%d" % i, [128, 2, 512]) for i in range(2)]
            pb = self.ps("mpb", [128, 32])
            stg = [self.sb("mstg%d" % i, [128, 256]) for i in range(2)]
            kst = self.sb("mkst", [128, 2, 512])
            gst = [self.sb("mgst%d" % i, [128, D]) for i in range(4)]
            gls = self.sb("mgls", [128, 32])
            bt = self.sb("mbt", [128, 4, 16])
            ci = 0
            wi = 0
            gi = 0
            for b in range(NB):
                for w in range(NT // 2):
                    h = h2[wi % 2]
                    hk = 'mh%d' % (wi % 2)
                    wi += 1
                    c0 = self.hcol(b, 2 * w)
                    tok0 = b * TOK + 2 * w * 128
                    kb.dma('sp', h[:], hv[:, :, c0:c0 + 256], writes=[hk])
                    for f in range(8):
                        a = ci % 2
                        ci += 1
                        pzk = 'mpz%d' % a
                        for k in range(8):
                            self.mm(pz[a][:, 0:256], win[:, k, f * 128:(f + 1) * 128], h[:, k, :], k == 0, k == 7, ['mwin', hk], [pzk])
                        gk = 'mstg%d' % a
                        kb.op('act', lambda e: e.activation(out=stg[a][:], in_=pz[a][:, 0:256], func=AF.Identity, scale=(0.125 if f < 4 else 1.0)),
                              reads=[pzk], writes=[gk])
                        kb.dma('pool', self.m_qkT[f // 4, (f % 4) * 128:(f % 4 + 1) * 128, tok0:tok0 + 256], stg[a][:], reads=[gk],
                               writes=['m_qkT_%d' % ci])
                        if f >= 4:
                            for t2 in range(2):
                                self.tr(pT[:, t2, :], stg[a][:, t2 * 128:(t2 + 1) * 128], identf[:], [gk, 'identf'], ['mpT'])
                            kb.op('dve', lambda e: e.tensor_copy(out=kst[:, :, (f - 4) * 128:(f - 3) * 128], in_=pT[:]), reads=['mpT'], writes=['mkst'])
                    for t2 in range(2):
                        r0 = tok0 + t2 * 128
                        kb.dma('pool', self.m_ktok[r0:r0 + 128, :], kst[:, t2, :], reads=['mkst'], writes=['m_ktok_%d' % r0])
                        hs = h[:, :, t2 * 128:(t2 + 1) * 128]
                        for part in range(2):
                            p_ = pg[part]
                            pk = 'mpg%d' % part
                            for n in range(2):
                                for k in range(8):
                                    c1 = 1024 + part * 1024 + n * 512
                                    self.mm(p_[:, n, :], hs[:, k, :], win[:, k, c1:c1 + 512], k == 0, k == 7, [hk, 'mwin'], [pk])
                            g_ = gst[gi % 4]
                            gk2 = 'mgst%d' % (gi % 4)
                            gi += 1
                            if part == 0:
                                kb.op('dve', lambda e: e.tensor_copy(out=g_[:], in_=p_[:].rearrange("p a b -> p (a b)")), reads=[pk], writes=[gk2])
                                kb.dma('pool', self.g_vtok[r0:r0 + 128, :], g_[:], reads=[gk2], writes=['g_vtok_%d' % r0])
                            else:
                                kb.op('act', lambda e: e.activation(out=g_[:], in_=p_[:].rearrange("p a b -> p (a b)"), func=AF.Sigmoid),
                                      reads=[pk], writes=[gk2])
                                kb.dma('pool', self.g_sgate[r0:r0 + 128, :], g_[:], reads=[gk2], writes=['g_sgate_%d' % r0])
                        for k in range(8):
                            self.mm(pb[:], hs[:, k, :], win[:, k, 3072:3104], k == 0, k == 7, [hk, 'mwin'], ['mpb'])
                        x4 = bt[:, 0:2, :].rearrange("p a c -> p (a c)")
                        kb.op('dve', lambda e: e.tensor_tensor(out=x4, in0=pb[:], in1=GB[:], op=ALU.add), reads=['mpb', 'mgb'], writes=['mbt'])
                        x5 = x4.rearrange("p (d t h) -> p d t h", d=2, t=2)
                        kb.op('pool', lambda e: e.tensor_copy(out=gls[:, 0:16].rearrange("p (d h) -> p d h", d=2), in_=x5[:, :, 0, :]),
                              reads=['mbt'], writes=['mgls'])
                        xf = x5[:, :, 1, :]
                        t3 = lambda i_: bt[:, i_, :].rearrange("p (d h) -> p d h", d=2)
                        kb.op('dve', lambda e: e.scalar_tensor_tensor(out=t3(2), in0=xf, scalar=-1.0, in1=xf, op0=ALU.mult, op1=ALU.max),
                              reads=['mbt'], writes=['mbt'])
                        kb.op('act', lambda e: e.activation(out=bt[:, 2, :], in_=bt[:, 2, :], func=AF.Exp, scale=-1.0), reads=['mbt'], writes=['mbt'])
                        kb.op('act', lambda e: e.activation(out=bt[:, 3, :], in_=bt[:, 2, :], func=AF.Ln, bias=1.0), reads=['mbt'], writes=['mbt'])
                        kb.op('dve', lambda e: e.scalar_tensor_tensor(out=gls[:, 16:32].rearrange("p (d h) -> p d h", d=2), in0=xf, scalar=0.0,
                                                                      in1=t3(3), op0=ALU.min, op1=ALU.subtract), reads=['mbt'], writes=['mgls'])
                        kb.dma('pool', self.g_bg[r0:r0 + 128, :], gls[:], reads=['mgls'], writes=['g_bg_%d' % r0])

    def mlstm_scan(self, li, jx):
        kb, NB = self.kb, self.NB
        HB = 4
        qkTv = self.m_qkT.rearrange("q (h p) c -> q p h c", p=64)
        order = [list(range(NT)), [1, 0] + list(range(NT - 1, NTC - 1, -1))]
        with self.phase():
            MS = self.sb("mmask", [128, 13, 128])
            for m0 in range(0, 13, 4):
                m1 = min(13, m0 + 4)
                kb.dma('sp', MS[:, m0:m1, :], self.c_masks[m0:m1].rearrange("m p c -> p m c"), writes=['mmask'])
            identf = self.sb("identf", [128, 128])
            kb.dma('sp', identf[:], self.c_ident, writes=['identf'])
            onesf = self.sb("onesf", [128, 128])
            kb.op('dve', lambda e: e.memset(onesf[:], 1.0), writes=['onesf'])
            PS = [self.ps("mps%d" % i, [128, 512]) for i in range(8)]
            PK = ['mps%d' % i for i in range(8)]

            def v4(t):
                return t[:].rearrange("p (h c) -> p h c", h=HB)
            sets = []
            for si in range(2):
                T = {}
                for nm in ('LF', 'Dig', 'DL', 'P', 'PT'):
                    T[nm] = self.sb("m%s%d" % (nm, si), [128, HB * 128])
                T['qT'] = self.sb("mqT%d" % si, [64, HB, 128])
                T['kT'] = self.sb("mkT%d" % si, [64, HB, 128])
                T['ktok'] = self.sb("mktok%d" % si, [128, HB, 64])
                T['kw'] = self.sb("mkw%d" % si, [128, HB, 64])
                T['v1'] = self.sb("mv1%d" % si, [128, HB, 132])
                T['NI'] = self.sb("mNI%d" % si, [128, HB, 132])
                T['t1'] = self.sb("mt1%d" % si, [128, HB, 132])
                T['t2'] = self.sb("mt2%d" % si, [128, HB, 132])
                T['c'] = self.sb("mc%d" % si, [128, 16, HB])
                T['mloc'] = self.sb("mmloc%d" % si, [128, HB, 2])
                T['blast'] = self.sb("mblast%d" % si, [128, 2, HB])
                T['e3'] = self.sb("me3%d" % si, [128, 3, HB])
                T['fi'] = self.sb("mfi%d" % si, [128, 2, HB])
                T['k'] = 'm%d_' % si
                kb.op('pool', lambda e: e.memset(T['v1'][:, :, 128:129], 1.0), writes=[T['k'] + 'v1'])
                sets.append(T)
            glt = [self.sb("mgl%d" % i, [128, 32]) for i in range(4)]
            ost = [self.sb("most%d" % i, [128, D]) for i in range(4)]
            Cn = [[self.sb("mCn_%d_%d" % (d, hf), [64, HB, 132]) for hf in range(2)] for d in range(2)]
            Mm = [[self.sb("mM_%d_%d" % (d, hf), [128, HB]) for hf in range(2)] for d in range(2)]
            stepi = 0
            bi = 0
            for b in range(NB):
                for d in range(2):
                    for hf in range(2):
                        kb.op('pool', lambda e: e.memset(Cn[d][hf][:], 0.0), writes=['mCn_%d_%d' % (d, hf)])
                        kb.op('pool', lambda e: e.memset(Mm[d][hf][:], 0.0), writes=['mM_%d_%d' % (d, hf)])
                for n in range(NT):
                    for d in range(2):
                        j = order[d][n]
                        tok0 = b * TOK + j * 128
                        gl = glt[bi % 4]
                        glk = 'mgl%d' % (bi % 4)
                        os_ = ost[bi % 4]
                        osk = 'most%d' % (bi % 4)
                        bi += 1
                        kb.dma('sp', gl[:], self.g_bg[tok0:tok0 + 128, :], writes=[glk])
                        Ud, nUd, BO, BmU, NEG = MS[:, d, :], MS[:, 4 + d, :], MS[:, 6, :], MS[:, 9 + d, :], MS[:, 11 + d, :]
                        for hf in range(2):
                            T = sets[stepi % 2]
                            K = lambda nm: T['k'] + nm
                            stepi += 1
                            h0 = hf * HB
                            Ck, Mk = 'mCn_%d_%d' % (d, hf), 'mM_%d_%d' % (d, hf)
                            Ct, Mt = Cn[d][hf], Mm[d][hf]
                            c = T['c']
                            kb.dma('sp', T['qT'][:], qkTv[0, :, h0:h0 + HB, tok0:tok0 + 128], writes=[K('qT')])
                            kb.dma('sp', T['kT'][:], qkTv[1, :, h0:h0 + HB, tok0:tok0 + 128], writes=[K('kT')])
                            kb.dma('sp', T['ktok'][:], self.m_ktok[tok0:tok0 + 128, h0 * 64:(h0 + HB) * 64].rearrange("p (h c) -> p h c", h=HB),
                                   writes=[K('ktok')])
                            kb.dma('sp', T['v1'][:, :, 0:128], self.g_vtok[tok0:tok0 + 128, h0 * 128:(h0 + HB) * 128].rearrange("p (h c) -> p h c", h=HB),
                                   writes=[K('v1')])
                            ig4 = gl[:, d * 8 + h0:d * 8 + h0 + HB]
                            lf4 = gl[:, 16 + d * 8 + h0:16 + d * 8 + h0 + HB]
                            bc4 = lambda ap: ap.unsqueeze(2).to_broadcast([128, HB, 128])
                            mk4 = lambda ap: ap.unsqueeze(1).to_broadcast([128, HB, 128])
                            kb.op('dve', lambda e: e.tensor_copy(out=v4(T['LF']), in_=bc4(lf4)), reads=[glk], writes=[K('LF')])
                            kb.op('pool', lambda e: e.tensor_tensor(out=v4(T['Dig']), in0=mk4(identf[:]), in1=bc4(ig4), op=ALU.mult),
                                  reads=[glk, 'identf'], writes=[K('Dig')])
                            LF, Dig = v4(T['LF']), v4(T['Dig'])
                            for h in range(HB):
                                o_ = PS[0][:, h * 128:(h + 1) * 128]
                                self.mm(o_, Ud, LF[:, h, :], True, False, ['mmask', K('LF')], [PK[0]])
                                self.mm(o_, LF[:, h, :], nUd, False, False, ['mmask', K('LF')], [PK[0]])
                                self.mm(o_, onesf[:], Dig[:, h, :], False, True, ['onesf', K('Dig')], [PK[0]])
                            for h in range(HB):
                                o_ = PS[1][:, h * 128:(h + 1) * 128]
                                self.mm(o_, LF[:, h, :], BmU, True, False, ['mmask', K('LF')], [PK[1]])
                                self.mm(o_, onesf[:], Dig[:, h, :], False, True, ['onesf', K('Dig')], [PK[1]])
                            self.mm(PS[4][:, 0:HB], Ud, lf4, True, True, ['mmask', glk], [PK[4]])
                            self.mm(PS[4][:, HB:2 * HB], BO, lf4, True, True, ['mmask', glk], [PK[4]])
                            for cc in range(2):
                                self.mm(PS[4][:, 8 + cc * HB:8 + (cc + 1) * HB], MS[:, 7 + cc, :], lf4, True, True, ['mmask', glk], [PK[4]])
                            for h in range(HB):
                                self.mm(PS[2][:, h * 128:(h + 1) * 128], T['qT'][:, h, :], T['kT'][:, h, :], True, True, [K('qT'), K('kT')], [PK[2]])
                            kb.op('act', lambda e: e.copy(out=c[:, 0:2, :].rearrange("p a h -> p (a h)"), in_=PS[4][:, 0:2 * HB]), reads=[PK[4]], writes=[K('c')])
                            kb.op('act', lambda e: e.copy(out=T['blast'][:].rearrange("p a h -> p (a h)"), in_=PS[4][:, 8:8 + 2 * HB]),
                                  reads=[PK[4]], writes=[K('blast')])
                            kb.op('dve', lambda e: e.tensor_tensor(out=v4(T['DL']), in0=PS[0][:].rearrange("p (h c) -> p h c", h=HB), in1=mk4(NEG), op=ALU.add),
                                  reads=[PK[0], 'mmask'], writes=[K('DL')])
                            kb.op('dve', lambda e: e.tensor_reduce(out=c[:, 2, :], in_=v4(T['DL']), axis=AX.X, op=ALU.max), reads=[K('DL')], writes=[K('c')])
                            kb.op('dve', lambda e: e.tensor_tensor(out=v4(T['DL']), in0=v4(T['DL']), in1=bc4(c[:, 2, :]), op=ALU.subtract),
                                  reads=[K('DL'), K('c')], writes=[K('DL')])
                            kb.op('act', lambda e: e.activation(out=T['P'][:], in_=T['DL'][:], func=AF.Exp), reads=[K('DL')], writes=[K('P')])
                            kb.op('dve', lambda e: e.tensor_tensor(out=T['P'][:], in0=T['P'][:], in1=PS[2][:], op=ALU.mult), reads=[K('P'), PK[2]], writes=[K('P')])
                            P4 = v4(T['P'])
                            for h in range(HB):
                                self.tr(PS[3][:, h * 128:(h + 1) * 128], P4[:, h, :], identf[:], [K('P'), 'identf'], [PK[3]])
                            kb.op('act', lambda e: e.copy(out=T['PT'][:], in_=PS[3][:]), reads=[PK[3]], writes=[K('PT')])
                            PT4 = v4(T['PT'])
                            for h in range(HB):
                                self.mm(PS[6 + h // 2][:, (h % 2) * 129:(h % 2) * 129 + 129], PT4[:, h, :], T['v1'][:, h, 0:129], True, True,
                                        [K('PT'), K('v1')], [PK[6 + h // 2]])
                            for q in range(2):
                                kb.op('act' if q == 0 else 'dve',
                                      (lambda e: e.copy(out=T['NI'][:, 0:2, 0:129], in_=PS[6][:, 0:258].rearrange("p (h c) -> p h c", h=2))) if q == 0 else
                                      (lambda e: e.tensor_copy(out=T['NI'][:, 2:4, 0:129], in_=PS[7][:, 0:258].rearrange("p (h c) -> p h c", h=2))),
                                      reads=[PK[6 + q]], writes=[K('NI')])
                            kb.op('dve', lambda e: e.tensor_reduce(out=T['mloc'][:], in_=PS[1][:].rearrange("p (h c j) -> p h c j", h=HB, c=2), axis=AX.X, op=ALU.max),
                                  reads=[PK[1]], writes=[K('mloc')])
                            kb.op('dve', lambda e: e.tensor_tensor(out=c[:, 3, :], in0=c[:, 1, :], in1=c[:, 0, :], op=ALU.subtract), reads=[K('c')], writes=[K('c')])
                            kb.op('dve', lambda e: e.tensor_tensor(out=c[:, 3, :], in0=c[:, 3, :], in1=ig4, op=ALU.add), reads=[K('c'), glk], writes=[K('c')])
                            for cc in range(2):
                                rw = slice(64 * cc, 64 * cc + 64)
                                kb.op('dve', lambda e: e.tensor_tensor(out=c[rw, 4, :], in0=c[rw, 3, :], in1=T['mloc'][rw, :, cc], op=ALU.subtract),
                                      reads=[K('c'), K('mloc')], writes=[K('c')])
                            kb.op('act', lambda e: e.activation(out=c[:, 5, :], in_=c[:, 4, :], func=AF.Exp), reads=[K('c')], writes=[K('c')])
                            kb.op('pool', lambda e: e.tensor_tensor(out=T['kw'][:], in0=T['ktok'][:], in1=c[:, 5, :].unsqueeze(2).to_broadcast([128, HB, 64]), op=ALU.mult),
                                  reads=[K('ktok'), K('c')], writes=[K('kw')])
                            for cc in ([0, 1] if d == 0 else [1, 0]):
                                rw = slice(64 * cc, 64 * cc + 64)
                                for h in range(HB):
                                    self.mm(PS[h // 2][rw, (h % 2) * 129:(h % 2) * 129 + 129], T['qT'][:, h, rw], Ct[:, h, 0:129], True, True,
                                            [K('qT'), Ck], [PK[h // 2]])
                                kb.op('dve', lambda e: e.tensor_tensor(out=c[rw, 6, :], in0=c[rw, 0, :], in1=Mt[rw, :], op=ALU.add), reads=[K('c'), Mk], writes=[K('c')])
                                kb.op('dve', lambda e: e.tensor_tensor(out=c[rw, 7, :], in0=c[rw, 2, :], in1=c[rw, 6, :], op=ALU.max), reads=[K('c')], writes=[K('c')])
                                e3 = T['e3']
                                kb.op('dve', lambda e: e.tensor_tensor(out=e3[rw, 0, :], in0=c[rw, 6, :], in1=c[rw, 7, :], op=ALU.subtract), reads=[K('c')], writes=[K('e3')])
                                kb.op('dve', lambda e: e.tensor_tensor(out=e3[rw, 1, :], in0=c[rw, 2, :], in1=c[rw, 7, :], op=ALU.subtract), reads=[K('c')], writes=[K('e3')])
                                kb.op('dve', lambda e: e.tensor_scalar(out=e3[rw, 2, :], in0=c[rw, 7, :], scalar1=-1.0, scalar2=None, op0=ALU.mult), reads=[K('c')], writes=[K('e3')])
                                kb.op('act', lambda e: e.activation(out=e3[rw, :, :], in_=e3[rw, :, :], func=AF.Exp), reads=[K('e3')], writes=[K('e3')])
                                for q in range(2):
                                    kb.op('dve', lambda e: e.tensor_tensor(out=T['t1'][rw, 2 * q:2 * q + 2, 0:129], in0=PS[q][rw, 0:258].rearrange("p (h c) -> p h c", h=2),
                                                                           in1=e3[rw, 0, 2 * q:2 * q + 2].unsqueeze(2).to_broadcast([64, 2, 129]), op=ALU.mult),
                                          reads=[PK[q], K('e3')], writes=[K('t1')])
                                kb.op('pool', lambda e: e.tensor_tensor(out=T['t2'][rw, :, 0:129], in0=T['NI'][rw, :, 0:129],
                                                                        in1=e3[rw, 1, :].unsqueeze(2).to_broadcast([64, HB, 129]), op=ALU.mult),
                                      reads=[K('NI'), K('e3')], writes=[K('t2')])
                                kb.op('dve', lambda e: e.tensor_tensor(out=T['t1'][rw, :, 0:129], in0=T['t1'][rw, :, 0:129], in1=T['t2'][rw, :, 0:129], op=ALU.add),
                                      reads=[K('t1'), K('t2')], writes=[K('t1')])
                                den = T['t1'][rw, :, 128]
                                kb.op('dve', lambda e: e.scalar_tensor_tensor(out=c[rw, 8, :], in0=den, scalar=-1.0, in1=den, op0=ALU.mult, op1=ALU.max),
                                      reads=[K('t1')], writes=[K('c')])
                                kb.op('dve', lambda e: e.tensor_tensor(out=c[rw, 9, :], in0=c[rw, 8, :], in1=e3[rw, 2, :], op=ALU.max), reads=[K('c'), K('e3')], writes=[K('c')])
                                kb.op('dve', lambda e: e.reciprocal(out=c[rw, 10, :], in_=c[rw, 9, :]), reads=[K('c')], writes=[K('c')])
                                kb.op('dve', lambda e: e.tensor_tensor(out=os_[rw, h0 * 128:(h0 + HB) * 128].rearrange("p (h c) -> p h c", h=HB), in0=T['t1'][rw, :, 0:128],
                                                                       in1=c[rw, 10, :].unsqueeze(2).to_broadcast([64, HB, 128]), op=ALU.mult),
                                      reads=[K('t1'), K('c')], writes=[osk])
                                for h in range(HB):
                                    self.mm(PS[2 + h // 2][0:64, (h % 2) * 129:(h % 2) * 129 + 129], T['kw'][rw, h, :], T['v1'][rw, h, 0:129], True, True,
                                            [K('kw'), K('v1')], [PK[2 + h // 2]])
                                fi = T['fi']
                                kb.op('dve', lambda e: e.tensor_tensor(out=c[:, 11, :], in0=T['blast'][:, cc, :], in1=Mt[:], op=ALU.add), reads=[K('blast'), Mk], writes=[K('c')])
                                kb.op('dve', lambda e: e.tensor_tensor(out=c[:, 12, :], in0=c[:, 11, :], in1=T['mloc'][:, :, cc], op=ALU.max), reads=[K('c'), K('mloc')], writes=[K('c')])
                                kb.op('dve', lambda e: e.tensor_tensor(out=fi[:, 0, :], in0=c[:, 11, :], in1=c[:, 12, :], op=ALU.subtract), reads=[K('c')], writes=[K('fi')])
                                kb.op('dve', lambda e: e.tensor_tensor(out=fi[:, 1, :], in0=T['mloc'][:, :, cc], in1=c[:, 12, :], op=ALU.subtract), reads=[K('c'), K('mloc')], writes=[K('fi')])
                                kb.op('act', lambda e: e.activation(out=fi[:], in_=fi[:], func=AF.Exp), reads=[K('fi')], writes=[K('fi')])
                                kb.op('dve', lambda e: e.tensor_copy(out=Mt[:], in_=c[:, 12, :]), reads=[K('c')], writes=[Mk])
                                kb.op('dve', lambda e: e.tensor_tensor(out=Ct[:, :, 0:129], in0=Ct[:, :, 0:129], in1=fi[0:64, 0, :].unsqueeze(2).to_broadcast([64, HB, 129]), op=ALU.mult),
                                      reads=[Ck, K('fi')], writes=[Ck])
                                for q in range(2):
                                    kb.op('dve', lambda e: e.tensor_tensor(out=T['t2'][0:64, 2 * q:2 * q + 2, 0:129], in0=PS[2 + q][0:64, 0:258].rearrange("p (h c) -> p h c", h=2),
                                                                           in1=fi[0:64, 1, 2 * q:2 * q + 2].unsqueeze(2).to_broadcast([64, 2, 129]), op=ALU.mult),
                                          reads=[PK[2 + q], K('fi'), K('t2')], writes=[K('t2')])
                                kb.op('dve', lambda e: e.tensor_tensor(out=Ct[:, :, 0:129], in0=Ct[:, :, 0:129], in1=T['t2'][0:64, :, 0:129], op=ALU.add),
                                      reads=[Ck, K('t2')], writes=[Ck])
                        kb.dma('pool', self.g_odir[d, tok0:tok0 + 128, :], os_[:], reads=[osk], writes=['g_odir_%d_%d' % (d, tok0)])

    def build(self):
        self.declare()
        self.setup()
        cnt = {0: 0, 1: 0, 2: 0}
        for li, kind in enumerate(self.kinds):
            last = self.last_flags[li]
            layer0 = (li == 0)
            jx = cnt[kind]
            cnt[kind] += 1
            self.mod_phase(li)
            self.norm_phase(li, 1, layer0)
            if kind == 2:
                self.attn_phase(li, jx, layer0, not last)
            elif kind == 0:
                self.gdn_phase(li, jx, layer0, not last)
            else:
                self.mlstm_phase(li, jx, layer0, not last)
            self.norm_phase(li, 2, False, ctx_needed=not last)
            self.ffn_phase(li, ctx_needed=not last)
        self.final_phase()
        self.kb.barrier()
        self.kb.close()


def host_consts():
    ident = np.eye(128, dtype=np.float32)
    n_pair = 32
    inv = (10000.0 ** (-np.arange(n_pair, dtype=np.float32) / n_pair)).astype(np.float32)
    pos = np.arange(LAT)
    row = (pos // 64).astype(np.float32)
    col = (pos % 64).astype(np.float32)
    ang = np.concatenate([row[:, None] * inv, col[:, None] * inv], axis=-1).astype(np.float32)
    rope = np.concatenate([np.cos(ang), np.sin(ang)], axis=-1).astype(np.float32)
    masks = np.zeros((16, 128, 128), np.float32)
    t = np.arange(128)
    same = (t[:, None] // 64) == (t[None, :] // 64)
    uf = (same & (t[:, None] <= t[None, :])).astype(np.float32)
    ub = (same & (t[:, None] >= t[None, :])).astype(np.float32)
    masks[0], masks[1] = uf, ub
    masks[2], masks[3] = uf - np.eye(128, dtype=np.float32), ub - np.eye(128, dtype=np.float32)
    masks[4], masks[5] = -uf, -ub
    masks[6] = same.astype(np.float32)
    masks[7] = np.repeat((t < 64).astype(np.float32)[:, None], 128, axis=1)
    masks[8] = np.repeat((t >= 64).astype(np.float32)[:, None], 128, axis=1)
    masks[9], masks[10] = masks[6] - uf, masks[6] - ub
    masks[11] = (1.0 - ub) * np.float32(-1e30)
    masks[12] = (1.0 - uf) * np.float32(-1e30)
    return {"c_ident": ident, "c_rope": rope, "c_masks": masks}


def make_in_maps(inputs, NB, n_cores, kinds):
    f = lambda a: np.ascontiguousarray(np.asarray(a, dtype=np.float32))
    consts = host_consts()
    shared = {}
    for k in ("norm1_g", "norm2_g", "w_mod", "b_mod", "ffn_w_in", "ffn_conv_w", "ffn_conv_b", "ffn_w_out",
              "gdn_w_in", "gdn_conv_w", "gdn_norm_g", "gdn_w_out", "mlstm_w_in", "mlstm_norm_g", "mlstm_w_out",
              "attn_w_in", "attn_q_norm_g", "attn_k_norm_g", "attn_w_out"):
        shared[k] = f(inputs[k])
    shared["gdn_a_log"] = f(inputs["gdn_a_log"]).reshape(-1, 16)
    shared["gdn_dt_bias"] = f(inputs["gdn_dt_bias"]).reshape(-1, 16)
    shared["mlstm_gate_b"] = f(inputs["mlstm_gate_b"]).reshape(-1, 32)
    shared["final_norm_g"] = f(inputs["final_norm_g"]).reshape(1, D)
    shared.update(consts)
    x, c, ctx, c_ctx = f(inputs["x"]), f(inputs["c"]), f(inputs["ctx"]), f(inputs["c_ctx"])
    maps = []
    for i in range(n_cores):
        m = dict(shared)
        m["x"] = x[i * NB:(i + 1) * NB]
        m["ctx"] = ctx[i * NB:(i + 1) * NB]
        m["cvec"] = np.ascontiguousarray(np.concatenate([c[i * NB:(i + 1) * NB], c_ctx[None, :]], axis=0))
        maps.append(m)
    return maps


def kernel(**inputs):
    NB = 2
    n_cores = 8
    nc = bass.Bass("TRN2", target_bir_lowering=False)
    mk = MK(nc, NB, KINDS, [False, False, False, True])
    mk.build()
    maps = make_in_maps(inputs, NB, n_cores, KINDS)
    res = run_bass_kernel_spmd(nc, maps, core_ids=list(range(n_cores)))
    return np.concatenate([r["out"] for r in res.results], axis=0).astype(np.float32)
```

```python
import contextlib
import math
import numpy as np
import concourse.bass as bass
import concourse.mybir as mybir
from concourse.bass_utils import run_bass_kernel_spmd

F32 = mybir.dt.float32
F32R = mybir.dt.float32r
BF16 = mybir.dt.bfloat16
AF = mybir.ActivationFunctionType
ALU = mybir.AluOpType
AX = mybir.AxisListType

D = 1024
LAT = 4096
CTXL = 256
NTC = 2
NTL = 32
NT = 34
TOK = NT * 128
FFN = 2816
EPS = 1e-6
HALO = 2
REG_C = CTXL + 2 * HALO
REG_L = LAT + 2 * HALO
REG = REG_C + REG_L
KINDS = [0, 1, 2, 0]


class KB:
    def __init__(self, nc, ring=6, same_engine_sync=True):
        self.nc = nc
        self.eng = {'pe': nc.tensor, 'dve': nc.vector, 'act': nc.scalar,
                    'pool': nc.gpsimd, 'sp': nc.sync}
        self.same_engine_sync = same_engine_sync
        self.sem = {}
        self.cnt = {}
        self.seen = {e: {} for e in self.eng}
        self.res = {}
        self._ctx = []
        for e in ('pe', 'dve', 'act', 'pool'):
            self._mksem('c_' + e)
        self.rings = {}
        for q in ('sp', 'act', 'pool'):
            names = []
            for i in range(ring):
                n = 'd_%s_%d' % (q, i)
                self._mksem(n)
                names.append(n)
            self.rings[q] = [names, 0]
        self.n_inst = 0
        self.n_wait = 0

    def _mksem(self, name):
        cm = self.nc.semaphore(name)
        h = cm.__enter__()
        self._ctx.append(cm)
        self.sem[name] = h
        self.cnt[name] = 0

    def close(self):
        for cm in reversed(self._ctx):
            cm.__exit__(None, None, None)
        self._ctx = []

    def _R(self, key):
        r = self.res.get(key)
        if r is None:
            r = {'w': None, 'r': {}}
            self.res[key] = r
        return r

    def _deps(self, reads, writes):
        deps = {}

        def add(tok):
            if tok is None:
                return
            s, v = tok
            if deps.get(s, 0) < v:
                deps[s] = v
        for k in reads:
            add(self._R(k)['w'])
        for k in writes:
            r = self._R(k)
            add(r['w'])
            for s, v in r['r'].items():
                add((s, v))
        return deps

    def _wait(self, e, deps, is_dma=False):
        own = 'c_' + e
        seen = self.seen[e]
        for s, v in deps.items():
            if s == own and not is_dma and (e == 'pe' or not self.same_engine_sync):
                continue
            if seen.get(s, 0) >= v:
                continue
            self.eng[e].wait_ge(self.sem[s], v)
            self.n_wait += 1
            seen[s] = v

    def _commit(self, tok, reads, writes):
        s, v = tok
        for k in reads:
            r = self._R(k)
            if r['r'].get(s, 0) < v:
                r['r'][s] = v
        for k in writes:
            r = self._R(k)
            r['w'] = tok
            r['r'] = {}

    def op(self, e, fn, reads=(), writes=()):
        deps = self._deps(reads, writes)
        self._wait(e, deps)
        inst = fn(self.eng[e])
        s = 'c_' + e
        self.cnt[s] += 1
        inst.then_inc(self.sem[s], 1)
        self.n_inst += 1
        self._commit((s, self.cnt[s]), reads, writes)
        return inst

    def dma(self, q, out, in_, reads=(), writes=(), **kw):
        names, idx = self.rings[q]
        s = names[idx % len(names)]
        self.rings[q][1] = idx + 1
        deps = self._deps(reads, writes)
        if self.cnt[s] > 0 and deps.get(s, 0) < self.cnt[s]:
            deps[s] = self.cnt[s]
        self._wait(q, deps, is_dma=True)
        inst = self.eng[q].dma_start(out=out, in_=in_, **kw)
        self.cnt[s] += 16
        inst.then_inc(self.sem[s], 16)
        self.n_inst += 1
        self._commit((s, self.cnt[s]), reads, writes)
        return inst

    def barrier(self):
        for e in self.eng:
            for s, v in self.cnt.items():
                if v > 0 and self.seen[e].get(s, 0) < v:
                    self.eng[e].wait_ge(self.sem[s], v)
                    self.seen[e][s] = v
        self.res = {}


class MK:
    def __init__(self, nc, NB, kinds, last_flags, debug=False):
        self.debug = debug
        self.nc = nc
        self.NB = NB
        self.kinds = kinds
        self.last_flags = last_flags
        self.kb = KB(nc)
        self.stack = None
        self.uid = 0

    @contextlib.contextmanager
    def phase(self):
        prev = self.stack
        with contextlib.ExitStack() as es:
            self.stack = es
            yield
            self.kb.barrier()
        self.stack = prev

    @contextlib.contextmanager
    def nosame(self):
        yield

    def sb(self, name, shape, dt=F32):
        self.uid += 1
        return self.stack.enter_context(self.nc.sbuf_tensor("%s_%d" % (name, self.uid), list(shape), dt))

    def ps(self, name, shape, dt=F32):
        self.uid += 1
        return self.stack.enter_context(self.nc.psum_tensor("%s_%d" % (name, self.uid), list(shape), dt))

    def dram(self, name, shape, dt, kind="Internal"):
        return self.nc.dram_tensor(name, list(shape), dt, kind=kind).ap()

    def mm(self, out, lhsT, rhs, start, stop, reads, writes):
        return self.kb.op('pe', lambda e: e.matmul(out, lhsT=lhsT, rhs=rhs, start=start, stop=stop),
                          reads=reads, writes=writes)

    def mmr(self, out, lhsT, rhs, start, stop, reads, writes):
        F32R = mybir.dt.float32r
        return self.kb.op('pe', lambda e: e.matmul(out, lhsT=lhsT.bitcast(F32R), rhs=rhs.bitcast(F32R), start=start, stop=stop),
                          reads=reads, writes=writes)

    def tr(self, out, in_, ident, reads, writes):
        return self.kb.op('pe', lambda e: e.transpose(out, in_, ident), reads=reads, writes=writes)

    def declare(self):
        NB = self.NB
        n0 = sum(1 for k in self.kinds if k == 0)
        n1 = sum(1 for k in self.kinds if k == 1)
        n2 = sum(1 for k in self.kinds if k == 2)
        DEPTH = len(self.kinds)
        I = lambda n, s: self.dram(n, s, F32, kind="ExternalInput")
        self.x = I("x", [NB, LAT, D])
        self.ctx = I("ctx", [NB, CTXL, D])
        self.cvec = I("cvec", [NB + 1, D])
        self.norm1_g = I("norm1_g", [DEPTH, D])
        self.norm2_g = I("norm2_g", [DEPTH, D])
        self.w_mod = I("w_mod", [DEPTH, D, 6 * D])
        self.b_mod = I("b_mod", [DEPTH, 6 * D])
        self.ffn_w_in = I("ffn_w_in", [DEPTH, D, 2 * FFN])
        self.ffn_conv_w = I("ffn_conv_w", [DEPTH, 3, FFN])
        self.ffn_conv_b = I("ffn_conv_b", [DEPTH, FFN])
        self.ffn_w_out = I("ffn_w_out", [DEPTH, FFN, D])
        self.gdn_w_in = I("gdn_w_in", [max(n0, 1), D, 4128])
        self.gdn_conv_w = I("gdn_conv_w", [max(n0, 1), 5, 3072])
        self.gdn_a_log = I("gdn_a_log", [max(n0, 1), 16])
        self.gdn_dt_bias = I("gdn_dt_bias", [max(n0, 1), 16])
        self.gdn_norm_g = I("gdn_norm_g", [max(n0, 1), 128])
        self.gdn_w_out = I("gdn_w_out", [max(n0, 1), D, D])
        self.mlstm_w_in = I("mlstm_w_in", [max(n1, 1), D, 3104])
        self.mlstm_gate_b = I("mlstm_gate_b", [max(n1, 1), 32])
        self.mlstm_norm_g = I("mlstm_norm_g", [max(n1, 1), 128])
        self.mlstm_w_out = I("mlstm_w_out", [max(n1, 1), D, D])
        self.attn_w_in = I("attn_w_in", [max(n2, 1), D, 1536])
        self.attn_q_norm_g = I("attn_q_norm_g", [max(n2, 1), 128])
        self.attn_k_norm_g = I("attn_k_norm_g", [max(n2, 1), 128])
        self.attn_w_out = I("attn_w_out", [max(n2, 1), D, D])
        self.final_norm_g = I("final_norm_g", [1, D])
        self.c_ident = I("c_ident", [128, 128])
        self.c_rope = I("c_rope", [LAT, 128])
        self.c_masks = I("c_masks", [16, 128, 128])
        self.out = self.dram("out", [NB, LAT, D], F32, kind="ExternalOutput")
        sk = "ExternalOutput" if self.debug else "Internal"
        self.xs = self.dram("xs", [NB, TOK, D], F32, kind=sk)
        self.hTs = self.dram("hTs", [D, NB * REG], BF16, kind=sk)
        self.modv = self.dram("modv", [DEPTH, NB + 1, 6, D], F32, kind=sk)

    def src_x(self, layer0, b, j):
        if layer0:
            if j < NTC:
                return self.ctx[b, j * 128:(j + 1) * 128, :], 'in_ctx'
            return self.x[b, (j - NTC) * 128:(j - NTC + 1) * 128, :], 'in_x'
        return self.xs[b, j * 128:(j + 1) * 128, :], 'xs_%d_%d' % (b, j)

    def hcol(self, b, j):
        base = b * REG
        if j < NTC:
            return base + HALO + j * 128
        return base + REG_C + HALO + (j - NTC) * 128

    def hT_view(self):
        return self.hTs.rearrange("(k p) c -> p k c", p=128)

    def setup(self):
        kb = self.kb
        with self.phase():
            z = self.sb("zero", [128, 8, 2 * HALO], BF16)
            kb.op('dve', lambda e: e.memset(z[:], 0.0), writes=['zero'])
            hv = self.hT_view()
            for b in range(self.NB):
                base = b * REG
                for c0 in (base, base + REG_C - HALO):
                    pass
                kb.dma('pool', hv[:, :, base:base + HALO], z[:, :, 0:HALO], reads=['zero'], writes=['hTs_halo'])
                kb.dma('pool', hv[:, :, base + REG_C - HALO:base + REG_C + HALO], z[:, :, :], reads=['zero'], writes=['hTs_halo'])
                kb.dma('pool', hv[:, :, base + REG - HALO:base + REG], z[:, :, 0:HALO], reads=['zero'], writes=['hTs_halo'])

    def mod_phase(self, li):
        kb, NB = self.kb, self.NB
        R = NB + 1
        with self.phase():
            cT = self.sb("cT", [128, 8, R])
            sT = self.sb("sT", [128, 8, R])
            ones = self.sb("ones", [1, 4])
            brow = self.sb("brow", [1, 6 * D])
            mrow = self.sb("mrow", [R, 6 * D])
            g12 = self.sb("g12", [R, 2, D])
            pm = [self.ps("pm%d" % i, [R, 512]) for i in range(2)]
            wm = [self.sb("wm%d" % i, [128, 8, 512]) for i in range(2)]
            with self.nc.allow_non_contiguous_dma(reason="tiny transposed load of conditioning vectors"):
                for r in range(R):
                    kb.dma('sp', cT[:, :, r], self.cvec[r, :].rearrange("(k p) -> p k", p=128), writes=['cT'])
            kb.op('act', lambda e: e.activation(out=sT[:], in_=cT[:], func=AF.Silu), reads=['cT'], writes=['sT'])
            kb.op('dve', lambda e: e.memset(ones[:], 1.0), writes=['ones'])
            kb.dma('sp', brow[:], self.b_mod[li:li + 1, :], writes=['brow'])
            kb.dma('sp', g12[:, 0, :], self.norm1_g[li:li + 1, :].to_broadcast([R, D]), writes=['g12'])
            kb.dma('sp', g12[:, 1, :], self.norm2_g[li:li + 1, :].to_broadcast([R, D]), writes=['g12'])
            for n in range(12):
                w = wm[n % 2]
                wk = 'wm%d' % (n % 2)
                pk = 'pm%d' % (n % 2)
                kb.dma('sp', w[:], self.w_mod[li, :, n * 512:(n + 1) * 512].rearrange("(k p) c -> p k c", p=128),
                       writes=[wk])
                for k in range(8):
                    self.mm(pm[n % 2][:], sT[:, k, :], w[:, k, :], k == 0, False, ['sT', wk], [pk])
                self.mm(pm[n % 2][:], ones[0:1, 0:R], brow[0:1, n * 512:(n + 1) * 512], False, True,
                        ['ones', 'brow'], [pk])
                kb.op('act', lambda e: e.copy(out=mrow[:, n * 512:(n + 1) * 512], in_=pm[n % 2][:]),
                      reads=[pk], writes=['mrow'])
            for (gi, sc) in ((0, 1), (1, 4)):
                kb.op('dve', lambda e: e.scalar_tensor_tensor(
                    out=mrow[:, sc * D:(sc + 1) * D], in0=mrow[:, sc * D:(sc + 1) * D], scalar=1.0,
                    in1=g12[:, gi, :], op0=ALU.add, op1=ALU.mult), reads=['mrow', 'g12'], writes=['mrow'])
            kb.dma('pool', self.modv[li].rearrange("r s d -> r (s d)"), mrow[:], reads=['mrow'], writes=['modv'])

    def load_bc(self, t, li, r, s, key):
        self.kb.dma('sp', t[:], self.modv[li, r, s:s + 1, :].to_broadcast([128, D]), reads=['modv'], writes=[key])

    def norm_phase(self, li, which, layer0, ctx_needed=True):
        kb, NB = self.kb, self.NB
        s_sh, s_g = (0, 1) if which == 1 else (3, 4)
        with self.phase():
            ident = self.sb("identb", [128, 128], BF16)
            kb.dma('pool', ident[:], self.c_ident, writes=['ident'])
            Gt = [self.sb("G%d" % r, [128, D]) for r in range(NB + 1)]
            St = [self.sb("S%d" % r, [128, D]) for r in range(NB + 1)]
            for r in range(NB + 1):
                self.load_bc(Gt[r], li, r, s_g, 'G%d' % r)
                self.load_bc(St[r], li, r, s_sh, 'S%d' % r)
            NBUF = 3
            xt = [self.sb("xt%d" % i, [128, D]) for i in range(NBUF)]
            sq = self.sb("sq", [128, D])
            st = [self.sb("st%d" % i, [128, 4]) for i in range(NBUF)]
            hb = [self.sb("hb%d" % i, [128, D], BF16) for i in range(NBUF)]
            pt = [self.ps("pt%d" % i, [128, 8, 128], BF16) for i in range(2)]
            hw = [self.sb("hw%d" % i, [128, 8, 256], BF16) for i in range(2)]
            hv = self.hT_view()
            it = 0
            wi = 0
            pending = []

            def make_b(i, itv, wiv, t2, b, w):
                def stage_b():
                    p = pt[itv % 2]
                    pk = 'pt%d' % (itv % 2)
                    hwk = 'hw%d' % (wiv % 2)
                    for k in range(8):
                        self.tr(p[:, k, :], hb[i][:, k * 128:(k + 1) * 128], ident[:], ['hb%d' % i, 'ident'], [pk])
                    kb.op('act', lambda e: e.copy(out=hw[wiv % 2][:, :, t2 * 128:(t2 + 1) * 128], in_=p[:]),
                          reads=[pk], writes=[hwk])
                    if t2 == 1:
                        c0 = self.hcol(b, 2 * w)
                        kb.dma('pool', hv[:, :, c0:c0 + 256], hw[wiv % 2][:], reads=[hwk], writes=['hTs_%d' % wiv])
                return stage_b

            for b in range(NB):
                for w in range(NT // 2):
                    if w == 0 and not ctx_needed:
                        continue
                    for t2 in range(2):
                        j = 2 * w + t2
                        r = NB if j < NTC else b
                        i = it % NBUF
                        src, skey = self.src_x(layer0, b, j)
                        kb.dma('sp', xt[i][:], src, reads=[skey], writes=['xt%d' % i])
                        kb.op('act', lambda e: e.activation(out=sq[:], in_=xt[i][:], func=AF.Square,
                                                            accum_out=st[i][:, 0:1]),
                              reads=['xt%d' % i], writes=['sq', 'st%d' % i])
                        kb.op('dve', lambda e: e.tensor_scalar(out=st[i][:, 1:2], in0=st[i][:, 0:1], scalar1=1.0 / D,
                                                               scalar2=EPS, op0=ALU.mult, op1=ALU.add),
                              reads=['st%d' % i], writes=['st%d' % i])
                        kb.op('act', lambda e: e.activation(out=st[i][:, 2:3], in_=st[i][:, 1:2], func=AF.Sqrt),
                              reads=['st%d' % i], writes=['st%d' % i])
                        kb.op('dve', lambda e: e.reciprocal(out=st[i][:, 3:4], in_=st[i][:, 2:3]),
                              reads=['st%d' % i], writes=['st%d' % i])
                        kb.op('dve', lambda e: e.scalar_tensor_tensor(out=xt[i][:], in0=xt[i][:], scalar=st[i][:, 3:4],
                                                                      in1=Gt[r][:], op0=ALU.mult, op1=ALU.mult),
                              reads=['xt%d' % i, 'st%d' % i, 'G%d' % r], writes=['xt%d' % i])
                        kb.op('pool', lambda e: e.tensor_tensor(out=hb[i][:], in0=xt[i][:], in1=St[r][:], op=ALU.add),
                              reads=['xt%d' % i, 'S%d' % r], writes=['hb%d' % i])
                        while pending:
                            pending.pop(0)()
                        pending.append(make_b(i, it, wi, t2, b, w))
                        it += 1
                    wi += 1
            while pending:
                pending.pop(0)()

    def outproj_setup(self, w_out_ap, li):
        kb, NB = self.kb, self.NB
        o = {}
        o['w'] = self.sb("wout", [128, 8, D], BF16)
        kb.dma('pool', o['w'][:], w_out_ap.rearrange("(k p) c -> p k c", p=128), writes=['wout'])
        o['ident'] = self.sb("identb", [128, 128], BF16)
        kb.dma('pool', o['ident'][:], self.c_ident, writes=['identb'])
        o['M'] = [self.sb("M2_%d" % r, [128, D]) for r in range(NB + 1)]
        for r in range(NB + 1):
            self.load_bc(o['M'][r], li, r, 2, 'M2_%d' % r)
        o['pt'] = self.ps("opt", [128, 8, 128], BF16)
        o['py'] = self.ps("opy", [128, 2, 512])
        o['oT'] = self.sb("oT", [128, 8, 128], BF16)
        o['xt'] = [self.sb("oxt%d" % i, [128, D]) for i in range(2)]
        o['n'] = 0
        return o

    def outproj_tile(self, o, ob, obkey, layer0, b, j):
        kb = self.kb
        r = self.NB if j < NTC else b
        for k in range(8):
            self.tr(o['pt'][:, k, :], ob[:, k * 128:(k + 1) * 128], o['ident'][:], [obkey, 'identb'], ['opt'])
        kb.op('act', lambda e: e.copy(out=o['oT'][:], in_=o['pt'][:]), reads=['opt'], writes=['oT'])
        for n in range(2):
            for k in range(8):
                self.mm(o['py'][:, n, :], o['oT'][:, k, :], o['w'][:, k, n * 512:(n + 1) * 512], k == 0, k == 7,
                        ['oT', 'wout'], ['opy'])
        i = o['n'] % 2
        o['n'] += 1
        xt = o['xt'][i]
        xk = 'oxt%d' % i
        src, skey = self.src_x(layer0, b, j)
        kb.dma('sp', xt[:], src, reads=[skey], writes=[xk])
        yk = 'oy%d' % i
        kb.op('dve', lambda e: e.tensor_tensor(out=o['py'][:].rearrange("p a b -> p (a b)"),
                                               in0=o['py'][:].rearrange("p a b -> p (a b)"),
                                               in1=o['M'][r][:], op=ALU.mult),
              reads=['opy', 'M2_%d' % r], writes=['opy'])
        kb.op('dve', lambda e: e.tensor_tensor(out=xt[:], in0=o['py'][:].rearrange("p a b -> p (a b)"), in1=xt[:],
                                               op=ALU.add),
              reads=['opy', xk], writes=[xk])
        kb.dma('pool', self.xs[b, j * 128:(j + 1) * 128, :], xt[:], reads=[xk], writes=['xs_%d_%d' % (b, j)])

    def ffn_phase(self, li, ctx_needed=True):
        kb, NB = self.kb, self.NB
        NF = FFN // 128
        HF = NF // 2
        hv = self.hT_view()
        for ps_ in range(2):
            with self.phase():
                f0 = ps_ * HF
                wv = self.sb("wv", [128, 8, HF * 128], BF16)
                wg = self.sb("wg", [128, 8, HF * 128], BF16)
                wo = self.sb("wo", [128, HF, D], BF16)
                win = self.ffn_w_in[li].rearrange("(k p) c -> p k c", p=128)
                for k in range(8):
                    kb.dma('pool', wv[:, k, :], win[:, k, f0 * 128:(f0 + HF) * 128], writes=['wv'])
                    kb.dma('pool', wg[:, k, :], win[:, k, FFN + f0 * 128:FFN + (f0 + HF) * 128], writes=['wg'])
                kb.dma('pool', wo[:], self.ffn_w_out[li, f0 * 128:(f0 + HF) * 128, :].rearrange("(f p) c -> p f c", p=128),
                       writes=['wo'])
                cw = self.sb("cw", [128, HF, 4])
                with self.nc.allow_non_contiguous_dma(reason="tiny per-channel conv taps"):
                    for t in range(3):
                        kb.dma('sp', cw[:, :, t], self.ffn_conv_w[li, t, f0 * 128:(f0 + HF) * 128].rearrange("(f p) -> p f", p=128),
                               writes=['cw'])
                    kb.dma('sp', cw[:, :, 3], self.ffn_conv_b[li, f0 * 128:(f0 + HF) * 128].rearrange("(f p) -> p f", p=128),
                           writes=['cw'])
                M5 = [self.sb("M5_%d" % r, [128, D]) for r in range(NB + 1)]
                for r in range(NB + 1):
                    self.load_bc(M5[r], li, r, 5, 'M5_%d' % r)
                hT = [self.sb("fh%d" % i, [128, 8, 256 + 2 * HALO], BF16) for i in range(2)]
                u = [self.sb("fu%d" % i, [128, HF, 256], BF16) for i in range(2)]
                tA = [self.sb("ftA%d" % i, [128, 256]) for i in range(2)]
                tB = [self.sb("ftB%d" % i, [128, 256]) for i in range(2)]
                pv = [self.ps("fpv%d" % i, [128, 512]) for i in range(2)]
                pg = [self.ps("fpg%d" % i, [128, 512]) for i in range(2)]
                py = [self.ps("fpy%d" % i, [128, 2, 512]) for i in range(2)]
                xt = [self.sb("fx%d" % i, [128, D]) for i in range(2)]
                wi = 0
                ci = 0
                ti = 0
                for b in range(NB):
                    for w in range(NT // 2):
                        if w == 0 and not ctx_needed:
                            continue
                        h = hT[wi % 2]
                        hk = 'fh%d' % (wi % 2)
                        uu = u[wi % 2]
                        uk = 'fu%d' % (wi % 2)
                        c0 = self.hcol(b, 2 * w)
                        kb.dma('sp', h[:], hv[:, :, c0 - HALO:c0 + 256 + HALO], reads=['hTs'], writes=[hk])
                        with self.nosame():
                            for f in range(HF):
                                a = ci % 2
                                ci += 1
                                for k in range(8):
                                    self.mm(pv[a][:, 0:256], wv[:, k, f * 128:(f + 1) * 128], h[:, k, HALO:HALO + 256],
                                            k == 0, k == 7, ['wv', hk], ['fpv%d' % a])
                                for k in range(8):
                                    self.mm(pg[a][:, 0:258], wg[:, k, f * 128:(f + 1) * 128], h[:, k, HALO - 1:HALO + 257],
                                            k == 0, k == 7, ['wg', hk], ['fpg%d' % a])
                                kb.op('dve', lambda e: e.tensor_scalar(out=tA[a][:], in0=pg[a][:, 0:256], scalar1=cw[:, f, 0:1],
                                                                       scalar2=None, op0=ALU.mult),
                                      reads=['fpg%d' % a, 'cw'], writes=['ftA%d' % a])
                                kb.op('dve', lambda e: e.scalar_tensor_tensor(out=tA[a][:], in0=pg[a][:, 1:257], scalar=cw[:, f, 1:2],
                                                                              in1=tA[a][:], op0=ALU.mult, op1=ALU.add),
                                      reads=['fpg%d' % a, 'cw', 'ftA%d' % a], writes=['ftA%d' % a])
                                kb.op('dve', lambda e: e.scalar_tensor_tensor(out=tA[a][:], in0=pg[a][:, 2:258], scalar=cw[:, f, 2:3],
                                                                              in1=tA[a][:], op0=ALU.mult, op1=ALU.add),
                                      reads=['fpg%d' % a, 'cw', 'ftA%d' % a], writes=['ftA%d' % a])
                                kb.op('act', lambda e: e.activation(out=tB[a][:], in_=tA[a][:], func=AF.Silu, bias=cw[:, f, 3:4]),
                                      reads=['ftA%d' % a, 'cw'], writes=['ftB%d' % a])
                                kb.op('dve', lambda e: e.tensor_tensor(out=uu[:, f, :], in0=pv[a][:, 0:256], in1=tB[a][:], op=ALU.mult),
                                      reads=['fpv%d' % a, 'ftB%d' % a], writes=[uk])
                        for t2 in range(2):
                            j = 2 * w + t2
                            r = NB if j < NTC else b
                            i = ti % 2
                            ti += 1
                            for n in range(2):
                                for f in range(HF):
                                    self.mm(py[i][:, n, :], uu[:, f, t2 * 128:(t2 + 1) * 128], wo[:, f, n * 512:(n + 1) * 512],
                                            f == 0, f == HF - 1, [uk, 'wo'], ['fpy%d' % i])
                            kb.dma('sp', xt[i][:], self.xs[b, j * 128:(j + 1) * 128, :], reads=['xs_%d_%d' % (b, j)], writes=['fx%d' % i])
                            pyf = py[i][:].rearrange("p a b -> p (a b)")
                            kb.op('dve', lambda e: e.tensor_tensor(out=pyf, in0=pyf, in1=M5[r][:], op=ALU.mult),
                                  reads=['fpy%d' % i, 'M5_%d' % r], writes=['fpy%d' % i])
                            kb.op('dve', lambda e: e.tensor_tensor(out=xt[i][:], in0=pyf, in1=xt[i][:], op=ALU.add),
                                  reads=['fpy%d' % i, 'fx%d' % i], writes=['fx%d' % i])
                            kb.dma('pool', self.xs[b, j * 128:(j + 1) * 128, :], xt[i][:], reads=['fx%d' % i],
                                   writes=['xs_%d_%d' % (b, j)])
                        wi += 1

    def final_phase(self):
        kb, NB = self.kb, self.NB
        with self.phase():
            G = self.sb("fG", [128, D])
            kb.dma('sp', G[:], self.final_norm_g[0:1, :].to_broadcast([128, D]), writes=['fG'])
            xt = [self.sb("fx%d" % i, [128, D]) for i in range(3)]
            sq = self.sb("fsq", [128, D])
            st = [self.sb("fst%d" % i, [128, 4]) for i in range(3)]
            it = 0
            for b in range(NB):
                for j in range(NTC, NT):
                    i = it % 3
                    it += 1
                    kb.dma('sp', xt[i][:], self.xs[b, j * 128:(j + 1) * 128, :], reads=['xs_%d_%d' % (b, j)], writes=['fx%d' % i])
                    kb.op('act', lambda e: e.activation(out=sq[:], in_=xt[i][:], func=AF.Square, accum_out=st[i][:, 0:1]),
                          reads=['fx%d' % i], writes=['fsq', 'fst%d' % i])
                    kb.op('dve', lambda e: e.tensor_scalar(out=st[i][:, 1:2], in0=st[i][:, 0:1], scalar1=1.0 / D,
                                                           scalar2=EPS, op0=ALU.mult, op1=ALU.add),
                          reads=['fst%d' % i], writes=['fst%d' % i])
                    kb.op('act', lambda e: e.activation(out=st[i][:, 2:3], in_=st[i][:, 1:2], func=AF.Sqrt),
                          reads=['fst%d' % i], writes=['fst%d' % i])
                    kb.op('dve', lambda e: e.reciprocal(out=st[i][:, 3:4], in_=st[i][:, 2:3]),
                          reads=['fst%d' % i], writes=['fst%d' % i])
                    kb.op('dve', lambda e: e.scalar_tensor_tensor(out=xt[i][:], in0=xt[i][:], scalar=st[i][:, 3:4],
                                                                  in1=G[:], op0=ALU.mult, op1=ALU.mult),
                          reads=['fx%d' % i, 'fst%d' % i, 'fG'], writes=['fx%d' % i])
                    kb.dma('pool', self.out[b, (j - NTC) * 128:(j - NTC + 1) * 128, :], xt[i][:], reads=['fx%d' % i],
                           writes=['out_%d_%d' % (b, j)])

    def attn_phase(self, li, jx, layer0, want_ctx):
        kb, NB = self.kb, self.NB
        hv = self.hT_view()
        SC = 128 ** -0.5
        for b in range(NB):
            with self.phase():
                qT = self.sb("qT", [128, 8, TOK], BF16)
                kT = self.sb("kT", [128, 2, TOK], BF16)
                V1 = self.sb("V1", [128, NT, 2, 132], BF16)
                kb.op('pool', lambda e: e.memset(V1[:, :, :, 128:129], 1.0), writes=['V1'])
                ident = self.sb("identb", [128, 128], BF16)
                kb.dma('pool', ident[:], self.c_ident, writes=['ident'])
                with self.phase():
                    win = self.sb("awin", [128, 8, 1536], BF16)
                    kb.dma('pool', win[:], self.attn_w_in[jx].rearrange("(k p) c -> p k c", p=128), writes=['awin'])
                    Gqk = self.sb("Gqk", [128, 2, 128])
                    kb.dma('sp', Gqk[:, 0, :], self.attn_q_norm_g[jx:jx + 1, :].to_broadcast([128, 128]), writes=['Gqk'])
                    kb.dma('sp', Gqk[:, 1, :], self.attn_k_norm_g[jx:jx + 1, :].to_broadcast([128, 128]), writes=['Gqk'])
                    hT = [self.sb("ah%d" % i, [128, 8, 128], BF16) for i in range(2)]
                    pz = [self.ps("apz%d" % i, [128, 512]) for i in range(3)]
                    zs = self.sb("azs", [128, 1536])
                    sq = self.sb("asq", [128, 1280])
                    st = self.sb("ast", [128, 4, 10])
                    qn = self.sb("aqn", [128, 1280])
                    qb = self.sb("aqb", [128, 1280], BF16)
                    rp = [self.sb("arp%d" % i, [128, 128]) for i in range(2)]
                    t1 = self.sb("at1", [128, 640])
                    t2 = self.sb("at2", [128, 640])
                    ptq = self.ps("aptq", [128, 8, 128], BF16)
                    ptk = self.ps("aptk", [128, 2, 128], BF16)
                    for j in range(NT):
                        i = j % 2
                        c0 = self.hcol(b, j)
                        kb.dma('sp', hT[i][:], hv[:, :, c0:c0 + 128], reads=['hTs'], writes=['ah%d' % i])
                        for n in range(3):
                            for k in range(8):
                                self.mm(pz[n][:], hT[i][:, k, :], win[:, k, n * 512:(n + 1) * 512], k == 0, k == 7,
                                        ['ah%d' % i, 'awin'], ['apz%d' % n])
                            kb.op('act', lambda e: e.copy(out=zs[:, n * 512:(n + 1) * 512], in_=pz[n][:]),
                                  reads=['apz%d' % n], writes=['azs'])
                        kb.op('pool', lambda e: e.tensor_copy(out=V1[:, j, :, 0:128],
                                                              in_=zs[:, 1280:1536].rearrange("p (g d) -> p g d", g=2)),
                              reads=['azs'], writes=['V1'])
                        kb.op('dve', lambda e: e.tensor_tensor(out=sq[:], in0=zs[:, 0:1280], in1=zs[:, 0:1280], op=ALU.mult),
                              reads=['azs'], writes=['asq'])
                        kb.op('dve', lambda e: e.tensor_reduce(out=st[:, 0, :], in_=sq[:].rearrange("p (h d) -> p h d", h=10),
                                                               axis=AX.X, op=ALU.add),
                              reads=['asq'], writes=['ast'])
                        kb.op('dve', lambda e: e.tensor_scalar(out=st[:, 1, :], in0=st[:, 0, :], scalar1=1.0 / 128, scalar2=EPS,
                                                               op0=ALU.mult, op1=ALU.add), reads=['ast'], writes=['ast'])
                        kb.op('act', lambda e: e.activation(out=st[:, 2, :], in_=st[:, 1, :], func=AF.Sqrt),
                              reads=['ast'], writes=['ast'])
                        kb.op('dve', lambda e: e.reciprocal(out=st[:, 3, :], in_=st[:, 2, :]), reads=['ast'], writes=['ast'])
                        z3 = zs[:, 0:1280].rearrange("p (h d) -> p h d", h=10)
                        q3 = qn[:].rearrange("p (h d) -> p h d", h=10)
                        kb.op('dve', lambda e: e.tensor_tensor(out=q3, in0=z3, in1=st[:, 3, :].unsqueeze(2).to_broadcast([128, 10, 128]),
                                                               op=ALU.mult), reads=['azs', 'ast'], writes=['aqn'])
                        kb.op('dve', lambda e: e.tensor_tensor(out=q3[:, 0:8, :], in0=q3[:, 0:8, :],
                                                               in1=Gqk[:, 0:1, :].to_broadcast([128, 8, 128]), op=ALU.mult),
                              reads=['aqn', 'Gqk'], writes=['aqn'])
                        kb.op('dve', lambda e: e.tensor_tensor(out=q3[:, 8:10, :], in0=q3[:, 8:10, :],
                                                               in1=Gqk[:, 1:2, :].to_broadcast([128, 2, 128]), op=ALU.mult),
                              reads=['aqn', 'Gqk'], writes=['aqn'])
                        if j >= NTC:
                            rr = rp[j % 2]
                            rk = 'arp%d' % (j % 2)
                            kb.dma('sp', rr[:], self.c_rope[(j - NTC) * 128:(j - NTC + 1) * 128, :], writes=[rk])
                            q4 = qn[:].rearrange("p (h d t) -> p h d t", h=10, t=2)
                            b4 = qb[:].rearrange("p (h d t) -> p h d t", h=10, t=2)
                            x0, x1 = q4[:, :, :, 0], q4[:, :, :, 1]
                            cosb = rr[:, 0:64].unsqueeze(1).to_broadcast([128, 10, 64])
                            sinb = rr[:, 64:128].unsqueeze(1).to_broadcast([128, 10, 64])
                            t13 = t1[:].rearrange("p (h d) -> p h d", h=10)
                            t23 = t2[:].rearrange("p (h d) -> p h d", h=10)
                            kb.op('dve', lambda e: e.tensor_tensor(out=t13, in0=x0, in1=cosb, op=ALU.mult), reads=['aqn', rk], writes=['at1'])
                            kb.op('pool', lambda e: e.tensor_tensor(out=t23, in0=x1, in1=sinb, op=ALU.mult), reads=['aqn', rk], writes=['at2'])
                            kb.op('dve', lambda e: e.tensor_tensor(out=b4[:, :, :, 0], in0=t13, in1=t23, op=ALU.subtract),
                                  reads=['at1', 'at2'], writes=['aqb'])
                            kb.op('dve', lambda e: e.tensor_tensor(out=t13, in0=x0, in1=sinb, op=ALU.mult), reads=['aqn', rk, 'aqb'], writes=['at1'])
                            kb.op('pool', lambda e: e.tensor_tensor(out=t23, in0=x1, in1=cosb, op=ALU.mult), reads=['aqn', rk, 'aqb'], writes=['at2'])
                            kb.op('dve', lambda e: e.tensor_tensor(out=b4[:, :, :, 1], in0=t13, in1=t23, op=ALU.add),
                                  reads=['at1', 'at2'], writes=['aqb'])
                        else:
                            kb.op('dve', lambda e: e.tensor_copy(out=qb[:], in_=qn[:]), reads=['aqn'], writes=['aqb'])
                        for h in range(8):
                            self.tr(ptq[:, h, :], qb[:, h * 128:(h + 1) * 128], ident[:], ['aqb', 'ident'], ['aptq'])
                        for g in range(2):
                            self.tr(ptk[:, g, :], qb[:, (8 + g) * 128:(9 + g) * 128], ident[:], ['aqb', 'ident'], ['aptk'])
                        kb.op('act', lambda e: e.copy(out=qT[:, :, j * 128:(j + 1) * 128], in_=ptq[:]), reads=['aptq'], writes=['qT'])
                        kb.op('act', lambda e: e.copy(out=kT[:, :, j * 128:(j + 1) * 128], in_=ptk[:]), reads=['aptk'], writes=['kT'])
                with self.phase():
                    o = self.outproj_setup(self.attn_w_out[jx], li)
                    E = self.sb("aE", [128, NT, 512], BF16)
                    pS = [self.ps("apS%d" % i, [128, 512]) for i in range(2)]
                    pO = [self.ps("apO%d" % i, [128, 512]) for i in range(2)]
                    ob = [self.sb("aob%d" % i, [128, D], BF16) for i in range(2)]
                    rc = self.sb("arc", [128, 8])
                    si = 0
                    oi = 0
                    for jq in range(NT):
                        if jq < NTC and not want_ctx:
                            continue
                        nk = NTC if jq < NTC else NT
                        obt = ob[jq % 2]
                        obk = 'aob%d' % (jq % 2)
                        for g in range(2):
                            for kt in range(nk):
                                p = pS[si % 2]
                                pk = 'apS%d' % (si % 2)
                                si += 1
                                self.mm(p[:].rearrange("p (h q) -> p h q", h=4), kT[:, g, kt * 128:(kt + 1) * 128], qT[:, 4 * g:4 * g + 4, jq * 128:(jq + 1) * 128],
                                        True, True, ['kT', 'qT'], [pk])
                                kb.op('act', lambda e: e.activation(out=E[:, kt, :], in_=p[:], func=AF.Exp, scale=SC),
                                      reads=[pk], writes=['aE'])
                            for hh in range(4):
                                po = pO[oi % 2]
                                pok = 'apO%d' % (oi % 2)
                                oi += 1
                                for kt in range(nk):
                                    self.mm(po[:, 0:129], E[:, kt, hh * 128:(hh + 1) * 128], V1[:, kt, g, 0:129],
                                            kt == 0, kt == nk - 1, ['aE', 'V1'], [pok])
                                hd = 4 * g + hh
                                kb.op('dve', lambda e: e.reciprocal(out=rc[:, hd:hd + 1], in_=po[:, 128:129]),
                                      reads=[pok], writes=['arc'])
                                kb.op('dve', lambda e: e.tensor_scalar(out=obt[:, hd * 128:(hd + 1) * 128], in0=po[:, 0:128],
                                                                       scalar1=rc[:, hd:hd + 1], scalar2=None, op0=ALU.mult),
                                      reads=[pok, 'arc'], writes=[obk])
                        self.outproj_tile(o, obt, obk, layer0, b, jq)

    def gdn_decl(self):
        if hasattr(self, 'g_qkT'):
            return
        NB = self.NB
        self.g_qkT = self.dram("g_qkT", [2, D, NB * TOK], F32)
        self.g_ktok = self.dram("g_ktok", [NB * TOK, D], F32)
        self.g_vtok = self.dram("g_vtok", [NB * TOK, D], F32)
        self.g_sgate = self.dram("g_sgate", [NB * TOK, D], F32)
        self.g_bg = self.dram("g_bg", [NB * TOK, 32], F32)
        self.g_odir = self.dram("g_odir", [2, NB * TOK, D], F32)

    def gdn_phase(self, li, jx, layer0, want_ctx):
        self.gdn_decl()
        self.gdn_proj(li, jx)
        self.gdn_scan(li, jx)
        self.gdn_finish(li, jx, layer0, want_ctx)

    def gdn_proj(self, li, jx):
        kb, NB = self.kb, self.NB
        hv = self.hT_view()
        with self.phase():
            win = self.sb("gwin", [128, 8, 4128], BF16)
            wsrc = self.gdn_w_in[jx].rearrange("(k p) c -> p k c", p=128)
            for k in range(8):
                kb.dma('pool', win[:, k, :], wsrc[:, k, :], writes=['gwin'])
            cw = self.sb("gcw", [128, 24, 5])
            with self.nc.allow_non_contiguous_dma(reason="tiny per-channel conv taps"):
                for t in range(5):
                    for f0 in range(0, 24, 8):
                        kb.dma('sp', cw[:, f0:f0 + 8, t], self.gdn_conv_w[jx, t, f0 * 128:(f0 + 8) * 128].rearrange("(f p) -> p f", p=128),
                               writes=['gcw'])
            identf = self.sb("identf", [128, 128])
            kb.dma('sp', identf[:], self.c_ident, writes=['identf'])
            onesf = self.sb("onesf", [128, 128])
            kb.op('dve', lambda e: e.memset(onesf[:], 1.0), writes=['onesf'])
            DTB = self.sb("gdtb", [128, 16])
            NA = self.sb("gna", [128, 16])
            kb.dma('sp', DTB[:], self.gdn_dt_bias[jx:jx + 1, :].to_broadcast([128, 16]), writes=['gdtb'])
            kb.dma('sp', NA[:], self.gdn_a_log[jx:jx + 1, :].to_broadcast([128, 16]), writes=['gna'])
            kb.op('act', lambda e: e.activation(out=NA[:], in_=NA[:], func=AF.Exp), reads=['gna'], writes=['gna'])
            kb.op('dve', lambda e: e.tensor_scalar(out=NA[:], in0=NA[:], scalar1=-1.0, scalar2=None, op0=ALU.mult),
                  reads=['gna'], writes=['gna'])
            h2 = [self.sb("gh%d" % i, [128, 8, 256 + 2 * HALO], BF16) for i in range(2)]
            NPZ = 2
            NB4 = 4
            pz = [self.ps("gpz%d" % i, [128, 512]) for i in range(NPZ)]
            pnbk = [self.ps("gpnb%d" % i, [128, 512]) for i in range(2)]
            pnl = [pnbk[0][:, 0:256], pnbk[1][:, 0:256]]
            pXb = [self.ps("gpX%d" % i, [128, 512]) for i in range(2)]
            pTl = [pXb[0][:, 0:256].rearrange("p (a b) -> p a b", a=2), pXb[1][:, 0:256].rearrange("p (a b) -> p a b", a=2)]
            pg = self.ps("gpg", [128, 2, 512])
            pb = pXb[1][:, 256:288]
            tA = [self.sb("gtA%d" % i, [128, 256]) for i in range(NB4)]
            sAll = [self.sb("gsAll%d" % i, [128, 24, 256]) for i in range(2)]
            rAll = [self.sb("grAll%d" % i, [128, 16, 256]) for i in range(2)]
            sqb = [self.sb("gsqb%d" % i, [128, 256], BF16) for i in range(NB4)]
            onesb = self.sb("gonesb", [128, 128], BF16)
            kb.op('dve', lambda e: e.memset(onesb[:], 1.0), writes=['onesb'])
            kst = self.sb("gkst", [128, 2, D])
            vst = self.sb("gvst", [128, 2, D])
            gst = [self.sb("ggst%d" % i, [128, D]) for i in range(2)]
            bgs = self.sb("gbgs", [128, 32])
            bt = self.sb("gbt", [128, 4, 16])
            qkTv = self.g_qkT.rearrange("q (h p) c -> q p h c", p=128)
            ci = 0
            wi = 0
            for b in range(NB):
                for w in range(NT // 2):
                    h = h2[wi % 2]
                    hk = 'gh%d' % (wi % 2)
                    wi += 1
                    c0 = self.hcol(b, 2 * w)
                    tok0 = b * TOK + 2 * w * 128
                    kb.dma('sp', h[:], hv[:, :, c0 - HALO:c0 + 256 + HALO], writes=[hk])
                    ws = wi % 2
                    sA, rA = sAll[ws], rAll[ws]
                    sAk = lambda f_: 'gsA%d_%d' % (ws, f_)
                    rAk = 'grA%d' % ws

                    def stage_ones(f_):
                        an_ = f_ % 2
                        self.mm(pnl[an_], onesb[:], sqb[f_ % NB4][:], True, True, ['onesb', 'gsqb%d' % (f_ % NB4)], ['gpn%d' % an_])
                        m_ = 128.0 if f_ < 8 else 1.0
                        kb.op('dve', lambda e: e.tensor_scalar(out=rA[:, f_, :], in0=pnl[an_], scalar1=m_, scalar2=EPS * m_,
                                                               op0=ALU.mult, op1=ALU.add), reads=['gpn%d' % an_], writes=[rAk])

                    def stage_tr(f_):
                        dst = kst if f_ < 16 else vst
                        dk = 'gkst' if f_ < 16 else 'gvst'
                        an_ = f_ % 2
                        pT, pTk = pTl[an_], 'gpX%d' % an_
                        for t2_ in range(2):
                            self.tr(pT[:, t2_, :], sA[:, f_, t2_ * 128:(t2_ + 1) * 128], identf[:], [sAk(f_), 'identf'], [pTk])
                        kb.op('act', lambda e: e.copy(out=dst[:, :, (f_ % 8) * 128:(f_ % 8 + 1) * 128], in_=pT), reads=[pTk], writes=[dk])

                    with self.nosame():
                        for f in range(24):
                            a = ci % NB4
                            az = ci % NPZ
                            ci += 1
                            pzk = 'gpz%d' % az
                            for k in range(8):
                                self.mm(pz[az][:, 0:260], win[:, k, f * 128:(f + 1) * 128], h[:, k, :], k == 0, k == 7,
                                        ['gwin', hk], [pzk])
                            tk = 'gtA%d' % a
                            kb.op('dve', lambda e: e.tensor_scalar(out=tA[a][:], in0=pz[az][:, 0:256], scalar1=cw[:, f, 0:1],
                                                                   scalar2=None, op0=ALU.mult), reads=[pzk, 'gcw'], writes=[tk])
                            for t in range(1, 5):
                                kb.op('dve', lambda e: e.scalar_tensor_tensor(out=tA[a][:], in0=pz[az][:, t:t + 256], scalar=cw[:, f, t:t + 1],
                                                                              in1=tA[a][:], op0=ALU.mult, op1=ALU.add),
                                      reads=[pzk, 'gcw', tk], writes=[tk])
                            kb.op('act', lambda e: e.activation(out=sA[:, f, :], in_=tA[a][:], func=AF.Silu), reads=[tk], writes=[sAk(f)])
                            if f < 16:
                                kb.op('pool', lambda e: e.tensor_tensor(out=sqb[f % NB4][:], in0=sA[:, f, :], in1=sA[:, f, :], op=ALU.mult),
                                      reads=[sAk(f)], writes=['gsqb%d' % (f % NB4)])
                            if 2 <= f < 18:
                                stage_ones(f - 2)
                            if f >= 19:
                                stage_tr(f - 3)
                        for f_ in (21, 22, 23):
                            stage_tr(f_)
                        kb.op('act', lambda e: e.activation(out=rA[:], in_=rA[:], func=AF.Sqrt), reads=[rAk], writes=[rAk])
                        for q4 in range(4):
                            kb.op('dve', lambda e: e.reciprocal(out=rA[:, 4 * q4:4 * q4 + 4, :], in_=rA[:, 4 * q4:4 * q4 + 4, :]),
                                  reads=[rAk], writes=[rAk])
                        for f in range(16):
                            kb.op('pool', lambda e: e.tensor_tensor(out=sA[:, f, :], in0=sA[:, f, :], in1=rA[:, f, :], op=ALU.mult),
                                  reads=[sAk(f), rAk], writes=[sAk(f)])
                            kb.dma('pool', qkTv[f // 8, :, f % 8, tok0:tok0 + 256], sA[:, f, :], reads=[sAk(f)], writes=['g_qkT_%d_%d' % (wi, f)])
                            if f >= 8:
                                stage_tr(f)
                    for t2 in range(2):
                        r0 = tok0 + t2 * 128
                        kb.dma('pool', self.g_ktok[r0:r0 + 128, :], kst[:, t2, :], reads=['gkst'], writes=['g_ktok_%d' % r0])
                        kb.dma('pool', self.g_vtok[r0:r0 + 128, :], vst[:, t2, :], reads=['gvst'], writes=['g_vtok_%d' % r0])
                        hs = h[:, :, HALO + t2 * 128:HALO + (t2 + 1) * 128]
                        for n in range(2):
                            for k in range(8):
                                self.mm(pg[:, n, :], hs[:, k, :], win[:, k, 3072 + n * 512:3072 + (n + 1) * 512], k == 0, k == 7,
                                        [hk, 'gwin'], ['gpg'])
                        g_ = gst[t2]
                        gk2 = 'ggst%d' % t2
                        kb.op('act', lambda e: e.activation(out=g_[:], in_=pg[:].rearrange("p a b -> p (a b)"), func=AF.Silu),
                              reads=['gpg'], writes=[gk2])
                        kb.dma('pool', self.g_sgate[r0:r0 + 128, :], g_[:], reads=[gk2], writes=['g_sgate_%d' % r0])
                        for k in range(8):
                            self.mm(pb, hs[:, k, :], win[:, k, 4096:4128], k == 0, k == 7, [hk, 'gwin'], ['gpX1'])
                        pb4 = pb.rearrange("p (d t h) -> p d t h", d=2, t=2)
                        kb.op('act', lambda e: e.activation(out=bgs[:, 0:16].rearrange("p (d h) -> p d h", d=2), in_=pb4[:, :, 0, :],
                                                            func=AF.Sigmoid), reads=['gpX1'], writes=['gbgs'])
                        kb.op('dve', lambda e: e.tensor_tensor(out=bt[:, 0, :].rearrange("p (d h) -> p d h", d=2), in0=pb4[:, :, 1, :],
                                                               in1=DTB[:].rearrange("p (d h) -> p d h", d=2), op=ALU.add),
                              reads=['gpX1', 'gdtb'], writes=['gbt'])
                        kb.op('dve', lambda e: e.scalar_tensor_tensor(out=bt[:, 1, :], in0=bt[:, 0, :], scalar=-1.0, in1=bt[:, 0, :],
                                                                      op0=ALU.mult, op1=ALU.max), reads=['gbt'], writes=['gbt'])
                        kb.op('act', lambda e: e.activation(out=bt[:, 2, :], in_=bt[:, 1, :], func=AF.Exp, scale=-1.0),
                              reads=['gbt'], writes=['gbt'])
                        kb.op('act', lambda e: e.activation(out=bt[:, 3, :], in_=bt[:, 2, :], func=AF.Ln, bias=1.0),
                              reads=['gbt'], writes=['gbt'])
                        kb.op('dve', lambda e: e.scalar_tensor_tensor(out=bt[:, 1, :], in0=bt[:, 0, :], scalar=0.0, in1=bt[:, 3, :],
                                                                      op0=ALU.max, op1=ALU.add), reads=['gbt'], writes=['gbt'])
                        kb.op('dve', lambda e: e.tensor_tensor(out=bgs[:, 16:32], in0=bt[:, 1, :], in1=NA[:], op=ALU.mult),
                              reads=['gbt', 'gna'], writes=['gbgs'])
                        kb.dma('pool', self.g_bg[r0:r0 + 128, :], bgs[:], reads=['gbgs'], writes=['g_bg_%d' % r0])

    def gdn_scan(self, li, jx):
        kb, NB = self.kb, self.NB
        HB = 4
        qkTv = self.g_qkT.rearrange("q (h p) c -> q p h c", p=128)
        order = [list(range(NT)), [1, 0] + list(range(NT - 1, NTC - 1, -1))]
        with self.phase():
            MS = self.sb("gmask", [128, 8, 128])
            kb.dma('sp', MS[:], self.c_masks[0:8].rearrange("m p c -> p m c"), writes=['gmask'])
            identf = self.sb("identf", [128, 128])
            kb.dma('sp', identf[:], self.c_ident, writes=['identf'])
            PS = [self.ps("gps%d" % i, [128, 512]) for i in range(8)]
            PK = ['gps%d' % i for i in range(8)]

            def v4(t):
                return t[:].rearrange("p (h c) -> p h c", h=HB)
            names = ['qT', 'kT', 'ktok', 'vtok', 'Gbc', 'Dm', 'E', 'DT', 'DTs', 'EG', 'qd', 'W', 'WT', 'Wa0', 'Wa1', 'Wb0', 'Wb1',
                     'QKm', 'kdec', 'wT', 'vn']
            sets = []
            for si in range(2):
                T = {}
                for nm in names:
                    T[nm] = self.sb("g%s%d" % (nm, si), [128, HB * 128])
                for nm in ('y0', 'y1', 'UW'):
                    T[nm] = self.sb("g%s%d" % (nm, si), [128, HB, 256])
                T['cols'] = self.sb("gcols%d" % si, [128, 6, HB])
                T['alast'] = self.sb("galast%d" % si, [128, HB, 2])
                T['k'] = 's%d_' % si
                sets.append(T)
            bgt = [self.sb("gbg%d" % i, [128, 32]) for i in range(4)]
            ost = [self.sb("gost%d" % i, [128, D]) for i in range(4)]
            S = [[self.sb("gS_%d_%d" % (d, hf), [128, HB, 128]) for hf in range(2)] for d in range(2)]
            stepi = 0
            bi = 0
            for b in range(NB):
                for d in range(2):
                    for hf in range(2):
                        kb.op('pool', lambda e: e.memset(S[d][hf][:], 0.0), writes=['gS_%d_%d' % (d, hf)])
                for n in range(NT):
                    for d in range(2):
                        j = order[d][n]
                        tok0 = b * TOK + j * 128
                        bg = bgt[bi % 4]
                        bgk = 'gbg%d' % (bi % 4)
                        os_ = ost[bi % 4]
                        osk = 'gost%d' % (bi % 4)
                        bi += 1
                        kb.dma('sp', bg[:], self.g_bg[tok0:tok0 + 128, :], writes=[bgk])
                        Ud, Usd, nUd, BO = MS[:, d, :], MS[:, 2 + d, :], MS[:, 4 + d, :], MS[:, 6, :]
                        for hf in range(2):
                            T = sets[stepi % 2]
                            K = lambda nm: T['k'] + nm
                            stepi += 1
                            h0 = hf * HB
                            Sk = 'gS_%d_%d' % (d, hf)
                            St = S[d][hf]
                            kb.dma('sp', v4(T['qT']), qkTv[0, :, h0:h0 + HB, tok0:tok0 + 128], writes=[K('qT')])
                            kb.dma('sp', v4(T['kT']), qkTv[1, :, h0:h0 + HB, tok0:tok0 + 128], writes=[K('kT')])
                            kb.dma('sp', T['ktok'][:], self.g_ktok[tok0:tok0 + 128, h0 * 128:(h0 + HB) * 128], writes=[K('ktok')])
                            kb.dma('sp', T['vtok'][:], self.g_vtok[tok0:tok0 + 128, h0 * 128:(h0 + HB) * 128], writes=[K('vtok')])
                            g4 = bg[:, 16 + d * 8 + h0:16 + d * 8 + h0 + HB]
                            be4 = bg[:, d * 8 + h0:d * 8 + h0 + HB]
                            bc4 = lambda ap: ap.unsqueeze(2).to_broadcast([128, HB, 128])
                            mk4 = lambda ap: ap.unsqueeze(1).to_broadcast([128, HB, 128])
                            kb.op('dve', lambda e: e.tensor_copy(out=v4(T['Gbc']), in_=bc4(g4)), reads=[bgk], writes=[K('Gbc')])
                            Gbc = v4(T['Gbc'])
                            for h in range(HB):
                                self.mm(PS[0][:, h * 128:(h + 1) * 128], Gbc[:, h, :], Ud, True, True, [K('Gbc'), 'gmask'], [PK[0]])
                            self.mm(PS[5][:, 0:HB], Ud, g4, True, True, ['gmask', bgk], [PK[5]])
                            self.mm(PS[5][:, HB:2 * HB], BO, g4, True, True, ['gmask', bgk], [PK[5]])
                            cols = T['cols']
                            kb.op('act', lambda e: e.copy(out=cols[:, 0, :], in_=PS[5][:, 0:HB]), reads=[PK[5]], writes=[K('cols')])
                            kb.op('dve', lambda e: e.tensor_tensor(out=cols[:, 1, :], in0=PS[5][:, HB:2 * HB], in1=cols[:, 0, :], op=ALU.subtract),
                                  reads=[PK[5], K('cols')], writes=[K('cols')])
                            kb.op('act', lambda e: e.activation(out=cols[:, 2, :], in_=cols[:, 0, :], func=AF.Exp), reads=[K('cols')], writes=[K('cols')])
                            kb.op('act', lambda e: e.activation(out=cols[:, 3, :], in_=cols[:, 1, :], func=AF.Exp), reads=[K('cols')], writes=[K('cols')])
                            for h in range(HB):
                                kb.op('dve', lambda e: e.tensor_scalar(out=T['Dm'][:, h * 128:(h + 1) * 128], in0=PS[0][:, h * 128:(h + 1) * 128],
                                                                       scalar1=cols[:, 0, h:h + 1], scalar2=0.0, op0=ALU.subtract, op1=ALU.min),
                                      reads=[PK[0], K('cols')], writes=[K('Dm')])
                            kb.op('act', lambda e: e.activation(out=T['E'][:], in_=T['Dm'][:], func=AF.Exp), reads=[K('Dm')], writes=[K('E')])
                            kb.op('dve', lambda e: e.tensor_tensor(out=v4(T['DT']), in0=v4(T['E']), in1=mk4(Ud), op=ALU.mult),
                                  reads=[K('E'), 'gmask'], writes=[K('DT')])
                            kb.op('pool', lambda e: e.tensor_tensor(out=v4(T['DTs']), in0=v4(T['E']), in1=mk4(Usd), op=ALU.mult),
                                  reads=[K('E'), 'gmask'], writes=[K('DTs')])
                            kb.op('act', lambda e: e.activation(out=T['EG'][:], in_=PS[0][:], func=AF.Exp), reads=[PK[0]], writes=[K('EG')])
                            kb.op('pool', lambda e: e.tensor_tensor(out=T['qd'][:], in0=T['qT'][:], in1=T['EG'][:], op=ALU.mult),
                                  reads=[K('qT'), K('EG')], writes=[K('qd')])
                            kT4, qT4 = v4(T['kT']), v4(T['qT'])
                            for h in range(HB):
                                self.mm(PS[2][:, h * 128:(h + 1) * 128], kT4[:, h, :], kT4[:, h, :], True, True, [K('kT')], [PK[2]])
                            for h in range(HB):
                                self.mm(PS[3][:, h * 128:(h + 1) * 128], kT4[:, h, :], qT4[:, h, :], True, True, [K('kT'), K('qT')], [PK[3]])
                            kb.op('dve', lambda e: e.tensor_tensor(out=T['DTs'][:], in0=PS[2][:], in1=T['DTs'][:], op=ALU.mult),
                                  reads=[PK[2], K('DTs')], writes=[K('DTs')])
                            kb.op('dve', lambda e: e.tensor_tensor(out=v4(T['W']).bitcast(F32R), in0=v4(T['DTs']), in1=bc4(be4), op=ALU.mult),
                                  reads=[K('DTs'), bgk], writes=[K('W')])
                            kb.op('dve', lambda e: e.tensor_tensor(out=T['QKm'][:], in0=PS[3][:], in1=T['DT'][:], op=ALU.mult),
                                  reads=[PK[3], K('DT')], writes=[K('QKm')])
                            kb.op('act', lambda e: e.copy(out=T['y0'][:, :, 0:128].bitcast(F32R), in_=v4(T['vtok'])), reads=[K('vtok')], writes=[K('y0')])
                            kb.op('dve', lambda e: e.tensor_tensor(out=T['y0'][:, :, 128:256].bitcast(F32R), in0=v4(T['ktok']), in1=bc4(cols[:, 2, :]), op=ALU.mult),
                                  reads=[K('ktok'), K('cols')], writes=[K('y0')])
                            kb.op('pool', lambda e: e.tensor_tensor(out=v4(T['kdec']), in0=v4(T['ktok']), in1=bc4(cols[:, 3, :]), op=ALU.mult),
                                  reads=[K('ktok'), K('cols')], writes=[K('kdec')])
                            with self.nosame():
                                W4 = v4(T['W'])
                                for h in range(HB):
                                    self.tr(PS[4][:, h * 128:(h + 1) * 128], W4[:, h, :], identf[:], [K('W'), 'identf'], [PK[4]])
                                kb.op('act', lambda e: e.copy(out=T['WT'][:].bitcast(F32R), in_=PS[4][:]), reads=[PK[4]], writes=[K('WT')])
                                pA = [PS[6], PS[7]]
                                ycur, ynxt = 'y0', 'y1'
                                for h in range(HB):
                                    self.mmr(pA[h // 2][:, (h % 2) * 256:(h % 2 + 1) * 256], W4[:, h, :], T[ycur][:, h, :], True, True,
                                             [K('W'), K(ycur)], [PK[6 + h // 2]])
                                for q in range(2):
                                    kb.op('dve', lambda e: e.tensor_tensor(out=T[ynxt][:, 2 * q:2 * q + 2, :].rearrange("p h c -> p (h c)").bitcast(F32R),
                                                                           in0=T[ycur][:, 2 * q:2 * q + 2, :].rearrange("p h c -> p (h c)"),
                                                                           in1=pA[q][:], op=ALU.subtract),
                                          reads=[K(ycur), PK[6 + q]], writes=[K(ynxt)])
                                ycur, ynxt = ynxt, ycur
                                cur, curT = 'W', 'WT'
                                for lvl in range(1, 6):
                                    na, nb_ = 'Wa%d' % (lvl % 2), 'Wb%d' % (lvl % 2)
                                    c4, cT4 = v4(T[cur]), v4(T[curT])
                                    for h in range(HB):
                                        self.mmr(PS[4][:, h * 128:(h + 1) * 128], cT4[:, h, :], c4[:, h, :], True, True, [K(cur), K(curT)], [PK[4]])
                                    if lvl < 5:
                                        for h in range(HB):
                                            self.mmr(PS[5][:, h * 128:(h + 1) * 128], c4[:, h, :], cT4[:, h, :], True, True, [K(cur), K(curT)], [PK[5]])
                                    kb.op('act', lambda e: e.copy(out=T[na][:].bitcast(F32R), in_=PS[4][:]), reads=[PK[4]], writes=[K(na)])
                                    if lvl < 5:
                                        kb.op('dve', lambda e: e.tensor_copy(out=T[nb_][:].bitcast(F32R), in_=PS[5][:]), reads=[PK[5]], writes=[K(nb_)])
                                    n4 = v4(T[na])
                                    for h in range(HB):
                                        self.mmr(pA[h // 2][:, (h % 2) * 256:(h % 2 + 1) * 256], n4[:, h, :], T[ycur][:, h, :], True, True,
                                                 [K(na), K(ycur)], [PK[6 + h // 2]])
                                    for q in range(2):
                                        kb.op('dve', lambda e: e.tensor_tensor(out=T[ynxt][:, 2 * q:2 * q + 2, :].rearrange("p h c -> p (h c)").bitcast(F32R),
                                                                               in0=T[ycur][:, 2 * q:2 * q + 2, :].rearrange("p h c -> p (h c)"),
                                                                               in1=pA[q][:], op=ALU.add),
                                              reads=[K(ycur), PK[6 + q]], writes=[K(ynxt)])
                                    ycur, ynxt = ynxt, ycur
                                    cur, curT = na, nb_
                                kb.op('dve', lambda e: e.tensor_tensor(out=T['UW'][:], in0=T[ycur][:], in1=be4.unsqueeze(2).to_broadcast([128, HB, 256]),
                                                                       op=ALU.mult), reads=[K(ycur), bgk], writes=[K('UW')])
                            UW = T['UW']
                            for h in range(HB):
                                self.tr(PS[3][:, h * 128:(h + 1) * 128], UW[:, h, 128:256], identf[:], [K('UW'), 'identf'], [PK[3]])
                            kb.op('act', lambda e: e.copy(out=T['wT'][:], in_=PS[3][:]), reads=[PK[3]], writes=[K('wT')])
                            wT4, qd4, QK4, kd4, vn4 = v4(T['wT']), v4(T['qd']), v4(T['QKm']), v4(T['kdec']), v4(T['vn'])
                            for c in ([0, 1] if d == 0 else [1, 0]):
                                rw = slice(64 * c, 64 * c + 64)
                                for h in range(HB):
                                    self.mm(PS[0][rw, h * 128:(h + 1) * 128], wT4[:, h, rw], St[:, h, :], True, True, [K('wT'), Sk], [PK[0]])
                                kb.op('dve', lambda e: e.tensor_tensor(out=vn4[rw], in0=UW[rw, :, 0:128],
                                                                       in1=PS[0][rw, :].rearrange("p (h c) -> p h c", h=HB), op=ALU.subtract),
                                      reads=[K('UW'), PK[0]], writes=[K('vn')])
                                for h in range(HB):
                                    self.mm(PS[1][rw, h * 128:(h + 1) * 128], qd4[:, h, rw], St[:, h, :], True, False, [K('qd'), Sk], [PK[1]])
                                    self.mm(PS[1][rw, h * 128:(h + 1) * 128], QK4[rw, h, rw], vn4[rw, h, :], False, True, [K('QKm'), K('vn')], [PK[1]])
                                kb.op('act', lambda e: e.copy(out=os_[rw, h0 * 128:(h0 + HB) * 128], in_=PS[1][rw, :]), reads=[PK[1]], writes=[osk])
                                for h in range(HB):
                                    self.mm(PS[2][:, h * 128:(h + 1) * 128], kd4[rw, h, :], vn4[rw, h, :], True, True, [K('kdec'), K('vn')], [PK[2]])
                                ia = (63 + 64 * c) if d == 0 else (64 * c)
                                kb.op('dve', lambda e: e.tensor_tensor(out=St[:], in0=St[:], in1=v4(T['EG'])[:, :, ia].unsqueeze(2).to_broadcast([128, HB, 128]),
                                                                       op=ALU.mult), reads=[Sk, K('EG')], writes=[Sk])
                                kb.op('dve', lambda e: e.tensor_tensor(out=St[:].rearrange("p h c -> p (h c)"), in0=St[:].rearrange("p h c -> p (h c)"),
                                                                       in1=PS[2][:], op=ALU.add), reads=[Sk, PK[2]], writes=[Sk])
                        kb.dma('pool', self.g_odir[d, tok0:tok0 + 128, :], os_[:], reads=[osk], writes=['g_odir_%d_%d' % (d, tok0)])

    def gdn_finish(self, li, jx, layer0, want_ctx):
        kb, NB = self.kb, self.NB
        with self.phase():
            o = self.outproj_setup(self.gdn_w_out[jx], li)
            NG = self.sb("gng", [128, 128])
            kb.dma('sp', NG[:], self.gdn_norm_g[jx:jx + 1, :].to_broadcast([128, 128]), writes=['gng'])
            self.rec_finish(o, NG, 'gng', self.g_odir, self.g_sgate, layer0, want_ctx)

    def rec_finish(self, o, NG, ngk, odir, sgate, layer0, want_ctx):
        kb, NB = self.kb, self.NB
        oa = [self.sb("foa%d" % i, [128, D]) for i in range(2)]
        obt = [self.sb("fob%d" % i, [128, D]) for i in range(2)]
        sg = [self.sb("fsg%d" % i, [128, D]) for i in range(2)]
        sq = self.sb("fsq", [128, D])
        st = self.sb("fst", [128, 4, 8])
        ob = [self.sb("fobb%d" % i, [128, D], BF16) for i in range(2)]
        it = 0
        pend = []
        for b in range(NB):
            for j in range(NT):
                if j < NTC and not want_ctx:
                    continue
                i = it % 2
                it += 1
                tok0 = b * TOK + j * 128
                kb.dma('sp', oa[i][:], odir[0, tok0:tok0 + 128, :], writes=['foa%d' % i])
                kb.dma('sp', obt[i][:], odir[1, tok0:tok0 + 128, :], writes=['fob%d' % i])
                kb.dma('sp', sg[i][:], sgate[tok0:tok0 + 128, :], writes=['fsg%d' % i])
                kb.op('dve', lambda e: e.tensor_tensor(out=oa[i][:], in0=oa[i][:], in1=obt[i][:], op=ALU.add),
                      reads=['foa%d' % i, 'fob%d' % i], writes=['foa%d' % i])
                kb.op('pool', lambda e: e.tensor_tensor(out=sq[:], in0=oa[i][:], in1=oa[i][:], op=ALU.mult), reads=['foa%d' % i], writes=['fsq'])
                kb.op('dve', lambda e: e.tensor_reduce(out=st[:, 0, :], in_=sq[:].rearrange("p (h d) -> p h d", h=8), axis=AX.X, op=ALU.add),
                      reads=['fsq'], writes=['fst'])
                kb.op('dve', lambda e: e.tensor_scalar(out=st[:, 1, :], in0=st[:, 0, :], scalar1=1.0 / 128, scalar2=EPS, op0=ALU.mult, op1=ALU.add),
                      reads=['fst'], writes=['fst'])
                kb.op('act', lambda e: e.activation(out=st[:, 2, :], in_=st[:, 1, :], func=AF.Sqrt), reads=['fst'], writes=['fst'])
                kb.op('dve', lambda e: e.reciprocal(out=st[:, 3, :], in_=st[:, 2, :]), reads=['fst'], writes=['fst'])
                o3 = oa[i][:].rearrange("p (h d) -> p h d", h=8)
                kb.op('dve', lambda e: e.tensor_tensor(out=o3, in0=o3, in1=st[:, 3, :].unsqueeze(2).to_broadcast([128, 8, 128]), op=ALU.mult),
                      reads=['foa%d' % i, 'fst'], writes=['foa%d' % i])
                kb.op('pool', lambda e: e.tensor_tensor(out=o3, in0=o3, in1=NG[:].unsqueeze(1).to_broadcast([128, 8, 128]), op=ALU.mult),
                      reads=['foa%d' % i, ngk], writes=['foa%d' % i])
                kb.op('dve', lambda e: e.tensor_tensor(out=ob[i][:], in0=oa[i][:], in1=sg[i][:], op=ALU.mult),
                      reads=['foa%d' % i, 'fsg%d' % i], writes=['fobb%d' % i])
                while pend:
                    pend.pop(0)()
                pend.append((lambda i_, b_, j_: (lambda: self.outproj_tile(o, ob[i_], 'fobb%d' % i_, layer0, b_, j_)))(i, b, j))
        while pend:
            pend.pop(0)()

    def mlstm_decl(self):
        self.gdn_decl()
        if hasattr(self, 'm_qkT'):
            return
        NB = self.NB
        self.m_qkT = self.dram("m_qkT", [2, 512, NB * TOK], F32)
        self.m_ktok = self.dram("m_ktok", [NB * TOK, 512], F32)

    def mlstm_phase(self, li, jx, layer0, want_ctx):
        self.mlstm_decl()
        self.mlstm_proj(li, jx)
        self.mlstm_scan(li, jx)
        kb = self.kb
        with self.phase():
            o = self.outproj_setup(self.mlstm_w_out[jx], li)
            NG = self.sb("mng", [128, 128])
            kb.dma('sp', NG[:], self.mlstm_norm_g[jx:jx + 1, :].to_broadcast([128, 128]), writes=['mng'])
            self.rec_finish(o, NG, 'mng', self.g_odir, self.g_sgate, layer0, want_ctx)

    def mlstm_proj(self, li, jx):
        kb, NB = self.kb, self.NB
        hv = self.hT_view()
        with self.phase():
            win = self.sb("mwin", [128, 8, 3104], BF16)
            wsrc = self.mlstm_w_in[jx].rearrange("(k p) c -> p k c", p=128)
            for k in range(8):
                kb.dma('pool', win[:, k, :], wsrc[:, k, :], writes=['mwin'])
            identf = self.sb("identf", [128, 128])
            kb.dma('sp', identf[:], self.c_ident, writes=['identf'])
            GB = self.sb("mgb", [128, 32])
            kb.dma('sp', GB[:], self.mlstm_gate_b[jx:jx + 1, :].to_broadcast([128, 32]), writes=['mgb'])
            h2 = [self.sb("mh%d" % i, [128, 8, 256], BF16) for i in range(2)]
            pz = [self.ps("mpz%d" % i, [128, 512]) for i in range(2)]
            pT = self.ps("mpT", [128, 2, 128])
            pg = [self.ps("mpg%d" % i, [128, 2, 512]) for i in range(2)]
            pb = self.ps("mpb", [128, 32])
            stg = [self.sb("mstg%d" % i, [128, 256]) for i in range(2)]
            kst = self.sb("mkst", [128, 2, 512])
            gst = [self.sb("mgst%d" % i, [128, D]) for i in range(4)]
            gls = self.sb("mgls", [128, 32])
            bt = self.sb("mbt", [128, 4, 16])
            ci = 0
            wi = 0
            gi = 0
            for b in range(NB):
                for w in range(NT // 2):
                    h = h2[wi % 2]
                    hk = 'mh%d' % (wi % 2)
                    wi += 1
                    c0 = self.hcol(b, 2 * w)
                    tok0 = b * TOK + 2 * w * 128
                    kb.dma('sp', h[:], hv[:, :, c0:c0 + 256], writes=[hk])
                    for f in range(8):
                        a = ci % 2
                        ci += 1
                        pzk = 'mpz%d' % a
                        for k in range(8):
                            self.mm(pz[a][:, 0:256], win[:, k, f * 128:(f + 1) * 128], h[:, k, :], k == 0, k == 7, ['mwin', hk], [pzk])
                        gk = 'mstg%d' % a
                        kb.op('act', lambda e: e.activation(out=stg[a][:], in_=pz[a][:, 0:256], func=AF.Identity, scale=(0.125 if f < 4 else 1.0)),
                              reads=[pzk], writes=[gk])
                        kb.dma('pool', self.m_qkT[f // 4, (f % 4) * 128:(f % 4 + 1) * 128, tok0:tok0 + 256], stg[a][:], reads=[gk],
                               writes=['m_qkT_%d' % ci])
                        if f >= 4:
                            for t2 in range(2):
                                self.tr(pT[:, t2, :], stg[a][:, t2 * 128:(t2 + 1) * 128], identf[:], [gk, 'identf'], ['mpT'])
                            kb.op('dve', lambda e: e.tensor_copy(out=kst[:, :, (f - 4) * 128:(f - 3) * 128], in_=pT[:]), reads=['mpT'], writes=['mkst'])
                    for t2 in range(2):
                        r0 = tok0 + t2 * 128
                        kb.dma('pool', self.m_ktok[r0:r0 + 128, :], kst[:, t2, :], reads=['mkst'], writes=['m_ktok_%d' % r0])
                        hs = h[:, :, t2 * 128:(t2 + 1) * 128]
                        for part in range(2):
                            p_ = pg[part]
                            pk = 'mpg%d' % part
                            for n in range(2):
                                for k in range(8):
                                    c1 = 1024 + part * 1024 + n * 512
                                    self.mm(p_[:, n, :], hs[:, k, :], win[:, k, c1:c1 + 512], k == 0, k == 7, [hk, 'mwin'], [pk])
                            g_ = gst[gi % 4]
                            gk2 = 'mgst%d' % (gi % 4)
                            gi += 1
                            if part == 0:
                                kb.op('dve', lambda e: e.tensor_copy(out=g_[:], in_=p_[:].rearrange("p a b -> p (a b)")), reads=[pk], writes=[gk2])
                                kb.dma('pool', self.g_vtok[r0:r0 + 128, :], g_[:], reads=[gk2], writes=['g_vtok_%d' % r0])
                            else:
                                kb.op('act', lambda e: e.activation(out=g_[:], in_=p_[:].rearrange("p a b -> p (a b)"), func=AF.Sigmoid),
                                      reads=[pk], writes=[gk2])
                                kb.dma('pool', self.g_sgate[r0:r0 + 128, :], g_[:], reads=[gk2], writes=['g_sgate_%d' % r0])
                        for k in range(8):
                            self.mm(pb[:], hs[:, k, :], win[:, k, 3072:3104], k == 0, k == 7, [hk, 'mwin'], ['mpb'])
                        x4 = bt[:, 0:2, :].rearrange("p a c -> p (a c)")
                        kb.op('dve', lambda e: e.tensor_tensor(out=x4, in0=pb[:], in1=GB[:], op=ALU.add), reads=['mpb', 'mgb'], writes=['mbt'])
                        x5 = x4.rearrange("p (d t h) -> p d t h", d=2, t=2)
                        kb.op('pool', lambda e: e.tensor_copy(out=gls[:, 0:16].rearrange("p (d h) -> p d h", d=2), in_=x5[:, :, 0, :]),
                              reads=['mbt'], writes=['mgls'])
                        xf = x5[:, :, 1, :]
                        t3 = lambda i_: bt[:, i_, :].rearrange("p (d h) -> p d h", d=2)
                        kb.op('dve', lambda e: e.scalar_tensor_tensor(out=t3(2), in0=xf, scalar=-1.0, in1=xf, op0=ALU.mult, op1=ALU.max),
                              reads=['mbt'], writes=['mbt'])
                        kb.op('act', lambda e: e.activation(out=bt[:, 2, :], in_=bt[:, 2, :], func=AF.Exp, scale=-1.0), reads=['mbt'], writes=['mbt'])
                        kb.op('act', lambda e: e.activation(out=bt[:, 3, :], in_=bt[:, 2, :], func=AF.Ln, bias=1.0), reads=['mbt'], writes=['mbt'])
                        kb.op('dve', lambda e: e.scalar_tensor_tensor(out=gls[:, 16:32].rearrange("p (d h) -> p d h", d=2), in0=xf, scalar=0.0,
                                                                      in1=t3(3), op0=ALU.min, op1=ALU.subtract), reads=['mbt'], writes=['mgls'])
                        kb.dma('pool', self.g_bg[r0:r0 + 128, :], gls[:], reads=['mgls'], writes=['g_bg_%d' % r0])

    def mlstm_scan(self, li, jx):
        kb, NB = self.kb, self.NB
        HB = 4
        qkTv = self.m_qkT.rearrange("q (h p) c -> q p h c", p=64)
        order = [list(range(NT)), [1, 0] + list(range(NT - 1, NTC - 1, -1))]
        with self.phase():
            MS = self.sb("mmask", [128, 13, 128])
            for m0 in range(0, 13, 4):
                m1 = min(13, m0 + 4)
                kb.dma('sp', MS[:, m0:m1, :], self.c_masks[m0:m1].rearrange("m p c -> p m c"), writes=['mmask'])
            identf = self.sb("identf", [128, 128])
            kb.dma('sp', identf[:], self.c_ident, writes=['identf'])
            onesf = self.sb("onesf", [128, 128])
            kb.op('dve', lambda e: e.memset(onesf[:], 1.0), writes=['onesf'])
            PS = [self.ps("mps%d" % i, [128, 512]) for i in range(8)]
            PK = ['mps%d' % i for i in range(8)]

            def v4(t):
                return t[:].rearrange("p (h c) -> p h c", h=HB)
            sets = []
            for si in range(2):
                T = {}
                for nm in ('LF', 'Dig', 'DL', 'P', 'PT'):
                    T[nm] = self.sb("m%s%d" % (nm, si), [128, HB * 128])
                T['qT'] = self.sb("mqT%d" % si, [64, HB, 128])
                T['kT'] = self.sb("mkT%d" % si, [64, HB, 128])
                T['ktok'] = self.sb("mktok%d" % si, [128, HB, 64])
                T['kw'] = self.sb("mkw%d" % si, [128, HB, 64])
                T['v1'] = self.sb("mv1%d" % si, [128, HB, 132])
                T['NI'] = self.sb("mNI%d" % si, [128, HB, 132])
                T['t1'] = self.sb("mt1%d" % si, [128, HB, 132])
                T['t2'] = self.sb("mt2%d" % si, [128, HB, 132])
                T['c'] = self.sb("mc%d" % si, [128, 16, HB])
                T['mloc'] = self.sb("mmloc%d" % si, [128, HB, 2])
                T['blast'] = self.sb("mblast%d" % si, [128, 2, HB])
                T['e3'] = self.sb("me3%d" % si, [128, 3, HB])
                T['fi'] = self.sb("mfi%d" % si, [128, 2, HB])
                T['k'] = 'm%d_' % si
                kb.op('pool', lambda e: e.memset(T['v1'][:, :, 128:129], 1.0), writes=[T['k'] + 'v1'])
                sets.append(T)
            glt = [self.sb("mgl%d" % i, [128, 32]) for i in range(4)]
            ost = [self.sb("most%d" % i, [128, D]) for i in range(4)]
            Cn = [[self.sb("mCn_%d_%d" % (d, hf), [64, HB, 132]) for hf in range(2)] for d in range(2)]
            Mm = [[self.sb("mM_%d_%d" % (d, hf), [128, HB]) for hf in range(2)] for d in range(2)]
            stepi = 0
            bi = 0
            for b in range(NB):
                for d in range(2):
                    for hf in range(2):
                        kb.op('pool', lambda e: e.memset(Cn[d][hf][:], 0.0), writes=['mCn_%d_%d' % (d, hf)])
                        kb.op('pool', lambda e: e.memset(Mm[d][hf][:], 0.0), writes=['mM_%d_%d' % (d, hf)])
                for n in range(NT):
                    for d in range(2):
                        j = order[d][n]
                        tok0 = b * TOK + j * 128
                        gl = glt[bi % 4]
                        glk = 'mgl%d' % (bi % 4)
                        os_ = ost[bi % 4]
                        osk = 'most%d' % (bi % 4)
                        bi += 1
                        kb.dma('sp', gl[:], self.g_bg[tok0:tok0 + 128, :], writes=[glk])
                        Ud, nUd, BO, BmU, NEG = MS[:, d, :], MS[:, 4 + d, :], MS[:, 6, :], MS[:, 9 + d, :], MS[:, 11 + d, :]
                        for hf in range(2):
                            T = sets[stepi % 2]
                            K = lambda nm: T['k'] + nm
                            stepi += 1
                            h0 = hf * HB
                            Ck, Mk = 'mCn_%d_%d' % (d, hf), 'mM_%d_%d' % (d, hf)
                            Ct, Mt = Cn[d][hf], Mm[d][hf]
                            c = T['c']
                            kb.dma('sp', T['qT'][:], qkTv[0, :, h0:h0 + HB, tok0:tok0 + 128], writes=[K('qT')])
                            kb.dma('sp', T['kT'][:], qkTv[1, :, h0:h0 + HB, tok0:tok0 + 128], writes=[K('kT')])
                            kb.dma('sp', T['ktok'][:], self.m_ktok[tok0:tok0 + 128, h0 * 64:(h0 + HB) * 64].rearrange("p (h c) -> p h c", h=HB),
                                   writes=[K('ktok')])
                            kb.dma('sp', T['v1'][:, :, 0:128], self.g_vtok[tok0:tok0 + 128, h0 * 128:(h0 + HB) * 128].rearrange("p (h c) -> p h c", h=HB),
                                   writes=[K('v1')])
                            ig4 = gl[:, d * 8 + h0:d * 8 + h0 + HB]
                            lf4 = gl[:, 16 + d * 8 + h0:16 + d * 8 + h0 + HB]
                            bc4 = lambda ap: ap.unsqueeze(2).to_broadcast([128, HB, 128])
                            mk4 = lambda ap: ap.unsqueeze(1).to_broadcast([128, HB, 128])
                            kb.op('dve', lambda e: e.tensor_copy(out=v4(T['LF']), in_=bc4(lf4)), reads=[glk], writes=[K('LF')])
                            kb.op('pool', lambda e: e.tensor_tensor(out=v4(T['Dig']), in0=mk4(identf[:]), in1=bc4(ig4), op=ALU.mult),
                                  reads=[glk, 'identf'], writes=[K('Dig')])
                            LF, Dig = v4(T['LF']), v4(T['Dig'])
                            for h in range(HB):
                                o_ = PS[0][:, h * 128:(h + 1) * 128]
                                self.mm(o_, Ud, LF[:, h, :], True, False, ['mmask', K('LF')], [PK[0]])
                                self.mm(o_, LF[:, h, :], nUd, False, False, ['mmask', K('LF')], [PK[0]])
                                self.mm(o_, onesf[:], Dig[:, h, :], False, True, ['onesf', K('Dig')], [PK[0]])
                            for h in range(HB):
                                o_ = PS[1][:, h * 128:(h + 1) * 128]
                                self.mm(o_, LF[:, h, :], BmU, True, False, ['mmask', K('LF')], [PK[1]])
                                self.mm(o_, onesf[:], Dig[:, h, :], False, True, ['onesf', K('Dig')], [PK[1]])
                            self.mm(PS[4][:, 0:HB], Ud, lf4, True, True, ['mmask', glk], [PK[4]])
                            self.mm(PS[4][:, HB:2 * HB], BO, lf4, True, True, ['mmask', glk], [PK[4]])
                            for cc in range(2):
                                self.mm(PS[4][:, 8 + cc * HB:8 + (cc + 1) * HB], MS[:, 7 + cc, :], lf4, True, True, ['mmask', glk], [PK[4]])
                            for h in range(HB):
                                self.mm(PS[2][:, h * 128:(h + 1) * 128], T['qT'][:, h, :], T['kT'][:, h, :], True, True, [K('qT'), K('kT')], [PK[2]])
                            kb.op('act', lambda e: e.copy(out=c[:, 0:2, :].rearrange("p a h -> p (a h)"), in_=PS[4][:, 0:2 * HB]), reads=[PK[4]], writes=[K('c')])
                            kb.op('act', lambda e: e.copy(out=T['blast'][:].rearrange("p a h -> p (a h)"), in_=PS[4][:, 8:8 + 2 * HB]),
                                  reads=[PK[4]], writes=[K('blast')])
                            kb.op('dve', lambda e: e.tensor_tensor(out=v4(T['DL']), in0=PS[0][:].rearrange("p (h c) -> p h c", h=HB), in1=mk4(NEG), op=ALU.add),
                                  reads=[PK[0], 'mmask'], writes=[K('DL')])
                            kb.op('dve', lambda e: e.tensor_reduce(out=c[:, 2, :], in_=v4(T['DL']), axis=AX.X, op=ALU.max), reads=[K('DL')], writes=[K('c')])
                            kb.op('dve', lambda e: e.tensor_tensor(out=v4(T['DL']), in0=v4(T['DL']), in1=bc4(c[:, 2, :]), op=ALU.subtract),
                                  reads=[K('DL'), K('c')], writes=[K('DL')])
                            kb.op('act', lambda e: e.activation(out=T['P'][:], in_=T['DL'][:], func=AF.Exp), reads=[K('DL')], writes=[K('P')])
                            kb.op('dve', lambda e: e.tensor_tensor(out=T['P'][:], in0=T['P'][:], in1=PS[2][:], op=ALU.mult), reads=[K('P'), PK[2]], writes=[K('P')])
                            P4 = v4(T['P'])
                            for h in range(HB):
                                self.tr(PS[3][:, h * 128:(h + 1) * 128], P4[:, h, :], identf[:], [K('P'), 'identf'], [PK[3]])
                            kb.op('act', lambda e: e.copy(out=T['PT'][:], in_=PS[3][:]), reads=[PK[3]], writes=[K('PT')])
                            PT4 = v4(T['PT'])
                            for h in range(HB):
                                self.mm(PS[6 + h // 2][:, (h % 2) * 129:(h % 2) * 129 + 129], PT4[:, h, :], T['v1'][:, h, 0:129], True, True,
                                        [K('PT'), K('v1')], [PK[6 + h // 2]])
                            for q in range(2):
                                kb.op('act' if q == 0 else 'dve',
                                      (lambda e: e.copy(out=T['NI'][:, 0:2, 0:129], in_=PS[6][:, 0:258].rearrange("p (h c) -> p h c", h=2))) if q == 0 else
                                      (lambda e: e.tensor_copy(out=T['NI'][:, 2:4, 0:129], in_=PS[7][:, 0:258].rearrange("p (h c) -> p h c", h=2))),
                                      reads=[PK[6 + q]], writes=[K('NI')])
                            kb.op('dve', lambda e: e.tensor_reduce(out=T['mloc'][:], in_=PS[1][:].rearrange("p (h c j) -> p h c j", h=HB, c=2), axis=AX.X, op=ALU.max),
                                  reads=[PK[1]], writes=[K('mloc')])
                            kb.op('dve', lambda e: e.tensor_tensor(out=c[:, 3, :], in0=c[:, 1, :], in1=c[:, 0, :], op=ALU.subtract), reads=[K('c')], writes=[K('c')])
                            kb.op('dve', lambda e: e.tensor_tensor(out=c[:, 3, :], in0=c[:, 3, :], in1=ig4, op=ALU.add), reads=[K('c'), glk], writes=[K('c')])
                            for cc in range(2):
                                rw = slice(64 * cc, 64 * cc + 64)
                                kb.op('dve', lambda e: e.tensor_tensor(out=c[rw, 4, :], in0=c[rw, 3, :], in1=T['mloc'][rw, :, cc], op=ALU.subtract),
                                      reads=[K('c'), K('mloc')], writes=[K('c')])
                            kb.op('act', lambda e: e.activation(out=c[:, 5, :], in_=c[:, 4, :], func=AF.Exp), reads=[K('c')], writes=[K('c')])
                            kb.op('pool', lambda e: e.tensor_tensor(out=T['kw'][:], in0=T['ktok'][:], in1=c[:, 5, :].unsqueeze(2).to_broadcast([128, HB, 64]), op=ALU.mult),
                                  reads=[K('ktok'), K('c')], writes=[K('kw')])
                            for cc in ([0, 1] if d == 0 else [1, 0]):
                                rw = slice(64 * cc, 64 * cc + 64)
                                for h in range(HB):
                                    self.mm(PS[h // 2][rw, (h % 2) * 129:(h % 2) * 129 + 129], T['qT'][:, h, rw], Ct[:, h, 0:129], True, True,
                                            [K('qT'), Ck], [PK[h // 2]])
                                kb.op('dve', lambda e: e.tensor_tensor(out=c[rw, 6, :], in0=c[rw, 0, :], in1=Mt[rw, :], op=ALU.add), reads=[K('c'), Mk], writes=[K('c')])
                                kb.op('dve', lambda e: e.tensor_tensor(out=c[rw, 7, :], in0=c[rw, 2, :], in1=c[rw, 6, :], op=ALU.max), reads=[K('c')], writes=[K('c')])
                                e3 = T['e3']
                                kb.op('dve', lambda e: e.tensor_tensor(out=e3[rw, 0, :], in0=c[rw, 6, :], in1=c[rw, 7, :], op=ALU.subtract), reads=[K('c')], writes=[K('e3')])
                                kb.op('dve', lambda e: e.tensor_tensor(out=e3[rw, 1, :], in0=c[rw, 2, :], in1=c[rw, 7, :], op=ALU.subtract), reads=[K('c')], writes=[K('e3')])
                                kb.op('dve', lambda e: e.tensor_scalar(out=e3[rw, 2, :], in0=c[rw, 7, :], scalar1=-1.0, scalar2=None, op0=ALU.mult), reads=[K('c')], writes=[K('e3')])
                                kb.op('act', lambda e: e.activation(out=e3[rw, :, :], in_=e3[rw, :, :], func=AF.Exp), reads=[K('e3')], writes=[K('e3')])
                                for q in range(2):
                                    kb.op('dve', lambda e: e.tensor_tensor(out=T['t1'][rw, 2 * q:2 * q + 2, 0:129], in0=PS[q][rw, 0:258].rearrange("p (h c) -> p h c", h=2),
                                                                           in1=e3[rw, 0, 2 * q:2 * q + 2].unsqueeze(2).to_broadcast([64, 2, 129]), op=ALU.mult),
                                          reads=[PK[q], K('e3')], writes=[K('t1')])
                                kb.op('pool', lambda e: e.tensor_tensor(out=T['t2'][rw, :, 0:129], in0=T['NI'][rw, :, 0:129],
                                                                        in1=e3[rw, 1, :].unsqueeze(2).to_broadcast([64, HB, 129]), op=ALU.mult),
                                      reads=[K('NI'), K('e3')], writes=[K('t2')])
                                kb.op('dve', lambda e: e.tensor_tensor(out=T['t1'][rw, :, 0:129], in0=T['t1'][rw, :, 0:129], in1=T['t2'][rw, :, 0:129], op=ALU.add),
                                      reads=[K('t1'), K('t2')], writes=[K('t1')])
                                den = T['t1'][rw, :, 128]
                                kb.op('dve', lambda e: e.scalar_tensor_tensor(out=c[rw, 8, :], in0=den, scalar=-1.0, in1=den, op0=ALU.mult, op1=ALU.max),
                                      reads=[K('t1')], writes=[K('c')])
                                kb.op('dve', lambda e: e.tensor_tensor(out=c[rw, 9, :], in0=c[rw, 8, :], in1=e3[rw, 2, :], op=ALU.max), reads=[K('c'), K('e3')], writes=[K('c')])
                                kb.op('dve', lambda e: e.reciprocal(out=c[rw, 10, :], in_=c[rw, 9, :]), reads=[K('c')], writes=[K('c')])
                                kb.op('dve', lambda e: e.tensor_tensor(out=os_[rw, h0 * 128:(h0 + HB) * 128].rearrange("p (h c) -> p h c", h=HB), in0=T['t1'][rw, :, 0:128],
                                                                       in1=c[rw, 10, :].unsqueeze(2).to_broadcast([64, HB, 128]), op=ALU.mult),
                                      reads=[K('t1'), K('c')], writes=[osk])
                                for h in range(HB):
                                    self.mm(PS[2 + h // 2][0:64, (h % 2) * 129:(h % 2) * 129 + 129], T['kw'][rw, h, :], T['v1'][rw, h, 0:129], True, True,
                                            [K('kw'), K('v1')], [PK[2 + h // 2]])
                                fi = T['fi']
                                kb.op('dve', lambda e: e.tensor_tensor(out=c[:, 11, :], in0=T['blast'][:, cc, :], in1=Mt[:], op=ALU.add), reads=[K('blast'), Mk], writes=[K('c')])
                                kb.op('dve', lambda e: e.tensor_tensor(out=c[:, 12, :], in0=c[:, 11, :], in1=T['mloc'][:, :, cc], op=ALU.max), reads=[K('c'), K('mloc')], writes=[K('c')])
                                kb.op('dve', lambda e: e.tensor_tensor(out=fi[:, 0, :], in0=c[:, 11, :], in1=c[:, 12, :], op=ALU.subtract), reads=[K('c')], writes=[K('fi')])
                                kb.op('dve', lambda e: e.tensor_tensor(out=fi[:, 1, :], in0=T['mloc'][:, :, cc], in1=c[:, 12, :], op=ALU.subtract), reads=[K('c'), K('mloc')], writes=[K('fi')])
                                kb.op('act', lambda e: e.activation(out=fi[:], in_=fi[:], func=AF.Exp), reads=[K('fi')], writes=[K('fi')])
                                kb.op('dve', lambda e: e.tensor_copy(out=Mt[:], in_=c[:, 12, :]), reads=[K('c')], writes=[Mk])
                                kb.op('dve', lambda e: e.tensor_tensor(out=Ct[:, :, 0:129], in0=Ct[:, :, 0:129], in1=fi[0:64, 0, :].unsqueeze(2).to_broadcast([64, HB, 129]), op=ALU.mult),
                                      reads=[Ck, K('fi')], writes=[Ck])
                                for q in range(2):
                                    kb.op('dve', lambda e: e.tensor_tensor(out=T['t2'][0:64, 2 * q:2 * q + 2, 0:129], in0=PS[2 + q][0:64, 0:258].rearrange("p (h c) -> p h c", h=2),
                                                                           in1=fi[0:64, 1, 2 * q:2 * q + 2].unsqueeze(2).to_broadcast([64, 2, 129]), op=ALU.mult),
                                          reads=[PK[2 + q], K('fi'), K('t2')], writes=[K('t2')])
                                kb.op('dve', lambda e: e.tensor_tensor(out=Ct[:, :, 0:129], in0=Ct[:, :, 0:129], in1=T['t2'][0:64, :, 0:129], op=ALU.add),
                                      reads=[Ck, K('t2')], writes=[Ck])
                        kb.dma('pool', self.g_odir[d, tok0:tok0 + 128, :], os_[:], reads=[osk], writes=['g_odir_%d_%d' % (d, tok0)])

    def build(self):
        self.declare()
        self.setup()
        cnt = {0: 0, 1: 0, 2: 0}
        for li, kind in enumerate(self.kinds):
            last = self.last_flags[li]
            layer0 = (li == 0)
            jx = cnt[kind]
            cnt[kind] += 1
            self.mod_phase(li)
            self.norm_phase(li, 1, layer0)
            if kind == 2:
                self.attn_phase(li, jx, layer0, not last)
            elif kind == 0:
                self.gdn_phase(li, jx, layer0, not last)
            else:
                self.mlstm_phase(li, jx, layer0, not last)
            self.norm_phase(li, 2, False, ctx_needed=not last)
            self.ffn_phase(li, ctx_needed=not last)
        self.final_phase()
        self.kb.barrier()
        self.kb.close()


def host_consts():
    ident = np.eye(128, dtype=np.float32)
    n_pair = 32
    inv = (10000.0 ** (-np.arange(n_pair, dtype=np.float32) / n_pair)).astype(np.float32)
    pos = np.arange(LAT)
    row = (pos // 64).astype(np.float32)
    col = (pos % 64).astype(np.float32)
    ang = np.concatenate([row[:, None] * inv, col[:, None] * inv], axis=-1).astype(np.float32)
    rope = np.concatenate([np.cos(ang), np.sin(ang)], axis=-1).astype(np.float32)
    masks = np.zeros((16, 128, 128), np.float32)
    t = np.arange(128)
    same = (t[:, None] // 64) == (t[None, :] // 64)
    uf = (same & (t[:, None] <= t[None, :])).astype(np.float32)
    ub = (same & (t[:, None] >= t[None, :])).astype(np.float32)
    masks[0], masks[1] = uf, ub
    masks[2], masks[3] = uf - np.eye(128, dtype=np.float32), ub - np.eye(128, dtype=np.float32)
    masks[4], masks[5] = -uf, -ub
    masks[6] = same.astype(np.float32)
    masks[7] = np.repeat((t < 64).astype(np.float32)[:, None], 128, axis=1)
    masks[8] = np.repeat((t >= 64).astype(np.float32)[:, None], 128, axis=1)
    masks[9], masks[10] = masks[6] - uf, masks[6] - ub
    masks[11] = (1.0 - ub) * np.float32(-1e30)
    masks[12] = (1.0 - uf) * np.float32(-1e30)
    return {"c_ident": ident, "c_rope": rope, "c_masks": masks}


def make_in_maps(inputs, NB, n_cores, kinds):
    f = lambda a: np.ascontiguousarray(np.asarray(a, dtype=np.float32))
    consts = host_consts()
    shared = {}
    for k in ("norm1_g", "norm2_g", "w_mod", "b_mod", "ffn_w_in", "ffn_conv_w", "ffn_conv_b", "ffn_w_out",
              "gdn_w_in", "gdn_conv_w", "gdn_norm_g", "gdn_w_out", "mlstm_w_in", "mlstm_norm_g", "mlstm_w_out",
              "attn_w_in", "attn_q_norm_g", "attn_k_norm_g", "attn_w_out"):
        shared[k] = f(inputs[k])
    shared["gdn_a_log"] = f(inputs["gdn_a_log"]).reshape(-1, 16)
    shared["gdn_dt_bias"] = f(inputs["gdn_dt_bias"]).reshape(-1, 16)
    shared["mlstm_gate_b"] = f(inputs["mlstm_gate_b"]).reshape(-1, 32)
    shared["final_norm_g"] = f(inputs["final_norm_g"]).reshape(1, D)
    shared.update(consts)
    x, c, ctx, c_ctx = f(inputs["x"]), f(inputs["c"]), f(inputs["ctx"]), f(inputs["c_ctx"])
    maps = []
    for i in range(n_cores):
        m = dict(shared)
        m["x"] = x[i * NB:(i + 1) * NB]
        m["ctx"] = ctx[i * NB:(i + 1) * NB]
        m["cvec"] = np.ascontiguousarray(np.concatenate([c[i * NB:(i + 1) * NB], c_ctx[None, :]], axis=0))
        maps.append(m)
    return maps


def kernel(**inputs):
    NB = 2
    n_cores = 8
    nc = bass.Bass("TRN2", target_bir_lowering=False)
    mk = MK(nc, NB, KINDS, [False, False, False, True])
    mk.build()
    maps = make_in_maps(inputs, NB, n_cores, KINDS)
    res = run_bass_kernel_spmd(nc, maps, core_ids=list(range(n_cores)))
    return np.concatenate([r["out"] for r in res.results], axis=0).astype(np.float32)
```

```python
import contextlib
import math
import numpy as np
import concourse.bass as bass
import concourse.mybir as mybir
from concourse.bass_utils import run_bass_kernel_spmd

F32 = mybir.dt.float32
F32R = mybir.dt.float32r
BF16 = mybir.dt.bfloat16
AF = mybir.ActivationFunctionType
ALU = mybir.AluOpType
AX = mybir.AxisListType

D = 1024
LAT = 4096
CTXL = 256
NTC = 2
NTL = 32
NT = 34
TOK = NT * 128
FFN = 2816
EPS = 1e-6
HALO = 2
REG_C = CTXL + 2 * HALO
REG_L = LAT + 2 * HALO
REG = REG_C + REG_L
KINDS = [0, 1, 2, 0]


class KB:
    def __init__(self, nc, ring=6, same_engine_sync=True):
        self.nc = nc
        self.eng = {'pe': nc.tensor, 'dve': nc.vector, 'act': nc.scalar,
                    'pool': nc.gpsimd, 'sp': nc.sync}
        self.same_engine_sync = same_engine_sync
        self.sem = {}
        self.cnt = {}
        self.seen = {e: {} for e in self.eng}
        self.res = {}
        self._ctx = []
        for e in ('pe', 'dve', 'act', 'pool'):
            self._mksem('c_' + e)
        self.rings = {}
        for q in ('sp', 'act', 'pool'):
            names = []
            for i in range(ring):
                n = 'd_%s_%d' % (q, i)
                self._mksem(n)
                names.append(n)
            self.rings[q] = [names, 0]
        self.n_inst = 0
        self.n_wait = 0

    def _mksem(self, name):
        cm = self.nc.semaphore(name)
        h = cm.__enter__()
        self._ctx.append(cm)
        self.sem[name] = h
        self.cnt[name] = 0

    def close(self):
        for cm in reversed(self._ctx):
            cm.__exit__(None, None, None)
        self._ctx = []

    def _R(self, key):
        r = self.res.get(key)
        if r is None:
            r = {'w': None, 'r': {}}
            self.res[key] = r
        return r

    def _deps(self, reads, writes):
        deps = {}

        def add(tok):
            if tok is None:
                return
            s, v = tok
            if deps.get(s, 0) < v:
                deps[s] = v
        for k in reads:
            add(self._R(k)['w'])
        for k in writes:
            r = self._R(k)
            add(r['w'])
            for s, v in r['r'].items():
                add((s, v))
        return deps

    def _wait(self, e, deps, is_dma=False):
        own = 'c_' + e
        seen = self.seen[e]
        for s, v in deps.items():
            if s == own and not is_dma and (e == 'pe' or not self.same_engine_sync):
                continue
            if seen.get(s, 0) >= v:
                continue
            self.eng[e].wait_ge(self.sem[s], v)
            self.n_wait += 1
            seen[s] = v

    def _commit(self, tok, reads, writes):
        s, v = tok
        for k in reads:
            r = self._R(k)
            if r['r'].get(s, 0) < v:
                r['r'][s] = v
        for k in writes:
            r = self._R(k)
            r['w'] = tok
            r['r'] = {}

    def op(self, e, fn, reads=(), writes=()):
        deps = self._deps(reads, writes)
        self._wait(e, deps)
        inst = fn(self.eng[e])
        s = 'c_' + e
        self.cnt[s] += 1
        inst.then_inc(self.sem[s], 1)
        self.n_inst += 1
        self._commit((s, self.cnt[s]), reads, writes)
        return inst

    def dma(self, q, out, in_, reads=(), writes=(), **kw):
        names, idx = self.rings[q]
        s = names[idx % len(names)]
        self.rings[q][1] = idx + 1
        deps = self._deps(reads, writes)
        if self.cnt[s] > 0 and deps.get(s, 0) < self.cnt[s]:
            deps[s] = self.cnt[s]
        self._wait(q, deps, is_dma=True)
        inst = self.eng[q].dma_start(out=out, in_=in_, **kw)
        self.cnt[s] += 16
        inst.then_inc(self.sem[s], 16)
        self.n_inst += 1
        self._commit((s, self.cnt[s]), reads, writes)
        return inst

    def barrier(self):
        for e in self.eng:
            for s, v in self.cnt.items():
                if v > 0 and self.seen[e].get(s, 0) < v:
                    self.eng[e].wait_ge(self.sem[s], v)
                    self.seen[e][s] = v
        self.res = {}


class MK:
    def __init__(self, nc, NB, kinds, last_flags, debug=False):
        self.debug = debug
        self.nc = nc
        self.NB = NB
        self.kinds = kinds
        self.last_flags = last_flags
        self.kb = KB(nc)
        self.stack = None
        self.uid = 0

    @contextlib.contextmanager
    def phase(self):
        prev = self.stack
        with contextlib.ExitStack() as es:
            self.stack = es
            yield
            self.kb.barrier()
        self.stack = prev

    @contextlib.contextmanager
    def nosame(self):
        yield

    def sb(self, name, shape, dt=F32):
        self.uid += 1
        return self.stack.enter_context(self.nc.sbuf_tensor("%s_%d" % (name, self.uid), list(shape), dt))

    def ps(self, name, shape, dt=F32):
        self.uid += 1
        return self.stack.enter_context(self.nc.psum_tensor("%s_%d" % (name, self.uid), list(shape), dt))

    def dram(self, name, shape, dt, kind="Internal"):
        return self.nc.dram_tensor(name, list(shape), dt, kind=kind).ap()

    def mm(self, out, lhsT, rhs, start, stop, reads, writes):
        return self.kb.op('pe', lambda e: e.matmul(out, lhsT=lhsT, rhs=rhs, start=start, stop=stop),
                          reads=reads, writes=writes)

    def mmr(self, out, lhsT, rhs, start, stop, reads, writes):
        F32R = mybir.dt.float32r
        return self.kb.op('pe', lambda e: e.matmul(out, lhsT=lhsT.bitcast(F32R), rhs=rhs.bitcast(F32R), start=start, stop=stop),
                          reads=reads, writes=writes)

    def tr(self, out, in_, ident, reads, writes):
        return self.kb.op('pe', lambda e: e.transpose(out, in_, ident), reads=reads, writes=writes)

    def declare(self):
        NB = self.NB
        n0 = sum(1 for k in self.kinds if k == 0)
        n1 = sum(1 for k in self.kinds if k == 1)
        n2 = sum(1 for k in self.kinds if k == 2)
        DEPTH = len(self.kinds)
        I = lambda n, s: self.dram(n, s, F32, kind="ExternalInput")
        self.x = I("x", [NB, LAT, D])
        self.ctx = I("ctx", [NB, CTXL, D])
        self.cvec = I("cvec", [NB + 1, D])
        self.norm1_g = I("norm1_g", [DEPTH, D])
        self.norm2_g = I("norm2_g", [DEPTH, D])
        self.w_mod = I("w_mod", [DEPTH, D, 6 * D])
        self.b_mod = I("b_mod", [DEPTH, 6 * D])
        self.ffn_w_in = I("ffn_w_in", [DEPTH, D, 2 * FFN])
        self.ffn_conv_w = I("ffn_conv_w", [DEPTH, 3, FFN])
        self.ffn_conv_b = I("ffn_conv_b", [DEPTH, FFN])
        self.ffn_w_out = I("ffn_w_out", [DEPTH, FFN, D])
        self.gdn_w_in = I("gdn_w_in", [max(n0, 1), D, 4128])
        self.gdn_conv_w = I("gdn_conv_w", [max(n0, 1), 5, 3072])
        self.gdn_a_log = I("gdn_a_log", [max(n0, 1), 16])
        self.gdn_dt_bias = I("gdn_dt_bias", [max(n0, 1), 16])
        self.gdn_norm_g = I("gdn_norm_g", [max(n0, 1), 128])
        self.gdn_w_out = I("gdn_w_out", [max(n0, 1), D, D])
        self.mlstm_w_in = I("mlstm_w_in", [max(n1, 1), D, 3104])
        self.mlstm_gate_b = I("mlstm_gate_b", [max(n1, 1), 32])
        self.mlstm_norm_g = I("mlstm_norm_g", [max(n1, 1), 128])
        self.mlstm_w_out = I("mlstm_w_out", [max(n1, 1), D, D])
        self.attn_w_in = I("attn_w_in", [max(n2, 1), D, 1536])
        self.attn_q_norm_g = I("attn_q_norm_g", [max(n2, 1), 128])
        self.attn_k_norm_g = I("attn_k_norm_g", [max(n2, 1), 128])
        self.attn_w_out = I("attn_w_out", [max(n2, 1), D, D])
        self.final_norm_g = I("final_norm_g", [1, D])
        self.c_ident = I("c_ident", [128, 128])
        self.c_rope = I("c_rope", [LAT, 128])
        self.c_masks = I("c_masks", [16, 128, 128])
        self.out = self.dram("out", [NB, LAT, D], F32, kind="ExternalOutput")
        sk = "ExternalOutput" if self.debug else "Internal"
        self.xs = self.dram("xs", [NB, TOK, D], F32, kind=sk)
        self.hTs = self.dram("hTs", [D, NB * REG], BF16, kind=sk)
        self.modv = self.dram("modv", [DEPTH, NB + 1, 6, D], F32, kind=sk)

    def src_x(self, layer0, b, j):
        if layer0:
            if j < NTC:
                return self.ctx[b, j * 128:(j + 1) * 128, :], 'in_ctx'
            return self.x[b, (j - NTC) * 128:(j - NTC + 1) * 128, :], 'in_x'
        return self.xs[b, j * 128:(j + 1) * 128, :], 'xs_%d_%d' % (b, j)

    def hcol(self, b, j):
        base = b * REG
        if j < NTC:
            return base + HALO + j * 128
        return base + REG_C + HALO + (j - NTC) * 128

    def hT_view(self):
        return self.hTs.rearrange("(k p) c -> p k c", p=128)

    def setup(self):
        kb = self.kb
        with self.phase():
            z = self.sb("zero", [128, 8, 2 * HALO], BF16)
            kb.op('dve', lambda e: e.memset(z[:], 0.0), writes=['zero'])
            hv = self.hT_view()
            for b in range(self.NB):
                base = b * REG
                for c0 in (base, base + REG_C - HALO):
                    pass
                kb.dma('pool', hv[:, :, base:base + HALO], z[:, :, 0:HALO], reads=['zero'], writes=['hTs_halo'])
                kb.dma('pool', hv[:, :, base + REG_C - HALO:base + REG_C + HALO], z[:, :, :], reads=['zero'], writes=['hTs_halo'])
                kb.dma('pool', hv[:, :, base + REG - HALO:base + REG], z[:, :, 0:HALO], reads=['zero'], writes=['hTs_halo'])

    def mod_phase(self, li):
        kb, NB = self.kb, self.NB
        R = NB + 1
        with self.phase():
            cT = self.sb("cT", [128, 8, R])
            sT = self.sb("sT", [128, 8, R])
            ones = self.sb("ones", [1, 4])
            brow = self.sb("brow", [1, 6 * D])
            mrow = self.sb("mrow", [R, 6 * D])
            g12 = self.sb("g12", [R, 2, D])
            pm = [self.ps("pm%d" % i, [R, 512]) for i in range(2)]
            wm = [self.sb("wm%d" % i, [128, 8, 512]) for i in range(2)]
            with self.nc.allow_non_contiguous_dma(reason="tiny transposed load of conditioning vectors"):
                for r in range(R):
                    kb.dma('sp', cT[:, :, r], self.cvec[r, :].rearrange("(k p) -> p k", p=128), writes=['cT'])
            kb.op('act', lambda e: e.activation(out=sT[:], in_=cT[:], func=AF.Silu), reads=['cT'], writes=['sT'])
            kb.op('dve', lambda e: e.memset(ones[:], 1.0), writes=['ones'])
            kb.dma('sp', brow[:], self.b_mod[li:li + 1, :], writes=['brow'])
            kb.dma('sp', g12[:, 0, :], self.norm1_g[li:li + 1, :].to_broadcast([R, D]), writes=['g12'])
            kb.dma('sp', g12[:, 1, :], self.norm2_g[li:li + 1, :].to_broadcast([R, D]), writes=['g12'])
            for n in range(12):
                w = wm[n % 2]
                wk = 'wm%d' % (n % 2)
                pk = 'pm%d' % (n % 2)
                kb.dma('sp', w[:], self.w_mod[li, :, n * 512:(n + 1) * 512].rearrange("(k p) c -> p k c", p=128),
                       writes=[wk])
                for k in range(8):
                    self.mm(pm[n % 2][:], sT[:, k, :], w[:, k, :], k == 0, False, ['sT', wk], [pk])
                self.mm(pm[n % 2][:], ones[0:1, 0:R], brow[0:1, n * 512:(n + 1) * 512], False, True,
                        ['ones', 'brow'], [pk])
                kb.op('act', lambda e: e.copy(out=mrow[:, n * 512:(n + 1) * 512], in_=pm[n % 2][:]),
                      reads=[pk], writes=['mrow'])
            for (gi, sc) in ((0, 1), (1, 4)):
                kb.op('dve', lambda e: e.scalar_tensor_tensor(
                    out=mrow[:, sc * D:(sc + 1) * D], in0=mrow[:, sc * D:(sc + 1) * D], scalar=1.0,
                    in1=g12[:, gi, :], op0=ALU.add, op1=ALU.mult), reads=['mrow', 'g12'], writes=['mrow'])
            kb.dma('pool', self.modv[li].rearrange("r s d -> r (s d)"), mrow[:], reads=['mrow'], writes=['modv'])

    def load_bc(self, t, li, r, s, key):
        self.kb.dma('sp', t[:], self.modv[li, r, s:s + 1, :].to_broadcast([128, D]), reads=['modv'], writes=[key])

    def norm_phase(self, li, which, layer0, ctx_needed=True):
        kb, NB = self.kb, self.NB
        s_sh, s_g = (0, 1) if which == 1 else (3, 4)
        with self.phase():
            ident = self.sb("identb", [128, 128], BF16)
            kb.dma('pool', ident[:], self.c_ident, writes=['ident'])
            Gt = [self.sb("G%d" % r, [128, D]) for r in range(NB + 1)]
            St = [self.sb("S%d" % r, [128, D]) for r in range(NB + 1)]
            for r in range(NB + 1):
                self.load_bc(Gt[r], li, r, s_g, 'G%d' % r)
                self.load_bc(St[r], li, r, s_sh, 'S%d' % r)
            NBUF = 3
            xt = [self.sb("xt%d" % i, [128, D]) for i in range(NBUF)]
            sq = self.sb("sq", [128, D])
            st = [self.sb("st%d" % i, [128, 4]) for i in range(NBUF)]
            hb = [self.sb("hb%d" % i, [128, D], BF16) for i in range(NBUF)]
            pt = [self.ps("pt%d" % i, [128, 8, 128], BF16) for i in range(2)]
            hw = [self.sb("hw%d" % i, [128, 8, 256], BF16) for i in range(2)]
            hv = self.hT_view()
            it = 0
            wi = 0
            pending = []

            def make_b(i, itv, wiv, t2, b, w):
                def stage_b():
                    p = pt[itv % 2]
                    pk = 'pt%d' % (itv % 2)
                    hwk = 'hw%d' % (wiv % 2)
                    for k in range(8):
                        self.tr(p[:, k, :], hb[i][:, k * 128:(k + 1) * 128], ident[:], ['hb%d' % i, 'ident'], [pk])
                    kb.op('act', lambda e: e.copy(out=hw[wiv % 2][:, :, t2 * 128:(t2 + 1) * 128], in_=p[:]),
                          reads=[pk], writes=[hwk])
                    if t2 == 1:
                        c0 = self.hcol(b, 2 * w)
                        kb.dma('pool', hv[:, :, c0:c0 + 256], hw[wiv % 2][:], reads=[hwk], writes=['hTs_%d' % wiv])
                return stage_b

            for b in range(NB):
                for w in range(NT // 2):
                    if w == 0 and not ctx_needed:
                        continue
                    for t2 in range(2):
                        j = 2 * w + t2
                        r = NB if j < NTC else b
                        i = it % NBUF
                        src, skey = self.src_x(layer0, b, j)
                        kb.dma('sp', xt[i][:], src, reads=[skey], writes=['xt%d' % i])
                        kb.op('act', lambda e: e.activation(out=sq[:], in_=xt[i][:], func=AF.Square,
                                                            accum_out=st[i][:, 0:1]),
                              reads=['xt%d' % i], writes=['sq', 'st%d' % i])
                        kb.op('dve', lambda e: e.tensor_scalar(out=st[i][:, 1:2], in0=st[i][:, 0:1], scalar1=1.0 / D,
                                                               scalar2=EPS, op0=ALU.mult, op1=ALU.add),
                              reads=['st%d' % i], writes=['st%d' % i])
                        kb.op('act', lambda e: e.activation(out=st[i][:, 2:3], in_=st[i][:, 1:2], func=AF.Sqrt),
                              reads=['st%d' % i], writes=['st%d' % i])
                        kb.op('dve', lambda e: e.reciprocal(out=st[i][:, 3:4], in_=st[i][:, 2:3]),
                              reads=['st%d' % i], writes=['st%d' % i])
                        kb.op('dve', lambda e: e.scalar_tensor_tensor(out=xt[i][:], in0=xt[i][:], scalar=st[i][:, 3:4],
                                                                      in1=Gt[r][:], op0=ALU.mult, op1=ALU.mult),
                              reads=['xt%d' % i, 'st%d' % i, 'G%d' % r], writes=['xt%d' % i])
                        kb.op('pool', lambda e: e.tensor_tensor(out=hb[i][:], in0=xt[i][:], in1=St[r][:], op=ALU.add),
                              reads=['xt%d' % i, 'S%d' % r], writes=['hb%d' % i])
                        while pending:
                            pending.pop(0)()
                        pending.append(make_b(i, it, wi, t2, b, w))
                        it += 1
                    wi += 1
            while pending:
                pending.pop(0)()

    def outproj_setup(self, w_out_ap, li):
        kb, NB = self.kb, self.NB
        o = {}
        o['w'] = self.sb("wout", [128, 8, D], BF16)
        kb.dma('pool', o['w'][:], w_out_ap.rearrange("(k p) c -> p k c", p=128), writes=['wout'])
        o['ident'] = self.sb("identb", [128, 128], BF16)
        kb.dma('pool', o['ident'][:], self.c_ident, writes=['identb'])
        o['M'] = [self.sb("M2_%d" % r, [128, D]) for r in range(NB + 1)]
        for r in range(NB + 1):
            self.load_bc(o['M'][r], li, r, 2, 'M2_%d' % r)
        o['pt'] = self.ps("opt", [128, 8, 128], BF16)
        o['py'] = self.ps("opy", [128, 2, 512])
        o['oT'] = self.sb("oT", [128, 8, 128], BF16)
        o['xt'] = [self.sb("oxt%d" % i, [128, D]) for i in range(2)]
        o['n'] = 0
        return o

    def outproj_tile(self, o, ob, obkey, layer0, b, j):
        kb = self.kb
        r = self.NB if j < NTC else b
        for k in range(8):
            self.tr(o['pt'][:, k, :], ob[:, k * 128:(k + 1) * 128], o['ident'][:], [obkey, 'identb'], ['opt'])
        kb.op('act', lambda e: e.copy(out=o['oT'][:], in_=o['pt'][:]), reads=['opt'], writes=['oT'])
        for n in range(2):
            for k in range(8):
                self.mm(o['py'][:, n, :], o['oT'][:, k, :], o['w'][:, k, n * 512:(n + 1) * 512], k == 0, k == 7,
                        ['oT', 'wout'], ['opy'])
        i = o['n'] % 2
        o['n'] += 1
        xt = o['xt'][i]
        xk = 'oxt%d' % i
        src, skey = self.src_x(layer0, b, j)
        kb.dma('sp', xt[:], src, reads=[skey], writes=[xk])
        yk = 'oy%d' % i
        kb.op('dve', lambda e: e.tensor_tensor(out=o['py'][:].rearrange("p a b -> p (a b)"),
                                               in0=o['py'][:].rearrange("p a b -> p (a b)"),
                                               in1=o['M'][r][:], op=ALU.mult),
              reads=['opy', 'M2_%d' % r], writes=['opy'])
        kb.op('dve', lambda e: e.tensor_tensor(out=xt[:], in0=o['py'][:].rearrange("p a b -> p (a b)"), in1=xt[:],
                                               op=ALU.add),
              reads=['opy', xk], writes=[xk])
        kb.dma('pool', self.xs[b, j * 128:(j + 1) * 128, :], xt[:], reads=[xk], writes=['xs_%d_%d' % (b, j)])

    def ffn_phase(self, li, ctx_needed=True):
        kb, NB = self.kb, self.NB
        NF = FFN // 128
        HF = NF // 2
        hv = self.hT_view()
        for ps_ in range(2):
            with self.phase():
                f0 = ps_ * HF
                wv = self.sb("wv", [128, 8, HF * 128], BF16)
                wg = self.sb("wg", [128, 8, HF * 128], BF16)
                wo = self.sb("wo", [128, HF, D], BF16)
                win = self.ffn_w_in[li].rearrange("(k p) c -> p k c", p=128)
                for k in range(8):
                    kb.dma('pool', wv[:, k, :], win[:, k, f0 * 128:(f0 + HF) * 128], writes=['wv'])
                    kb.dma('pool', wg[:, k, :], win[:, k, FFN + f0 * 128:FFN + (f0 + HF) * 128], writes=['wg'])
                kb.dma('pool', wo[:], self.ffn_w_out[li, f0 * 128:(f0 + HF) * 128, :].rearrange("(f p) c -> p f c", p=128),
                       writes=['wo'])
                cw = self.sb("cw", [128, HF, 4])
                with self.nc.allow_non_contiguous_dma(reason="tiny per-channel conv taps"):
                    for t in range(3):
                        kb.dma('sp', cw[:, :, t], self.ffn_conv_w[li, t, f0 * 128:(f0 + HF) * 128].rearrange("(f p) -> p f", p=128),
                               writes=['cw'])
                    kb.dma('sp', cw[:, :, 3], self.ffn_conv_b[li, f0 * 128:(f0 + HF) * 128].rearrange("(f p) -> p f", p=128),
                           writes=['cw'])
                M5 = [self.sb("M5_%d" % r, [128, D]) for r in range(NB + 1)]
                for r in range(NB + 1):
                    self.load_bc(M5[r], li, r, 5, 'M5_%d' % r)
                hT = [self.sb("fh%d" % i, [128, 8, 256 + 2 * HALO], BF16) for i in range(2)]
                u = [self.sb("fu%d" % i, [128, HF, 256], BF16) for i in range(2)]
                tA = [self.sb("ftA%d" % i, [128, 256]) for i in range(2)]
                tB = [self.sb("ftB%d" % i, [128, 256]) for i in range(2)]
                pv = [self.ps("fpv%d" % i, [128, 512]) for i in range(2)]
                pg = [self.ps("fpg%d" % i, [128, 512]) for i in range(2)]
                py = [self.ps("fpy%d" % i, [128, 2, 512]) for i in range(2)]
                xt = [self.sb("fx%d" % i, [128, D]) for i in range(2)]
                wi = 0
                ci = 0
                ti = 0
                fpend = []
                for b in range(NB):
                    for w in range(NT // 2):
                        if w == 0 and not ctx_needed:
                            continue
                        h = hT[wi % 2]
                        hk = 'fh%d' % (wi % 2)
                        uu = u[wi % 2]
                        uk = 'fu%d' % (wi % 2)
                        c0 = self.hcol(b, 2 * w)
                        kb.dma('sp', h[:], hv[:, :, c0 - HALO:c0 + 256 + HALO], reads=['hTs'], writes=[hk])
                        with self.nosame():
                            for f in range(HF):
                                a = ci % 2
                                ci += 1
                                for k in range(8):
                                    self.mm(pv[a][:, 0:256], wv[:, k, f * 128:(f + 1) * 128], h[:, k, HALO:HALO + 256],
                                            k == 0, k == 7, ['wv', hk], ['fpv%d' % a])
                                for k in range(8):
                                    self.mm(pg[a][:, 0:258], wg[:, k, f * 128:(f + 1) * 128], h[:, k, HALO - 1:HALO + 257],
                                            k == 0, k == 7, ['wg', hk], ['fpg%d' % a])
                                kb.op('dve', lambda e: e.tensor_scalar(out=tA[a][:], in0=pg[a][:, 0:256], scalar1=cw[:, f, 0:1],
                                                                       scalar2=None, op0=ALU.mult),
                                      reads=['fpg%d' % a, 'cw'], writes=['ftA%d' % a])
                                kb.op('dve', lambda e: e.scalar_tensor_tensor(out=tA[a][:], in0=pg[a][:, 1:257], scalar=cw[:, f, 1:2],
                                                                              in1=tA[a][:], op0=ALU.mult, op1=ALU.add),
                                      reads=['fpg%d' % a, 'cw', 'ftA%d' % a], writes=['ftA%d' % a])
                                kb.op('dve', lambda e: e.scalar_tensor_tensor(out=tA[a][:], in0=pg[a][:, 2:258], scalar=cw[:, f, 2:3],
                                                                              in1=tA[a][:], op0=ALU.mult, op1=ALU.add),
                                      reads=['fpg%d' % a, 'cw', 'ftA%d' % a], writes=['ftA%d' % a])
                                kb.op('act', lambda e: e.activation(out=tB[a][:], in_=tA[a][:], func=AF.Silu, bias=cw[:, f, 3:4]),
                                      reads=['ftA%d' % a, 'cw'], writes=['ftB%d' % a])
                                kb.op('dve', lambda e: e.tensor_tensor(out=uu[:, f, :], in0=pv[a][:, 0:256], in1=tB[a][:], op=ALU.mult),
                                      reads=['fpv%d' % a, 'ftB%d' % a], writes=[uk])
                        def make_out(uu, uk, b, w):
                            def stage_out():
                                nonlocal ti
                                for t2 in range(2):
                                    j = 2 * w + t2
                                    r = NB if j < NTC else b
                                    i = ti % 2
                                    ti += 1
                                    for n in range(2):
                                        for f in range(HF):
                                            self.mm(py[i][:, n, :], uu[:, f, t2 * 128:(t2 + 1) * 128], wo[:, f, n * 512:(n + 1) * 512],
                                                    f == 0, f == HF - 1, [uk, 'wo'], ['fpy%d' % i])
                                    kb.dma('sp', xt[i][:], self.xs[b, j * 128:(j + 1) * 128, :], reads=['xs_%d_%d' % (b, j)], writes=['fx%d' % i])
                                    pyf = py[i][:].rearrange("p a b -> p (a b)")
                                    kb.op('dve', lambda e: e.tensor_tensor(out=pyf, in0=pyf, in1=M5[r][:], op=ALU.mult),
                                          reads=['fpy%d' % i, 'M5_%d' % r], writes=['fpy%d' % i])
                                    kb.op('dve', lambda e: e.tensor_tensor(out=xt[i][:], in0=pyf, in1=xt[i][:], op=ALU.add),
                                          reads=['fpy%d' % i, 'fx%d' % i], writes=['fx%d' % i])
                                    kb.dma('pool', self.xs[b, j * 128:(j + 1) * 128, :], xt[i][:], reads=['fx%d' % i],
                                           writes=['xs_%d_%d' % (b, j)])
                            return stage_out
                        fpend.append(make_out(uu, uk, b, w))
                        if len(fpend) > 1:
                            fpend.pop(0)()
                        wi += 1
                while fpend:
                    fpend.pop(0)()

    def final_phase(self):
        kb, NB = self.kb, self.NB
        with self.phase():
            G = self.sb("fG", [128, D])
            kb.dma('sp', G[:], self.final_norm_g[0:1, :].to_broadcast([128, D]), writes=['fG'])
            xt = [self.sb("fx%d" % i, [128, D]) for i in range(3)]
            sq = self.sb("fsq", [128, D])
            st = [self.sb("fst%d" % i, [128, 4]) for i in range(3)]
            it = 0
            for b in range(NB):
                for j in range(NTC, NT):
                    i = it % 3
                    it += 1
                    kb.dma('sp', xt[i][:], self.xs[b, j * 128:(j + 1) * 128, :], reads=['xs_%d_%d' % (b, j)], writes=['fx%d' % i])
                    kb.op('act', lambda e: e.activation(out=sq[:], in_=xt[i][:], func=AF.Square, accum_out=st[i][:, 0:1]),
                          reads=['fx%d' % i], writes=['fsq', 'fst%d' % i])
                    kb.op('dve', lambda e: e.tensor_scalar(out=st[i][:, 1:2], in0=st[i][:, 0:1], scalar1=1.0 / D,
                                                           scalar2=EPS, op0=ALU.mult, op1=ALU.add),
                          reads=['fst%d' % i], writes=['fst%d' % i])
                    kb.op('act', lambda e: e.activation(out=st[i][:, 2:3], in_=st[i][:, 1:2], func=AF.Sqrt),
                          reads=['fst%d' % i], writes=['fst%d' % i])
                    kb.op('dve', lambda e: e.reciprocal(out=st[i][:, 3:4], in_=st[i][:, 2:3]),
                          reads=['fst%d' % i], writes=['fst%d' % i])
                    kb.op('dve', lambda e: e.scalar_tensor_tensor(out=xt[i][:], in0=xt[i][:], scalar=st[i][:, 3:4],
                                                                  in1=G[:], op0=ALU.mult, op1=ALU.mult),
                          reads=['fx%d' % i, 'fst%d' % i, 'fG'], writes=['fx%d' % i])
                    kb.dma('pool', self.out[b, (j - NTC) * 128:(j - NTC + 1) * 128, :], xt[i][:], reads=['fx%d' % i],
                           writes=['out_%d_%d' % (b, j)])

    def attn_phase(self, li, jx, layer0, want_ctx):
        kb, NB = self.kb, self.NB
        hv = self.hT_view()
        SC = 128 ** -0.5
        for b in range(NB):
            with self.phase():
                qT = self.sb("qT", [128, 8, TOK], BF16)
                kT = self.sb("kT", [128, 2, TOK], BF16)
                V1 = self.sb("V1", [128, NT, 2, 132], BF16)
                kb.op('pool', lambda e: e.memset(V1[:, :, :, 128:129], 1.0), writes=['V1'])
                ident = self.sb("identb", [128, 128], BF16)
                kb.dma('pool', ident[:], self.c_ident, writes=['ident'])
                with self.phase():
                    win = self.sb("awin", [128, 8, 1536], BF16)
                    kb.dma('pool', win[:], self.attn_w_in[jx].rearrange("(k p) c -> p k c", p=128), writes=['awin'])
                    Gqk = self.sb("Gqk", [128, 2, 128])
                    kb.dma('sp', Gqk[:, 0, :], self.attn_q_norm_g[jx:jx + 1, :].to_broadcast([128, 128]), writes=['Gqk'])
                    kb.dma('sp', Gqk[:, 1, :], self.attn_k_norm_g[jx:jx + 1, :].to_broadcast([128, 128]), writes=['Gqk'])
                    hT = [self.sb("ah%d" % i, [128, 8, 128], BF16) for i in range(2)]
                    pz = [self.ps("apz%d" % i, [128, 512]) for i in range(3)]
                    zs = self.sb("azs", [128, 1536])
                    sq = self.sb("asq", [128, 1280])
                    st = self.sb("ast", [128, 4, 10])
                    qn = self.sb("aqn", [128, 1280])
                    qb = self.sb("aqb", [128, 1280], BF16)
                    rp = [self.sb("arp%d" % i, [128, 128]) for i in range(2)]
                    t1 = self.sb("at1", [128, 640])
                    t2 = self.sb("at2", [128, 640])
                    ptq = self.ps("aptq", [128, 8, 128], BF16)
                    ptk = self.ps("aptk", [128, 2, 128], BF16)
                    for j in range(NT):
                        i = j % 2
                        c0 = self.hcol(b, j)
                        kb.dma('sp', hT[i][:], hv[:, :, c0:c0 + 128], reads=['hTs'], writes=['ah%d' % i])
                        for n in range(3):
                            for k in range(8):
                                self.mm(pz[n][:], hT[i][:, k, :], win[:, k, n * 512:(n + 1) * 512], k == 0, k == 7,
                                        ['ah%d' % i, 'awin'], ['apz%d' % n])
                            kb.op('act', lambda e: e.copy(out=zs[:, n * 512:(n + 1) * 512], in_=pz[n][:]),
                                  reads=['apz%d' % n], writes=['azs'])
                        kb.op('pool', lambda e: e.tensor_copy(out=V1[:, j, :, 0:128],
                                                              in_=zs[:, 1280:1536].rearrange("p (g d) -> p g d", g=2)),
                              reads=['azs'], writes=['V1'])
                        kb.op('dve', lambda e: e.tensor_tensor(out=sq[:], in0=zs[:, 0:1280], in1=zs[:, 0:1280], op=ALU.mult),
                              reads=['azs'], writes=['asq'])
                        kb.op('dve', lambda e: e.tensor_reduce(out=st[:, 0, :], in_=sq[:].rearrange("p (h d) -> p h d", h=10),
                                                               axis=AX.X, op=ALU.add),
                              reads=['asq'], writes=['ast'])
                        kb.op('dve', lambda e: e.tensor_scalar(out=st[:, 1, :], in0=st[:, 0, :], scalar1=1.0 / 128, scalar2=EPS,
                                                               op0=ALU.mult, op1=ALU.add), reads=['ast'], writes=['ast'])
                        kb.op('act', lambda e: e.activation(out=st[:, 2, :], in_=st[:, 1, :], func=AF.Sqrt),
                              reads=['ast'], writes=['ast'])
                        kb.op('dve', lambda e: e.reciprocal(out=st[:, 3, :], in_=st[:, 2, :]), reads=['ast'], writes=['ast'])
                        z3 = zs[:, 0:1280].rearrange("p (h d) -> p h d", h=10)
                        q3 = qn[:].rearrange("p (h d) -> p h d", h=10)
                        kb.op('dve', lambda e: e.tensor_tensor(out=q3, in0=z3, in1=st[:, 3, :].unsqueeze(2).to_broadcast([128, 10, 128]),
                                                               op=ALU.mult), reads=['azs', 'ast'], writes=['aqn'])
                        kb.op('dve', lambda e: e.tensor_tensor(out=q3[:, 0:8, :], in0=q3[:, 0:8, :],
                                                               in1=Gqk[:, 0:1, :].to_broadcast([128, 8, 128]), op=ALU.mult),
                              reads=['aqn', 'Gqk'], writes=['aqn'])
                        kb.op('dve', lambda e: e.tensor_tensor(out=q3[:, 8:10, :], in0=q3[:, 8:10, :],
                                                               in1=Gqk[:, 1:2, :].to_broadcast([128, 2, 128]), op=ALU.mult),
                              reads=['aqn', 'Gqk'], writes=['aqn'])
                        if j >= NTC:
                            rr = rp[j % 2]
                            rk = 'arp%d' % (j % 2)
                            kb.dma('sp', rr[:], self.c_rope[(j - NTC) * 128:(j - NTC + 1) * 128, :], writes=[rk])
                            q4 = qn[:].rearrange("p (h d t) -> p h d t", h=10, t=2)
                            b4 = qb[:].rearrange("p (h d t) -> p h d t", h=10, t=2)
                            x0, x1 = q4[:, :, :, 0], q4[:, :, :, 1]
                            cosb = rr[:, 0:64].unsqueeze(1).to_broadcast([128, 10, 64])
                            sinb = rr[:, 64:128].unsqueeze(1).to_broadcast([128, 10, 64])
                            t13 = t1[:].rearrange("p (h d) -> p h d", h=10)
                            t23 = t2[:].rearrange("p (h d) -> p h d", h=10)
                            kb.op('dve', lambda e: e.tensor_tensor(out=t13, in0=x0, in1=cosb, op=ALU.mult), reads=['aqn', rk], writes=['at1'])
                            kb.op('pool', lambda e: e.tensor_tensor(out=t23, in0=x1, in1=sinb, op=ALU.mult), reads=['aqn', rk], writes=['at2'])
                            kb.op('dve', lambda e: e.tensor_tensor(out=b4[:, :, :, 0], in0=t13, in1=t23, op=ALU.subtract),
                                  reads=['at1', 'at2'], writes=['aqb'])
                            kb.op('dve', lambda e: e.tensor_tensor(out=t13, in0=x0, in1=sinb, op=ALU.mult), reads=['aqn', rk, 'aqb'], writes=['at1'])
                            kb.op('pool', lambda e: e.tensor_tensor(out=t23, in0=x1, in1=cosb, op=ALU.mult), reads=['aqn', rk, 'aqb'], writes=['at2'])
                            kb.op('dve', lambda e: e.tensor_tensor(out=b4[:, :, :, 1], in0=t13, in1=t23, op=ALU.add),
                                  reads=['at1', 'at2'], writes=['aqb'])
                        else:
                            kb.op('dve', lambda e: e.tensor_copy(out=qb[:], in_=qn[:]), reads=['aqn'], writes=['aqb'])
                        for h in range(8):
                            self.tr(ptq[:, h, :], qb[:, h * 128:(h + 1) * 128], ident[:], ['aqb', 'ident'], ['aptq'])
                        for g in range(2):
                            self.tr(ptk[:, g, :], qb[:, (8 + g) * 128:(9 + g) * 128], ident[:], ['aqb', 'ident'], ['aptk'])
                        kb.op('act', lambda e: e.copy(out=qT[:, :, j * 128:(j + 1) * 128], in_=ptq[:]), reads=['aptq'], writes=['qT'])
                        kb.op('act', lambda e: e.copy(out=kT[:, :, j * 128:(j + 1) * 128], in_=ptk[:]), reads=['aptk'], writes=['kT'])
                with self.phase():
                    o = self.outproj_setup(self.attn_w_out[jx], li)
                    E = self.sb("aE", [128, NT, 512], BF16)
                    pS = [self.ps("apS%d" % i, [128, 512]) for i in range(2)]
                    pO = [self.ps("apO%d" % i, [128, 512]) for i in range(2)]
                    ob = [self.sb("aob%d" % i, [128, D], BF16) for i in range(2)]
                    rc = self.sb("arc", [128, 8])
                    si = 0
                    oi = 0
                    apend = []
                    for jq in range(NT):
                        if jq < NTC and not want_ctx:
                            continue
                        nk = NTC if jq < NTC else NT
                        obt = ob[jq % 2]
                        obk = 'aob%d' % (jq % 2)
                        for g in range(2):
                            for kt in range(nk):
                                p = pS[si % 2]
                                pk = 'apS%d' % (si % 2)
                                si += 1
                                self.mm(p[:].rearrange("p (h q) -> p h q", h=4), kT[:, g, kt * 128:(kt + 1) * 128], qT[:, 4 * g:4 * g + 4, jq * 128:(jq + 1) * 128],
                                        True, True, ['kT', 'qT'], [pk])
                                kb.op('act', lambda e: e.activation(out=E[:, kt, :], in_=p[:], func=AF.Exp, scale=SC),
                                      reads=[pk], writes=['aE'])
                            for hh in range(4):
                                po = pO[oi % 2]
                                pok = 'apO%d' % (oi % 2)
                                oi += 1
                                for kt in range(nk):
                                    self.mm(po[:, 0:129], E[:, kt, hh * 128:(hh + 1) * 128], V1[:, kt, g, 0:129],
                                            kt == 0, kt == nk - 1, ['aE', 'V1'], [pok])
                                hd = 4 * g + hh
                                kb.op('dve', lambda e: e.reciprocal(out=rc[:, hd:hd + 1], in_=po[:, 128:129]),
                                      reads=[pok], writes=['arc'])
                                kb.op('dve', lambda e: e.tensor_scalar(out=obt[:, hd * 128:(hd + 1) * 128], in0=po[:, 0:128],
                                                                       scalar1=rc[:, hd:hd + 1], scalar2=None, op0=ALU.mult),
                                      reads=[pok, 'arc'], writes=[obk])
                        apend.append((lambda obt_, obk_, jq_: (lambda: self.outproj_tile(o, obt_, obk_, layer0, b, jq_)))(obt, obk, jq))
                        if len(apend) > 1:
                            apend.pop(0)()
                    while apend:
                        apend.pop(0)()

    def gdn_decl(self):
        if hasattr(self, 'g_qkT'):
            return
        NB = self.NB
        self.g_qkT = self.dram("g_qkT", [2, D, NB * TOK], F32)
        self.g_ktok = self.dram("g_ktok", [NB * TOK, D], F32)
        self.g_vtok = self.dram("g_vtok", [NB * TOK, D], F32)
        self.g_sgate = self.dram("g_sgate", [NB * TOK, D], F32)
        self.g_bg = self.dram("g_bg", [NB * TOK, 32], F32)
        self.g_odir = self.dram("g_odir", [2, NB * TOK, D], F32)

    def gdn_phase(self, li, jx, layer0, want_ctx):
        self.gdn_decl()
        self.gdn_proj(li, jx)
        self.gdn_scan(li, jx)
        self.gdn_finish(li, jx, layer0, want_ctx)

    def gdn_proj(self, li, jx):
        kb, NB = self.kb, self.NB
        hv = self.hT_view()
        with self.phase():
            win = self.sb("gwin", [128, 8, 4128], BF16)
            wsrc = self.gdn_w_in[jx].rearrange("(k p) c -> p k c", p=128)
            for k in range(8):
                kb.dma('pool', win[:, k, :], wsrc[:, k, :], writes=['gwin'])
            cw = self.sb("gcw", [128, 24, 5])
            with self.nc.allow_non_contiguous_dma(reason="tiny per-channel conv taps"):
                for t in range(5):
                    for f0 in range(0, 24, 8):
                        kb.dma('sp', cw[:, f0:f0 + 8, t], self.gdn_conv_w[jx, t, f0 * 128:(f0 + 8) * 128].rearrange("(f p) -> p f", p=128),
                               writes=['gcw'])
            identf = self.sb("identf", [128, 128])
            kb.dma('sp', identf[:], self.c_ident, writes=['identf'])
            onesf = self.sb("onesf", [128, 128])
            kb.op('dve', lambda e: e.memset(onesf[:], 1.0), writes=['onesf'])
            DTB = self.sb("gdtb", [128, 16])
            NA = self.sb("gna", [128, 16])
            kb.dma('sp', DTB[:], self.gdn_dt_bias[jx:jx + 1, :].to_broadcast([128, 16]), writes=['gdtb'])
            kb.dma('sp', NA[:], self.gdn_a_log[jx:jx + 1, :].to_broadcast([128, 16]), writes=['gna'])
            kb.op('act', lambda e: e.activation(out=NA[:], in_=NA[:], func=AF.Exp), reads=['gna'], writes=['gna'])
            kb.op('dve', lambda e: e.tensor_scalar(out=NA[:], in0=NA[:], scalar1=-1.0, scalar2=None, op0=ALU.mult),
                  reads=['gna'], writes=['gna'])
            h2 = [self.sb("gh%d" % i, [128, 8, 256 + 2 * HALO], BF16) for i in range(2)]
            NPZ = 2
            NB4 = 4
            pz = [self.ps("gpz%d" % i, [128, 512]) for i in range(NPZ)]
            pnbk = [self.ps("gpnb%d" % i, [128, 512]) for i in range(2)]
            pnl = [pnbk[0][:, 0:256], pnbk[1][:, 0:256]]
            pXb = [self.ps("gpX%d" % i, [128, 512]) for i in range(2)]
            pTl = [pXb[0][:, 0:256].rearrange("p (a b) -> p a b", a=2), pXb[1][:, 0:256].rearrange("p (a b) -> p a b", a=2)]
            pg = self.ps("gpg", [128, 2, 512])
            pb = pXb[1][:, 256:288]
            tA = [self.sb("gtA%d" % i, [128, 256]) for i in range(NB4)]
            sAll = [self.sb("gsAll%d" % i, [128, 24, 256]) for i in range(2)]
            rAll = [self.sb("grAll%d" % i, [128, 16, 256]) for i in range(2)]
            sqb = [self.sb("gsqb%d" % i, [128, 256], BF16) for i in range(NB4)]
            onesb = self.sb("gonesb", [128, 128], BF16)
            kb.op('dve', lambda e: e.memset(onesb[:], 1.0), writes=['onesb'])
            kst = self.sb("gkst", [128, 2, D])
            vst = self.sb("gvst", [128, 2, D])
            gst = [self.sb("ggst%d" % i, [128, D]) for i in range(2)]
            bgs = self.sb("gbgs", [128, 32])
            bt = self.sb("gbt", [128, 4, 16])
            qkTv = self.g_qkT.rearrange("q (h p) c -> q p h c", p=128)
            ci = 0
            wi = 0
            for b in range(NB):
                for w in range(NT // 2):
                    h = h2[wi % 2]
                    hk = 'gh%d' % (wi % 2)
                    wi += 1
                    c0 = self.hcol(b, 2 * w)
                    tok0 = b * TOK + 2 * w * 128
                    kb.dma('sp', h[:], hv[:, :, c0 - HALO:c0 + 256 + HALO], writes=[hk])
                    ws = wi % 2
                    sA, rA = sAll[ws], rAll[ws]
                    sAk = lambda f_: 'gsA%d_%d' % (ws, f_)
                    rAk = 'grA%d' % ws

                    def stage_ones(f_):
                        an_ = f_ % 2
                        self.mm(pnl[an_], onesb[:], sqb[f_ % NB4][:], True, True, ['onesb', 'gsqb%d' % (f_ % NB4)], ['gpn%d' % an_])
                        m_ = 128.0 if f_ < 8 else 1.0
                        kb.op('dve', lambda e: e.tensor_scalar(out=rA[:, f_, :], in0=pnl[an_], scalar1=m_, scalar2=EPS * m_,
                                                               op0=ALU.mult, op1=ALU.add), reads=['gpn%d' % an_], writes=[rAk])

                    def stage_tr(f_):
                        dst = kst if f_ < 16 else vst
                        dk = 'gkst' if f_ < 16 else 'gvst'
                        an_ = f_ % 2
                        pT, pTk = pTl[an_], 'gpX%d' % an_
                        for t2_ in range(2):
                            self.tr(pT[:, t2_, :], sA[:, f_, t2_ * 128:(t2_ + 1) * 128], identf[:], [sAk(f_), 'identf'], [pTk])
                        kb.op('act', lambda e: e.copy(out=dst[:, :, (f_ % 8) * 128:(f_ % 8 + 1) * 128], in_=pT), reads=[pTk], writes=[dk])

                    with self.nosame():
                        for f in range(24):
                            a = ci % NB4
                            az = ci % NPZ
                            ci += 1
                            pzk = 'gpz%d' % az
                            for k in range(8):
                                self.mm(pz[az][:, 0:260], win[:, k, f * 128:(f + 1) * 128], h[:, k, :], k == 0, k == 7,
                                        ['gwin', hk], [pzk])
                            tk = 'gtA%d' % a
                            kb.op('dve', lambda e: e.tensor_scalar(out=tA[a][:], in0=pz[az][:, 0:256], scalar1=cw[:, f, 0:1],
                                                                   scalar2=None, op0=ALU.mult), reads=[pzk, 'gcw'], writes=[tk])
                            for t in range(1, 5):
                                kb.op('dve', lambda e: e.scalar_tensor_tensor(out=tA[a][:], in0=pz[az][:, t:t + 256], scalar=cw[:, f, t:t + 1],
                                                                              in1=tA[a][:], op0=ALU.mult, op1=ALU.add),
                                      reads=[pzk, 'gcw', tk], writes=[tk])
                            kb.op('act', lambda e: e.activation(out=sA[:, f, :], in_=tA[a][:], func=AF.Silu), reads=[tk], writes=[sAk(f)])
                            if f < 16:
                                kb.op('pool', lambda e: e.tensor_tensor(out=sqb[f % NB4][:], in0=sA[:, f, :], in1=sA[:, f, :], op=ALU.mult),
                                      reads=[sAk(f)], writes=['gsqb%d' % (f % NB4)])
                            if 2 <= f < 18:
                                stage_ones(f - 2)
                            if f >= 19:
                                stage_tr(f - 3)
                        for f_ in (21, 22, 23):
                            stage_tr(f_)
                        kb.op('act', lambda e: e.activation(out=rA[:], in_=rA[:], func=AF.Sqrt), reads=[rAk], writes=[rAk])
                        for q4 in range(4):
                            kb.op('dve', lambda e: e.reciprocal(out=rA[:, 4 * q4:4 * q4 + 4, :], in_=rA[:, 4 * q4:4 * q4 + 4, :]),
                                  reads=[rAk], writes=[rAk])
                        for f in range(16):
                            kb.op('pool', lambda e: e.tensor_tensor(out=sA[:, f, :], in0=sA[:, f, :], in1=rA[:, f, :], op=ALU.mult),
                                  reads=[sAk(f), rAk], writes=[sAk(f)])
                            kb.dma('pool', qkTv[f // 8, :, f % 8, tok0:tok0 + 256], sA[:, f, :], reads=[sAk(f)], writes=['g_qkT_%d_%d' % (wi, f)])
                            if f >= 8:
                                stage_tr(f)
                    for t2 in range(2):
                        r0 = tok0 + t2 * 128
                        kb.dma('pool', self.g_ktok[r0:r0 + 128, :], kst[:, t2, :], reads=['gkst'], writes=['g_ktok_%d' % r0])
                        kb.dma('pool', self.g_vtok[r0:r0 + 128, :], vst[:, t2, :], reads=['gvst'], writes=['g_vtok_%d' % r0])
                        hs = h[:, :, HALO + t2 * 128:HALO + (t2 + 1) * 128]
                        for n in range(2):
                            for k in range(8):
                                self.mm(pg[:, n, :], hs[:, k, :], win[:, k, 3072 + n * 512:3072 + (n + 1) * 512], k == 0, k == 7,
                                        [hk, 'gwin'], ['gpg'])
                        g_ = gst[t2]
                        gk2 = 'ggst%d' % t2
                        kb.op('act', lambda e: e.activation(out=g_[:], in_=pg[:].rearrange("p a b -> p (a b)"), func=AF.Silu),
                              reads=['gpg'], writes=[gk2])
                        kb.dma('pool', self.g_sgate[r0:r0 + 128, :], g_[:], reads=[gk2], writes=['g_sgate_%d' % r0])
                        for k in range(8):
                            self.mm(pb, hs[:, k, :], win[:, k, 4096:4128], k == 0, k == 7, [hk, 'gwin'], ['gpX1'])
                        pb4 = pb.rearrange("p (d t h) -> p d t h", d=2, t=2)
                        kb.op('act', lambda e: e.activation(out=bgs[:, 0:16].rearrange("p (d h) -> p d h", d=2), in_=pb4[:, :, 0, :],
                                                            func=AF.Sigmoid), reads=['gpX1'], writes=['gbgs'])
                        kb.op('dve', lambda e: e.tensor_tensor(out=bt[:, 0, :].rearrange("p (d h) -> p d h", d=2), in0=pb4[:, :, 1, :],
                                                               in1=DTB[:].rearrange("p (d h) -> p d h", d=2), op=ALU.add),
                              reads=['gpX1', 'gdtb'], writes=['gbt'])
                        kb.op('dve', lambda e: e.scalar_tensor_tensor(out=bt[:, 1, :], in0=bt[:, 0, :], scalar=-1.0, in1=bt[:, 0, :],
                                                                      op0=ALU.mult, op1=ALU.max), reads=['gbt'], writes=['gbt'])
                        kb.op('act', lambda e: e.activation(out=bt[:, 2, :], in_=bt[:, 1, :], func=AF.Exp, scale=-1.0),
                              reads=['gbt'], writes=['gbt'])
                        kb.op('act', lambda e: e.activation(out=bt[:, 3, :], in_=bt[:, 2, :], func=AF.Ln, bias=1.0),
                              reads=['gbt'], writes=['gbt'])
                        kb.op('dve', lambda e: e.scalar_tensor_tensor(out=bt[:, 1, :], in0=bt[:, 0, :], scalar=0.0, in1=bt[:, 3, :],
                                                                      op0=ALU.max, op1=ALU.add), reads=['gbt'], writes=['gbt'])
                        kb.op('dve', lambda e: e.tensor_tensor(out=bgs[:, 16:32], in0=bt[:, 1, :], in1=NA[:], op=ALU.mult),
                              reads=['gbt', 'gna'], writes=['gbgs'])
                        kb.dma('pool', self.g_bg[r0:r0 + 128, :], bgs[:], reads=['gbgs'], writes=['g_bg_%d' % r0])

    def gdn_scan(self, li, jx):
        kb, NB = self.kb, self.NB
        HB = 4
        qkTv = self.g_qkT.rearrange("q (h p) c -> q p h c", p=128)
        order = [list(range(NT)), [1, 0] + list(range(NT - 1, NTC - 1, -1))]
        with self.phase():
            MS = self.sb("gmask", [128, 8, 128])
            kb.dma('sp', MS[:], self.c_masks[0:8].rearrange("m p c -> p m c"), writes=['gmask'])
            identf = self.sb("identf", [128, 128])
            kb.dma('sp', identf[:], self.c_ident, writes=['identf'])
            PS = [self.ps("gps%d" % i, [128, 512]) for i in range(8)]
            PK = ['gps%d' % i for i in range(8)]

            def v4(t):
                return t[:].rearrange("p (h c) -> p h c", h=HB)
            names = ['qT', 'kT', 'ktok', 'vtok', 'Gbc', 'Dm', 'E', 'DT', 'DTs', 'EG', 'qd', 'W', 'WT', 'Wa0', 'Wa1', 'Wb0', 'Wb1',
                     'QKm', 'kdec', 'wT', 'vn']
            sets = []
            for si in range(2):
                T = {}
                for nm in names:
                    T[nm] = self.sb("g%s%d" % (nm, si), [128, HB * 128])
                for nm in ('y0', 'y1', 'UW'):
                    T[nm] = self.sb("g%s%d" % (nm, si), [128, HB, 256])
                T['cols'] = self.sb("gcols%d" % si, [128, 6, HB])
                T['alast'] = self.sb("galast%d" % si, [128, HB, 2])
                T['k'] = 's%d_' % si
                sets.append(T)
            bgt = [self.sb("gbg%d" % i, [128, 32]) for i in range(4)]
            ost = [self.sb("gost%d" % i, [128, D]) for i in range(4)]
            S = [[self.sb("gS_%d_%d" % (d, hf), [128, HB, 128]) for hf in range(2)] for d in range(2)]
            stepi = 0
            bi = 0
            for b in range(NB):
                for d in range(2):
                    for hf in range(2):
                        kb.op('pool', lambda e: e.memset(S[d][hf][:], 0.0), writes=['gS_%d_%d' % (d, hf)])
                for n in range(NT):
                    for d in range(2):
                        j = order[d][n]
                        tok0 = b * TOK + j * 128
                        bg = bgt[bi % 4]
                        bgk = 'gbg%d' % (bi % 4)
                        os_ = ost[bi % 4]
                        osk = 'gost%d' % (bi % 4)
                        bi += 1
                        kb.dma('sp', bg[:], self.g_bg[tok0:tok0 + 128, :], writes=[bgk])
                        Ud, Usd, nUd, BO = MS[:, d, :], MS[:, 2 + d, :], MS[:, 4 + d, :], MS[:, 6, :]
                        for hf in range(2):
                            T = sets[stepi % 2]
                            K = lambda nm: T['k'] + nm
                            stepi += 1
                            h0 = hf * HB
                            Sk = 'gS_%d_%d' % (d, hf)
                            St = S[d][hf]
                            kb.dma('sp', v4(T['qT']), qkTv[0, :, h0:h0 + HB, tok0:tok0 + 128], writes=[K('qT')])
                            kb.dma('sp', v4(T['kT']), qkTv[1, :, h0:h0 + HB, tok0:tok0 + 128], writes=[K('kT')])
                            kb.dma('sp', T['ktok'][:], self.g_ktok[tok0:tok0 + 128, h0 * 128:(h0 + HB) * 128], writes=[K('ktok')])
                            kb.dma('sp', T['vtok'][:], self.g_vtok[tok0:tok0 + 128, h0 * 128:(h0 + HB) * 128], writes=[K('vtok')])
                            g4 = bg[:, 16 + d * 8 + h0:16 + d * 8 + h0 + HB]
                            be4 = bg[:, d * 8 + h0:d * 8 + h0 + HB]
                            bc4 = lambda ap: ap.unsqueeze(2).to_broadcast([128, HB, 128])
                            mk4 = lambda ap: ap.unsqueeze(1).to_broadcast([128, HB, 128])
                            kb.op('dve', lambda e: e.tensor_copy(out=v4(T['Gbc']), in_=bc4(g4)), reads=[bgk], writes=[K('Gbc')])
                            Gbc = v4(T['Gbc'])
                            for h in range(HB):
                                self.mm(PS[0][:, h * 128:(h + 1) * 128], Gbc[:, h, :], Ud, True, True, [K('Gbc'), 'gmask'], [PK[0]])
                            self.mm(PS[5][:, 0:HB], Ud, g4, True, True, ['gmask', bgk], [PK[5]])
                            self.mm(PS[5][:, HB:2 * HB], BO, g4, True, True, ['gmask', bgk], [PK[5]])
                            cols = T['cols']
                            kb.op('act', lambda e: e.copy(out=cols[:, 0, :], in_=PS[5][:, 0:HB]), reads=[PK[5]], writes=[K('cols')])
                            kb.op('dve', lambda e: e.tensor_tensor(out=cols[:, 1, :], in0=PS[5][:, HB:2 * HB], in1=cols[:, 0, :], op=ALU.subtract),
                                  reads=[PK[5], K('cols')], writes=[K('cols')])
                            kb.op('act', lambda e: e.activation(out=cols[:, 2, :], in_=cols[:, 0, :], func=AF.Exp), reads=[K('cols')], writes=[K('cols')])
                            kb.op('act', lambda e: e.activation(out=cols[:, 3, :], in_=cols[:, 1, :], func=AF.Exp), reads=[K('cols')], writes=[K('cols')])
                            for h in range(HB):
                                kb.op('dve', lambda e: e.tensor_scalar(out=T['Dm'][:, h * 128:(h + 1) * 128], in0=PS[0][:, h * 128:(h + 1) * 128],
                                                                       scalar1=cols[:, 0, h:h + 1], scalar2=0.0, op0=ALU.subtract, op1=ALU.min),
                                      reads=[PK[0], K('cols')], writes=[K('Dm')])
                            kb.op('act', lambda e: e.activation(out=T['E'][:], in_=T['Dm'][:], func=AF.Exp), reads=[K('Dm')], writes=[K('E')])
                            kb.op('dve', lambda e: e.tensor_tensor(out=v4(T['DT']), in0=v4(T['E']), in1=mk4(Ud), op=ALU.mult),
                                  reads=[K('E'), 'gmask'], writes=[K('DT')])
                            kb.op('pool', lambda e: e.tensor_tensor(out=v4(T['DTs']), in0=v4(T['E']), in1=mk4(Usd), op=ALU.mult),
                                  reads=[K('E'), 'gmask'], writes=[K('DTs')])
                            kb.op('act', lambda e: e.activation(out=T['EG'][:], in_=PS[0][:], func=AF.Exp), reads=[PK[0]], writes=[K('EG')])
                            kb.op('pool', lambda e: e.tensor_tensor(out=T['qd'][:], in0=T['qT'][:], in1=T['EG'][:], op=ALU.mult),
                                  reads=[K('qT'), K('EG')], writes=[K('qd')])
                            kT4, qT4 = v4(T['kT']), v4(T['qT'])
                            for h in range(HB):
                                self.mm(PS[2][:, h * 128:(h + 1) * 128], kT4[:, h, :], kT4[:, h, :], True, True, [K('kT')], [PK[2]])
                            for h in range(HB):
                                self.mm(PS[3][:, h * 128:(h + 1) * 128], kT4[:, h, :], qT4[:, h, :], True, True, [K('kT'), K('qT')], [PK[3]])
                            kb.op('dve', lambda e: e.tensor_tensor(out=T['DTs'][:], in0=PS[2][:], in1=T['DTs'][:], op=ALU.mult),
                                  reads=[PK[2], K('DTs')], writes=[K('DTs')])
                            kb.op('dve', lambda e: e.tensor_tensor(out=v4(T['W']).bitcast(F32R), in0=v4(T['DTs']), in1=bc4(be4), op=ALU.mult),
                                  reads=[K('DTs'), bgk], writes=[K('W')])
                            kb.op('dve', lambda e: e.tensor_tensor(out=T['QKm'][:], in0=PS[3][:], in1=T['DT'][:], op=ALU.mult),
                                  reads=[PK[3], K('DT')], writes=[K('QKm')])
                            kb.op('act', lambda e: e.copy(out=T['y0'][:, :, 0:128].bitcast(F32R), in_=v4(T['vtok'])), reads=[K('vtok')], writes=[K('y0')])
                            kb.op('dve', lambda e: e.tensor_tensor(out=T['y0'][:, :, 128:256].bitcast(F32R), in0=v4(T['ktok']), in1=bc4(cols[:, 2, :]), op=ALU.mult),
                                  reads=[K('ktok'), K('cols')], writes=[K('y0')])
                            kb.op('pool', lambda e: e.tensor_tensor(out=v4(T['kdec']), in0=v4(T['ktok']), in1=bc4(cols[:, 3, :]), op=ALU.mult),
                                  reads=[K('ktok'), K('cols')], writes=[K('kdec')])
                            with self.nosame():
                                W4 = v4(T['W'])
                                for h in range(HB):
                                    self.tr(PS[4][:, h * 128:(h + 1) * 128], W4[:, h, :], identf[:], [K('W'), 'identf'], [PK[4]])
                                kb.op('act', lambda e: e.copy(out=T['WT'][:].bitcast(F32R), in_=PS[4][:]), reads=[PK[4]], writes=[K('WT')])
                                pA = [PS[6], PS[7]]
                                ycur, ynxt = 'y0', 'y1'
                                for h in range(HB):
                                    self.mmr(pA[h // 2][:, (h % 2) * 256:(h % 2 + 1) * 256], W4[:, h, :], T[ycur][:, h, :], True, True,
                                             [K('W'), K(ycur)], [PK[6 + h // 2]])
                                for q in range(2):
                                    kb.op('dve', lambda e: e.tensor_tensor(out=T[ynxt][:, 2 * q:2 * q + 2, :].rearrange("p h c -> p (h c)").bitcast(F32R),
                                                                           in0=T[ycur][:, 2 * q:2 * q + 2, :].rearrange("p h c -> p (h c)"),
                                                                           in1=pA[q][:], op=ALU.subtract),
                                          reads=[K(ycur), PK[6 + q]], writes=[K(ynxt)])
                                ycur, ynxt = ynxt, ycur
                                cur, curT = 'W', 'WT'
                                for lvl in range(1, 6):
                                    na, nb_ = 'Wa%d' % (lvl % 2), 'Wb%d' % (lvl % 2)
                                    c4, cT4 = v4(T[cur]), v4(T[curT])
                                    for h in range(HB):
                                        self.mmr(PS[4][:, h * 128:(h + 1) * 128], cT4[:, h, :], c4[:, h, :], True, True, [K(cur), K(curT)], [PK[4]])
                                    if lvl < 5:
                                        for h in range(HB):
                                            self.mmr(PS[5][:, h * 128:(h + 1) * 128], c4[:, h, :], cT4[:, h, :], True, True, [K(cur), K(curT)], [PK[5]])
                                    kb.op('act', lambda e: e.copy(out=T[na][:].bitcast(F32R), in_=PS[4][:]), reads=[PK[4]], writes=[K(na)])
                                    if lvl < 5:
                                        kb.op('dve', lambda e: e.tensor_copy(out=T[nb_][:].bitcast(F32R), in_=PS[5][:]), reads=[PK[5]], writes=[K(nb_)])
                                    n4 = v4(T[na])
                                    for h in range(HB):
                                        self.mmr(pA[h // 2][:, (h % 2) * 256:(h % 2 + 1) * 256], n4[:, h, :], T[ycur][:, h, :], True, True,
                                                 [K(na), K(ycur)], [PK[6 + h // 2]])
                                    for q in range(2):
                                        kb.op('dve', lambda e: e.tensor_tensor(out=T[ynxt][:, 2 * q:2 * q + 2, :].rearrange("p h c -> p (h c)").bitcast(F32R),
                                                                               in0=T[ycur][:, 2 * q:2 * q + 2, :].rearrange("p h c -> p (h c)"),
                                                                               in1=pA[q][:], op=ALU.add),
                                              reads=[K(ycur), PK[6 + q]], writes=[K(ynxt)])
                                    ycur, ynxt = ynxt, ycur
                                    cur, curT = na, nb_
                                kb.op('dve', lambda e: e.tensor_tensor(out=T['UW'][:], in0=T[ycur][:], in1=be4.unsqueeze(2).to_broadcast([128, HB, 256]),
                                                                       op=ALU.mult), reads=[K(ycur), bgk], writes=[K('UW')])
                            UW = T['UW']
                            for h in range(HB):
                                self.tr(PS[3][:, h * 128:(h + 1) * 128], UW[:, h, 128:256], identf[:], [K('UW'), 'identf'], [PK[3]])
                            kb.op('act', lambda e: e.copy(out=T['wT'][:], in_=PS[3][:]), reads=[PK[3]], writes=[K('wT')])
                            wT4, qd4, QK4, kd4, vn4 = v4(T['wT']), v4(T['qd']), v4(T['QKm']), v4(T['kdec']), v4(T['vn'])
                            for c in ([0, 1] if d == 0 else [1, 0]):
                                rw = slice(64 * c, 64 * c + 64)
                                for h in range(HB):
                                    self.mm(PS[0][rw, h * 128:(h + 1) * 128], wT4[:, h, rw], St[:, h, :], True, True, [K('wT'), Sk], [PK[0]])
                                kb.op('dve', lambda e: e.tensor_tensor(out=vn4[rw], in0=UW[rw, :, 0:128],
                                                                       in1=PS[0][rw, :].rearrange("p (h c) -> p h c", h=HB), op=ALU.subtract),
                                      reads=[K('UW'), PK[0]], writes=[K('vn')])
                                for h in range(HB):
                                    self.mm(PS[1][rw, h * 128:(h + 1) * 128], qd4[:, h, rw], St[:, h, :], True, False, [K('qd'), Sk], [PK[1]])
                                    self.mm(PS[1][rw, h * 128:(h + 1) * 128], QK4[rw, h, rw], vn4[rw, h, :], False, True, [K('QKm'), K('vn')], [PK[1]])
                                kb.op('act', lambda e: e.copy(out=os_[rw, h0 * 128:(h0 + HB) * 128], in_=PS[1][rw, :]), reads=[PK[1]], writes=[osk])
                                for h in range(HB):
                                    self.mm(PS[2][:, h * 128:(h + 1) * 128], kd4[rw, h, :], vn4[rw, h, :], True, True, [K('kdec'), K('vn')], [PK[2]])
                                ia = (63 + 64 * c) if d == 0 else (64 * c)
                                kb.op('dve', lambda e: e.tensor_tensor(out=St[:], in0=St[:], in1=v4(T['EG'])[:, :, ia].unsqueeze(2).to_broadcast([128, HB, 128]),
                                                                       op=ALU.mult), reads=[Sk, K('EG')], writes=[Sk])
                                kb.op('dve', lambda e: e.tensor_tensor(out=St[:].rearrange("p h c -> p (h c)"), in0=St[:].rearrange("p h c -> p (h c)"),
                                                                       in1=PS[2][:], op=ALU.add), reads=[Sk, PK[2]], writes=[Sk])
                        kb.dma('pool', self.g_odir[d, tok0:tok0 + 128, :], os_[:], reads=[osk], writes=['g_odir_%d_%d' % (d, tok0)])

    def gdn_finish(self, li, jx, layer0, want_ctx):
        kb, NB = self.kb, self.NB
        with self.phase():
            o = self.outproj_setup(self.gdn_w_out[jx], li)
            NG = self.sb("gng", [128, 128])
            kb.dma('sp', NG[:], self.gdn_norm_g[jx:jx + 1, :].to_broadcast([128, 128]), writes=['gng'])
            self.rec_finish(o, NG, 'gng', self.g_odir, self.g_sgate, layer0, want_ctx)

    def rec_finish(self, o, NG, ngk, odir, sgate, layer0, want_ctx):
        kb, NB = self.kb, self.NB
        oa = [self.sb("foa%d" % i, [128, D]) for i in range(2)]
        obt = [self.sb("fob%d" % i, [128, D]) for i in range(2)]
        sg = [self.sb("fsg%d" % i, [128, D]) for i in range(2)]
        sq = self.sb("fsq", [128, D])
        st = self.sb("fst", [128, 4, 8])
        ob = [self.sb("fobb%d" % i, [128, D], BF16) for i in range(2)]
        it = 0
        pend = []
        for b in range(NB):
            for j in range(NT):
                if j < NTC and not want_ctx:
                    continue
                i = it % 2
                it += 1
                tok0 = b * TOK + j * 128
                kb.dma('sp', oa[i][:], odir[0, tok0:tok0 + 128, :], writes=['foa%d' % i])
                kb.dma('sp', obt[i][:], odir[1, tok0:tok0 + 128, :], writes=['fob%d' % i])
                kb.dma('sp', sg[i][:], sgate[tok0:tok0 + 128, :], writes=['fsg%d' % i])
                kb.op('dve', lambda e: e.tensor_tensor(out=oa[i][:], in0=oa[i][:], in1=obt[i][:], op=ALU.add),
                      reads=['foa%d' % i, 'fob%d' % i], writes=['foa%d' % i])
                kb.op('pool', lambda e: e.tensor_tensor(out=sq[:], in0=oa[i][:], in1=oa[i][:], op=ALU.mult), reads=['foa%d' % i], writes=['fsq'])
                kb.op('dve', lambda e: e.tensor_reduce(out=st[:, 0, :], in_=sq[:].rearrange("p (h d) -> p h d", h=8), axis=AX.X, op=ALU.add),
                      reads=['fsq'], writes=['fst'])
                kb.op('dve', lambda e: e.tensor_scalar(out=st[:, 1, :], in0=st[:, 0, :], scalar1=1.0 / 128, scalar2=EPS, op0=ALU.mult, op1=ALU.add),
                      reads=['fst'], writes=['fst'])
                kb.op('act', lambda e: e.activation(out=st[:, 2, :], in_=st[:, 1, :], func=AF.Sqrt), reads=['fst'], writes=['fst'])
                kb.op('dve', lambda e: e.reciprocal(out=st[:, 3, :], in_=st[:, 2, :]), reads=['fst'], writes=['fst'])
                o3 = oa[i][:].rearrange("p (h d) -> p h d", h=8)
                kb.op('dve', lambda e: e.tensor_tensor(out=o3, in0=o3, in1=st[:, 3, :].unsqueeze(2).to_broadcast([128, 8, 128]), op=ALU.mult),
                      reads=['foa%d' % i, 'fst'], writes=['foa%d' % i])
                kb.op('pool', lambda e: e.tensor_tensor(out=o3, in0=o3, in1=NG[:].unsqueeze(1).to_broadcast([128, 8, 128]), op=ALU.mult),
                      reads=['foa%d' % i, ngk], writes=['foa%d' % i])
                kb.op('dve', lambda e: e.tensor_tensor(out=ob[i][:], in0=oa[i][:], in1=sg[i][:], op=ALU.mult),
                      reads=['foa%d' % i, 'fsg%d' % i], writes=['fobb%d' % i])
                while pend:
                    pend.pop(0)()
                pend.append((lambda i_, b_, j_: (lambda: self.outproj_tile(o, ob[i_], 'fobb%d' % i_, layer0, b_, j_)))(i, b, j))
        while pend:
            pend.pop(0)()

    def mlstm_decl(self):
        self.gdn_decl()
        if hasattr(self, 'm_qkT'):
            return
        NB = self.NB
        self.m_qkT = self.dram("m_qkT", [2, 512, NB * TOK], F32)
        self.m_ktok = self.dram("m_ktok", [NB * TOK, 512], F32)

    def mlstm_phase(self, li, jx, layer0, want_ctx):
        self.mlstm_decl()
        self.mlstm_proj(li, jx)
        self.mlstm_scan(li, jx)
        kb = self.kb
        with self.phase():
            o = self.outproj_setup(self.mlstm_w_out[jx], li)
            NG = self.sb("mng", [128, 128])
            kb.dma('sp', NG[:], self.mlstm_norm_g[jx:jx + 1, :].to_broadcast([128, 128]), writes=['mng'])
            self.rec_finish(o, NG, 'mng', self.g_odir, self.g_sgate, layer0, want_ctx)

    def mlstm_proj(self, li, jx):
        kb, NB = self.kb, self.NB
        hv = self.hT_view()
        with self.phase():
            win = self.sb("mwin", [128, 8, 3104], BF16)
            wsrc = self.mlstm_w_in[jx].rearrange("(k p) c -> p k c", p=128)
            for k in range(8):
                kb.dma('pool', win[:, k, :], wsrc[:, k, :], writes=['mwin'])
            identf = self.sb("identf", [128, 128])
            kb.dma('sp', identf[:], self.c_ident, writes=['identf'])
            GB = self.sb("mgb", [128, 32])
            kb.dma('sp', GB[:], self.mlstm_gate_b[jx:jx + 1, :].to_broadcast([128, 32]), writes=['mgb'])
            h2 = [self.sb("mh%d" % i, [128, 8, 256], BF16) for i in range(2)]
            pz = [self.ps("mpz%d" % i, [128, 512]) for i in range(2)]
            pT = self.ps("mpT", [128, 2, 128])
            pg = [self.ps("mpg%d" % i, [128, 2, 512]) for i in range(2)]
            pb = self.ps("mpb", [128, 32])
            stg = [self.sb("mstg%d" % i, [128, 256]) for i in range(2)]
            kst = self.sb("mkst", [128, 2, 512])
            gst = [self.sb("mgst%d" % i, [128, D]) for i in range(4)]
            gls = self.sb("mgls", [128, 32])
            bt = self.sb("mbt", [128, 4, 16])
            ci = 0
            wi = 0
            gi = 0
            for b in range(NB):
                for w in range(NT // 2):
                    h = h2[wi % 2]
                    hk = 'mh%d' % (wi % 2)
                    wi += 1
                    c0 = self.hcol(b, 2 * w)
                    tok0 = b * TOK + 2 * w * 128
                    kb.dma('sp', h[:], hv[:, :, c0:c0 + 256], writes=[hk])
                    for f in range(8):
                        a = ci % 2
                        ci += 1
                        pzk = 'mpz%d' % a
                        for k in range(8):
                            self.mm(pz[a][:, 0:256], win[:, k, f * 128:(f + 1) * 128], h[:, k, :], k == 0, k == 7, ['mwin', hk], [pzk])
                        gk = 'mstg%d' % a
                        kb.op('act', lambda e: e.activation(out=stg[a][:], in_=pz[a][:, 0:256], func=AF.Identity, scale=(0.125 if f < 4 else 1.0)),
                              reads=[pzk], writes=[gk])
                        kb.dma('pool', self.m_qkT[f // 4, (f % 4) * 128:(f % 4 + 1) * 128, tok0:tok0 + 256], stg[a][:], reads=[gk],
                               writes=['m_qkT_%d' % ci])
                        if f >= 4:
                            for t2 in range(2):
                                self.tr(pT[:, t2, :], stg[a][:, t2 * 128:(t2 + 1) * 128], identf[:], [gk, 'identf'], ['mpT'])
                            kb.op('dve', lambda e: e.tensor_copy(out=kst[:, :, (f - 4) * 128:(f - 3) * 128], in_=pT[:]), reads=['mpT'], writes=['mkst'])
                    for t2 in range(2):
                        r0 = tok0 + t2 * 128
                        kb.dma('pool', self.m_ktok[r0:r0 + 128, :], kst[:, t2, :], reads=['mkst'], writes=['m_ktok_%d' % r0])
                        hs = h[:, :, t2 * 128:(t2 + 1) * 128]
                        for part in range(2):
                            p_ = pg[part]
                            pk = 'mpg%d' % part
                            for n in range(2):
                                for k in range(8):
                                    c1 = 1024 + part * 1024 + n * 512
                                    self.mm(p_[:, n, :], hs[:, k, :], win[:, k, c1:c1 + 512], k == 0, k == 7, [hk, 'mwin'], [pk])
                            g_ = gst[gi % 4]
                            gk2 = 'mgst%d' % (gi % 4)
                            gi += 1
                            if part == 0:
                                kb.op('dve', lambda e: e.tensor_copy(out=g_[:], in_=p_[:].rearrange("p a b -> p (a b)")), reads=[pk], writes=[gk2])
                                kb.dma('pool', self.g_vtok[r0:r0 + 128, :], g_[:], reads=[gk2], writes=['g_vtok_%d' % r0])
                            else:
                                kb.op('act', lambda e: e.activation(out=g_[:], in_=p_[:].rearrange("p a b -> p (a b)"), func=AF.Sigmoid),
                                      reads=[pk], writes=[gk2])
                                kb.dma('pool', self.g_sgate[r0:r0 + 128, :], g_[:], reads=[gk2], writes=['g_sgate_%d' % r0])
                        for k in range(8):
                            self.mm(pb[:], hs[:, k, :], win[:, k, 3072:3104], k == 0, k == 7, [hk, 'mwin'], ['mpb'])
                        x4 = bt[:, 0:2, :].rearrange("p a c -> p (a c)")
                        kb.op('dve', lambda e: e.tensor_tensor(out=x4, in0=pb[:], in1=GB[:], op=ALU.add), reads=['mpb', 'mgb'], writes=['mbt'])
                        x5 = x4.rearrange("p (d t h) -> p d t h", d=2, t=2)
                        kb.op('pool', lambda e: e.tensor_copy(out=gls[:, 0:16].rearrange("p (d h) -> p d h", d=2), in_=x5[:, :, 0, :]),
                              reads=['mbt'], writes=['mgls'])
                        xf = x5[:, :, 1, :]
                        t3 = lambda i_: bt[:, i_, :].rearrange("p (d h) -> p d h", d=2)
                        kb.op('dve', lambda e: e.scalar_tensor_tensor(out=t3(2), in0=xf, scalar=-1.0, in1=xf, op0=ALU.mult, op1=ALU.max),
                              reads=['mbt'], writes=['mbt'])
                        kb.op('act', lambda e: e.activation(out=bt[:, 2, :], in_=bt[:, 2, :], func=AF.Exp, scale=-1.0), reads=['mbt'], writes=['mbt'])
                        kb.op('act', lambda e: e.activation(out=bt[:, 3, :], in_=bt[:, 2, :], func=AF.Ln, bias=1.0), reads=['mbt'], writes=['mbt'])
                        kb.op('dve', lambda e: e.scalar_tensor_tensor(out=gls[:, 16:32].rearrange("p (d h) -> p d h", d=2), in0=xf, scalar=0.0,
                                                                      in1=t3(3), op0=ALU.min, op1=ALU.subtract), reads=['mbt'], writes=['mgls'])
                        kb.dma('pool', self.g_bg[r0:r0 + 128, :], gls[:], reads=['mgls'], writes=['g_bg_%d' % r0])

    def mlstm_scan(self, li, jx):
        kb, NB = self.kb, self.NB
        HB = 4
        qkTv = self.m_qkT.rearrange("q (h p) c -> q p h c", p=64)
        order = [list(range(NT)), [1, 0] + list(range(NT - 1, NTC - 1, -1))]
        with self.phase():
            MS = self.sb("mmask", [128, 13, 128])
            for m0 in range(0, 13, 4):
                m1 = min(13, m0 + 4)
                kb.dma('sp', MS[:, m0:m1, :], self.c_masks[m0:m1].rearrange("m p c -> p m c"), writes=['mmask'])
            identf = self.sb("identf", [128, 128])
            kb.dma('sp', identf[:], self.c_ident, writes=['identf'])
            onesf = self.sb("onesf", [128, 128])
            kb.op('dve', lambda e: e.memset(onesf[:], 1.0), writes=['onesf'])
            PS = [self.ps("mps%d" % i, [128, 512]) for i in range(8)]
            PK = ['mps%d' % i for i in range(8)]

            def v4(t):
                return t[:].rearrange("p (h c) -> p h c", h=HB)
            sets = []
            for si in range(2):
                T = {}
                for nm in ('LF', 'Dig', 'DL', 'P', 'PT'):
                    T[nm] = self.sb("m%s%d" % (nm, si), [128, HB * 128])
                T['qT'] = self.sb("mqT%d" % si, [64, HB, 128])
                T['kT'] = self.sb("mkT%d" % si, [64, HB, 128])
                T['ktok'] = self.sb("mktok%d" % si, [128, HB, 64])
                T['kw'] = self.sb("mkw%d" % si, [128, HB, 64])
                T['v1'] = self.sb("mv1%d" % si, [128, HB, 132])
                T['NI'] = self.sb("mNI%d" % si, [128, HB, 132])
                T['t1'] = self.sb("mt1%d" % si, [128, HB, 132])
                T['t2'] = self.sb("mt2%d" % si, [128, HB, 132])
                T['c'] = self.sb("mc%d" % si, [128, 16, HB])
                T['mloc'] = self.sb("mmloc%d" % si, [128, HB, 2])
                T['blast'] = self.sb("mblast%d" % si, [128, 2, HB])
                T['e3'] = self.sb("me3%d" % si, [128, 3, HB])
                T['fi'] = self.sb("mfi%d" % si, [128, 2, HB])
                T['k'] = 'm%d_' % si
                kb.op('pool', lambda e: e.memset(T['v1'][:, :, 128:129], 1.0), writes=[T['k'] + 'v1'])
                sets.append(T)
            glt = [self.sb("mgl%d" % i, [128, 32]) for i in range(4)]
            ost = [self.sb("most%d" % i, [128, D]) for i in range(4)]
            Cn = [[self.sb("mCn_%d_%d" % (d, hf), [64, HB, 132]) for hf in range(2)] for d in range(2)]
            Mm = [[self.sb("mM_%d_%d" % (d, hf), [128, HB]) for hf in range(2)] for d in range(2)]
            stepi = 0
            bi = 0
            for b in range(NB):
                for d in range(2):
                    for hf in range(2):
                        kb.op('pool', lambda e: e.memset(Cn[d][hf][:], 0.0), writes=['mCn_%d_%d' % (d, hf)])
                        kb.op('pool', lambda e: e.memset(Mm[d][hf][:], 0.0), writes=['mM_%d_%d' % (d, hf)])
                for n in range(NT):
                    for d in range(2):
                        j = order[d][n]
                        tok0 = b * TOK + j * 128
                        gl = glt[bi % 4]
                        glk = 'mgl%d' % (bi % 4)
                        os_ = ost[bi % 4]
                        osk = 'most%d' % (bi % 4)
                        bi += 1
                        kb.dma('sp', gl[:], self.g_bg[tok0:tok0 + 128, :], writes=[glk])
                        Ud, nUd, BO, BmU, NEG = MS[:, d, :], MS[:, 4 + d, :], MS[:, 6, :], MS[:, 9 + d, :], MS[:, 11 + d, :]
                        for hf in range(2):
                            T = sets[stepi % 2]
                            K = lambda nm: T['k'] + nm
                            stepi += 1
                            h0 = hf * HB
                            Ck, Mk = 'mCn_%d_%d' % (d, hf), 'mM_%d_%d' % (d, hf)
                            Ct, Mt = Cn[d][hf], Mm[d][hf]
                            c = T['c']
                            kb.dma('sp', T['qT'][:], qkTv[0, :, h0:h0 + HB, tok0:tok0 + 128], writes=[K('qT')])
                            kb.dma('sp', T['kT'][:], qkTv[1, :, h0:h0 + HB, tok0:tok0 + 128], writes=[K('kT')])
                            kb.dma('sp', T['ktok'][:], self.m_ktok[tok0:tok0 + 128, h0 * 64:(h0 + HB) * 64].rearrange("p (h c) -> p h c", h=HB),
                                   writes=[K('ktok')])
                            kb.dma('sp', T['v1'][:, :, 0:128], self.g_vtok[tok0:tok0 + 128, h0 * 128:(h0 + HB) * 128].rearrange("p (h c) -> p h c", h=HB),
                                   writes=[K('v1')])
                            ig4 = gl[:, d * 8 + h0:d * 8 + h0 + HB]
                            lf4 = gl[:, 16 + d * 8 + h0:16 + d * 8 + h0 + HB]
                            bc4 = lambda ap: ap.unsqueeze(2).to_broadcast([128, HB, 128])
                            mk4 = lambda ap: ap.unsqueeze(1).to_broadcast([128, HB, 128])
                            kb.op('dve', lambda e: e.tensor_copy(out=v4(T['LF']), in_=bc4(lf4)), reads=[glk], writes=[K('LF')])
                            kb.op('pool', lambda e: e.tensor_tensor(out=v4(T['Dig']), in0=mk4(identf[:]), in1=bc4(ig4), op=ALU.mult),
                                  reads=[glk, 'identf'], writes=[K('Dig')])
                            LF, Dig = v4(T['LF']), v4(T['Dig'])
                            for h in range(HB):
                                o_ = PS[0][:, h * 128:(h + 1) * 128]
                                self.mm(o_, Ud, LF[:, h, :], True, False, ['mmask', K('LF')], [PK[0]])
                                self.mm(o_, LF[:, h, :], nUd, False, False, ['mmask', K('LF')], [PK[0]])
                                self.mm(o_, onesf[:], Dig[:, h, :], False, True, ['onesf', K('Dig')], [PK[0]])
                            for h in range(HB):
                                o_ = PS[1][:, h * 128:(h + 1) * 128]
                                self.mm(o_, LF[:, h, :], BmU, True, False, ['mmask', K('LF')], [PK[1]])
                                self.mm(o_, onesf[:], Dig[:, h, :], False, True, ['onesf', K('Dig')], [PK[1]])
                            self.mm(PS[4][:, 0:HB], Ud, lf4, True, True, ['mmask', glk], [PK[4]])
                            self.mm(PS[4][:, HB:2 * HB], BO, lf4, True, True, ['mmask', glk], [PK[4]])
                            for cc in range(2):
                                self.mm(PS[4][:, 8 + cc * HB:8 + (cc + 1) * HB], MS[:, 7 + cc, :], lf4, True, True, ['mmask', glk], [PK[4]])
                            for h in range(HB):
                                self.mm(PS[2][:, h * 128:(h + 1) * 128], T['qT'][:, h, :], T['kT'][:, h, :], True, True, [K('qT'), K('kT')], [PK[2]])
                            kb.op('act', lambda e: e.copy(out=c[:, 0:2, :].rearrange("p a h -> p (a h)"), in_=PS[4][:, 0:2 * HB]), reads=[PK[4]], writes=[K('c')])
                            kb.op('act', lambda e: e.copy(out=T['blast'][:].rearrange("p a h -> p (a h)"), in_=PS[4][:, 8:8 + 2 * HB]),
                                  reads=[PK[4]], writes=[K('blast')])
                            kb.op('dve', lambda e: e.tensor_tensor(out=v4(T['DL']), in0=PS[0][:].rearrange("p (h c) -> p h c", h=HB), in1=mk4(NEG), op=ALU.add),
                                  reads=[PK[0], 'mmask'], writes=[K('DL')])
                            kb.op('dve', lambda e: e.tensor_reduce(out=c[:, 2, :], in_=v4(T['DL']), axis=AX.X, op=ALU.max), reads=[K('DL')], writes=[K('c')])
                            kb.op('dve', lambda e: e.tensor_tensor(out=v4(T['DL']), in0=v4(T['DL']), in1=bc4(c[:, 2, :]), op=ALU.subtract),
                                  reads=[K('DL'), K('c')], writes=[K('DL')])
                            kb.op('act', lambda e: e.activation(out=T['P'][:], in_=T['DL'][:], func=AF.Exp), reads=[K('DL')], writes=[K('P')])
                            kb.op('dve', lambda e: e.tensor_tensor(out=T['P'][:], in0=T['P'][:], in1=PS[2][:], op=ALU.mult), reads=[K('P'), PK[2]], writes=[K('P')])
                            P4 = v4(T['P'])
                            for h in range(HB):
                                self.tr(PS[3][:, h * 128:(h + 1) * 128], P4[:, h, :], identf[:], [K('P'), 'identf'], [PK[3]])
                            kb.op('act', lambda e: e.copy(out=T['PT'][:], in_=PS[3][:]), reads=[PK[3]], writes=[K('PT')])
                            PT4 = v4(T['PT'])
                            for h in range(HB):
                                self.mm(PS[6 + h // 2][:, (h % 2) * 129:(h % 2) * 129 + 129], PT4[:, h, :], T['v1'][:, h, 0:129], True, True,
                                        [K('PT'), K('v1')], [PK[6 + h // 2]])
                            for q in range(2):
                                kb.op('act' if q == 0 else 'dve',
                                      (lambda e: e.copy(out=T['NI'][:, 0:2, 0:129], in_=PS[6][:, 0:258].rearrange("p (h c) -> p h c", h=2))) if q == 0 else
                                      (lambda e: e.tensor_copy(out=T['NI'][:, 2:4, 0:129], in_=PS[7][:, 0:258].rearrange("p (h c) -> p h c", h=2))),
                                      reads=[PK[6 + q]], writes=[K('NI')])
                            kb.op('dve', lambda e: e.tensor_reduce(out=T['mloc'][:], in_=PS[1][:].rearrange("p (h c j) -> p h c j", h=HB, c=2), axis=AX.X, op=ALU.max),
                                  reads=[PK[1]], writes=[K('mloc')])
                            kb.op('dve', lambda e: e.tensor_tensor(out=c[:, 3, :], in0=c[:, 1, :], in1=c[:, 0, :], op=ALU.subtract), reads=[K('c')], writes=[K('c')])
                            kb.op('dve', lambda e: e.tensor_tensor(out=c[:, 3, :], in0=c[:, 3, :], in1=ig4, op=ALU.add), reads=[K('c'), glk], writes=[K('c')])
                            for cc in range(2):
                                rw = slice(64 * cc, 64 * cc + 64)
                                kb.op('dve', lambda e: e.tensor_tensor(out=c[rw, 4, :], in0=c[rw, 3, :], in1=T['mloc'][rw, :, cc], op=ALU.subtract),
                                      reads=[K('c'), K('mloc')], writes=[K('c')])
                            kb.op('act', lambda e: e.activation(out=c[:, 5, :], in_=c[:, 4, :], func=AF.Exp), reads=[K('c')], writes=[K('c')])
                            kb.op('pool', lambda e: e.tensor_tensor(out=T['kw'][:], in0=T['ktok'][:], in1=c[:, 5, :].unsqueeze(2).to_broadcast([128, HB, 64]), op=ALU.mult),
                                  reads=[K('ktok'), K('c')], writes=[K('kw')])
                            for cc in ([0, 1] if d == 0 else [1, 0]):
                                rw = slice(64 * cc, 64 * cc + 64)
                                for h in range(HB):
                                    self.mm(PS[h // 2][rw, (h % 2) * 129:(h % 2) * 129 + 129], T['qT'][:, h, rw], Ct[:, h, 0:129], True, True,
                                            [K('qT'), Ck], [PK[h // 2]])
                                kb.op('dve', lambda e: e.tensor_tensor(out=c[rw, 6, :], in0=c[rw, 0, :], in1=Mt[rw, :], op=ALU.add), reads=[K('c'), Mk], writes=[K('c')])
                                kb.op('dve', lambda e: e.tensor_tensor(out=c[rw, 7, :], in0=c[rw, 2, :], in1=c[rw, 6, :], op=ALU.max), reads=[K('c')], writes=[K('c')])
                                e3 = T['e3']
                                kb.op('dve', lambda e: e.tensor_tensor(out=e3[rw, 0, :], in0=c[rw, 6, :], in1=c[rw, 7, :], op=ALU.subtract), reads=[K('c')], writes=[K('e3')])
                                kb.op('dve', lambda e: e.tensor_tensor(out=e3[rw, 1, :], in0=c[rw, 2, :], in1=c[rw, 7, :], op=ALU.subtract), reads=[K('c')], writes=[K('e3')])
                                kb.op('dve', lambda e: e.tensor_scalar(out=e3[rw, 2, :], in0=c[rw, 7, :], scalar1=-1.0, scalar2=None, op0=ALU.mult), reads=[K('c')], writes=[K('e3')])
                                kb.op('act', lambda e: e.activation(out=e3[rw, :, :], in_=e3[rw, :, :], func=AF.Exp), reads=[K('e3')], writes=[K('e3')])
                                for q in range(2):
                                    kb.op('dve', lambda e: e.tensor_tensor(out=T['t1'][rw, 2 * q:2 * q + 2, 0:129], in0=PS[q][rw, 0:258].rearrange("p (h c) -> p h c", h=2),
                                                                           in1=e3[rw, 0, 2 * q:2 * q + 2].unsqueeze(2).to_broadcast([64, 2, 129]), op=ALU.mult),
                                          reads=[PK[q], K('e3')], writes=[K('t1')])
                                kb.op('pool', lambda e: e.tensor_tensor(out=T['t2'][rw, :, 0:129], in0=T['NI'][rw, :, 0:129],
                                                                        in1=e3[rw, 1, :].unsqueeze(2).to_broadcast([64, HB, 129]), op=ALU.mult),
                                      reads=[K('NI'), K('e3')], writes=[K('t2')])
                                kb.op('dve', lambda e: e.tensor_tensor(out=T['t1'][rw, :, 0:129], in0=T['t1'][rw, :, 0:129], in1=T['t2'][rw, :, 0:129], op=ALU.add),
                                      reads=[K('t1'), K('t2')], writes=[K('t1')])
                                den = T['t1'][rw, :, 128]
                                kb.op('dve', lambda e: e.scalar_tensor_tensor(out=c[rw, 8, :], in0=den, scalar=-1.0, in1=den, op0=ALU.mult, op1=ALU.max),
                                      reads=[K('t1')], writes=[K('c')])
                                kb.op('dve', lambda e: e.tensor_tensor(out=c[rw, 9, :], in0=c[rw, 8, :], in1=e3[rw, 2, :], op=ALU.max), reads=[K('c'), K('e3')], writes=[K('c')])
                                kb.op('dve', lambda e: e.reciprocal(out=c[rw, 10, :], in_=c[rw, 9, :]), reads=[K('c')], writes=[K('c')])
                                kb.op('dve', lambda e: e.tensor_tensor(out=os_[rw, h0 * 128:(h0 + HB) * 128].rearrange("p (h c) -> p h c", h=HB), in0=T['t1'][rw, :, 0:128],
                                                                       in1=c[rw, 10, :].unsqueeze(2).to_broadcast([64, HB, 128]), op=ALU.mult),
                                      reads=[K('t1'), K('c')], writes=[osk])
                                for h in range(HB):
                                    self.mm(PS[2 + h // 2][0:64, (h % 2) * 129:(h % 2) * 129 + 129], T['kw'][rw, h, :], T['v1'][rw, h, 0:129], True, True,
                                            [K('kw'), K('v1')], [PK[2 + h // 2]])
                                fi = T['fi']
                                kb.op('dve', lambda e: e.tensor_tensor(out=c[:, 11, :], in0=T['blast'][:, cc, :], in1=Mt[:], op=ALU.add), reads=[K('blast'), Mk], writes=[K('c')])
                                kb.op('dve', lambda e: e.tensor_tensor(out=c[:, 12, :], in0=c[:, 11, :], in1=T['mloc'][:, :, cc], op=ALU.max), reads=[K('c'), K('mloc')], writes=[K('c')])
                                kb.op('dve', lambda e: e.tensor_tensor(out=fi[:, 0, :], in0=c[:, 11, :], in1=c[:, 12, :], op=ALU.subtract), reads=[K('c')], writes=[K('fi')])
                                kb.op('dve', lambda e: e.tensor_tensor(out=fi[:, 1, :], in0=T['mloc'][:, :, cc], in1=c[:, 12, :], op=ALU.subtract), reads=[K('c'), K('mloc')], writes=[K('fi')])
                                kb.op('act', lambda e: e.activation(out=fi[:], in_=fi[:], func=AF.Exp), reads=[K('fi')], writes=[K('fi')])
                                kb.op('dve', lambda e: e.tensor_copy(out=Mt[:], in_=c[:, 12, :]), reads=[K('c')], writes=[Mk])
                                kb.op('dve', lambda e: e.tensor_tensor(out=Ct[:, :, 0:129], in0=Ct[:, :, 0:129], in1=fi[0:64, 0, :].unsqueeze(2).to_broadcast([64, HB, 129]), op=ALU.mult),
                                      reads=[Ck, K('fi')], writes=[Ck])
                                for q in range(2):
                                    kb.op('dve', lambda e: e.tensor_tensor(out=T['t2'][0:64, 2 * q:2 * q + 2, 0:129], in0=PS[2 + q][0:64, 0:258].rearrange("p (h c) -> p h c", h=2),
                                                                           in1=fi[0:64, 1, 2 * q:2 * q + 2].unsqueeze(2).to_broadcast([64, 2, 129]), op=ALU.mult),
                                          reads=[PK[2 + q], K('fi'), K('t2')], writes=[K('t2')])
                                kb.op('dve', lambda e: e.tensor_tensor(out=Ct[:, :, 0:129], in0=Ct[:, :, 0:129], in1=T['t2'][0:64, :, 0:129], op=ALU.add),
                                      reads=[Ck, K('t2')], writes=[Ck])
                        kb.dma('pool', self.g_odir[d, tok0:tok0 + 128, :], os_[:], reads=[osk], writes=['g_odir_%d_%d' % (d, tok0)])

    def build(self):
        self.declare()
        self.setup()
        cnt = {0: 0, 1: 0, 2: 0}
        for li, kind in enumerate(self.kinds):
            last = self.last_flags[li]
            layer0 = (li == 0)
            jx = cnt[kind]
            cnt[kind] += 1
            self.mod_phase(li)
            self.norm_phase(li, 1, layer0)
            if kind == 2:
                self.attn_phase(li, jx, layer0, not last)
            elif kind == 0:
                self.gdn_phase(li, jx, layer0, not last)
            else:
                self.mlstm_phase(li, jx, layer0, not last)
            self.norm_phase(li, 2, False, ctx_needed=not last)
            self.ffn_phase(li, ctx_needed=not last)
        self.final_phase()
        self.kb.barrier()
        self.kb.close()


def host_consts():
    ident = np.eye(128, dtype=np.float32)
    n_pair = 32
    inv = (10000.0 ** (-np.arange(n_pair, dtype=np.float32) / n_pair)).astype(np.float32)
    pos = np.arange(LAT)
    row = (pos // 64).astype(np.float32)
    col = (pos % 64).astype(np.float32)
    ang = np.concatenate([row[:, None] * inv, col[:, None] * inv], axis=-1).astype(np.float32)
    rope = np.concatenate([np.cos(ang), np.sin(ang)], axis=-1).astype(np.float32)
    masks = np.zeros((16, 128, 128), np.float32)
    t = np.arange(128)
    same = (t[:, None] // 64) == (t[None, :] // 64)
    uf = (same & (t[:, None] <= t[None, :])).astype(np.float32)
    ub = (same & (t[:, None] >= t[None, :])).astype(np.float32)
    masks[0], masks[1] = uf, ub
    masks[2], masks[3] = uf - np.eye(128, dtype=np.float32), ub - np.eye(128, dtype=np.float32)
    masks[4], masks[5] = -uf, -ub
    masks[6] = same.astype(np.float32)
    masks[7] = np.repeat((t < 64).astype(np.float32)[:, None], 128, axis=1)
    masks[8] = np.repeat((t >= 64).astype(np.float32)[:, None], 128, axis=1)
    masks[9], masks[10] = masks[6] - uf, masks[6] - ub
    masks[11] = (1.0 - ub) * np.float32(-1e30)
    masks[12] = (1.0 - uf) * np.float32(-1e30)
    return {"c_ident": ident, "c_rope": rope, "c_masks": masks}


def make_in_maps(inputs, NB, n_cores, kinds):
    f = lambda a: np.ascontiguousarray(np.asarray(a, dtype=np.float32))
    consts = host_consts()
    shared = {}
    for k in ("norm1_g", "norm2_g", "w_mod", "b_mod", "ffn_w_in", "ffn_conv_w", "ffn_conv_b", "ffn_w_out",
              "gdn_w_in", "gdn_conv_w", "gdn_norm_g", "gdn_w_out", "mlstm_w_in", "mlstm_norm_g", "mlstm_w_out",
              "attn_w_in", "attn_q_norm_g", "attn_k_norm_g", "attn_w_out"):
        shared[k] = f(inputs[k])
    shared["gdn_a_log"] = f(inputs["gdn_a_log"]).reshape(-1, 16)
    shared["gdn_dt_bias"] = f(inputs["gdn_dt_bias"]).reshape(-1, 16)
    shared["mlstm_gate_b"] = f(inputs["mlstm_gate_b"]).reshape(-1, 32)
    shared["final_norm_g"] = f(inputs["final_norm_g"]).reshape(1, D)
    shared.update(consts)
    x, c, ctx, c_ctx = f(inputs["x"]), f(inputs["c"]), f(inputs["ctx"]), f(inputs["c_ctx"])
    maps = []
    for i in range(n_cores):
        m = dict(shared)
        m["x"] = x[i * NB:(i + 1) * NB]
        m["ctx"] = ctx[i * NB:(i + 1) * NB]
        m["cvec"] = np.ascontiguousarray(np.concatenate([c[i * NB:(i + 1) * NB], c_ctx[None, :]], axis=0))
        maps.append(m)
    return maps


def kernel(**inputs):
    NB = 2
    n_cores = 8
    nc = bass.Bass("TRN2", target_bir_lowering=False)
    mk = MK(nc, NB, KINDS, [False, False, False, True])
    mk.build()
    maps = make_in_maps(inputs, NB, n_cores, KINDS)
    res = run_bass_kernel_spmd(nc, maps, core_ids=list(range(n_cores)))
    return np.concatenate([r["out"] for r in res.results], axis=0).astype(np.float32)
```
